# Optimizing a Trainium2 kernel written in Bass

```python
import math
import jax, jax.numpy as jnp
from jax import lax
import numpy as np

D_MODEL = 1024
BATCH = 8
SEQ = 2048
DEPTH = 1

A_HEADS = 8
A_LAT = 128
A_HEAD_DIM = 64
IDX_HEADS = 8
IDX_DIM = 64
TOPK_MAX = 256
B_HEADS = 8
B_KV_HEADS = 2
B_HEAD_DIM = 64
WINDOW = 128
BLOCK = 128
N_BUCKETS = 32
MAX_DISTANCE = 128
N_BIAS_HEADS = A_HEADS + B_HEADS
N_GROUPS = 4
EXPERTS_PER_GROUP = 8
N_EXPERTS = N_GROUPS * EXPERTS_PER_GROUP
EXPERT_TOPK = 2
D_EXPERT = 256
ALPHA = (2 * DEPTH) ** 0.25
BETA = (8 * DEPTH) ** -0.25
LN_EPS = 1e-5
RMS_EPS = 1e-6

IN_WIDTHS = (A_HEADS * A_LAT, A_LAT, IDX_HEADS * IDX_DIM, IDX_DIM, IDX_HEADS,
             B_HEADS * B_HEAD_DIM, B_KV_HEADS * B_HEAD_DIM, B_KV_HEADS * B_HEAD_DIM, D_MODEL, D_MODEL)
IN_WIDTH = (A_HEADS * A_LAT + A_LAT + IDX_HEADS * IDX_DIM + IDX_DIM + IDX_HEADS
            + B_HEADS * B_HEAD_DIM + 2 * B_KV_HEADS * B_HEAD_DIM + 2 * D_MODEL)

kernel_name = "hybrid_dsa_swa_sink_hmoe_deepnorm"


def _split_points():
    return [int(v) for v in np.cumsum(IN_WIDTHS)[:-1]]


def layer_norm(x, g, b):
    xf = x.astype(jnp.float32)
    mu = xf.mean(-1, keepdims=True)
    var = jnp.square(xf - mu).mean(-1, keepdims=True)
    return ((xf - mu) * lax.rsqrt(var + LN_EPS) * g.astype(jnp.float32) + b.astype(jnp.float32)).astype(x.dtype)


def rms_norm(x, g):
    xf = x.astype(jnp.float32)
    ms = jnp.square(xf).mean(-1, keepdims=True)
    return (xf * lax.rsqrt(ms + RMS_EPS) * g.astype(jnp.float32)).astype(x.dtype)


def t5_bucket(dist):
    n = jnp.maximum(dist, 0)
    max_exact = N_BUCKETS // 2
    nf = jnp.maximum(n, 1).astype(jnp.float32)
    large = max_exact + (jnp.log(nf / max_exact) / math.log(MAX_DISTANCE / max_exact)
                         * (N_BUCKETS - max_exact)).astype(jnp.int32)
    large = jnp.minimum(large, N_BUCKETS - 1)
    return jnp.where(n < max_exact, n, large).astype(jnp.int32)


def dsa_branch(q_lat, c_kv, q_idx, k_idx, w_idx, w_uv, bias_tab):
    bsz, L = c_kv.shape[:2]
    k_sel = min(TOPK_MAX, L // 4)
    n_blk = L // BLOCK
    key_pos = jnp.arange(L, dtype=jnp.int32)
    idx_scale = IDX_DIM ** -0.5
    w_scale = IDX_HEADS ** -0.5
    att_scale = A_LAT ** -0.5
    bias_a = bias_tab[:, :A_HEADS]
    gather = jax.vmap(lambda kv_b, i_b: kv_b[i_b])

    def to_blocks(a):
        return jnp.moveaxis(a.reshape((bsz, n_blk, BLOCK) + a.shape[2:]), 1, 0)

    def one_block(args):
        blk, qa, qi, wi = args
        q_pos = blk * BLOCK + jnp.arange(BLOCK, dtype=jnp.int32)
        s = jnp.einsum('bthd,bsd->bths', qi, k_idx) * idx_scale
        score = jnp.einsum('bths,bth->bts', jax.nn.relu(s), wi * w_scale).astype(jnp.float32)
        causal = key_pos[None, :] <= q_pos[:, None]
        score = jnp.where(causal[None], score, -jnp.inf)
        _, sel = lax.top_k(score, k_sel)
        kv = gather(c_kv, sel)
        dist = q_pos[None, :, None] - sel
        valid = dist >= 0
        bias = jnp.moveaxis(bias_a[t5_bucket(dist)], -1, 2).astype(jnp.float32)
        logits = jnp.einsum('bthc,btkc->bthk', qa, kv).astype(jnp.float32) * att_scale + bias
        logits = jnp.where(valid[:, :, None, :], logits, -jnp.inf)
        p = jax.nn.softmax(logits, axis=-1).astype(kv.dtype)
        o = jnp.einsum('bthk,btkc->bthc', p, kv)
        return jnp.einsum('bthc,hcd->bthd', o, w_uv)

    out = lax.map(one_block, (jnp.arange(n_blk, dtype=jnp.int32), to_blocks(q_lat),
                              to_blocks(q_idx), to_blocks(w_idx)))
    return jnp.moveaxis(out, 0, 1).reshape(bsz, L, A_HEADS * A_HEAD_DIM)


def swa_branch(q, k, v, sinks, bias_tab):
    bsz, L = q.shape[:2]
    n_blk = L // BLOCK
    grp = B_HEADS // B_KV_HEADS
    qb = q.reshape(bsz, n_blk, BLOCK, B_KV_HEADS, grp, B_HEAD_DIM)

    def band(a):
        a = jnp.pad(a, ((0, 0), (BLOCK, 0), (0, 0), (0, 0)))
        a = a.reshape(bsz, n_blk + 1, BLOCK, B_KV_HEADS, B_HEAD_DIM)
        return jnp.concatenate([a[:, :-1], a[:, 1:]], axis=2)

    kb, vb = band(k), band(v)
    t_loc = jnp.arange(BLOCK, dtype=jnp.int32)[:, None]
    s_loc = jnp.arange(2 * BLOCK, dtype=jnp.int32)[None, :]
    dist = t_loc + BLOCK - s_loc
    in_window = (dist >= 0) & (dist < WINDOW)
    blk_ids = jnp.arange(n_blk, dtype=jnp.int32)[:, None, None]
    mask = in_window[None] & ((blk_ids > 0) | (s_loc >= BLOCK)[None])
    bias = bias_tab[:, A_HEADS:][t5_bucket(dist)].astype(jnp.float32)
    bias = jnp.transpose(bias, (2, 0, 1)).reshape(B_KV_HEADS, grp, BLOCK, 2 * BLOCK)
    logits = jnp.einsum('bntkgd,bnskd->bnkgts', qb, kb).astype(jnp.float32) * (B_HEAD_DIM ** -0.5) + bias
    logits = jnp.where(mask[None, :, None, None], logits, -jnp.inf)
    sink = sinks.astype(jnp.float32).reshape(1, 1, B_KV_HEADS, grp, 1, 1)
    m = jnp.maximum(logits.max(-1, keepdims=True), sink)
    p = jnp.exp(logits - m)
    p = (p / (p.sum(-1, keepdims=True) + jnp.exp(sink - m))).astype(v.dtype)
    o = jnp.einsum('bnkgts,bnskd->bntkgd', p, vb)
    return o.reshape(bsz, L, B_HEADS * B_HEAD_DIM)


def hier_moe(x, w_group, b_group, w_router, b_router, w_gate, w_up, w_down):
    bsz, L, d = x.shape
    xt = x.reshape(-1, d)
    g_logits = (xt @ w_group).astype(jnp.float32) + b_group.astype(jnp.float32)
    g_prob = jax.nn.softmax(g_logits, axis=-1)
    g_sel = jnp.argmax(g_logits, axis=-1).astype(jnp.int32)
    g_w = jnp.take_along_axis(g_prob, g_sel[:, None], axis=-1)
    e_logits = ((xt @ w_router).astype(jnp.float32) + b_router.astype(jnp.float32))
    e_logits = e_logits.reshape(-1, N_GROUPS, EXPERTS_PER_GROUP)
    e_logits = jnp.take_along_axis(e_logits, g_sel[:, None, None], axis=1)[:, 0]
    e_prob = jax.nn.softmax(e_logits, axis=-1)
    top_p, top_i = lax.top_k(e_prob, EXPERT_TOPK)
    top_w = g_w * top_p / top_p.sum(-1, keepdims=True)
    expert_id = g_sel[:, None] * EXPERTS_PER_GROUP + top_i
    combine = (jax.nn.one_hot(expert_id, N_EXPERTS, dtype=jnp.float32) * top_w[..., None]).sum(1)
    combine = combine.astype(x.dtype)
    out = jnp.zeros_like(xt)
    for e in range(N_EXPERTS):
        h = jax.nn.silu(xt @ w_gate[e]) * (xt @ w_up[e])
        out = out + combine[:, e:e + 1] * (h @ w_down[e])
    return out.reshape(bsz, L, d)


def setup_inputs(seed: int = 0) -> dict:
    key = jax.random.key(seed)
    ks = jax.random.split(key, 20)
    f32 = jnp.float32

    def nrm(k, shape, scale):
        return jax.random.normal(k, shape, f32) * scale

    a_width = A_HEADS * A_HEAD_DIM
    b_width = B_HEADS * B_HEAD_DIM
    return {
        "x": nrm(ks[0], (BATCH, SEQ, D_MODEL), 1.0),
        "w_in": nrm(ks[1], (DEPTH, D_MODEL, IN_WIDTH), D_MODEL ** -0.5),
        "kv_norm_g": 1.0 + nrm(ks[2], (DEPTH, A_LAT), 0.02),
        "w_uv": nrm(ks[3], (DEPTH, A_HEADS, A_LAT, A_HEAD_DIM), A_LAT ** -0.5),
        "w_branch_a": nrm(ks[4], (DEPTH, a_width, D_MODEL), a_width ** -0.5),
        "sinks": nrm(ks[5], (DEPTH, B_HEADS), 0.5),
        "w_branch_b": nrm(ks[6], (DEPTH, b_width, D_MODEL), b_width ** -0.5),
        "w_out": nrm(ks[7], (DEPTH, D_MODEL, D_MODEL), BETA * D_MODEL ** -0.5),
        "rel_bias": nrm(ks[8], (N_BUCKETS, N_BIAS_HEADS), 0.5),
        "ln1_g": 1.0 + nrm(ks[9], (DEPTH, D_MODEL), 0.02),
        "ln1_b": nrm(ks[10], (DEPTH, D_MODEL), 0.02),
        "w_group": nrm(ks[11], (DEPTH, D_MODEL, N_GROUPS), D_MODEL ** -0.5),
        "b_group": nrm(ks[12], (DEPTH, N_GROUPS), 0.01),
        "w_router": nrm(ks[13], (DEPTH, D_MODEL, N_EXPERTS), D_MODEL ** -0.5),
        "b_router": nrm(ks[14], (DEPTH, N_EXPERTS), 0.01),
        "w_gate": nrm(ks[15], (DEPTH, N_EXPERTS, D_MODEL, D_EXPERT), D_MODEL ** -0.5),
        "w_up": nrm(ks[16], (DEPTH, N_EXPERTS, D_MODEL, D_EXPERT), D_MODEL ** -0.5),
        "w_down": nrm(ks[17], (DEPTH, N_EXPERTS, D_EXPERT, D_MODEL), BETA * D_EXPERT ** -0.5),
        "ln2_g": 1.0 + nrm(ks[18], (DEPTH, D_MODEL), 0.02),
        "ln2_b": nrm(ks[19], (DEPTH, D_MODEL), 0.02),
    }


def reference(x, w_in, kv_norm_g, w_uv, w_branch_a, sinks, w_branch_b, w_out, rel_bias,
              ln1_g, ln1_b, w_group, b_group, w_router, b_router, w_gate, w_up, w_down,
              ln2_g, ln2_b):
    bsz, L, _ = x.shape
    splits = _split_points()
    for layer in range(DEPTH):
        proj = x @ w_in[layer]
        (q_lat, c_kv, q_idx, k_idx, w_idx, q_b, k_b, v_b, gate_a, gate_b) = jnp.split(proj, splits, axis=-1)
        c_kv = rms_norm(c_kv, kv_norm_g[layer])
        y_a = dsa_branch(q_lat.reshape(bsz, L, A_HEADS, A_LAT), c_kv,
                         q_idx.reshape(bsz, L, IDX_HEADS, IDX_DIM), k_idx, w_idx,
                         w_uv[layer], rel_bias)
        y_b = swa_branch(q_b.reshape(bsz, L, B_HEADS, B_HEAD_DIM),
                         k_b.reshape(bsz, L, B_KV_HEADS, B_HEAD_DIM),
                         v_b.reshape(bsz, L, B_KV_HEADS, B_HEAD_DIM),
                         sinks[layer], rel_bias)
        merged = (jax.nn.sigmoid(gate_a) * (y_a @ w_branch_a[layer])
                  + jax.nn.sigmoid(gate_b) * (y_b @ w_branch_b[layer]))
        x = layer_norm(ALPHA * x + merged @ w_out[layer], ln1_g[layer], ln1_b[layer])
        ffn = hier_moe(x, w_group[layer], b_group[layer], w_router[layer], b_router[layer],
                       w_gate[layer], w_up[layer], w_down[layer])
        x = layer_norm(ALPHA * x + ffn, ln2_g[layer], ln2_b[layer])
    return x
```

```python
import math
import contextlib
import numpy as np
import concourse.bass as bass
import concourse.mybir as mybir
from concourse.bass_utils import run_bass_kernel_spmd

F32 = mybir.dt.float32
BF16 = mybir.dt.bfloat16
AF = mybir.ActivationFunctionType
ALU = mybir.AluOpType
AX = mybir.AxisListType

D = 1024
L = 2048
NT = 16
NEXP = 32
DE = 256
ALPHA = 2.0 ** 0.25
LN_EPS = 1e-5
RMS_EPS = 1e-6
ATT_SCALE = 128.0 ** -0.5
NEG = -30000.0
N_BISECT = 16
W1COLS = 2568
SB_BASE = 16640
SB_END = 229368


class Buf:
    __slots__ = ("name", "writer", "readers")

    def __init__(self, name):
        self.name = name
        self.writer = None
        self.readers = []


class Sched:
    ENGS = ("pe", "act", "dve", "pool", "sp")

    def __init__(self, nc, n_dma_sems=24):
        self.nc = nc
        self.ops = {e: [] for e in self.ENGS}
        self.cnt = {e: 0 for e in self.ENGS}
        self.seen = {e: {} for e in self.ENGS}
        self.n_dma_sems = n_dma_sems
        self.dma_i = {}
        self.dma_val = {}
        self.sems = {}

    def _deps(self, eng, reads, writes):
        need = {}

        def add(tok, skip_same):
            if tok is None:
                return
            e, key, val = tok
            if e == eng and skip_same:
                return
            if need.get(key, 0) < val:
                need[key] = val

        pe = eng == "pe"
        for b in reads:
            add(b.writer, pe)
        for b in writes:
            add(b.writer, pe)
            for r in b.readers:
                add(r, True)
        waits = []
        seen = self.seen[eng]
        for key, val in need.items():
            if seen.get(key, 0) < val:
                seen[key] = val
                waits.append((key, val))
        return waits

    def _commit(self, tok, reads, writes):
        for b in reads:
            b.readers.append(tok)
        for b in writes:
            b.writer = tok
            b.readers = []

    def group(self, eng, fns, reads=(), writes=()):
        waits = self._deps(eng, reads, writes)
        self.cnt[eng] += 1
        tok = (eng, "e_" + eng, self.cnt[eng])
        n = len(fns)
        for i, fn in enumerate(fns):
            self.ops[eng].append((fn, waits if i == 0 else (), ("e_" + eng, 1) if i == n - 1 else None))
        self._commit(tok, reads, writes)
        return tok

    def op(self, eng, fn, reads=(), writes=()):
        return self.group(eng, [fn], reads, writes)

    def dma(self, eng, fn, reads=(), writes=()):
        i = self.dma_i.get(eng, 0) % self.n_dma_sems
        self.dma_i[eng] = self.dma_i.get(eng, 0) + 1
        key = "d_%s_%d" % (eng, i)
        waits = list(self._deps(eng, reads, writes))
        prev = self.dma_val.get(key, 0)
        if prev > 0 and self.seen[eng].get(key, 0) < prev:
            self.seen[eng][key] = prev
            waits.append((key, prev))
        self.dma_val[key] = prev + 16
        tok = ("dma", key, self.dma_val[key])
        self.ops[eng].append((fn, waits, (key, 16)))
        self._commit(tok, reads, writes)
        return tok

    def barrier(self):
        targets = [("e_" + e, self.cnt[e]) for e in self.ENGS if self.cnt[e] > 0]
        targets += [(k, v) for k, v in self.dma_val.items() if v > 0]
        for e in self.ENGS:
            waits = []
            for key, val in targets:
                if key == "e_" + e and e != "pe":
                    pass
                if self.seen[e].get(key, 0) < val:
                    self.seen[e][key] = val
                    waits.append((key, val))
            if waits:
                self.ops[e].append((None, waits, None))

    def emit(self):
        nc = self.nc
        keys = ["e_" + e for e in self.ENGS] + sorted(self.dma_val.keys())
        with contextlib.ExitStack() as st:
            for k in keys:
                self.sems[k] = st.enter_context(nc.semaphore(k))
            block = st.enter_context(nc.Block())
            sems = self.sems

            def run(eng_name):
                def body(eng):
                    for fn, waits, inc in self.ops[eng_name]:
                        for key, val in waits:
                            eng.wait_ge(sems[key], val)
                        if fn is None:
                            continue
                        ins = fn(eng)
                        if inc is not None:
                            ins.then_inc(sems[inc[0]], inc[1])
                return body

            block.tensor(run("pe"))
            block.scalar(run("act"))
            block.vector(run("dve"))
            block.gpsimd(run("pool"))
            block.sync(run("sp"))


class Mem:
    def __init__(self, nc):
        self.nc = nc
        self.allocs = []

    def __call__(self, name, shape, dtype, off, life):
        esz = 4 if dtype == F32 else 2
        n = esz
        for s in shape[1:]:
            n *= s
        a0, a1 = SB_BASE + off, SB_BASE + off + n
        assert a1 <= SB_END, (name, a1)
        for (nm, b0, b1, lf) in self.allocs:
            if a0 < b1 and b0 < a1 and lf[0] <= life[1] and life[0] <= lf[1]:
                raise AssertionError("SBUF overlap %s vs %s" % (name, nm))
        self.allocs.append((name, a0, a1, life))
        return self.nc.alloc_sbuf_tensor_at(name, list(shape), dtype, offset=a0)


def t5_bucket_np(dist):
    n = np.maximum(dist, 0)
    nf = np.maximum(n, 1).astype(np.float32)
    large = 16 + (np.log(nf / np.float32(16)) / np.float32(math.log(128 / 16)) * np.float32(16)).astype(np.int32)
    large = np.minimum(large, 31)
    return np.where(n < 16, n, large).astype(np.int32)


def build_nc(debug=False, stop_after=99):
    nc = bass.Bass("TRN2", target_bir_lowering=False)

    def din(name, shape, dt=F32):
        return nc.dram_tensor(name, list(shape), dt, kind="ExternalInput").ap()

    xT_d = din("xT", [D, L])
    x_d = din("x", [L, D])
    w1_d = din("w1", [D, W1COLS])
    wg_d = din("wg", [D, 2048])
    kvg_d = din("kvg", [1, 128])
    wuv_d = din("wuv", [128, 512])
    wa_d = din("wa", [512, D])
    wb_d = din("wb", [512, D])
    wo_d = din("wo", [D, D])
    sinks_d = din("sinks", [1, 8])
    ln1g_d = din("ln1g", [1, D])
    ln1b_d = din("ln1b", [1, D])
    ln2g_d = din("ln2g", [1, D])
    ln2b_d = din("ln2b", [1, D])
    wr_d = din("wr", [D, 36])
    br_d = din("br", [1, 36])
    wgr_d = din("wgr", [NEXP * 128, 2048])
    wur_d = din("wur", [NEXP * 128, 2048])
    wdr_d = din("wdr", [NEXP * 128, 2048])
    wgb_d = nc.dram_tensor("wg_bf16", [NEXP * 128, 2048], BF16, kind="Internal").ap()
    wub_d = nc.dram_tensor("wu_bf16", [NEXP * 128, 2048], BF16, kind="Internal").ap()
    wdb_d = nc.dram_tensor("wd_bf16", [NEXP * 128, 2048], BF16, kind="Internal").ap()
    gates_d = nc.dram_tensor("gates_scr", [2048, L], BF16, kind="Internal").ap()
    xs_d = nc.dram_tensor("xs_scr", [64 * 128, D], BF16, kind="Internal").ap()
    ys_d = nc.dram_tensor("ys_scr", [64 * 128, D], F32, kind="Internal").ap()
    swab_d = din("swab", [4, 128, 512])
    dsab_d = din("dsab", [2, 128, 1024])
    c31_d = din("c31", [1, 8])
    out_d = nc.dram_tensor("out", [L, D], F32, kind="ExternalOutput").ap()
    dbg = {}
    if debug:
        for nm, shp in [("d_yb", [512, L]), ("d_ya", [512, L]), ("d_mergedT", [D, L]), ("d_acc", [L, D]),
                        ("d_comb", [L, 32]), ("d_ckvT", [128, L]), ("d_qlatT", [128, L])]:
            dbg[nm] = nc.dram_tensor(nm, shp, BF16 if nm in ("d_yb", "d_ya", "d_mergedT", "d_ckvT", "d_qlatT") else F32,
                                     kind="ExternalOutput").ap()

    S = Sched(nc)
    M = Mem(nc)
    _regs = {}

    def preg(e, val):
        if val not in _regs:
            _regs[val] = e.to_reg(val)
        return _regs[val]
    PS = [nc.alloc_psum_tensor("ps%d" % i, [128, 512], F32) for i in range(8)]
    Bps = [Buf("ps%d" % i) for i in range(8)]
    Bout = Buf("out")

    ident = M("ident", [128, 128], BF16, 0, (1, 7))
    gkv_bc = M("gkv_bc", [128, 128], F32, 256, (1, 7))
    esink = M("esink", [128, 8], F32, 768, (1, 7))
    negc31 = M("negc31", [128, 8], F32, 800, (1, 7))
    identf = M("identf", [128, 128], F32, 1024, (1, 7))
    causal01 = M("causal01", [128, 128], BF16, 1536, (1, 7))
    Bconst = Buf("const")

    S.op("pool", lambda e: e.memset(identf[:], 1.0), writes=[Bconst])
    S.op("pool", lambda e: e.affine_select(out=identf[:], in_=identf[:], pattern=[[1, 128]],
                                           compare_op=ALU.is_equal, fill=preg(e, 0.0), base=0, channel_multiplier=-1),
         reads=[Bconst], writes=[Bconst])
    S.op("pool", lambda e: e.tensor_copy(out=ident[:], in_=identf[:]), reads=[Bconst], writes=[Bconst])
    S.op("pool", lambda e: e.memset(causal01[:], 1.0), writes=[Bconst])
    S.op("pool", lambda e: e.affine_select(out=causal01[:], in_=causal01[:], pattern=[[1, 128]],
                                           compare_op=ALU.is_ge, fill=preg(e, 0.0), base=0, channel_multiplier=-1),
         reads=[Bconst], writes=[Bconst])
    S.dma("sp", lambda e: e.dma_start(out=gkv_bc[:], in_=kvg_d.to_broadcast([128, 128])), writes=[Bconst])
    S.dma("sp", lambda e: e.dma_start(out=esink[:], in_=sinks_d.to_broadcast([128, 8])), writes=[Bconst])
    S.dma("sp", lambda e: e.dma_start(out=negc31[:], in_=c31_d.to_broadcast([128, 8])), writes=[Bconst])
    S.op("act", lambda e: e.activation(out=esink[:], in_=esink[:], func=AF.Exp), reads=[Bconst], writes=[Bconst])
    S.op("dve", lambda e: e.tensor_scalar(out=negc31[:], in0=negc31[:], scalar1=-1.0, scalar2=None, op0=ALU.mult),
         reads=[Bconst], writes=[Bconst])

    xT = M("xT", [128, 8, L], BF16, 2048, (1, 1))
    o = 34816
    qlatT = M("qlatT", [128, 8, L], BF16, o, (1, 2.5)); o += 32768
    qidxT = M("qidxT", [128, 4, L], BF16, o, (1, 2.5)); o += 16384
    qbT = M("qbT", [128, 4, L], BF16, o, (1, 2)); o += 16384
    kidxT = M("kidxT", [128, L], BF16, o, (1, 2.5)); o += 4096
    kbT = M("kbT", [128, L], BF16, o, (1, 2)); o += 4096
    ckvT = M("ckvT", [128, L], BF16, o, (1, 2.5)); o += 4096
    kvW = M("kvW", [128, NT, 8, 65], BF16, o, (1, 2.5)); o += 16640
    vaug = M("vaug", [128, NT, 2, 65], BF16, o, (1, 2)); o += 4160
    widx = M("widx", [128, NT, 8], F32, o, (1, 2.5)); o += 512
    assert o == 133952
    R3 = 133952
    w1b = M("w1b", [128, 8, W1COLS], BF16, R3, (1, 1))
    wuvs = M("wuvs", [128, 512], F32, R3 + 41088, (1, 1))
    ckvtok = M("ckvtok", [128, NT, 128], BF16, R3 + 43136, (1, 1))
    wuvb = M("wuvb", [128, 512], BF16, R3 + 47232, (1, 1))
    p1tmp = M("p1tmp", [128, 128], F32, R3 + 48256, (1, 1))
    p1junk = M("p1junk", [128, 128], F32, R3 + 48768, (1, 1))

    BxT = [Buf("xT%d" % k) for k in range(8)]
    Bw1 = [Buf("w1_%d" % c) for c in range(6)]
    w1v = w1_d.rearrange("(k p) n -> p k n", p=128)
    xTv = xT_d.rearrange("(k p) n -> p k n", p=128)
    for k in range(8):
        S.dma("pool", lambda e, k=k: e.dma_start(out=xT[:, k, :], in_=xTv[:, k, :]), writes=[BxT[k]])
    w1blocks = [(0, 512), (512, 1024), (1024, 1536), (1536, 2048), (2048, 2304), (2304, W1COLS)]
    for c, (c0, c1) in enumerate(w1blocks):
        S.dma("pool", lambda e, c0=c0, c1=c1: e.dma_start(out=w1b[:, :, c0:c1], in_=w1v[:, :, c0:c1]),
              writes=[Bw1[c]])
    wgs = [M("wgs0", [128, 8, 512], BF16, 184320, (1, 1)), M("wgs1", [128, 8, 512], BF16, 192512, (1, 1))]
    gst = M("gst", [128, 2, 512], BF16, 200704, (1, 1))
    Bwgs = [Buf("wgs0"), Buf("wgs1")]; Bgst = [Buf("gst0"), Buf("gst1")]; Bgates = Buf("gates_d")
    wgv = wg_d.rearrange("(k p) n -> p k n", p=128)

    def load_wgs(c):
        wb_ = c % 2
        S.dma("pool", lambda e: e.dma_start(out=wgs[wb_][:], in_=wgv[:, :, c * 512:(c + 1) * 512]), writes=[Bwgs[wb_]])

    load_wgs(0)
    load_wgs(1)
    Bwuv = Buf("wuv")
    S.dma("sp", lambda e: e.dma_start(out=wuvs[:], in_=wuv_d), writes=[Bwuv])
    S.op("dve", lambda e: e.tensor_copy(out=wuvb[:], in_=wuvs[:]), reads=[Bwuv], writes=[Bwuv])

    def w1buf(col):
        for c, (c0, c1) in enumerate(w1blocks):
            if c0 <= col < c1:
                return Bw1[c]

    Bqlat = [Buf("qlat%d" % h) for h in range(8)]
    Bqidx = Buf("qidx"); Bqb = Buf("qb"); Bkidx = Buf("kidx"); Bkb = Buf("kb")
    Bckv = Buf("ckvT"); BkvW = Buf("kvW"); Bvaug = Buf("vaug"); Bwidx = Buf("widx"); Bcktok = Buf("ckvtok")

    fm_tiles = []
    for h in range(8):
        fm_tiles.append((lambda tb, h=h: qlatT[:, h, tb * 512:(tb + 1) * 512], ATT_SCALE, Bqlat[h]))
    for j in range(4):
        fm_tiles.append((lambda tb, j=j: qidxT[:, j, tb * 512:(tb + 1) * 512], 1.0, Bqidx))
    fm_tiles.append((lambda tb: kidxT[:, tb * 512:(tb + 1) * 512], 1.0, Bkidx))
    for j in range(4):
        fm_tiles.append((lambda tb, j=j: qbT[:, j, tb * 512:(tb + 1) * 512], 0.125, Bqb))
    fm_tiles.append((lambda tb: kbT[:, tb * 512:(tb + 1) * 512], 1.0, Bkb))
    ev = 0
    for ti, (dst, scale, bdst) in enumerate(fm_tiles):
        for tb in range(4):
            bank = ev % 4
            fns = []
            for k in range(8):
                fns.append(lambda e, k=k, ti=ti, tb=tb, bank=bank: e.matmul(
                    PS[bank][:], lhsT=w1b[:, k, ti * 128:(ti + 1) * 128], rhs=xT[:, k, tb * 512:(tb + 1) * 512],
                    start=(k == 0), stop=(k == 7)))
            S.group("pe", fns, reads=BxT + [w1buf(ti * 128)], writes=[Bps[bank]])
            if ev % 2 == 0:
                S.op("act", lambda e, dst=dst, tb=tb, bank=bank, scale=scale: e.activation(
                    out=dst(tb), in_=PS[bank][:], func=AF.Copy, scale=scale), reads=[Bps[bank]], writes=[bdst])
            else:
                S.op("dve", lambda e, dst=dst, tb=tb, bank=bank, scale=scale: e.tensor_scalar(
                    out=dst(tb), in0=PS[bank][:], scalar1=scale, scalar2=None, op0=ALU.mult),
                    reads=[Bps[bank]], writes=[bdst])
            ev += 1

    S.op("pool", lambda e: e.memset(vaug[:], 1.0), writes=[Bvaug])
    S.op("pool", lambda e: e.memset(kvW[:], 1.0), writes=[BkvW])
    Bp1tmp = Buf("p1tmp"); Bp1junk = Buf("p1junk")
    for tt in range(NT):
        bank = 4 + (tt % 2)
        fns = []
        for k in range(8):
            fns.append(lambda e, k=k, tt=tt, bank=bank: e.matmul(
                PS[bank][:, 0:264], lhsT=xT[:, k, tt * 128:(tt + 1) * 128], rhs=w1b[:, k, 2304:2568],
                start=(k == 0), stop=(k == 7)))
        S.group("pe", fns, reads=BxT + [Bw1[5]], writes=[Bps[bank]])
        S.op("act", lambda e, tt=tt, bank=bank: e.activation(
            out=p1junk[:, 0:128], in_=PS[bank][:, 0:128], func=AF.Square, accum_out=p1tmp[:, tt:tt + 1]),
            reads=[Bps[bank]], writes=[Bp1junk, Bp1tmp])
        S.op("act", lambda e, tt=tt, bank=bank: e.activation(
            out=vaug[:, tt, :, 0:64], in_=PS[bank][:, 128:256].rearrange("p (g d) -> p g d", g=2), func=AF.Copy),
            reads=[Bps[bank]], writes=[Bvaug])
        S.op("act", lambda e, tt=tt, bank=bank: e.activation(
            out=widx[:, tt, :], in_=PS[bank][:, 256:264], func=AF.Copy), reads=[Bps[bank]], writes=[Bwidx])
        S.op("dve", lambda e, tt=tt: e.tensor_scalar(
            out=p1tmp[:, 16 + tt:17 + tt], in0=p1tmp[:, tt:tt + 1], scalar1=1.0 / 128.0, scalar2=RMS_EPS,
            op0=ALU.mult, op1=ALU.add), reads=[Bp1tmp], writes=[Bp1tmp])
        S.op("act", lambda e, tt=tt: e.activation(
            out=p1tmp[:, 16 + tt:17 + tt], in_=p1tmp[:, 16 + tt:17 + tt], func=AF.Sqrt),
            reads=[Bp1tmp], writes=[Bp1tmp])
        S.op("dve", lambda e, tt=tt: e.reciprocal(
            out=p1tmp[:, 32 + tt:33 + tt], in_=p1tmp[:, 16 + tt:17 + tt]), reads=[Bp1tmp], writes=[Bp1tmp])
        S.op("dve", lambda e, tt=tt, bank=bank: e.scalar_tensor_tensor(
            out=ckvtok[:, tt, :], in0=PS[bank][:, 0:128], scalar=p1tmp[:, 32 + tt:33 + tt], in1=gkv_bc[:],
            op0=ALU.mult, op1=ALU.mult), reads=[Bps[bank], Bp1tmp, Bconst], writes=[Bcktok])
        tb_ = 6 + (tt % 2)
        S.op("pe", lambda e, tt=tt, tb_=tb_: e.transpose(
            out=PS[tb_][:].bitcast(BF16)[:, 0:128], in_=ckvtok[:, tt, :], identity=ident[:]),
            reads=[Bcktok, Bconst], writes=[Bps[tb_]])
        S.op("dve", lambda e, tt=tt, tb_=tb_: e.tensor_copy(
            out=ckvT[:, tt * 128:(tt + 1) * 128], in_=PS[tb_][:].bitcast(BF16)[:, 0:128]),
            reads=[Bps[tb_]], writes=[Bckv])
        S.op("pe", lambda e, tt=tt, bank=bank: e.matmul(
            PS[bank][:], lhsT=ckvT[:, tt * 128:(tt + 1) * 128], rhs=wuvb[:], start=True, stop=True),
            reads=[Bckv, Bwuv], writes=[Bps[bank]])
        S.op("act", lambda e, tt=tt, bank=bank: e.activation(
            out=kvW[:, tt, :, 0:64], in_=PS[bank][:].rearrange("p (h d) -> p h d", h=8), func=AF.Copy),
            reads=[Bps[bank]], writes=[BkvW])


    def gen_gates():
        gi_ = 0
        for c in range(4):
            wb_ = c % 2
            if c >= 1 and c + 1 < 4:
                load_wgs(c + 1)
            for j in range(4):
                for tb in range(4):
                    bank = 5 + (gi_ % 2)
                    sb_ = gi_ % 2
                    gi_ += 1
                    fns = [lambda e, k=k, j=j, tb=tb, bank=bank, wb_=wb_: e.matmul(
                        PS[bank][:], lhsT=wgs[wb_][:, k, j * 128:(j + 1) * 128], rhs=xT[:, k, tb * 512:(tb + 1) * 512],
                        start=(k == 0), stop=(k == 7)) for k in range(8)]
                    S.group("pe", fns, reads=BxT + [Bwgs[wb_]], writes=[Bps[bank]])
                    S.op("act", lambda e, bank=bank, sb_=sb_: e.activation(out=gst[:, sb_, :], in_=PS[bank][:], func=AF.Sigmoid),
                         reads=[Bps[bank]], writes=[Bgst[sb_]])
                    r0 = c * 512 + j * 128
                    S.dma("sp", lambda e, r0=r0, tb=tb, sb_=sb_: e.dma_start(
                        out=gates_d[r0:r0 + 128, tb * 512:(tb + 1) * 512], in_=gst[:, sb_, :]),
                        reads=[Bgst[sb_]], writes=[Bgates])
                    yield

    for _ in gen_gates():
        pass
    if debug:
        S.dma("sp", lambda e: e.dma_start(out=dbg["d_ckvT"], in_=ckvT[:]), reads=[Bckv], writes=[Bout])
        S.dma("sp", lambda e: e.dma_start(out=dbg["d_qlatT"], in_=qlatT[:, 0, :]), reads=Bqlat, writes=[Bout])
    S.barrier()
    if stop_after <= 1:
        S.emit()
        return nc

    yaT = M("yaT", [128, 4, L], BF16, 179904, (2, 3))
    ybT = M("ybT", [128, 4, L], BF16, 179904 + 16384, (2, 3))
    Eswa = M("Eswa", [128, 4, 512], BF16, R3, (2, 2))
    ytokA = M("ytokA", [128, 2, 512], BF16, R3 + 4096, (2, 2))
    st2a = M("st2a", [128, 128], F32, R3 + 6144, (2, 2))
    Edsa = M("Edsa", [128, 2, 1024], BF16, R3 + 20480, (2, 2.5))
    eTb = M("eTb", [128, 4, 512], BF16, R3 + 32768, (2, 2.5))
    pTb = M("pTb", [128, 4, 512], BF16, R3 + 36864, (2, 2.5))
    scr = M("scr", [128, 1024], F32, R3 + 40960, (2, 2))
    BEswa = Buf("Eswa"); BEdsa = Buf("Edsa"); Bscr = Buf("scr")
    BeT = [Buf("eT%d" % i) for i in range(4)]
    BpT = [Buf("pT%d" % i) for i in range(4)]
    BytokA = [Buf("ytokA0"), Buf("ytokA1")]
    Bst2a = Buf("st2a")
    ByaT = Buf("yaT"); BybT = Buf("ybT")

    for idx in range(4):
        typ = idx // 2
        S.dma("sp", lambda e, idx=idx: e.dma_start(out=scr[:, 0:512], in_=swab_d[idx]), writes=[Bscr])
        S.op("act", lambda e, idx=idx: e.activation(out=Eswa[:, idx, :], in_=scr[:, 0:512], func=AF.Exp),
             reads=[Bscr], writes=[BEswa])
        for j in range(4):
            if typ == 1:
                S.op("pool", lambda e, idx=idx, j=j: e.tensor_tensor(
                    out=Eswa[:, idx, j * 128:(j + 1) * 128], in0=Eswa[:, idx, j * 128:(j + 1) * 128],
                    in1=causal01[:], op=ALU.mult), reads=[BEswa, Bconst], writes=[BEswa])
            else:
                S.op("pool", lambda e, idx=idx, j=j: e.affine_select(
                    out=Eswa[:, idx, j * 128:(j + 1) * 128], in_=Eswa[:, idx, j * 128:(j + 1) * 128],
                    pattern=[[-1, 128]], compare_op=ALU.is_ge, fill=preg(e, 0.0), base=-1, channel_multiplier=1),
                    reads=[BEswa], writes=[BEswa])
    for typ in range(2):
        S.dma("sp", lambda e, typ=typ: e.dma_start(out=scr[:], in_=dsab_d[typ]), writes=[Bscr])
        for h in range(8):
            S.op("act", lambda e, typ=typ, h=h: e.activation(
                out=Edsa[:, typ, h * 128:(h + 1) * 128], in_=scr[:, h * 128:(h + 1) * 128], func=AF.Exp,
                bias=negc31[:, h:h + 1], scale=1.0), reads=[Bscr, Bconst], writes=[BEdsa])
            if typ == 1:
                S.op("pool", lambda e, h=h: e.tensor_tensor(
                    out=Edsa[:, 1, h * 128:(h + 1) * 128], in0=Edsa[:, 1, h * 128:(h + 1) * 128],
                    in1=causal01[:], op=ALU.mult), reads=[BEdsa, Bconst], writes=[BEdsa])

    def ytok_to_T(nblk, ysrc, ybufB, dstT, bdst):
        pv = PS[7][:].bitcast(BF16)
        fns = [lambda e, c=c: e.transpose(out=pv[:, c * 128:(c + 1) * 128],
                                         in_=ysrc[:, c * 128:(c + 1) * 128], identity=ident[:])
               for c in range(4)]
        S.group("pe", fns, reads=[ybufB, Bconst], writes=[Bps[7]])
        S.op("act", lambda e: e.activation(
            out=dstT[:, :, nblk * 128:(nblk + 1) * 128], in_=pv[:, 0:512].rearrange("p (c t) -> p c t", c=4),
            func=AF.Copy), reads=[Bps[7]], writes=[bdst])

    items = []
    for n in range(NT):
        for g in range(2):
            chunks = ([(n - 1, 0)] if n > 0 else []) + [(n, 1)]
            for ci, (kb, typ) in enumerate(chunks):
                items.append((n, g, kb, typ, ci == 0, ci == len(chunks) - 1))

    def swa_stage1(idx):
        n, g, kb, typ, first, last = items[idx]
        r = idx % 4
        lb = idx % 3
        S.op("pe", lambda e: e.matmul(
            PS[lb][:], lhsT=kbT[g * 64:(g + 1) * 64, kb * 128:(kb + 1) * 128],
            rhs=qbT[g * 64:(g + 1) * 64, :, n * 128:(n + 1) * 128], start=True, stop=True),
            reads=[Bkb, Bqb], writes=[Bps[lb]])
        S.op("act", lambda e: e.activation(out=eTb[:, r, :], in_=PS[lb][:], func=AF.Exp),
             reads=[Bps[lb]], writes=[BeT[r]])
        S.op("dve", lambda e: e.tensor_tensor(
            out=pTb[:, r, :], in0=eTb[:, r, :], in1=Eswa[:, typ * 2 + g, :], op=ALU.mult),
            reads=[BeT[r], BEswa], writes=[BpT[r]])

    def swa_stage2(idx):
        n, g, kb, typ, first, last = items[idx]
        r = idx % 4
        ybuf = n % 2
        obank = 3 + g
        fns = [lambda e, j=j: e.matmul(
            PS[obank][:, j * 65:(j + 1) * 65], lhsT=pTb[:, r, j * 128:(j + 1) * 128],
            rhs=vaug[:, kb, g, :], start=(first and j == 0), stop=(last and j == 3),
            skip_group_check=True) for j in range(4)]
        S.group("pe", fns, reads=[BpT[r], Bvaug], writes=[Bps[obank]])
        if not last:
            return
        ov = PS[obank][:, 0:260].rearrange("p (j c) -> p j c", j=4)
        c0 = ybuf * 16 + g * 4
        S.op("dve", lambda e: e.tensor_tensor(
            out=st2a[:, c0:c0 + 4].rearrange("p (j o) -> p j o", o=1), in0=ov[:, :, 64:65],
            in1=esink[:, g * 4:(g + 1) * 4].rearrange("p (j o) -> p j o", o=1), op=ALU.add),
            reads=[Bps[obank], Bconst], writes=[Bst2a])
        S.op("dve", lambda e: e.reciprocal(out=st2a[:, 32 + c0:32 + c0 + 4], in_=st2a[:, c0:c0 + 4]),
             reads=[Bst2a], writes=[Bst2a])
        S.op("dve", lambda e: e.tensor_tensor(
            out=ytokA[:, ybuf, g * 256:(g + 1) * 256].rearrange("p (j d) -> p j d", j=4), in0=ov[:, :, 0:64],
            in1=st2a[:, 32 + c0:32 + c0 + 4].rearrange("p (j o) -> p j o", o=1).to_broadcast([128, 4, 64]),
            op=ALU.mult), reads=[Bps[obank], Bst2a], writes=[BytokA[ybuf]])
        if g == 1:
            ytok_to_T(n, ytokA[:, ybuf, :], BytokA[ybuf], ybT, BybT)

    LAG = 2
    for idx in range(len(items) + LAG):
        if idx < len(items):
            swa_stage1(idx)
        if idx >= LAG:
            swa_stage2(idx - LAG)
    if debug:
        S.dma("sp", lambda e: e.dma_start(out=dbg["d_yb"].rearrange("(c p) t -> p c t", p=128), in_=ybT[:]),
              reads=[BybT], writes=[Bout])
    S.barrier()

    QB = 83968
    scoresA = M("scoresA", [128, L], F32, R3, (2.5, 2.5))
    scoresB = M("scoresB", [128, L], F32, QB, (2.5, 2.5))
    scoresC = M("scoresC", [128, L], F32, 2048, (2.5, 2.5))
    scoresD = M("scoresD", [128, L], F32, 2048 + 8192, (2.5, 2.5))
    maskbA = M("maskbA", [128, L], BF16, R3 + 8192, (2.5, 2.5))
    maskbB = M("maskbB", [128, L], BF16, 2048 + 16384, (2.5, 2.5))
    osb = M("osb", [128, 2, 520], F32, 129280, (2.5, 2.5))
    maskT_lo = M("maskT_lo", [128, 2, NT, 128], BF16, R3 + 12288, (2.5, 2.5))
    maskT_hi = M("maskT_hi", [128, 2, NT, 128], BF16, 2048 + 20480, (2.5, 2.5))
    rbuf = M("rbuf", [128, 4, 512], BF16, R3 + 40960, (2.5, 2.5))
    dg = M("dg", [128, 2, 8, 128], BF16, QB + 8192, (2.5, 2.5))
    ytokB = M("ytokB", [128, 2, 512], BF16, QB + 12288, (2.5, 2.5))
    st2 = M("st2", [128, 4, 64], F32, QB + 14336, (2.5, 2.5))
    steps = M("steps", [128, 32], F32, QB + 15360, (2.5, 2.5))
    sd0 = M("sd0", [128, 4, 32], F32, QB + 15488, (2.5, 2.5))
    zt = M("zt", [128, D], BF16, R3 + 24576, (2.5, 2.5))
    Bzero = Buf("zero")
    S.op("pool", lambda e: e.memset(zt[:], 0.0), writes=[Bzero])
    xs_v = xs_d.rearrange("(j p) n -> p j n", p=128)
    for j0 in range(0, 64, 8):
        S.dma("sp", lambda e, j0=j0: e.dma_start(
            out=xs_v[:, j0:j0 + 8, :], in_=zt[:].rearrange("p (o n) -> p o n", o=1).to_broadcast([128, 8, D])),
            reads=[Bzero], writes=[Bzero])
    Bwbf = Buf("wbf16")
    bg_jobs = []
    for r0 in range(0, NEXP * 128, 512):
        for src_, dst_ in ((wgr_d, wgb_d), (wur_d, wub_d), (wdr_d, wdb_d)):
            bg_jobs.append((src_, dst_, r0))

    def bg_convert(n=1):
        for _ in range(n):
            if not bg_jobs:
                return
            src_, dst_, r0 = bg_jobs.pop(0)
            S.dma("pool", lambda e, src_=src_, dst_=dst_, r0=r0: e.dma_start(
                out=dst_[r0:r0 + 512, :], in_=src_[r0:r0 + 512, :]), writes=[Bwbf])
    kblk0 = M("kblk0", [128, L], BF16, 104448, (2.5, 2.5))
    kblk1 = M("kblk1", [128, L], BF16, 2048 + 28672, (2.5, 2.5))
    kblk = [kblk0, kblk1]
    Bkblk = Buf("kblk")
    S.op("pool", lambda e: e.memset(kblk0[:], 0.0), writes=[Bkblk])
    S.op("pool", lambda e: e.memset(kblk1[:], 0.0), writes=[Bkblk])
    S.op("dve", lambda e: e.tensor_copy(out=kblk0[0:64, :], in_=kidxT[0:64, :]), reads=[Bkidx, Bkblk], writes=[Bkblk])
    S.op("dve", lambda e: e.tensor_copy(out=kblk1[64:128, :], in_=kidxT[64:128, :]), reads=[Bkidx, Bkblk], writes=[Bkblk])
    scoresX = [scoresA, scoresB, scoresC, scoresD]
    maskbX = [maskbA, maskbB]
    Bscore = [Buf("scores%d" % i) for i in range(4)]
    BmaskX = [Buf("mask0"), Buf("mask1")]; BmaskT = [Buf("maskT%d" % i) for i in range(4)]
    Brb = [Buf("rb%d" % i) for i in range(4)]
    Bdg = [Buf("dg0"), Buf("dg1")]
    BytokB = [Buf("ytokB0"), Buf("ytokB1")]
    Bbis = [Buf("bis%d" % i) for i in range(4)]
    Bst = [Buf("st%d" % i) for i in range(4)]
    Bosb = [Buf("osb0"), Buf("osb1")]
    Bsteps = Buf("steps")
    for k in range(N_BISECT):
        S.op("pool", lambda e, k=k: e.memset(steps[:, k:k + 1], 2.0 ** -(k + 1)), writes=[Bsteps])
    rotA = {"s1": 0, "rb": 0}

    def genS(i):
        Si = (i + 1) * 128
        sb = i % 2
        q4 = i % 4
        sc_t = scoresX[q4]
        for h in range(8):
            S.op("act", lambda e, h=h: e.activation(
                out=dg[:, sb, h, :], in_=ident[:], func=AF.Copy, scale=widx[:, i, h:h + 1]),
                reads=[Bconst, Bwidx], writes=[Bdg[sb]])
        yield
        nsc = (Si + 511) // 512
        stepsS = [(sc, h) for sc in range(nsc) for h in range(8)]
        slots = {}

        def S1(n):
            sc, h = stepsS[n]
            c0, c1 = sc * 512, min(Si, sc * 512 + 512)
            w = c1 - c0
            rr = rotA["rb"] % 4
            ab = 2 + (rotA["rb"] % 2)
            rotA["rb"] += 1
            slots[n] = rr
            hp = (h % 2) * 64
            S.op("pe", lambda e: e.matmul(
                PS[ab][:, 0:w], lhsT=qidxT[:, h // 2, i * 128:(i + 1) * 128],
                rhs=kblk[h % 2][:, c0:c1], start=True, stop=True),
                reads=[Bqidx, Bkblk], writes=[Bps[ab]])
            S.op("act", lambda e: e.activation(
                out=rbuf[:, rr, 0:w], in_=PS[ab][:, 0:w], func=AF.Relu), reads=[Bps[ab]], writes=[Brb[rr]])

        def S2(n):
            sc, h = stepsS[n]
            c0, c1 = sc * 512, min(Si, sc * 512 + 512)
            w = c1 - c0
            rr = slots[n]
            S.op("pe", lambda e: e.matmul(
                PS[6][:, 0:w], lhsT=dg[:, sb, h, :], rhs=rbuf[:, rr, 0:w], start=(h == 0), stop=(h == 7)),
                reads=[Bdg[sb], Brb[rr]], writes=[Bps[6]])
            if h == 7:
                S.op("act", lambda e: e.activation(out=sc_t[:, c0:c1], in_=PS[6][:, 0:w], func=AF.Copy),
                     reads=[Bps[6]], writes=[Bscore[q4]])

        S1(0)
        for n in range(len(stepsS)):
            if n + 1 < len(stepsS):
                S1(n + 1)
            S2(n)
            yield

    def genB(i):
        Si = (i + 1) * 128
        sb = i % 2
        q4 = i % 4
        sc_t = scoresX[q4]
        maskb = maskbX[sb]
        maskT = maskT_lo if q4 < 2 else maskT_hi
        Bmask = BmaskX[sb]
        stv = st2[:, q4, :]
        BB = [Bbis[q4]]
        S.op("dve", lambda e: e.tensor_reduce(out=stv[:, 0:1], in_=sc_t[:, 0:Si], axis=AX.X, op=ALU.max),
             reads=[Bscore[q4]], writes=BB)
        yield
        S.op("dve", lambda e: e.tensor_reduce(out=stv[:, 1:2], in_=sc_t[:, 0:Si], axis=AX.X, op=ALU.min),
             reads=[Bscore[q4]] + BB, writes=BB)
        yield
        S.op("dve", lambda e: e.scalar_tensor_tensor(
            out=stv[:, 2:3], in0=stv[:, 0:1], scalar=1.0, in1=stv[:, 1:2], op0=ALU.add, op1=ALU.subtract),
            reads=BB, writes=BB)
        S.op("dve", lambda e: e.tensor_scalar(
            out=sd0[:, q4, 0:N_BISECT], in0=steps[:, 0:N_BISECT], scalar1=stv[:, 2:3], scalar2=None, op0=ALU.mult),
            reads=BB + [Bsteps], writes=BB)
        S.op("dve", lambda e: e.tensor_tensor(out=stv[:, 3:4], in0=sd0[:, q4, 0:1], in1=stv[:, 1:2], op=ALU.add),
             reads=BB, writes=BB)
        S.op("pool", lambda e: e.affine_select(
            out=sc_t[:, i * 128:(i + 1) * 128], in_=sc_t[:, i * 128:(i + 1) * 128], pattern=[[-1, 128]],
            compare_op=ALU.is_ge, fill=preg(e, -1.0e30), base=0, channel_multiplier=1),
            reads=[Bscore[q4]] + BB, writes=[Bscore[q4]])
        yield
        for it in range(N_BISECT):
            S.op("dve", lambda e: e.tensor_scalar(
                out=maskb[:, 0:Si], in0=sc_t[:, 0:Si], scalar1=stv[:, 3:4], scalar2=None,
                op0=ALU.is_ge, op1=ALU.add, accum_out=stv[:, 4:5]),
                reads=[Bscore[q4]] + BB, writes=[Bmask] + BB)
            yield
            S.op("dve", lambda e: e.tensor_scalar(
                out=stv[:, 5:6], in0=stv[:, 4:5], scalar1=255.5, scalar2=0.5, op0=ALU.is_ge, op1=ALU.subtract),
                reads=BB, writes=BB)
            S.op("dve", lambda e, it=it: e.scalar_tensor_tensor(
                out=stv[:, 3:4], in0=stv[:, 5:6], scalar=sd0[:, q4, it:it + 1], in1=stv[:, 3:4],
                op0=ALU.mult, op1=ALU.add), reads=BB, writes=BB)
            yield
        S.op("dve", lambda e: e.scalar_tensor_tensor(
            out=stv[:, 6:7], in0=sd0[:, q4, N_BISECT - 1:N_BISECT], scalar=-0.5, in1=stv[:, 3:4],
            op0=ALU.mult, op1=ALU.add), reads=BB, writes=BB)
        S.op("dve", lambda e: e.tensor_scalar(
            out=maskb[:, 0:Si], in0=sc_t[:, 0:Si], scalar1=stv[:, 6:7], scalar2=None, op0=ALU.is_ge),
            reads=[Bscore[q4]] + BB, writes=[Bmask])
        yield
        pv = PS[7][:].bitcast(BF16)
        for q0 in range(0, i + 1, 4):
            q1 = min(i + 1, q0 + 4)
            fns = [lambda e, kb=kb, q0=q0: e.transpose(
                out=pv[:, (kb - q0) * 128:(kb - q0 + 1) * 128], in_=maskb[:, kb * 128:(kb + 1) * 128],
                identity=ident[:]) for kb in range(q0, q1)]
            S.group("pe", fns, reads=[Bmask, Bconst], writes=[Bps[7]])
            S.op("act", lambda e, q0=q0, q1=q1: e.activation(
                out=maskT[:, sb, q0:q1, :], in_=pv[:, 0:(q1 - q0) * 128].rearrange("p (c t) -> p c t", t=128),
                func=AF.Identity, scale=100.0, bias=-100.0), reads=[Bps[7]], writes=[BmaskT[q4]])
            yield

    def genA(i):
        sb = i % 2
        maskT = maskT_lo if (i % 4) < 2 else maskT_hi
        pairs = [(kb, hg) for kb in range(i + 1) for hg in range(2)]
        info = {}

        def stage1(pi):
            kb, hg = pairs[pi]
            near = kb >= i - 1
            typ = 1 if kb == i else 0
            r = rotA["s1"] % 4
            lb = rotA["s1"] % 2
            rotA["s1"] += 1
            info[pi] = r
            masked = i >= 2
            fns = [lambda e: e.matmul(
                PS[lb][:], lhsT=ckvT[:, kb * 128:(kb + 1) * 128],
                rhs=qlatT[:, hg * 4:(hg + 1) * 4, i * 128:(i + 1) * 128], start=True, stop=(not masked),
                skip_group_check=True)]
            rd = [Bckv] + Bqlat[hg * 4:(hg + 1) * 4]
            if masked:
                for j in range(4):
                    fns.append(lambda e, j=j: e.matmul(
                        PS[lb][:, j * 128:(j + 1) * 128], lhsT=ident[:], rhs=maskT[:, sb, kb, :], start=False,
                        stop=(j == 3), skip_group_check=True))
                rd = rd + [BmaskT[i % 4], Bconst]
            S.group("pe", fns, reads=rd, writes=[Bps[lb]])
            if near:
                S.op("act", lambda e: e.activation(out=eTb[:, r, :], in_=PS[lb][:], func=AF.Exp),
                     reads=[Bps[lb]], writes=[BeT[r]])
                S.op("pool", lambda e: e.tensor_tensor(out=pTb[:, r, :], in0=eTb[:, r, :],
                                                       in1=Edsa[:, typ, hg * 512:(hg + 1) * 512], op=ALU.mult),
                     reads=[BeT[r], BEdsa], writes=[BpT[r]])
            else:
                S.op("act", lambda e: e.activation(out=pTb[:, r, :], in_=PS[lb][:], func=AF.Exp),
                     reads=[Bps[lb]], writes=[BpT[r]])

        def stage2(pi):
            kb, hg = pairs[pi]
            r = info[pi]
            obank = 4 + hg
            fns = [lambda e, j=j: e.matmul(
                PS[obank][:, j * 65:(j + 1) * 65], lhsT=pTb[:, r, j * 128:(j + 1) * 128],
                rhs=kvW[:, kb, hg * 4 + j, :], start=(kb == 0 and j == 0), stop=(kb == i and j == 3),
                skip_group_check=True) for j in range(4)]
            S.group("pe", fns, reads=[BpT[r], BkvW], writes=[Bps[obank]])

        LAGA = 1
        for pi in range(len(pairs) + LAGA):
            if pi < len(pairs):
                stage1(pi)
            if pi >= LAGA:
                stage2(pi - LAGA)
            yield
        for hg in range(2):
            obank = 4 + hg
            S.op("act", lambda e, hg=hg, obank=obank: e.activation(
                out=osb[:, sb, hg * 260:(hg + 1) * 260], in_=PS[obank][:, 0:260], func=AF.Copy),
                reads=[Bps[obank]], writes=[Bosb[sb]])
        yield

        def finish():
            q4 = i % 4
            for hg in range(2):
                ov = osb[:, sb, hg * 260:(hg + 1) * 260].rearrange("p (j c) -> p j c", j=4)
                c0 = 32 + hg * 4
                S.op("dve", lambda e, ov=ov, c0=c0: e.reciprocal(
                    out=st2[:, q4, c0:c0 + 4].rearrange("p (j o) -> p j o", o=1), in_=ov[:, :, 64:65]),
                    reads=[Bosb[sb]], writes=[Bst[q4]])
                S.op("dve", lambda e, ov=ov, c0=c0, hg=hg: e.tensor_tensor(
                    out=ytokB[:, sb, hg * 256:(hg + 1) * 256].rearrange("p (j d) -> p j d", j=4), in0=ov[:, :, 0:64],
                    in1=st2[:, q4, c0:c0 + 4].rearrange("p (j o) -> p j o", o=1).to_broadcast([128, 4, 64]),
                    op=ALU.mult), reads=[Bosb[sb], Bst[q4]], writes=[BytokB[sb]])
            ytok_to_T(i, ytokB[:, sb, :], BytokB[sb], yaT, ByaT)
        post_round.append(finish)

    tick = {"n": 0}

    def interleave(gens):
        gens = [g for g in gens if g is not None]
        while gens:
            tick["n"] += 1
            if tick["n"] % 25 == 0:
                bg_convert(1)
            for g in list(gens):
                try:
                    next(g)
                except StopIteration:
                    gens.remove(g)

    import itertools
    Bjunk = Buf("junk")

    def warm_pe(n):
        fns = [lambda e: e.matmul(PS[7][:, 256:512], lhsT=ident[:], rhs=Edsa[:, 0, 0:256], start=True, stop=True,
                                  skip_group_check=True) for _ in range(n)]
        S.group("pe", fns, reads=[BEdsa, Bconst], writes=[Bjunk])

    post_round = []
    NP = NT // 2
    for rnd in range(0, NP + 2):
        gs = []
        if 1 <= rnd + 1 < NP:
            gs.append(itertools.chain(genS(2 * rnd + 2), genS(2 * rnd + 3)))
        if 1 <= rnd < NP:
            gs.append(genB(2 * rnd))
            gs.append(genB(2 * rnd + 1))
        if 0 <= rnd - 1 < NP:
            gs.append(itertools.chain(genA(2 * rnd - 2), genA(2 * rnd - 1)))
        interleave(gs)
        for f_ in post_round:
            f_()
        del post_round[:]

    if debug:
        S.dma("sp", lambda e: e.dma_start(out=dbg["d_ya"].rearrange("(c p) t -> p c t", p=128), in_=yaT[:]),
              reads=[ByaT], writes=[Bout])
    S.barrier()
    if stop_after <= 2:
        S.emit()
        return nc

    wab = M("wab", [128, 4, D], BF16, 67584, (3, 3))
    wbb = M("wbb", [128, 4, D], BF16, 75776, (3, 3))
    mergedT = M("mergedT", [128, 8, L], BF16, 83968, (3, 4))
    sgate = M("sgate", [128, 6, 512], BF16, 116736, (3, 3))
    mtmp = M("mtmp", [128, 2, 2, 512], F32, 124928, (3, 3))
    Bwa = Buf("wa"); Bwb = Buf("wb")
    S.dma("pool", lambda e: e.dma_start(out=wab[:], in_=wa_d.rearrange("(k p) n -> p k n", p=128)), writes=[Bwa])
    S.dma("pool", lambda e: e.dma_start(out=wbb[:], in_=wb_d.rearrange("(k p) n -> p k n", p=128)), writes=[Bwb])
    Bsg = [Buf("sg%d" % i) for i in range(6)]
    Bmt = [Buf("mt0"), Buf("mt1")]
    Bmerged = [Buf("merged%d" % f) for f in range(8)]

    def p3a_load(n):
        f, tb = n // 4, n % 4
        for gi in range(2):
            sl = (n % 3) * 2 + gi
            r0 = gi * 1024 + f * 128
            S.dma("sp", lambda e, r0=r0, tb=tb, sl=sl: e.dma_start(
                out=sgate[:, sl, :], in_=gates_d[r0:r0 + 128, tb * 512:(tb + 1) * 512]), reads=[Bgates], writes=[Bsg[sl]])

    p3a_load(0)
    p3a_load(1)
    it3 = 0
    for n in range(32):
        f, tb = n // 4, n % 4
        ts_ = slice(tb * 512, (tb + 1) * 512)
        if n + 2 < 32:
            p3a_load(n + 2)
        pb = (n % 2) * 2
        mi = n % 2
        for bi, (wt, yT, bw, by) in enumerate(((wab, yaT, Bwa, ByaT), (wbb, ybT, Bwb, BybT))):
            fns = [lambda e, k=k, wt=wt, yT=yT, f=f, ts_=ts_, pb=pb, bi=bi: e.matmul(
                PS[pb + bi][:], lhsT=wt[:, k, f * 128:(f + 1) * 128], rhs=yT[:, k, ts_],
                start=(k == 0), stop=(k == 3)) for k in range(4)]
            S.group("pe", fns, reads=[bw, by], writes=[Bps[pb + bi]])
            sl = (n % 3) * 2 + bi
            S.op("dve", lambda e, pb=pb, bi=bi, sl=sl, mi=mi: e.tensor_tensor(
                out=mtmp[:, mi, bi, :], in0=sgate[:, sl, :], in1=PS[pb + bi][:], op=ALU.mult),
                reads=[Bsg[sl], Bps[pb + bi]], writes=[Bmt[mi]])
        S.op("pool", lambda e, mi=mi, f=f, ts_=ts_: e.tensor_tensor(
            out=mergedT[:, f, ts_], in0=mtmp[:, mi, 0, :], in1=mtmp[:, mi, 1, :], op=ALU.add),
            reads=[Bmt[mi]], writes=[Bmerged[f]])
        bg_convert(1)
    if debug:
        S.dma("sp", lambda e: e.dma_start(out=dbg["d_mergedT"].rearrange("(c p) t -> p c t", p=128), in_=mergedT[:]),
              reads=Bmerged, writes=[Bout])
    S.barrier()
    if stop_after <= 3:
        S.emit()
        return nc

    ACC_OFF = 147136
    acc = M("acc", [128, NT, D], F32, ACC_OFF, (4, 7))
    x1T = M("x1T", [128, 8, L], BF16, 2048, (4, 5))
    x1tok = M("x1tok", [128, NT, D], BF16, 34816, (4, 6))
    woutb = M("woutb", [128, 8, D], BF16, 67584, (4, 4))
    ln1g = M("ln1g", [128, D], F32, 116736, (4, 4))
    ln1bA = M("ln1bA", [128, D], F32, 120832, (4, 4))
    xtok = M("xtok", [128, 2, D], F32, 124928, (4, 4))
    rres = M("rres", [128, 3, D], F32, 133120, (4, 4))
    lnst = M("lnst", [128, 3, 32], F32, 145408, (4, 4))
    Bwout = Buf("wout"); Bln1 = Buf("ln1")
    Bxtok = [Buf("xtok0"), Buf("xtok1")]
    Brres = [Buf("r%d" % i) for i in range(3)]
    Blnst = [Buf("lnst%d" % i) for i in range(3)]
    Bacc = [Buf("acc%d" % t) for t in range(NT)]
    Bx1tok = [Buf("x1tok%d" % t) for t in range(NT)]
    Bx1T = Buf("x1T")
    wov = wo_d.rearrange("(k p) n -> p k n", p=128)
    S.dma("pool", lambda e: e.dma_start(out=woutb[:, 0:4, :], in_=wov[:, 0:4, :]), writes=[Bwout])
    S.dma("pool", lambda e: e.dma_start(out=woutb[:, 4:8, :], in_=wov[:, 4:8, :]), writes=[Bwout])
    S.dma("sp", lambda e: e.dma_start(out=ln1g[:], in_=ln1g_d.to_broadcast([128, D])), writes=[Bln1])
    S.dma("sp", lambda e: e.dma_start(out=ln1bA[:], in_=ln1b_d.to_broadcast([128, D])), writes=[Bln1])
    S.op("act", lambda e: e.activation(out=ln1bA[:], in_=ln1bA[:], func=AF.Copy, scale=ALPHA),
         reads=[Bln1], writes=[Bln1])

    def ln_scaled(src, srcB, stv, stB, gt, btA, gB, dst, dstB, alpha=ALPHA):
        S.op("dve", lambda e: e.bn_stats(out=stv[:, 0:6], in_=src[:, 0:512]), reads=srcB, writes=[stB])
        S.op("dve", lambda e: e.bn_stats(out=stv[:, 6:12], in_=src[:, 512:1024]), reads=srcB + [stB], writes=[stB])
        S.op("dve", lambda e: e.bn_aggr(out=stv[:, 12:14], in_=stv[:, 0:12]), reads=[stB], writes=[stB])
        S.op("dve", lambda e: e.tensor_scalar(out=stv[:, 14:15], in0=stv[:, 13:14], scalar1=LN_EPS, scalar2=None,
                                              op0=ALU.add), reads=[stB], writes=[stB])
        S.op("act", lambda e: e.activation(out=stv[:, 14:15], in_=stv[:, 14:15], func=AF.Sqrt), reads=[stB], writes=[stB])
        S.op("dve", lambda e: e.reciprocal(out=stv[:, 15:16], in_=stv[:, 14:15]), reads=[stB], writes=[stB])
        S.op("dve", lambda e: e.tensor_scalar(out=stv[:, 16:17], in0=stv[:, 15:16], scalar1=alpha, scalar2=None,
                                              op0=ALU.mult), reads=[stB], writes=[stB])
        S.op("dve", lambda e: e.scalar_tensor_tensor(out=src, in0=src, scalar=stv[:, 12:13], in1=gt[:],
                                                     op0=ALU.subtract, op1=ALU.mult), reads=srcB + [stB, gB], writes=srcB)
        S.op("dve", lambda e: e.scalar_tensor_tensor(out=dst, in0=src, scalar=stv[:, 16:17], in1=btA[:],
                                                     op0=ALU.mult, op1=ALU.add), reads=srcB + [stB, gB], writes=dstB)

    def p3b_A(tt):
        b2, b3 = tt % 2, tt % 3
        S.dma("sp", lambda e: e.dma_start(out=xtok[:, b2, :], in_=x_d[tt * 128:(tt + 1) * 128, :]), writes=[Bxtok[b2]])
        for half in range(2):
            bank = b3 * 2 + half
            fns = [lambda e, k=k, half=half, bank=bank: e.matmul(
                PS[bank][:], lhsT=mergedT[:, k, tt * 128:(tt + 1) * 128], rhs=woutb[:, k, half * 512:(half + 1) * 512],
                start=(k == 0), stop=(k == 7)) for k in range(8)]
            S.group("pe", fns, reads=Bmerged + [Bwout], writes=[Bps[bank]])
            S.op("dve", lambda e, half=half, bank=bank: e.scalar_tensor_tensor(
                out=rres[:, b3, half * 512:(half + 1) * 512], in0=xtok[:, b2, half * 512:(half + 1) * 512], scalar=ALPHA,
                in1=PS[bank][:], op0=ALU.mult, op1=ALU.add), reads=[Bxtok[b2], Bps[bank]], writes=[Brres[b3]])

    def p3b_B(tt):
        b3 = tt % 3
        ln_scaled(rres[:, b3, :], [Brres[b3]], lnst[:, b3, :], Blnst[b3], ln1g, ln1bA, Bln1, acc[:, tt, :], [Bacc[tt]])
        S.op("act", lambda e: e.activation(out=x1tok[:, tt, :], in_=acc[:, tt, :], func=AF.Copy, scale=1.0 / ALPHA),
             reads=[Bacc[tt]], writes=[Bx1tok[tt]])

    def p3b_C(tt):
        for half in range(2):
            tbk = 6 + half
            pv = PS[tbk][:].bitcast(BF16)
            fns = [lambda e, c=c, half=half, pv=pv: e.transpose(
                out=pv[:, c * 128:(c + 1) * 128], in_=x1tok[:, tt, (half * 4 + c) * 128:(half * 4 + c + 1) * 128],
                identity=ident[:]) for c in range(4)]
            S.group("pe", fns, reads=[Bx1tok[tt], Bconst], writes=[Bps[tbk]])
            S.op("act", lambda e, half=half, pv=pv: e.activation(
                out=x1T[:, half * 4:(half + 1) * 4, tt * 128:(tt + 1) * 128],
                in_=pv[:, 0:512].rearrange("p (c t) -> p c t", c=4), func=AF.Copy), reads=[Bps[tbk]], writes=[Bx1T])

    for s_ in range(NT + 2):
        if s_ < NT:
            p3b_A(s_)
        if 1 <= s_ <= NT:
            p3b_B(s_ - 1)
        if s_ >= 2:
            p3b_C(s_ - 2)
    if debug:
        S.dma("sp", lambda e: e.dma_start(out=dbg["d_acc"].rearrange("(t p) d -> p t d", p=128), in_=acc[:]),
              reads=Bacc, writes=[Bout])
    S.barrier()
    if stop_after <= 4:
        S.emit()
        return nc

    bg_convert(len(bg_jobs))
    o = 67584
    wrb = M("wrb", [128, 8, 36], BF16, o, (5, 5)); o += 1024
    brbc = M("brbc", [128, 36], F32, o, (5, 5)); o += 256
    ustr = M("ustr", [128, 128], BF16, o, (5, 5)); o += 256
    onesb = M("onesb", [128, 128], BF16, o, (5, 5)); o += 256
    jrow = M("jrow", [128, 64], F32, o, (5, 5)); o += 256
    thr16 = M("thr16", [128, 16], F32, o, (5, 5)); o += 64
    piota = M("piota", [128, 2], F32, o, (5, 5)); o += 64
    lg = M("lg", [128, NT, 36], F32, o, (5, 5)); o += 2304
    elm = M("elm", [128, NT, 32], F32, o, (5, 5)); o += 2048
    exr = M("exr", [128, NT, 32], F32, o, (5, 5)); o += 2048
    sel = M("sel", [128, NT, 32], F32, o, (5, 5)); o += 2048
    selb = M("selb", [128, NT, 32], BF16, o, (5, 5)); o += 1024
    comb = M("comb", [128, NT, 32], F32, o, (5, 5)); o += 2048
    posf = M("posf", [128, NT, 32], F32, o, (5, 5)); o += 2048
    tmpa = M("tmpa", [128, NT, 32], F32, o, (5, 5)); o += 2048
    tmpb = M("tmpb", [128, 64, 32], F32, o, (5, 5)); o += 8192
    sm = M("sm", [128, 24, NT], F32, o, (5, 5)); o += 1536
    pen = M("pen", [128, NT, 4], F32, o, (5, 5)); o += 256
    gex = M("gex", [128, NT, 4], F32, o, (5, 5)); o += 256
    ntr = M("ntr", [128, 128], BF16, o, (5, 5)); o += 256
    cnt32 = M("cnt32", [128, 24], F32, o, (5, 5)); o += 96
    toffs = M("toffs", [128, 64], F32, o, (5, 5)); o += 256
    ejf = M("ejf", [128, 64], F32, o, (5, 5)); o += 256
    assert o < 116736
    posu = M("posu", [128, NT, 2], mybir.dt.uint32, 100352, (5, 7))
    wsl = M("wsl", [128, NT, 2], F32, 100480, (5, 7))
    widxu = M("widxu", [128, 64], mybir.dt.uint32, 100608, (5, 7))
    Bwr = Buf("wr"); Bc5 = Buf("c5"); BR = Buf("route"); Bpos = Buf("posu")
    S.dma("pool", lambda e: e.dma_start(out=wrb[:], in_=wr_d.rearrange("(k p) n -> p k n", p=128)), writes=[Bwr])
    S.dma("sp", lambda e: e.dma_start(out=brbc[:], in_=br_d.to_broadcast([128, 36])), writes=[Bwr])
    S.op("pool", lambda e: e.memset(ustr[:], 1.0), writes=[Bc5])
    S.op("pool", lambda e: e.affine_select(out=ustr[:], in_=ustr[:], pattern=[[1, 128]], compare_op=ALU.is_ge,
                                           fill=preg(e, 0.0), base=-1, channel_multiplier=-1), reads=[Bc5], writes=[Bc5])
    S.op("pool", lambda e: e.memset(onesb[:], 1.0), writes=[Bc5])
    S.op("pool", lambda e: e.iota(jrow[:], pattern=[[1, 64]], base=0, channel_multiplier=0,
                                  allow_small_or_imprecise_dtypes=True), writes=[Bc5])
    S.op("pool", lambda e: e.iota(thr16[:], pattern=[[128, 16]], base=0, channel_multiplier=0,
                                  allow_small_or_imprecise_dtypes=True), writes=[Bc5])
    S.op("pool", lambda e: e.iota(piota[:], pattern=[[0, 2]], base=0, channel_multiplier=1,
                                  allow_small_or_imprecise_dtypes=True), writes=[Bc5])

    def bc(ap, shape):
        return ap.to_broadcast(shape)

    for tt in range(NT):
        bank = tt // 8
        fns = [lambda e, k=k, tt=tt, bank=bank: e.matmul(
            PS[bank][:, (tt % 8) * 36:(tt % 8 + 1) * 36], lhsT=x1T[:, k, tt * 128:(tt + 1) * 128], rhs=wrb[:, k, :],
            start=(k == 0), stop=(k == 7), skip_group_check=True) for k in range(8)]
        S.group("pe", fns, reads=[Bx1T, Bwr], writes=[Bps[bank]])
    for bank in range(2):
        S.op("dve", lambda e, bank=bank: e.tensor_tensor(
            out=lg[:, bank * 8:(bank + 1) * 8, :], in0=PS[bank][:, 0:288].rearrange("p (t c) -> p t c", c=36),
            in1=bc(brbc[:].rearrange("p (o c) -> p o c", o=1), [128, 8, 36]), op=ALU.add),
            reads=[Bps[bank], Bwr], writes=[BR])
    RB = [BR]
    def smv(r):
        return sm[:, r, :]

    def sm3(r, n):
        return bc(sm[:, r, :].rearrange("p (t o) -> p t o", o=1), [128, NT, n])

    S.op("dve", lambda e: e.tensor_reduce(out=smv(0), in_=lg[:, :, 0:4], axis=AX.X, op=ALU.max), reads=RB, writes=RB)
    S.op("dve", lambda e: e.tensor_tensor(out=pen[:], in0=lg[:, :, 0:4], in1=sm3(0, 4), op=ALU.is_lt), reads=RB, writes=RB)
    S.op("dve", lambda e: e.tensor_tensor(out=gex[:], in0=lg[:, :, 0:4], in1=sm3(0, 4), op=ALU.subtract), reads=RB, writes=RB)
    S.op("act", lambda e: e.activation(out=gex[:], in_=gex[:], func=AF.Exp), reads=RB, writes=RB)
    S.op("dve", lambda e: e.tensor_reduce(out=smv(1), in_=gex[:], axis=AX.X, op=ALU.add), reads=RB, writes=RB)
    S.op("dve", lambda e: e.tensor_scalar(out=pen[:], in0=pen[:], scalar1=NEG, scalar2=None, op0=ALU.mult), reads=RB, writes=RB)
    S.op("dve", lambda e: e.tensor_tensor(
        out=elm[:].rearrange("p t (g j) -> p t g j", g=4), in0=lg[:, :, 4:36].rearrange("p t (g j) -> p t g j", g=4),
        in1=bc(pen[:].rearrange("p t (g o) -> p t g o", o=1), [128, NT, 4, 8]), op=ALU.add), reads=RB, writes=RB)
    S.op("dve", lambda e: e.tensor_reduce(out=smv(2), in_=elm[:], axis=AX.X, op=ALU.max), reads=RB, writes=RB)
    S.op("dve", lambda e: e.tensor_tensor(out=tmpa[:], in0=elm[:], in1=sm3(2, 32), op=ALU.is_ge), reads=RB, writes=RB)
    S.op("dve", lambda e: e.scalar_tensor_tensor(out=tmpa[:], in0=tmpa[:], scalar=NEG, in1=elm[:], op0=ALU.mult, op1=ALU.add),
         reads=RB, writes=RB)
    S.op("dve", lambda e: e.tensor_reduce(out=smv(3), in_=tmpa[:], axis=AX.X, op=ALU.max), reads=RB, writes=RB)
    S.op("dve", lambda e: e.tensor_tensor(out=exr[:], in0=elm[:], in1=sm3(2, 32), op=ALU.subtract), reads=RB, writes=RB)
    S.op("act", lambda e: e.activation(out=exr[:], in_=exr[:], func=AF.Exp), reads=RB, writes=RB)
    S.op("dve", lambda e: e.tensor_tensor(out=sel[:], in0=elm[:], in1=sm3(3, 32), op=ALU.is_ge), reads=RB, writes=RB)
    S.op("dve", lambda e: e.tensor_copy(out=selb[:], in_=sel[:]), reads=RB, writes=RB)
    S.op("dve", lambda e: e.tensor_tensor(out=comb[:], in0=sel[:], in1=exr[:], op=ALU.mult), reads=RB, writes=RB)
    S.op("dve", lambda e: e.tensor_reduce(out=smv(4), in_=comb[:], axis=AX.X, op=ALU.add), reads=RB, writes=RB)
    S.op("dve", lambda e: e.tensor_tensor(out=smv(5), in0=smv(4), in1=smv(1), op=ALU.mult), reads=RB, writes=RB)
    S.op("dve", lambda e: e.reciprocal(out=smv(6), in_=smv(5)), reads=RB, writes=RB)
    S.op("dve", lambda e: e.tensor_tensor(out=comb[:], in0=comb[:], in1=sm3(6, 32), op=ALU.mult), reads=RB, writes=RB)
    if debug:
        S.dma("sp", lambda e: e.dma_start(out=dbg["d_comb"].rearrange("(t p) d -> p t d", p=128), in_=comb[:]),
              reads=RB, writes=[Bout])
    for tt in range(NT):
        fns = []
        for tp in range(tt):
            fns.append(lambda e, tt=tt, tp=tp: e.matmul(
                PS[2][:, tt * 32:(tt + 1) * 32], lhsT=onesb[:], rhs=selb[:, tp, :], start=(tp == 0), stop=False,
                skip_group_check=True))
        fns.append(lambda e, tt=tt: e.matmul(
            PS[2][:, tt * 32:(tt + 1) * 32], lhsT=ustr[:], rhs=selb[:, tt, :], start=(tt == 0), stop=True,
            skip_group_check=True))
        S.group("pe", fns, reads=RB + [Bc5], writes=[Bps[2]])
    fns = [lambda e, tt=tt: e.matmul(PS[3][0:32, 0:1], lhsT=selb[:, tt, :], rhs=onesb[:, 0:1],
                                     start=(tt == 0), stop=(tt == NT - 1)) for tt in range(NT)]
    S.group("pe", fns, reads=RB + [Bc5], writes=[Bps[3]])
    S.op("dve", lambda e: e.tensor_copy(out=cnt32[0:32, 0:1], in_=PS[3][0:32, 0:1]), reads=[Bps[3]], writes=RB)
    S.op("dve", lambda e: e.tensor_scalar(out=cnt32[0:32, 4:20], in0=thr16[0:32, :], scalar1=cnt32[0:32, 0:1], scalar2=None,
                                          op0=ALU.is_lt), reads=RB + [Bc5], writes=RB)
    S.op("dve", lambda e: e.tensor_reduce(out=cnt32[0:32, 1:2], in_=cnt32[0:32, 4:20], axis=AX.X, op=ALU.add), reads=RB, writes=RB)
    S.op("dve", lambda e: e.tensor_scalar(out=ntr[0:32, :], in0=onesb[0:32, :], scalar1=cnt32[0:32, 1:2], scalar2=None,
                                          op0=ALU.mult), reads=RB + [Bc5], writes=RB)
    S.op("pe", lambda e: e.matmul(PS[3][:, 64:96], lhsT=ntr[0:32, :], rhs=ustr[0:32, 0:32], start=True, stop=True),
         reads=RB + [Bc5], writes=[Bps[3]])
    S.op("pe", lambda e: e.matmul(PS[3][:, 96:128], lhsT=ntr[0:32, :], rhs=causal01[0:32, 0:32], start=True, stop=True),
         reads=RB + [Bconst], writes=[Bps[3]])
    S.op("dve", lambda e: e.tensor_copy(out=toffs[:], in_=PS[3][:, 64:128]), reads=[Bps[3]], writes=RB)
    S.op("dve", lambda e: e.scalar_tensor_tensor(
        out=posf[:], in0=bc(toffs[:, 0:32].rearrange("p (o c) -> p o c", o=1), [128, NT, 32]), scalar=128.0,
        in1=PS[2][:].rearrange("p (t c) -> p t c", c=32), op0=ALU.mult, op1=ALU.add), reads=RB + [Bps[2]], writes=RB)
    S.op("dve", lambda e: e.tensor_tensor(out=tmpa[:], in0=sel[:], in1=posf[:], op=ALU.mult), reads=RB, writes=RB)
    S.op("dve", lambda e: e.tensor_reduce(out=smv(8), in_=tmpa[:], axis=AX.X, op=ALU.max), reads=RB, writes=RB)
    S.op("dve", lambda e: e.tensor_scalar(out=tmpa[:], in0=sel[:], scalar1=-1.0e6, scalar2=1.0e6, op0=ALU.mult, op1=ALU.add),
         reads=RB, writes=RB)
    S.op("dve", lambda e: e.tensor_tensor(out=tmpa[:], in0=tmpa[:], in1=posf[:], op=ALU.add), reads=RB, writes=RB)
    S.op("dve", lambda e: e.tensor_reduce(out=smv(7), in_=tmpa[:], axis=AX.X, op=ALU.min), reads=RB, writes=RB)
    for r_pos, r_w in ((7, 9), (8, 10)):
        S.op("dve", lambda e, r_pos=r_pos: e.tensor_tensor(out=tmpa[:], in0=posf[:], in1=sm3(r_pos, 32), op=ALU.is_equal),
             reads=RB, writes=RB)
        S.op("dve", lambda e: e.tensor_tensor(out=tmpa[:], in0=tmpa[:], in1=comb[:], op=ALU.mult), reads=RB, writes=RB)
        S.op("dve", lambda e, r_w=r_w: e.tensor_reduce(out=smv(r_w), in_=tmpa[:], axis=AX.X, op=ALU.add), reads=RB, writes=RB)
    for slot in range(2):
        S.op("dve", lambda e, slot=slot: e.tensor_copy(out=posu[:, :, slot], in_=smv(7 + slot)), reads=RB, writes=[Bpos])
        S.op("dve", lambda e, slot=slot: e.tensor_copy(out=wsl[:, :, slot], in_=smv(9 + slot)), reads=RB, writes=[Bpos])
    S.op("dve", lambda e: e.tensor_tensor(
        out=tmpb[:], in0=bc(toffs[:, 32:64].rearrange("p (o c) -> p o c", o=1), [128, 64, 32]),
        in1=bc(jrow[:].rearrange("p (j o) -> p j o", o=1), [128, 64, 32]), op=ALU.is_le), reads=RB + [Bc5], writes=RB)
    S.op("dve", lambda e: e.tensor_reduce(out=ejf[:], in_=tmpb[:], axis=AX.X, op=ALU.add), reads=RB, writes=RB)
    S.op("dve", lambda e: e.scalar_tensor_tensor(
        out=ejf[:], in0=ejf[:], scalar=128.0, in1=bc(piota[:, 0:1], [128, 64]), op0=ALU.mult, op1=ALU.add),
        reads=RB + [Bc5], writes=RB)
    S.op("dve", lambda e: e.tensor_copy(out=widxu[:], in_=ejf[:]), reads=RB, writes=[Bpos])
    S.barrier()
    if stop_after <= 5:
        S.emit()
        return nc

    NTL = 64
    NB = 5
    NX = 6
    wgt = [M("wgt%d" % b, [128, 2048], BF16, 2048 + b * 4096, (6, 6)) for b in range(NB)]
    wut = [M("wut%d" % b, [128, 2048], BF16, 100864 + b * 4096, (6, 6)) for b in range(NB)]
    wdt = [M("wdt%d" % b, [128, 2048], BF16, 121344 + b * 4096, (6, 6)) for b in range(NB)]
    xst = M("xst", [128, NX, D], BF16, 22528, (6, 6))
    xsT = M("xsT", [128, 2, 8, 128], BF16, 71680, (6, 6))
    sgt = M("sgt", [128, 2, 256], BF16, 75776, (6, 6))
    hTt = M("hTt", [128, 2, 256], BF16, 76800, (6, 6))
    ysb = M("ysb", [128, 2, D], F32, 77824, (6, 6))
    Bwt = [[Buf("wt%d_%d" % (m, b)) for b in range(NB)] for m in range(3)]
    Bxst = [Buf("xst%d" % i) for i in range(NX)]; BxsT = [Buf("xsT0"), Buf("xsT1")]
    Bsgt = [Buf("sgt0"), Buf("sgt1")]; BhTt = [Buf("hTt0"), Buf("hTt1")]; Bysb = [Buf("ysb0"), Buf("ysb1")]
    Bxs = Buf("xs_d"); Bys = Buf("ys_d")
    for tt in range(NT):
        for slot in range(2):
            S.dma("pool", lambda e, tt=tt, slot=slot: e.indirect_dma_start(
                out=xs_d, out_offset=bass.IndirectOffsetOnAxis(ap=posu[:, tt, slot:slot + 1], axis=0),
                in_=x1tok[:, tt, :], in_offset=None), reads=[Bpos, Bx1tok[tt], Bzero], writes=[Bxs])

    def moe_load_w(j):
        b = j % NB
        for m, (wd_, wt_) in enumerate(((wgb_d, wgt), (wub_d, wut), (wdb_d, wdt))):
            S.dma("pool", lambda e, wd_=wd_, wt_=wt_: e.indirect_dma_start(
                out=wt_[b][:], out_offset=None, in_=wd_,
                in_offset=bass.IndirectOffsetOnAxis(ap=widxu[:, j:j + 1], axis=0), bounds_check=preg(e, NEXP * 128 - 1),
                oob_is_err=False), reads=[Bpos, Bwbf], writes=[Bwt[m][b]])

    def moe_load_x(j):
        bx = j % NX
        S.dma("sp", lambda e: e.dma_start(out=xst[:, bx, :], in_=xs_d[j * 128:(j + 1) * 128, :]), reads=[Bxs], writes=[Bxst[bx]])

    def moe_T(j):
        b2, bx = j % 2, j % NX
        pv = PS[b2][:].bitcast(BF16)
        fns = [lambda e, c=c: e.transpose(out=pv[:, c * 128:(c + 1) * 128], in_=xst[:, bx, c * 128:(c + 1) * 128],
                                         identity=ident[:]) for c in range(8)]
        S.group("pe", fns, reads=[Bxst[bx], Bconst], writes=[Bps[b2]])
        S.op("act", lambda e: e.activation(out=xsT[:, b2, :, :], in_=pv.rearrange("p (c t) -> p c t", c=8), func=AF.Copy),
             reads=[Bps[b2]], writes=[BxsT[b2]])

    def moe_GU(j):
        b2, b = j % 2, j % NB
        bank = 2 + b2
        fns = []
        for m, wt_ in ((0, wgt), (1, wut)):
            wv = wt_[b][:].rearrange("p (k n) -> p k n", k=8)
            for c in range(2):
                for k in range(8):
                    fns.append(lambda e, m=m, wv=wv, c=c, k=k: e.matmul(
                        PS[bank][:, (m * 2 + c) * 128:(m * 2 + c + 1) * 128], lhsT=wv[:, k, c * 128:(c + 1) * 128],
                        rhs=xsT[:, b2, k, :], start=(k == 0), stop=(k == 7), skip_group_check=True))
        S.group("pe", fns, reads=[BxsT[b2], Bwt[0][b], Bwt[1][b]], writes=[Bps[bank]])
        S.op("act", lambda e: e.activation(out=sgt[:, b2, :], in_=PS[bank][:, 0:256], func=AF.Silu),
             reads=[Bps[bank]], writes=[Bsgt[b2]])
        S.op("dve", lambda e: e.tensor_tensor(out=hTt[:, b2, :], in0=sgt[:, b2, :], in1=PS[bank][:, 256:512], op=ALU.mult),
             reads=[Bsgt[b2], Bps[bank]], writes=[BhTt[b2]])

    def moe_D(j):
        b2, b = j % 2, j % NB
        wv = wdt[b][:].rearrange("p (c n) -> p c n", c=2)
        for half in range(2):
            bank = 4 + b2 * 2 + half
            fns = [lambda e, c=c, half=half, bank=bank: e.matmul(
                PS[bank][:], lhsT=hTt[:, b2, c * 128:(c + 1) * 128], rhs=wv[:, c, half * 512:(half + 1) * 512],
                start=(c == 0), stop=(c == 1)) for c in range(2)]
            S.group("pe", fns, reads=[BhTt[b2], Bwt[2][b]], writes=[Bps[bank]])
            if half == 0:
                S.op("act", lambda e, bank=bank: e.activation(out=ysb[:, b2, 0:512], in_=PS[bank][:], func=AF.Copy),
                     reads=[Bps[bank]], writes=[Bysb[b2]])
            else:
                S.op("dve", lambda e, bank=bank: e.tensor_copy(out=ysb[:, b2, 512:1024], in_=PS[bank][:]),
                     reads=[Bps[bank]], writes=[Bysb[b2]])
        S.dma("sp", lambda e: e.dma_start(out=ys_d[j * 128:(j + 1) * 128, :], in_=ysb[:, b2, :]), reads=[Bysb[b2]], writes=[Bys])

    for j in range(NX):
        moe_load_x(j)
    for j in range(NB):
        moe_load_w(j)
    for s_ in range(NTL + 2):
        if s_ < NTL:
            moe_T(s_)
            if s_ + NX < NTL:
                moe_load_x(s_ + NX)
        if 1 <= s_ <= NTL:
            moe_GU(s_ - 1)
        if s_ >= 2:
            moe_D(s_ - 2)
            if s_ - 2 + NB < NTL:
                moe_load_w(s_ - 2 + NB)
    S.barrier()

    NYG = 12
    yg = M("yg", [128, NYG, D], F32, 34816, (7, 7))
    ln2g = M("ln2g", [128, D], F32, 2048, (7, 7))
    ln2bA = M("ln2bA", [128, D], F32, 6144, (7, 7))
    obuf = M("obuf", [128, 3, D], F32, 10240, (7, 7))
    lnst2 = M("lnst2", [128, 3, 32], F32, 22528, (7, 7))
    Bln2 = Buf("ln2"); Bob = [Buf("ob%d" % i) for i in range(3)]; Blnst2 = [Buf("ls%d" % i) for i in range(3)]
    Byg = [Buf("yg%d" % i) for i in range(NYG)]
    S.dma("sp", lambda e: e.dma_start(out=ln2g[:], in_=ln2g_d.to_broadcast([128, D])), writes=[Bln2])
    S.dma("sp", lambda e: e.dma_start(out=ln2bA[:], in_=ln2b_d.to_broadcast([128, D])), writes=[Bln2])

    def tail_gather(q):
        tt, slot = q // 2, q % 2
        yb_ = q % NYG
        S.dma("pool", lambda e: e.indirect_dma_start(
            out=yg[:, yb_, :], out_offset=None, in_=ys_d,
            in_offset=bass.IndirectOffsetOnAxis(ap=posu[:, tt, slot:slot + 1], axis=0)),
            reads=[Bpos, Bys], writes=[Byg[yb_]])

    for q in range(NYG):
        tail_gather(q)
    for tt in range(NT):
        b3 = tt % 3
        for slot in range(2):
            q = tt * 2 + slot
            yb_ = q % NYG
            S.op("dve", lambda e, tt=tt, slot=slot, yb_=yb_: e.scalar_tensor_tensor(
                out=acc[:, tt, :], in0=yg[:, yb_, :], scalar=wsl[:, tt, slot:slot + 1], in1=acc[:, tt, :],
                op0=ALU.mult, op1=ALU.add), reads=[Byg[yb_], Bpos, Bacc[tt]], writes=[Bacc[tt]])
            if q + NYG < 2 * NT:
                tail_gather(q + NYG)
        ln_scaled(acc[:, tt, :], [Bacc[tt]], lnst2[:, b3, :], Blnst2[b3], ln2g, ln2bA, Bln2, obuf[:, b3, :], [Bob[b3]],
                  alpha=1.0)
        S.dma("sp", lambda e, tt=tt, b3=b3: e.dma_start(out=out_d[tt * 128:(tt + 1) * 128, :], in_=obuf[:, b3, :]),
              reads=[Bob[b3]], writes=[Bout])
    S.barrier()
    S.emit()
    return nc


_NC_CACHE = {}


def _host_inputs(inp, b):
    f = np.float32
    w_in = inp["w_in"][0]
    cols = list(range(0, 1024))
    cols += list(range(1152, 1664))
    cols += list(range(1664, 1728)) * 2
    qb0 = 1736
    for j in range(4):
        cols += list(range(qb0 + j * 64, qb0 + (j + 1) * 64))
        cols += list(range(qb0 + (j + 4) * 64, qb0 + (j + 5) * 64))
    cols += list(range(2248, 2376))
    cols += list(range(1024, 1152))
    cols += list(range(2376, 2504))
    cols += list(range(1728, 1736))
    assert len(cols) == W1COLS
    rel = inp["rel_bias"].astype(f)
    s = np.arange(128)[:, None]
    t = np.arange(128)[None, :]
    bk_prev = t5_bucket_np(t - s + 128)
    bk_own = t5_bucket_np(t - s)
    swab = np.zeros((4, 128, 4, 128), f)
    for typ, bk in enumerate((bk_prev, bk_own)):
        for g in range(2):
            for j in range(4):
                swab[typ * 2 + g, :, j, :] = rel[bk, 8 + 4 * g + j]
    dsab = np.zeros((2, 128, 8, 128), f)
    for typ, bk in enumerate((bk_prev, bk_own)):
        for h in range(8):
            dsab[typ, :, h, :] = rel[bk, h]
    return {
        "xT": np.ascontiguousarray(inp["x"][b].T),
        "x": np.ascontiguousarray(inp["x"][b]),
        "w1": np.ascontiguousarray(w_in[:, cols]),
        "wg": np.ascontiguousarray(w_in[:, 2504:4552]),
        "kvg": np.ascontiguousarray(inp["kv_norm_g"][0].reshape(1, 128)),
        "wuv": np.ascontiguousarray(inp["w_uv"][0].transpose(1, 0, 2).reshape(128, 512)),
        "wa": np.ascontiguousarray(inp["w_branch_a"][0]),
        "wb": np.ascontiguousarray(inp["w_branch_b"][0]),
        "wo": np.ascontiguousarray(inp["w_out"][0]),
        "sinks": np.ascontiguousarray(inp["sinks"][0].reshape(1, 8)),
        "ln1g": np.ascontiguousarray(inp["ln1_g"][0].reshape(1, D)),
        "ln1b": np.ascontiguousarray(inp["ln1_b"][0].reshape(1, D)),
        "ln2g": np.ascontiguousarray(inp["ln2_g"][0].reshape(1, D)),
        "ln2b": np.ascontiguousarray(inp["ln2_b"][0].reshape(1, D)),
        "wr": np.ascontiguousarray(np.concatenate([inp["w_group"][0], inp["w_router"][0]], axis=1)),
        "br": np.ascontiguousarray(np.concatenate([inp["b_group"][0], inp["b_router"][0]]).reshape(1, 36)),
        "wgr": np.ascontiguousarray(inp["w_gate"][0].reshape(NEXP, 8, 128, DE).transpose(0, 2, 1, 3).reshape(NEXP * 128, 2048)),
        "wur": np.ascontiguousarray(inp["w_up"][0].reshape(NEXP, 8, 128, DE).transpose(0, 2, 1, 3).reshape(NEXP * 128, 2048)),
        "wdr": np.ascontiguousarray(inp["w_down"][0].reshape(NEXP, 2, 128, D).transpose(0, 2, 1, 3).reshape(NEXP * 128, 2048)),
        "swab": swab.reshape(4, 128, 512),
        "dsab": dsab.reshape(2, 128, 1024),
        "c31": np.ascontiguousarray(rel[31, 0:8].reshape(1, 8)),
    }


def kernel(**inputs):
    inp = {k: np.asarray(v, dtype=np.float32) for k, v in inputs.items()}
    n = 8
    if "nc" not in _NC_CACHE:
        _NC_CACHE["nc"] = build_nc(False)
    nc = _NC_CACHE["nc"]
    shared = None
    in_maps = []
    for b in range(n):
        m = _host_inputs(inp, b) if shared is None else dict(shared)
        if shared is None:
            shared = m
        else:
            m["xT"] = np.ascontiguousarray(inp["x"][b].T)
            m["x"] = np.ascontiguousarray(inp["x"][b])
        in_maps.append(m)
    res = run_bass_kernel_spmd(nc, in_maps, core_ids=list(range(n)))
    return np.stack([np.asarray(r["out"], dtype=np.float32) for r in res.results], axis=0)
```

```python
import math
import contextlib
import numpy as np
import concourse.bass as bass
import concourse.mybir as mybir
from concourse.bass_utils import run_bass_kernel_spmd

F32 = mybir.dt.float32
BF16 = mybir.dt.bfloat16
AF = mybir.ActivationFunctionType
ALU = mybir.AluOpType
AX = mybir.AxisListType

D = 1024
L = 2048
NT = 16
NEXP = 32
DE = 256
ALPHA = 2.0 ** 0.25
LN_EPS = 1e-5
RMS_EPS = 1e-6
ATT_SCALE = 128.0 ** -0.5
NEG = -30000.0
N_BISECT = 16
W1COLS = 2568
SB_BASE = 16640
SB_END = 229368


class Buf:
    __slots__ = ("name", "writer", "readers")

    def __init__(self, name):
        self.name = name
        self.writer = None
        self.readers = []


class Sched:
    ENGS = ("pe", "act", "dve", "pool", "sp")

    def __init__(self, nc, n_dma_sems=24):
        self.nc = nc
        self.ops = {e: [] for e in self.ENGS}
        self.cnt = {e: 0 for e in self.ENGS}
        self.seen = {e: {} for e in self.ENGS}
        self.n_dma_sems = n_dma_sems
        self.dma_i = {}
        self.dma_val = {}
        self.sems = {}

    def _deps(self, eng, reads, writes):
        need = {}

        def add(tok, skip_same):
            if tok is None:
                return
            e, key, val = tok
            if e == eng and skip_same:
                return
            if need.get(key, 0) < val:
                need[key] = val

        pe = eng == "pe"
        for b in reads:
            add(b.writer, pe)
        for b in writes:
            add(b.writer, pe)
            for r in b.readers:
                add(r, True)
        waits = []
        seen = self.seen[eng]
        for key, val in need.items():
            if seen.get(key, 0) < val:
                seen[key] = val
                waits.append((key, val))
        return waits

    def _commit(self, tok, reads, writes):
        for b in reads:
            b.readers.append(tok)
        for b in writes:
            b.writer = tok
            b.readers = []

    def group(self, eng, fns, reads=(), writes=()):
        waits = self._deps(eng, reads, writes)
        self.cnt[eng] += 1
        tok = (eng, "e_" + eng, self.cnt[eng])
        n = len(fns)
        for i, fn in enumerate(fns):
            self.ops[eng].append((fn, waits if i == 0 else (), ("e_" + eng, 1) if i == n - 1 else None))
        self._commit(tok, reads, writes)
        return tok

    def op(self, eng, fn, reads=(), writes=()):
        return self.group(eng, [fn], reads, writes)

    def dma(self, eng, fn, reads=(), writes=()):
        i = self.dma_i.get(eng, 0) % self.n_dma_sems
        self.dma_i[eng] = self.dma_i.get(eng, 0) + 1
        key = "d_%s_%d" % (eng, i)
        waits = list(self._deps(eng, reads, writes))
        prev = self.dma_val.get(key, 0)
        if prev > 0 and self.seen[eng].get(key, 0) < prev:
            self.seen[eng][key] = prev
            waits.append((key, prev))
        self.dma_val[key] = prev + 16
        tok = ("dma", key, self.dma_val[key])
        self.ops[eng].append((fn, waits, (key, 16)))
        self._commit(tok, reads, writes)
        return tok

    def barrier(self):
        targets = [("e_" + e, self.cnt[e]) for e in self.ENGS if self.cnt[e] > 0]
        targets += [(k, v) for k, v in self.dma_val.items() if v > 0]
        for e in self.ENGS:
            waits = []
            for key, val in targets:
                if key == "e_" + e and e != "pe":
                    pass
                if self.seen[e].get(key, 0) < val:
                    self.seen[e][key] = val
                    waits.append((key, val))
            if waits:
                self.ops[e].append((None, waits, None))

    def emit(self):
        nc = self.nc
        keys = ["e_" + e for e in self.ENGS] + sorted(self.dma_val.keys())
        with contextlib.ExitStack() as st:
            for k in keys:
                self.sems[k] = st.enter_context(nc.semaphore(k))
            block = st.enter_context(nc.Block())
            sems = self.sems

            def run(eng_name):
                def body(eng):
                    for fn, waits, inc in self.ops[eng_name]:
                        for key, val in waits:
                            eng.wait_ge(sems[key], val)
                        if fn is None:
                            continue
                        ins = fn(eng)
                        if inc is not None:
                            ins.then_inc(sems[inc[0]], inc[1])
                return body

            block.tensor(run("pe"))
            block.scalar(run("act"))
            block.vector(run("dve"))
            block.gpsimd(run("pool"))
            block.sync(run("sp"))


class Mem:
    def __init__(self, nc):
        self.nc = nc
        self.allocs = []

    def __call__(self, name, shape, dtype, off, life):
        esz = 4 if dtype == F32 else 2
        n = esz
        for s in shape[1:]:
            n *= s
        a0, a1 = SB_BASE + off, SB_BASE + off + n
        assert a1 <= SB_END, (name, a1)
        for (nm, b0, b1, lf) in self.allocs:
            if a0 < b1 and b0 < a1 and lf[0] <= life[1] and life[0] <= lf[1]:
                raise AssertionError("SBUF overlap %s vs %s" % (name, nm))
        self.allocs.append((name, a0, a1, life))
        return self.nc.alloc_sbuf_tensor_at(name, list(shape), dtype, offset=a0)


def t5_bucket_np(dist):
    n = np.maximum(dist, 0)
    nf = np.maximum(n, 1).astype(np.float32)
    large = 16 + (np.log(nf / np.float32(16)) / np.float32(math.log(128 / 16)) * np.float32(16)).astype(np.int32)
    large = np.minimum(large, 31)
    return np.where(n < 16, n, large).astype(np.int32)


def build_nc(debug=False, stop_after=99):
    nc = bass.Bass("TRN2", target_bir_lowering=False)

    def din(name, shape, dt=F32):
        return nc.dram_tensor(name, list(shape), dt, kind="ExternalInput").ap()

    xT_d = din("xT", [D, L])
    x_d = din("x", [L, D])
    w1_d = din("w1", [D, W1COLS])
    wg_d = din("wg", [D, 2048])
    kvg_d = din("kvg", [1, 128])
    wuv_d = din("wuv", [128, 512])
    wa_d = din("wa", [512, D])
    wb_d = din("wb", [512, D])
    wo_d = din("wo", [D, D])
    sinks_d = din("sinks", [1, 8])
    ln1g_d = din("ln1g", [1, D])
    ln1b_d = din("ln1b", [1, D])
    ln2g_d = din("ln2g", [1, D])
    ln2b_d = din("ln2b", [1, D])
    wr_d = din("wr", [D, 36])
    br_d = din("br", [1, 36])
    wgr_d = din("wgr", [NEXP * 128, 2048])
    wur_d = din("wur", [NEXP * 128, 2048])
    wdr_d = din("wdr", [NEXP * 128, 2048])
    wgb_d = nc.dram_tensor("wg_bf16", [NEXP * 128, 2048], BF16, kind="Internal").ap()
    wub_d = nc.dram_tensor("wu_bf16", [NEXP * 128, 2048], BF16, kind="Internal").ap()
    wdb_d = nc.dram_tensor("wd_bf16", [NEXP * 128, 2048], BF16, kind="Internal").ap()
    gates_d = nc.dram_tensor("gates_scr", [2048, L], BF16, kind="Internal").ap()
    xs_d = nc.dram_tensor("xs_scr", [64 * 128, D], BF16, kind="Internal").ap()
    ys_d = nc.dram_tensor("ys_scr", [64 * 128, D], F32, kind="Internal").ap()
    swab_d = din("swab", [4, 128, 512])
    dsab_d = din("dsab", [2, 128, 1024])
    c31_d = din("c31", [1, 8])
    out_d = nc.dram_tensor("out", [L, D], F32, kind="ExternalOutput").ap()
    dbg = {}
    if debug:
        for nm, shp in [("d_yb", [512, L]), ("d_ya", [512, L]), ("d_mergedT", [D, L]), ("d_acc", [L, D]),
                        ("d_comb", [L, 32]), ("d_ckvT", [128, L]), ("d_qlatT", [128, L])]:
            dbg[nm] = nc.dram_tensor(nm, shp, BF16 if nm in ("d_yb", "d_ya", "d_mergedT", "d_ckvT", "d_qlatT") else F32,
                                     kind="ExternalOutput").ap()

    S = Sched(nc)
    M = Mem(nc)
    _regs = {}

    def preg(e, val):
        if val not in _regs:
            _regs[val] = e.to_reg(val)
        return _regs[val]
    PS = [nc.alloc_psum_tensor("ps%d" % i, [128, 512], F32) for i in range(8)]
    Bps = [Buf("ps%d" % i) for i in range(8)]
    Bout = Buf("out")

    ident = M("ident", [128, 128], BF16, 0, (1, 7))
    gkv_bc = M("gkv_bc", [128, 128], F32, 256, (1, 7))
    esink = M("esink", [128, 8], F32, 768, (1, 7))
    negc31 = M("negc31", [128, 8], F32, 800, (1, 7))
    identf = M("identf", [128, 128], F32, 1024, (1, 7))
    causal01 = M("causal01", [128, 128], BF16, 1536, (1, 7))
    Bconst = Buf("const")

    S.op("pool", lambda e: e.memset(identf[:], 1.0), writes=[Bconst])
    S.op("pool", lambda e: e.affine_select(out=identf[:], in_=identf[:], pattern=[[1, 128]],
                                           compare_op=ALU.is_equal, fill=preg(e, 0.0), base=0, channel_multiplier=-1),
         reads=[Bconst], writes=[Bconst])
    S.op("pool", lambda e: e.tensor_copy(out=ident[:], in_=identf[:]), reads=[Bconst], writes=[Bconst])
    S.op("pool", lambda e: e.memset(causal01[:], 1.0), writes=[Bconst])
    S.op("pool", lambda e: e.affine_select(out=causal01[:], in_=causal01[:], pattern=[[1, 128]],
                                           compare_op=ALU.is_ge, fill=preg(e, 0.0), base=0, channel_multiplier=-1),
         reads=[Bconst], writes=[Bconst])
    S.dma("sp", lambda e: e.dma_start(out=gkv_bc[:], in_=kvg_d.to_broadcast([128, 128])), writes=[Bconst])
    S.dma("sp", lambda e: e.dma_start(out=esink[:], in_=sinks_d.to_broadcast([128, 8])), writes=[Bconst])
    S.dma("sp", lambda e: e.dma_start(out=negc31[:], in_=c31_d.to_broadcast([128, 8])), writes=[Bconst])
    S.op("act", lambda e: e.activation(out=esink[:], in_=esink[:], func=AF.Exp), reads=[Bconst], writes=[Bconst])
    S.op("dve", lambda e: e.tensor_scalar(out=negc31[:], in0=negc31[:], scalar1=-1.0, scalar2=None, op0=ALU.mult),
         reads=[Bconst], writes=[Bconst])

    xT = M("xT", [128, 8, L], BF16, 2048, (1, 1))
    o = 34816
    qlatT = M("qlatT", [128, 8, L], BF16, o, (1, 2.5)); o += 32768
    qidxT = M("qidxT", [128, 4, L], BF16, o, (1, 2.5)); o += 16384
    qbT = M("qbT", [128, 4, L], BF16, o, (1, 2)); o += 16384
    kidxT = M("kidxT", [128, L], BF16, o, (1, 2.5)); o += 4096
    kbT = M("kbT", [128, L], BF16, o, (1, 2)); o += 4096
    ckvT = M("ckvT", [128, L], BF16, o, (1, 2.5)); o += 4096
    kvW = M("kvW", [128, NT, 8, 65], BF16, o, (1, 2.5)); o += 16640
    vaug = M("vaug", [128, NT, 2, 65], BF16, o, (1, 2)); o += 4160
    widx = M("widx", [128, NT, 8], F32, o, (1, 2.5)); o += 512
    assert o == 133952
    R3 = 133952
    w1b = M("w1b", [128, 8, W1COLS], BF16, R3, (1, 1))
    wuvs = M("wuvs", [128, 512], F32, R3 + 41088, (1, 1))
    ckvtok = M("ckvtok", [128, NT, 128], BF16, R3 + 43136, (1, 1))
    wuvb = M("wuvb", [128, 512], BF16, R3 + 47232, (1, 1))
    p1tmp = M("p1tmp", [128, 128], F32, R3 + 48256, (1, 1))
    p1junk = M("p1junk", [128, 128], F32, R3 + 48768, (1, 1))

    BxT = [Buf("xT%d" % k) for k in range(8)]
    Bw1 = [Buf("w1_%d" % c) for c in range(6)]
    w1v = w1_d.rearrange("(k p) n -> p k n", p=128)
    xTv = xT_d.rearrange("(k p) n -> p k n", p=128)
    for k in range(8):
        S.dma("pool", lambda e, k=k: e.dma_start(out=xT[:, k, :], in_=xTv[:, k, :]), writes=[BxT[k]])
    w1blocks = [(0, 512), (512, 1024), (1024, 1536), (1536, 2048), (2048, 2304), (2304, W1COLS)]
    for c, (c0, c1) in enumerate(w1blocks):
        S.dma("pool", lambda e, c0=c0, c1=c1: e.dma_start(out=w1b[:, :, c0:c1], in_=w1v[:, :, c0:c1]),
              writes=[Bw1[c]])
    wgs = [M("wgs0", [128, 8, 512], BF16, 184320, (1, 1)), M("wgs1", [128, 8, 512], BF16, 192512, (1, 1))]
    gst = M("gst", [128, 2, 512], BF16, 200704, (1, 1))
    Bwgs = [Buf("wgs0"), Buf("wgs1")]; Bgst = [Buf("gst0"), Buf("gst1")]; Bgates = Buf("gates_d")
    wgv = wg_d.rearrange("(k p) n -> p k n", p=128)

    def load_wgs(c):
        wb_ = c % 2
        S.dma("pool", lambda e: e.dma_start(out=wgs[wb_][:], in_=wgv[:, :, c * 512:(c + 1) * 512]), writes=[Bwgs[wb_]])

    load_wgs(0)
    load_wgs(1)
    Bwuv = Buf("wuv")
    S.dma("sp", lambda e: e.dma_start(out=wuvs[:], in_=wuv_d), writes=[Bwuv])
    S.op("dve", lambda e: e.tensor_copy(out=wuvb[:], in_=wuvs[:]), reads=[Bwuv], writes=[Bwuv])

    def w1buf(col):
        for c, (c0, c1) in enumerate(w1blocks):
            if c0 <= col < c1:
                return Bw1[c]

    Bqlat = [Buf("qlat%d" % h) for h in range(8)]
    Bqidx = Buf("qidx"); Bqb = Buf("qb"); Bkidx = Buf("kidx"); Bkb = Buf("kb")
    Bckv = Buf("ckvT"); BkvW = Buf("kvW"); Bvaug = Buf("vaug"); Bwidx = Buf("widx"); Bcktok = Buf("ckvtok")

    fm_tiles = []
    for h in range(8):
        fm_tiles.append((lambda tb, h=h: qlatT[:, h, tb * 512:(tb + 1) * 512], ATT_SCALE, Bqlat[h]))
    for j in range(4):
        fm_tiles.append((lambda tb, j=j: qidxT[:, j, tb * 512:(tb + 1) * 512], 1.0, Bqidx))
    fm_tiles.append((lambda tb: kidxT[:, tb * 512:(tb + 1) * 512], 1.0, Bkidx))
    for j in range(4):
        fm_tiles.append((lambda tb, j=j: qbT[:, j, tb * 512:(tb + 1) * 512], 0.125, Bqb))
    fm_tiles.append((lambda tb: kbT[:, tb * 512:(tb + 1) * 512], 1.0, Bkb))
    ev = 0
    for ti, (dst, scale, bdst) in enumerate(fm_tiles):
        for tb in range(4):
            bank = ev % 4
            fns = []
            for k in range(8):
                fns.append(lambda e, k=k, ti=ti, tb=tb, bank=bank: e.matmul(
                    PS[bank][:], lhsT=w1b[:, k, ti * 128:(ti + 1) * 128], rhs=xT[:, k, tb * 512:(tb + 1) * 512],
                    start=(k == 0), stop=(k == 7)))
            S.group("pe", fns, reads=BxT + [w1buf(ti * 128)], writes=[Bps[bank]])
            if ev % 2 == 0:
                S.op("act", lambda e, dst=dst, tb=tb, bank=bank, scale=scale: e.activation(
                    out=dst(tb), in_=PS[bank][:], func=AF.Copy, scale=scale), reads=[Bps[bank]], writes=[bdst])
            else:
                S.op("dve", lambda e, dst=dst, tb=tb, bank=bank, scale=scale: e.tensor_scalar(
                    out=dst(tb), in0=PS[bank][:], scalar1=scale, scalar2=None, op0=ALU.mult),
                    reads=[Bps[bank]], writes=[bdst])
            ev += 1

    S.op("pool", lambda e: e.memset(vaug[:], 1.0), writes=[Bvaug])
    S.op("pool", lambda e: e.memset(kvW[:], 1.0), writes=[BkvW])
    Bp1tmp = Buf("p1tmp"); Bp1junk = Buf("p1junk")
    for tt in range(NT):
        bank = 4 + (tt % 2)
        fns = []
        for k in range(8):
            fns.append(lambda e, k=k, tt=tt, bank=bank: e.matmul(
                PS[bank][:, 0:264], lhsT=xT[:, k, tt * 128:(tt + 1) * 128], rhs=w1b[:, k, 2304:2568],
                start=(k == 0), stop=(k == 7)))
        S.group("pe", fns, reads=BxT + [Bw1[5]], writes=[Bps[bank]])
        S.op("act", lambda e, tt=tt, bank=bank: e.activation(
            out=p1junk[:, 0:128], in_=PS[bank][:, 0:128], func=AF.Square, accum_out=p1tmp[:, tt:tt + 1]),
            reads=[Bps[bank]], writes=[Bp1junk, Bp1tmp])
        S.op("act", lambda e, tt=tt, bank=bank: e.activation(
            out=vaug[:, tt, :, 0:64], in_=PS[bank][:, 128:256].rearrange("p (g d) -> p g d", g=2), func=AF.Copy),
            reads=[Bps[bank]], writes=[Bvaug])
        S.op("act", lambda e, tt=tt, bank=bank: e.activation(
            out=widx[:, tt, :], in_=PS[bank][:, 256:264], func=AF.Copy), reads=[Bps[bank]], writes=[Bwidx])
        S.op("dve", lambda e, tt=tt: e.tensor_scalar(
            out=p1tmp[:, 16 + tt:17 + tt], in0=p1tmp[:, tt:tt + 1], scalar1=1.0 / 128.0, scalar2=RMS_EPS,
            op0=ALU.mult, op1=ALU.add), reads=[Bp1tmp], writes=[Bp1tmp])
        S.op("act", lambda e, tt=tt: e.activation(
            out=p1tmp[:, 16 + tt:17 + tt], in_=p1tmp[:, 16 + tt:17 + tt], func=AF.Sqrt),
            reads=[Bp1tmp], writes=[Bp1tmp])
        S.op("dve", lambda e, tt=tt: e.reciprocal(
            out=p1tmp[:, 32 + tt:33 + tt], in_=p1tmp[:, 16 + tt:17 + tt]), reads=[Bp1tmp], writes=[Bp1tmp])
        S.op("dve", lambda e, tt=tt, bank=bank: e.scalar_tensor_tensor(
            out=ckvtok[:, tt, :], in0=PS[bank][:, 0:128], scalar=p1tmp[:, 32 + tt:33 + tt], in1=gkv_bc[:],
            op0=ALU.mult, op1=ALU.mult), reads=[Bps[bank], Bp1tmp, Bconst], writes=[Bcktok])
        tb_ = 6 + (tt % 2)
        S.op("pe", lambda e, tt=tt, tb_=tb_: e.transpose(
            out=PS[tb_][:].bitcast(BF16)[:, 0:128], in_=ckvtok[:, tt, :], identity=ident[:]),
            reads=[Bcktok, Bconst], writes=[Bps[tb_]])
        S.op("dve", lambda e, tt=tt, tb_=tb_: e.tensor_copy(
            out=ckvT[:, tt * 128:(tt + 1) * 128], in_=PS[tb_][:].bitcast(BF16)[:, 0:128]),
            reads=[Bps[tb_]], writes=[Bckv])
        S.op("pe", lambda e, tt=tt, bank=bank: e.matmul(
            PS[bank][:], lhsT=ckvT[:, tt * 128:(tt + 1) * 128], rhs=wuvb[:], start=True, stop=True),
            reads=[Bckv, Bwuv], writes=[Bps[bank]])
        S.op("act", lambda e, tt=tt, bank=bank: e.activation(
            out=kvW[:, tt, :, 0:64], in_=PS[bank][:].rearrange("p (h d) -> p h d", h=8), func=AF.Copy),
            reads=[Bps[bank]], writes=[BkvW])


    def gen_gates():
        gi_ = 0
        for c in range(4):
            wb_ = c % 2
            if c >= 1 and c + 1 < 4:
                load_wgs(c + 1)
            for j in range(4):
                for tb in range(4):
                    bank = 5 + (gi_ % 2)
                    sb_ = gi_ % 2
                    gi_ += 1
                    fns = [lambda e, k=k, j=j, tb=tb, bank=bank, wb_=wb_: e.matmul(
                        PS[bank][:], lhsT=wgs[wb_][:, k, j * 128:(j + 1) * 128], rhs=xT[:, k, tb * 512:(tb + 1) * 512],
                        start=(k == 0), stop=(k == 7)) for k in range(8)]
                    S.group("pe", fns, reads=BxT + [Bwgs[wb_]], writes=[Bps[bank]])
                    S.op("act", lambda e, bank=bank, sb_=sb_: e.activation(out=gst[:, sb_, :], in_=PS[bank][:], func=AF.Sigmoid),
                         reads=[Bps[bank]], writes=[Bgst[sb_]])
                    r0 = c * 512 + j * 128
                    S.dma("sp", lambda e, r0=r0, tb=tb, sb_=sb_: e.dma_start(
                        out=gates_d[r0:r0 + 128, tb * 512:(tb + 1) * 512], in_=gst[:, sb_, :]),
                        reads=[Bgst[sb_]], writes=[Bgates])
                    yield

    for _ in gen_gates():
        pass
    if debug:
        S.dma("sp", lambda e: e.dma_start(out=dbg["d_ckvT"], in_=ckvT[:]), reads=[Bckv], writes=[Bout])
        S.dma("sp", lambda e: e.dma_start(out=dbg["d_qlatT"], in_=qlatT[:, 0, :]), reads=Bqlat, writes=[Bout])
    S.barrier()
    if stop_after <= 1:
        S.emit()
        return nc

    yaT = M("yaT", [128, 4, L], BF16, 179904, (2, 3))
    ybT = M("ybT", [128, 4, L], BF16, 179904 + 16384, (2, 3))
    Eswa = M("Eswa", [128, 4, 512], BF16, R3, (2, 2))
    ytokA = M("ytokA", [128, 2, 512], BF16, R3 + 4096, (2, 2))
    st2a = M("st2a", [128, 128], F32, R3 + 6144, (2, 2))
    Edsa = M("Edsa", [128, 2, 1024], BF16, R3 + 20480, (2, 2.5))
    eTb = M("eTb", [128, 4, 512], BF16, R3 + 32768, (2, 2.5))
    pTb = M("pTb", [128, 4, 512], BF16, R3 + 36864, (2, 2.5))
    scr = M("scr", [128, 1024], F32, R3 + 40960, (2, 2))
    BEswa = Buf("Eswa"); BEdsa = Buf("Edsa"); Bscr = Buf("scr")
    BeT = [Buf("eT%d" % i) for i in range(4)]
    BpT = [Buf("pT%d" % i) for i in range(4)]
    BytokA = [Buf("ytokA0"), Buf("ytokA1")]
    Bst2a = Buf("st2a")
    ByaT = Buf("yaT"); BybT = Buf("ybT")

    for idx in range(4):
        typ = idx // 2
        S.dma("sp", lambda e, idx=idx: e.dma_start(out=scr[:, 0:512], in_=swab_d[idx]), writes=[Bscr])
        S.op("act", lambda e, idx=idx: e.activation(out=Eswa[:, idx, :], in_=scr[:, 0:512], func=AF.Exp),
             reads=[Bscr], writes=[BEswa])
        for j in range(4):
            if typ == 1:
                S.op("pool", lambda e, idx=idx, j=j: e.tensor_tensor(
                    out=Eswa[:, idx, j * 128:(j + 1) * 128], in0=Eswa[:, idx, j * 128:(j + 1) * 128],
                    in1=causal01[:], op=ALU.mult), reads=[BEswa, Bconst], writes=[BEswa])
            else:
                S.op("pool", lambda e, idx=idx, j=j: e.affine_select(
                    out=Eswa[:, idx, j * 128:(j + 1) * 128], in_=Eswa[:, idx, j * 128:(j + 1) * 128],
                    pattern=[[-1, 128]], compare_op=ALU.is_ge, fill=preg(e, 0.0), base=-1, channel_multiplier=1),
                    reads=[BEswa], writes=[BEswa])
    for typ in range(2):
        S.dma("sp", lambda e, typ=typ: e.dma_start(out=scr[:], in_=dsab_d[typ]), writes=[Bscr])
        for h in range(8):
            S.op("act", lambda e, typ=typ, h=h: e.activation(
                out=Edsa[:, typ, h * 128:(h + 1) * 128], in_=scr[:, h * 128:(h + 1) * 128], func=AF.Exp,
                bias=negc31[:, h:h + 1], scale=1.0), reads=[Bscr, Bconst], writes=[BEdsa])
            if typ == 1:
                S.op("pool", lambda e, h=h: e.tensor_tensor(
                    out=Edsa[:, 1, h * 128:(h + 1) * 128], in0=Edsa[:, 1, h * 128:(h + 1) * 128],
                    in1=causal01[:], op=ALU.mult), reads=[BEdsa, Bconst], writes=[BEdsa])

    def ytok_to_T(nblk, ysrc, ybufB, dstT, bdst):
        pv = PS[7][:].bitcast(BF16)
        fns = [lambda e, c=c: e.transpose(out=pv[:, c * 128:(c + 1) * 128],
                                         in_=ysrc[:, c * 128:(c + 1) * 128], identity=ident[:])
               for c in range(4)]
        S.group("pe", fns, reads=[ybufB, Bconst], writes=[Bps[7]])
        S.op("act", lambda e: e.activation(
            out=dstT[:, :, nblk * 128:(nblk + 1) * 128], in_=pv[:, 0:512].rearrange("p (c t) -> p c t", c=4),
            func=AF.Copy), reads=[Bps[7]], writes=[bdst])

    items = []
    for n in range(NT):
        for g in range(2):
            chunks = ([(n - 1, 0)] if n > 0 else []) + [(n, 1)]
            for ci, (kb, typ) in enumerate(chunks):
                items.append((n, g, kb, typ, ci == 0, ci == len(chunks) - 1))

    def swa_stage1(idx):
        n, g, kb, typ, first, last = items[idx]
        r = idx % 4
        lb = idx % 3
        S.op("pe", lambda e: e.matmul(
            PS[lb][:], lhsT=kbT[g * 64:(g + 1) * 64, kb * 128:(kb + 1) * 128],
            rhs=qbT[g * 64:(g + 1) * 64, :, n * 128:(n + 1) * 128], start=True, stop=True),
            reads=[Bkb, Bqb], writes=[Bps[lb]])
        S.op("act", lambda e: e.activation(out=eTb[:, r, :], in_=PS[lb][:], func=AF.Exp),
             reads=[Bps[lb]], writes=[BeT[r]])
        S.op("dve", lambda e: e.tensor_tensor(
            out=pTb[:, r, :], in0=eTb[:, r, :], in1=Eswa[:, typ * 2 + g, :], op=ALU.mult),
            reads=[BeT[r], BEswa], writes=[BpT[r]])

    def swa_stage2(idx):
        n, g, kb, typ, first, last = items[idx]
        r = idx % 4
        ybuf = n % 2
        obank = 3 + g
        fns = [lambda e, j=j: e.matmul(
            PS[obank][:, j * 65:(j + 1) * 65], lhsT=pTb[:, r, j * 128:(j + 1) * 128],
            rhs=vaug[:, kb, g, :], start=(first and j == 0), stop=(last and j == 3),
            skip_group_check=True) for j in range(4)]
        S.group("pe", fns, reads=[BpT[r], Bvaug], writes=[Bps[obank]])
        if not last:
            return
        ov = PS[obank][:, 0:260].rearrange("p (j c) -> p j c", j=4)
        c0 = ybuf * 16 + g * 4
        S.op("dve", lambda e: e.tensor_tensor(
            out=st2a[:, c0:c0 + 4].rearrange("p (j o) -> p j o", o=1), in0=ov[:, :, 64:65],
            in1=esink[:, g * 4:(g + 1) * 4].rearrange("p (j o) -> p j o", o=1), op=ALU.add),
            reads=[Bps[obank], Bconst], writes=[Bst2a])
        S.op("dve", lambda e: e.reciprocal(out=st2a[:, 32 + c0:32 + c0 + 4], in_=st2a[:, c0:c0 + 4]),
             reads=[Bst2a], writes=[Bst2a])
        S.op("dve", lambda e: e.tensor_tensor(
            out=ytokA[:, ybuf, g * 256:(g + 1) * 256].rearrange("p (j d) -> p j d", j=4), in0=ov[:, :, 0:64],
            in1=st2a[:, 32 + c0:32 + c0 + 4].rearrange("p (j o) -> p j o", o=1).to_broadcast([128, 4, 64]),
            op=ALU.mult), reads=[Bps[obank], Bst2a], writes=[BytokA[ybuf]])
        if g == 1:
            ytok_to_T(n, ytokA[:, ybuf, :], BytokA[ybuf], ybT, BybT)

    LAG = 2
    for idx in range(len(items) + LAG):
        if idx < len(items):
            swa_stage1(idx)
        if idx >= LAG:
            swa_stage2(idx - LAG)
    if debug:
        S.dma("sp", lambda e: e.dma_start(out=dbg["d_yb"].rearrange("(c p) t -> p c t", p=128), in_=ybT[:]),
              reads=[BybT], writes=[Bout])
    S.barrier()

    QB = 83968
    scoresA = M("scoresA", [128, L], F32, R3, (2.5, 2.5))
    scoresB = M("scoresB", [128, L], F32, QB, (2.5, 2.5))
    scoresC = M("scoresC", [128, L], F32, 2048, (2.5, 2.5))
    scoresD = M("scoresD", [128, L], F32, 2048 + 8192, (2.5, 2.5))
    maskbA = M("maskbA", [128, L], BF16, R3 + 8192, (2.5, 2.5))
    maskbB = M("maskbB", [128, L], BF16, 2048 + 16384, (2.5, 2.5))
    osb = M("osb", [128, 2, 520], F32, 129280, (2.5, 2.5))
    maskT_lo = M("maskT_lo", [128, 2, NT, 128], BF16, R3 + 12288, (2.5, 2.5))
    maskT_hi = M("maskT_hi", [128, 2, NT, 128], BF16, 2048 + 20480, (2.5, 2.5))
    rbuf = M("rbuf", [128, 4, 512], BF16, R3 + 40960, (2.5, 2.5))
    dg = M("dg", [128, 2, 8, 128], BF16, QB + 8192, (2.5, 2.5))
    ytokB = M("ytokB", [128, 2, 512], BF16, QB + 12288, (2.5, 2.5))
    st2 = M("st2", [128, 4, 64], F32, QB + 14336, (2.5, 2.5))
    steps = M("steps", [128, 32], F32, QB + 15360, (2.5, 2.5))
    sd0 = M("sd0", [128, 4, 32], F32, QB + 15488, (2.5, 2.5))
    zt = M("zt", [128, D], BF16, R3 + 24576, (2.5, 2.5))
    Bzero = Buf("zero")
    S.op("pool", lambda e: e.memset(zt[:], 0.0), writes=[Bzero])
    xs_v = xs_d.rearrange("(j p) n -> p j n", p=128)
    for j0 in range(0, 64, 8):
        S.dma("sp", lambda e, j0=j0: e.dma_start(
            out=xs_v[:, j0:j0 + 8, :], in_=zt[:].rearrange("p (o n) -> p o n", o=1).to_broadcast([128, 8, D])),
            reads=[Bzero], writes=[Bzero])
    Bwbf = Buf("wbf16")
    bg_jobs = []
    for r0 in range(0, NEXP * 128, 512):
        for src_, dst_ in ((wgr_d, wgb_d), (wur_d, wub_d), (wdr_d, wdb_d)):
            bg_jobs.append((src_, dst_, r0))

    def bg_convert(n=1):
        for _ in range(n):
            if not bg_jobs:
                return
            src_, dst_, r0 = bg_jobs.pop(0)
            S.dma("pool", lambda e, src_=src_, dst_=dst_, r0=r0: e.dma_start(
                out=dst_[r0:r0 + 512, :], in_=src_[r0:r0 + 512, :]), writes=[Bwbf])
    kblk0 = M("kblk0", [128, L], BF16, 104448, (2.5, 2.5))
    kblk1 = M("kblk1", [128, L], BF16, 2048 + 28672, (2.5, 2.5))
    kblk = [kblk0, kblk1]
    Bkblk = Buf("kblk")
    S.op("pool", lambda e: e.memset(kblk0[:], 0.0), writes=[Bkblk])
    S.op("pool", lambda e: e.memset(kblk1[:], 0.0), writes=[Bkblk])
    S.op("dve", lambda e: e.tensor_copy(out=kblk0[0:64, :], in_=kidxT[0:64, :]), reads=[Bkidx, Bkblk], writes=[Bkblk])
    S.op("dve", lambda e: e.tensor_copy(out=kblk1[64:128, :], in_=kidxT[64:128, :]), reads=[Bkidx, Bkblk], writes=[Bkblk])
    scoresX = [scoresA, scoresB, scoresC, scoresD]
    maskbX = [maskbA, maskbB]
    Bscore = [Buf("scores%d" % i) for i in range(4)]
    BmaskX = [Buf("mask0"), Buf("mask1")]; BmaskT = [Buf("maskT%d" % i) for i in range(4)]
    Brb = [Buf("rb%d" % i) for i in range(4)]
    Bdg = [Buf("dg0"), Buf("dg1")]
    BytokB = [Buf("ytokB0"), Buf("ytokB1")]
    Bbis = [Buf("bis%d" % i) for i in range(4)]
    Bst = [Buf("st%d" % i) for i in range(4)]
    Bosb = [Buf("osb0"), Buf("osb1")]
    Bsteps = Buf("steps")
    for k in range(N_BISECT):
        S.op("pool", lambda e, k=k: e.memset(steps[:, k:k + 1], 2.0 ** -(k + 1)), writes=[Bsteps])
    rotA = {"s1": 0, "rb": 0}

    def genS(i):
        Si = (i + 1) * 128
        sb = i % 2
        q4 = i % 4
        sc_t = scoresX[q4]
        for h in range(8):
            S.op("act", lambda e, h=h: e.activation(
                out=dg[:, sb, h, :], in_=ident[:], func=AF.Copy, scale=widx[:, i, h:h + 1]),
                reads=[Bconst, Bwidx], writes=[Bdg[sb]])
        yield
        nsc = (Si + 511) // 512
        stepsS = [(sc, h) for sc in range(nsc) for h in range(8)]
        slots = {}

        def S1(n):
            sc, h = stepsS[n]
            c0, c1 = sc * 512, min(Si, sc * 512 + 512)
            w = c1 - c0
            rr = rotA["rb"] % 4
            ab = 2 + (rotA["rb"] % 2)
            rotA["rb"] += 1
            slots[n] = rr
            hp = (h % 2) * 64
            S.op("pe", lambda e: e.matmul(
                PS[ab][:, 0:w], lhsT=qidxT[:, h // 2, i * 128:(i + 1) * 128],
                rhs=kblk[h % 2][:, c0:c1], start=True, stop=True),
                reads=[Bqidx, Bkblk], writes=[Bps[ab]])
            S.op("act", lambda e: e.activation(
                out=rbuf[:, rr, 0:w], in_=PS[ab][:, 0:w], func=AF.Relu), reads=[Bps[ab]], writes=[Brb[rr]])

        def S2(n):
            sc, h = stepsS[n]
            c0, c1 = sc * 512, min(Si, sc * 512 + 512)
            w = c1 - c0
            rr = slots[n]
            S.op("pe", lambda e: e.matmul(
                PS[6][:, 0:w], lhsT=dg[:, sb, h, :], rhs=rbuf[:, rr, 0:w], start=(h == 0), stop=(h == 7)),
                reads=[Bdg[sb], Brb[rr]], writes=[Bps[6]])
            if h == 7:
                S.op("act", lambda e: e.activation(out=sc_t[:, c0:c1], in_=PS[6][:, 0:w], func=AF.Copy),
                     reads=[Bps[6]], writes=[Bscore[q4]])

        S1(0)
        for n in range(len(stepsS)):
            if n + 1 < len(stepsS):
                S1(n + 1)
            S2(n)
            yield

    def genB(i):
        Si = (i + 1) * 128
        sb = i % 2
        q4 = i % 4
        sc_t = scoresX[q4]
        maskb = maskbX[sb]
        maskT = maskT_lo if q4 < 2 else maskT_hi
        Bmask = BmaskX[sb]
        stv = st2[:, q4, :]
        BB = [Bbis[q4]]
        S.op("dve", lambda e: e.tensor_reduce(out=stv[:, 0:1], in_=sc_t[:, 0:Si], axis=AX.X, op=ALU.max),
             reads=[Bscore[q4]], writes=BB)
        yield
        S.op("dve", lambda e: e.tensor_reduce(out=stv[:, 1:2], in_=sc_t[:, 0:Si], axis=AX.X, op=ALU.min),
             reads=[Bscore[q4]] + BB, writes=BB)
        yield
        S.op("dve", lambda e: e.scalar_tensor_tensor(
            out=stv[:, 2:3], in0=stv[:, 0:1], scalar=1.0, in1=stv[:, 1:2], op0=ALU.add, op1=ALU.subtract),
            reads=BB, writes=BB)
        S.op("dve", lambda e: e.tensor_scalar(
            out=sd0[:, q4, 0:N_BISECT], in0=steps[:, 0:N_BISECT], scalar1=stv[:, 2:3], scalar2=None, op0=ALU.mult),
            reads=BB + [Bsteps], writes=BB)
        S.op("dve", lambda e: e.tensor_tensor(out=stv[:, 3:4], in0=sd0[:, q4, 0:1], in1=stv[:, 1:2], op=ALU.add),
             reads=BB, writes=BB)
        S.op("pool", lambda e: e.affine_select(
            out=sc_t[:, i * 128:(i + 1) * 128], in_=sc_t[:, i * 128:(i + 1) * 128], pattern=[[-1, 128]],
            compare_op=ALU.is_ge, fill=preg(e, -1.0e30), base=0, channel_multiplier=1),
            reads=[Bscore[q4]] + BB, writes=[Bscore[q4]])
        yield
        for it in range(N_BISECT):
            S.op("dve", lambda e: e.tensor_scalar(
                out=maskb[:, 0:Si], in0=sc_t[:, 0:Si], scalar1=stv[:, 3:4], scalar2=None,
                op0=ALU.is_ge, op1=ALU.add, accum_out=stv[:, 4:5]),
                reads=[Bscore[q4]] + BB, writes=[Bmask] + BB)
            yield
            S.op("dve", lambda e: e.tensor_scalar(
                out=stv[:, 5:6], in0=stv[:, 4:5], scalar1=255.5, scalar2=0.5, op0=ALU.is_ge, op1=ALU.subtract),
                reads=BB, writes=BB)
            S.op("dve", lambda e, it=it: e.scalar_tensor_tensor(
                out=stv[:, 3:4], in0=stv[:, 5:6], scalar=sd0[:, q4, it:it + 1], in1=stv[:, 3:4],
                op0=ALU.mult, op1=ALU.add), reads=BB, writes=BB)
            yield
        S.op("dve", lambda e: e.scalar_tensor_tensor(
            out=stv[:, 6:7], in0=sd0[:, q4, N_BISECT - 1:N_BISECT], scalar=-0.5, in1=stv[:, 3:4],
            op0=ALU.mult, op1=ALU.add), reads=BB, writes=BB)
        S.op("dve", lambda e: e.tensor_scalar(
            out=maskb[:, 0:Si], in0=sc_t[:, 0:Si], scalar1=stv[:, 6:7], scalar2=None, op0=ALU.is_ge),
            reads=[Bscore[q4]] + BB, writes=[Bmask])
        yield
        pv = PS[7][:].bitcast(BF16)
        for q0 in range(0, i + 1, 4):
            q1 = min(i + 1, q0 + 4)
            fns = [lambda e, kb=kb, q0=q0: e.transpose(
                out=pv[:, (kb - q0) * 128:(kb - q0 + 1) * 128], in_=maskb[:, kb * 128:(kb + 1) * 128],
                identity=ident[:]) for kb in range(q0, q1)]
            S.group("pe", fns, reads=[Bmask, Bconst], writes=[Bps[7]])
            S.op("act", lambda e, q0=q0, q1=q1: e.activation(
                out=maskT[:, sb, q0:q1, :], in_=pv[:, 0:(q1 - q0) * 128].rearrange("p (c t) -> p c t", t=128),
                func=AF.Identity, scale=100.0, bias=-100.0), reads=[Bps[7]], writes=[BmaskT[q4]])
            yield

    def genA(i):
        sb = i % 2
        maskT = maskT_lo if (i % 4) < 2 else maskT_hi
        pairs = [(kb, hg) for kb in range(i + 1) for hg in range(2)]
        info = {}

        def stage1(pi):
            kb, hg = pairs[pi]
            near = kb >= i - 1
            typ = 1 if kb == i else 0
            r = rotA["s1"] % 4
            lb = rotA["s1"] % 2
            rotA["s1"] += 1
            info[pi] = r
            masked = i >= 2
            fns = [lambda e: e.matmul(
                PS[lb][:], lhsT=ckvT[:, kb * 128:(kb + 1) * 128],
                rhs=qlatT[:, hg * 4:(hg + 1) * 4, i * 128:(i + 1) * 128], start=True, stop=(not masked),
                skip_group_check=True)]
            rd = [Bckv] + Bqlat[hg * 4:(hg + 1) * 4]
            if masked:
                for j in range(4):
                    fns.append(lambda e, j=j: e.matmul(
                        PS[lb][:, j * 128:(j + 1) * 128], lhsT=ident[:], rhs=maskT[:, sb, kb, :], start=False,
                        stop=(j == 3), skip_group_check=True))
                rd = rd + [BmaskT[i % 4], Bconst]
            S.group("pe", fns, reads=rd, writes=[Bps[lb]])
            if near:
                S.op("act", lambda e: e.activation(out=eTb[:, r, :], in_=PS[lb][:], func=AF.Exp),
                     reads=[Bps[lb]], writes=[BeT[r]])
                S.op("pool", lambda e: e.tensor_tensor(out=pTb[:, r, :], in0=eTb[:, r, :],
                                                       in1=Edsa[:, typ, hg * 512:(hg + 1) * 512], op=ALU.mult),
                     reads=[BeT[r], BEdsa], writes=[BpT[r]])
            else:
                S.op("act", lambda e: e.activation(out=pTb[:, r, :], in_=PS[lb][:], func=AF.Exp),
                     reads=[Bps[lb]], writes=[BpT[r]])

        def stage2(pi):
            kb, hg = pairs[pi]
            r = info[pi]
            obank = 4 + hg
            fns = [lambda e, j=j: e.matmul(
                PS[obank][:, j * 65:(j + 1) * 65], lhsT=pTb[:, r, j * 128:(j + 1) * 128],
                rhs=kvW[:, kb, hg * 4 + j, :], start=(kb == 0 and j == 0), stop=(kb == i and j == 3),
                skip_group_check=True) for j in range(4)]
            S.group("pe", fns, reads=[BpT[r], BkvW], writes=[Bps[obank]])

        LAGA = 1
        for pi in range(len(pairs) + LAGA):
            if pi < len(pairs):
                stage1(pi)
            if pi >= LAGA:
                stage2(pi - LAGA)
            yield
        for hg in range(2):
            obank = 4 + hg
            S.op("act", lambda e, hg=hg, obank=obank: e.activation(
                out=osb[:, sb, hg * 260:(hg + 1) * 260], in_=PS[obank][:, 0:260], func=AF.Copy),
                reads=[Bps[obank]], writes=[Bosb[sb]])
        yield

        def finish():
            q4 = i % 4
            for hg in range(2):
                ov = osb[:, sb, hg * 260:(hg + 1) * 260].rearrange("p (j c) -> p j c", j=4)
                c0 = 32 + hg * 4
                S.op("dve", lambda e, ov=ov, c0=c0: e.reciprocal(
                    out=st2[:, q4, c0:c0 + 4].rearrange("p (j o) -> p j o", o=1), in_=ov[:, :, 64:65]),
                    reads=[Bosb[sb]], writes=[Bst[q4]])
                S.op("dve", lambda e, ov=ov, c0=c0, hg=hg: e.tensor_tensor(
                    out=ytokB[:, sb, hg * 256:(hg + 1) * 256].rearrange("p (j d) -> p j d", j=4), in0=ov[:, :, 0:64],
                    in1=st2[:, q4, c0:c0 + 4].rearrange("p (j o) -> p j o", o=1).to_broadcast([128, 4, 64]),
                    op=ALU.mult), reads=[Bosb[sb], Bst[q4]], writes=[BytokB[sb]])
            ytok_to_T(i, ytokB[:, sb, :], BytokB[sb], yaT, ByaT)
        post_round.append(finish)

    tick = {"n": 0}

    def interleave(gens):
        gens = [g for g in gens if g is not None]
        while gens:
            tick["n"] += 1
            if tick["n"] % 25 == 0:
                bg_convert(1)
            for g in list(gens):
                try:
                    next(g)
                except StopIteration:
                    gens.remove(g)

    import itertools
    Bjunk = Buf("junk")

    def warm_pe(n):
        fns = [lambda e: e.matmul(PS[7][:, 256:512], lhsT=ident[:], rhs=Edsa[:, 0, 0:256], start=True, stop=True,
                                  skip_group_check=True) for _ in range(n)]
        S.group("pe", fns, reads=[BEdsa, Bconst], writes=[Bjunk])

    post_round = []
    NP = NT // 2
    for rnd in range(0, NP + 2):
        gs = []
        if 1 <= rnd + 1 < NP:
            gs.append(itertools.chain(genS(2 * rnd + 2), genS(2 * rnd + 3)))
        if 1 <= rnd < NP:
            gs.append(genB(2 * rnd))
            gs.append(genB(2 * rnd + 1))
        if 0 <= rnd - 1 < NP:
            gs.append(itertools.chain(genA(2 * rnd - 2), genA(2 * rnd - 1)))
        interleave(gs)
        for f_ in post_round:
            f_()
        del post_round[:]

    if debug:
        S.dma("sp", lambda e: e.dma_start(out=dbg["d_ya"].rearrange("(c p) t -> p c t", p=128), in_=yaT[:]),
              reads=[ByaT], writes=[Bout])
    S.barrier()
    if stop_after <= 2:
        S.emit()
        return nc

    wab = M("wab", [128, 4, D], BF16, 34816, (3, 3))
    wbb = M("wbb", [128, 4, D], BF16, 43008, (3, 3))
    mergedT = M("mergedT", [128, 8, L], BF16, 83968, (3, 4))
    sgate = M("sgate", [128, 6, 512], BF16, 51200, (3, 3))
    mtmp = M("mtmp", [128, 2, 2, 512], F32, 57344, (3, 3))
    Bwa = Buf("wa"); Bwb = Buf("wb")
    S.dma("pool", lambda e: e.dma_start(out=wab[:], in_=wa_d.rearrange("(k p) n -> p k n", p=128)), writes=[Bwa])
    S.dma("pool", lambda e: e.dma_start(out=wbb[:], in_=wb_d.rearrange("(k p) n -> p k n", p=128)), writes=[Bwb])
    woutb = M("woutb", [128, 8, D], BF16, 67584, (3, 4))
    ln1g = M("ln1g", [128, D], F32, 116736, (3, 4))
    ln1bA = M("ln1bA", [128, D], F32, 120832, (3, 4))
    Bwout = Buf("wout"); Bln1 = Buf("ln1")
    wov = wo_d.rearrange("(k p) n -> p k n", p=128)

    def prefetch_p3b():
        S.dma("pool", lambda e: e.dma_start(out=woutb[:, 0:4, :], in_=wov[:, 0:4, :]), writes=[Bwout])
        S.dma("pool", lambda e: e.dma_start(out=woutb[:, 4:8, :], in_=wov[:, 4:8, :]), writes=[Bwout])
        S.dma("sp", lambda e: e.dma_start(out=ln1g[:], in_=ln1g_d.to_broadcast([128, D])), writes=[Bln1])
        S.dma("sp", lambda e: e.dma_start(out=ln1bA[:], in_=ln1b_d.to_broadcast([128, D])), writes=[Bln1])
        S.op("act", lambda e: e.activation(out=ln1bA[:], in_=ln1bA[:], func=AF.Copy, scale=ALPHA),
             reads=[Bln1], writes=[Bln1])

    Bsg = [Buf("sg%d" % i) for i in range(6)]
    Bmt = [Buf("mt0"), Buf("mt1")]
    Bmerged = [Buf("merged%d" % f) for f in range(8)]

    def p3a_load(n):
        f, tb = n // 4, n % 4
        for gi in range(2):
            sl = (n % 3) * 2 + gi
            r0 = gi * 1024 + f * 128
            S.dma("sp", lambda e, r0=r0, tb=tb, sl=sl: e.dma_start(
                out=sgate[:, sl, :], in_=gates_d[r0:r0 + 128, tb * 512:(tb + 1) * 512]), reads=[Bgates], writes=[Bsg[sl]])

    p3a_load(0)
    p3a_load(1)
    it3 = 0
    for n in range(32):
        f, tb = n // 4, n % 4
        ts_ = slice(tb * 512, (tb + 1) * 512)
        if n + 2 < 32:
            p3a_load(n + 2)
        pb = (n % 2) * 2
        mi = n % 2
        for bi, (wt, yT, bw, by) in enumerate(((wab, yaT, Bwa, ByaT), (wbb, ybT, Bwb, BybT))):
            fns = [lambda e, k=k, wt=wt, yT=yT, f=f, ts_=ts_, pb=pb, bi=bi: e.matmul(
                PS[pb + bi][:], lhsT=wt[:, k, f * 128:(f + 1) * 128], rhs=yT[:, k, ts_],
                start=(k == 0), stop=(k == 3)) for k in range(4)]
            S.group("pe", fns, reads=[bw, by], writes=[Bps[pb + bi]])
            sl = (n % 3) * 2 + bi
            S.op("dve", lambda e, pb=pb, bi=bi, sl=sl, mi=mi: e.tensor_tensor(
                out=mtmp[:, mi, bi, :], in0=sgate[:, sl, :], in1=PS[pb + bi][:], op=ALU.mult),
                reads=[Bsg[sl], Bps[pb + bi]], writes=[Bmt[mi]])
        S.op("pool", lambda e, mi=mi, f=f, ts_=ts_: e.tensor_tensor(
            out=mergedT[:, f, ts_], in0=mtmp[:, mi, 0, :], in1=mtmp[:, mi, 1, :], op=ALU.add),
            reads=[Bmt[mi]], writes=[Bmerged[f]])
        bg_convert(1)
        if n == 2:
            prefetch_p3b()
    if debug:
        S.dma("sp", lambda e: e.dma_start(out=dbg["d_mergedT"].rearrange("(c p) t -> p c t", p=128), in_=mergedT[:]),
              reads=Bmerged, writes=[Bout])
    S.barrier()
    if stop_after <= 3:
        S.emit()
        return nc

    ACC_OFF = 147136
    acc = M("acc", [128, NT, D], F32, ACC_OFF, (4, 7))
    x1T = M("x1T", [128, 8, L], BF16, 2048, (4, 5))
    x1tok = M("x1tok", [128, NT, D], BF16, 34816, (4, 6))
    xtok = M("xtok", [128, 2, D], F32, 124928, (4, 4))
    rres = M("rres", [128, 3, D], F32, 133120, (4, 4))
    lnst = M("lnst", [128, 3, 32], F32, 145408, (4, 4))
    Bxtok = [Buf("xtok0"), Buf("xtok1")]
    Brres = [Buf("r%d" % i) for i in range(3)]
    Blnst = [Buf("lnst%d" % i) for i in range(3)]
    Bacc = [Buf("acc%d" % t) for t in range(NT)]
    Bx1tok = [Buf("x1tok%d" % t) for t in range(NT)]
    Bx1T = Buf("x1T")

    def ln_scaled(src, srcB, stv, stB, gt, btA, gB, dst, dstB, alpha=ALPHA):
        S.op("dve", lambda e: e.bn_stats(out=stv[:, 0:6], in_=src[:, 0:512]), reads=srcB, writes=[stB])
        S.op("dve", lambda e: e.bn_stats(out=stv[:, 6:12], in_=src[:, 512:1024]), reads=srcB + [stB], writes=[stB])
        S.op("dve", lambda e: e.bn_aggr(out=stv[:, 12:14], in_=stv[:, 0:12]), reads=[stB], writes=[stB])
        S.op("dve", lambda e: e.tensor_scalar(out=stv[:, 14:15], in0=stv[:, 13:14], scalar1=LN_EPS, scalar2=None,
                                              op0=ALU.add), reads=[stB], writes=[stB])
        S.op("act", lambda e: e.activation(out=stv[:, 14:15], in_=stv[:, 14:15], func=AF.Sqrt), reads=[stB], writes=[stB])
        S.op("dve", lambda e: e.reciprocal(out=stv[:, 15:16], in_=stv[:, 14:15]), reads=[stB], writes=[stB])
        S.op("dve", lambda e: e.tensor_scalar(out=stv[:, 16:17], in0=stv[:, 15:16], scalar1=alpha, scalar2=None,
                                              op0=ALU.mult), reads=[stB], writes=[stB])
        S.op("dve", lambda e: e.scalar_tensor_tensor(out=src, in0=src, scalar=stv[:, 12:13], in1=gt[:],
                                                     op0=ALU.subtract, op1=ALU.mult), reads=srcB + [stB, gB], writes=srcB)
        S.op("dve", lambda e: e.scalar_tensor_tensor(out=dst, in0=src, scalar=stv[:, 16:17], in1=btA[:],
                                                     op0=ALU.mult, op1=ALU.add), reads=srcB + [stB, gB], writes=dstB)

    def p3b_A(tt):
        b2, b3 = tt % 2, tt % 3
        S.dma("sp", lambda e: e.dma_start(out=xtok[:, b2, :], in_=x_d[tt * 128:(tt + 1) * 128, :]), writes=[Bxtok[b2]])
        for half in range(2):
            bank = b3 * 2 + half
            fns = [lambda e, k=k, half=half, bank=bank: e.matmul(
                PS[bank][:], lhsT=mergedT[:, k, tt * 128:(tt + 1) * 128], rhs=woutb[:, k, half * 512:(half + 1) * 512],
                start=(k == 0), stop=(k == 7)) for k in range(8)]
            S.group("pe", fns, reads=Bmerged + [Bwout], writes=[Bps[bank]])
            S.op("dve", lambda e, half=half, bank=bank: e.scalar_tensor_tensor(
                out=rres[:, b3, half * 512:(half + 1) * 512], in0=xtok[:, b2, half * 512:(half + 1) * 512], scalar=ALPHA,
                in1=PS[bank][:], op0=ALU.mult, op1=ALU.add), reads=[Bxtok[b2], Bps[bank]], writes=[Brres[b3]])

    def p3b_B(tt):
        b3 = tt % 3
        ln_scaled(rres[:, b3, :], [Brres[b3]], lnst[:, b3, :], Blnst[b3], ln1g, ln1bA, Bln1, acc[:, tt, :], [Bacc[tt]])
        S.op("act", lambda e: e.activation(out=x1tok[:, tt, :], in_=acc[:, tt, :], func=AF.Copy, scale=1.0 / ALPHA),
             reads=[Bacc[tt]], writes=[Bx1tok[tt]])

    def p3b_C(tt):
        for half in range(2):
            tbk = 6 + half
            pv = PS[tbk][:].bitcast(BF16)
            fns = [lambda e, c=c, half=half, pv=pv: e.transpose(
                out=pv[:, c * 128:(c + 1) * 128], in_=x1tok[:, tt, (half * 4 + c) * 128:(half * 4 + c + 1) * 128],
                identity=ident[:]) for c in range(4)]
            S.group("pe", fns, reads=[Bx1tok[tt], Bconst], writes=[Bps[tbk]])
            S.op("act", lambda e, half=half, pv=pv: e.activation(
                out=x1T[:, half * 4:(half + 1) * 4, tt * 128:(tt + 1) * 128],
                in_=pv[:, 0:512].rearrange("p (c t) -> p c t", c=4), func=AF.Copy), reads=[Bps[tbk]], writes=[Bx1T])

    for s_ in range(NT + 2):
        if s_ < NT:
            p3b_A(s_)
        if 1 <= s_ <= NT:
            p3b_B(s_ - 1)
        if s_ >= 2:
            p3b_C(s_ - 2)
    if debug:
        S.dma("sp", lambda e: e.dma_start(out=dbg["d_acc"].rearrange("(t p) d -> p t d", p=128), in_=acc[:]),
              reads=Bacc, writes=[Bout])
    S.barrier()
    if stop_after <= 4:
        S.emit()
        return nc

    bg_convert(len(bg_jobs))
    o = 67584
    wrb = M("wrb", [128, 8, 36], BF16, o, (5, 5)); o += 1024
    brbc = M("brbc", [128, 36], F32, o, (5, 5)); o += 256
    ustr = M("ustr", [128, 128], BF16, o, (5, 5)); o += 256
    onesb = M("onesb", [128, 128], BF16, o, (5, 5)); o += 256
    jrow = M("jrow", [128, 64], F32, o, (5, 5)); o += 256
    thr16 = M("thr16", [128, 16], F32, o, (5, 5)); o += 64
    piota = M("piota", [128, 2], F32, o, (5, 5)); o += 64
    lg = M("lg", [128, NT, 36], F32, o, (5, 5)); o += 2304
    elm = M("elm", [128, NT, 32], F32, o, (5, 5)); o += 2048
    exr = M("exr", [128, NT, 32], F32, o, (5, 5)); o += 2048
    sel = M("sel", [128, NT, 32], F32, o, (5, 5)); o += 2048
    selb = M("selb", [128, NT, 32], BF16, o, (5, 5)); o += 1024
    comb = M("comb", [128, NT, 32], F32, o, (5, 5)); o += 2048
    posf = M("posf", [128, NT, 32], F32, o, (5, 5)); o += 2048
    tmpa = M("tmpa", [128, NT, 32], F32, o, (5, 5)); o += 2048
    tmpb = M("tmpb", [128, 64, 32], F32, o, (5, 5)); o += 8192
    sm = M("sm", [128, 24, NT], F32, o, (5, 5)); o += 1536
    pen = M("pen", [128, NT, 4], F32, o, (5, 5)); o += 256
    gex = M("gex", [128, NT, 4], F32, o, (5, 5)); o += 256
    ntr = M("ntr", [128, 128], BF16, o, (5, 5)); o += 256
    cnt32 = M("cnt32", [128, 24], F32, o, (5, 5)); o += 96
    toffs = M("toffs", [128, 64], F32, o, (5, 5)); o += 256
    ejf = M("ejf", [128, 64], F32, o, (5, 5)); o += 256
    assert o < 116736
    posu = M("posu", [128, NT, 2], mybir.dt.uint32, 100352, (5, 7))
    wsl = M("wsl", [128, NT, 2], F32, 100480, (5, 7))
    widxu = M("widxu", [128, 64], mybir.dt.uint32, 100608, (5, 7))
    Bwr = Buf("wr"); Bc5 = Buf("c5"); BR = Buf("route"); Bpos = Buf("posu")
    S.dma("pool", lambda e: e.dma_start(out=wrb[:], in_=wr_d.rearrange("(k p) n -> p k n", p=128)), writes=[Bwr])
    S.dma("sp", lambda e: e.dma_start(out=brbc[:], in_=br_d.to_broadcast([128, 36])), writes=[Bwr])
    S.op("pool", lambda e: e.memset(ustr[:], 1.0), writes=[Bc5])
    S.op("pool", lambda e: e.affine_select(out=ustr[:], in_=ustr[:], pattern=[[1, 128]], compare_op=ALU.is_ge,
                                           fill=preg(e, 0.0), base=-1, channel_multiplier=-1), reads=[Bc5], writes=[Bc5])
    S.op("pool", lambda e: e.memset(onesb[:], 1.0), writes=[Bc5])
    S.op("pool", lambda e: e.iota(jrow[:], pattern=[[1, 64]], base=0, channel_multiplier=0,
                                  allow_small_or_imprecise_dtypes=True), writes=[Bc5])
    S.op("pool", lambda e: e.iota(thr16[:], pattern=[[128, 16]], base=0, channel_multiplier=0,
                                  allow_small_or_imprecise_dtypes=True), writes=[Bc5])
    S.op("pool", lambda e: e.iota(piota[:], pattern=[[0, 2]], base=0, channel_multiplier=1,
                                  allow_small_or_imprecise_dtypes=True), writes=[Bc5])

    def bc(ap, shape):
        return ap.to_broadcast(shape)

    for tt in range(NT):
        bank = tt // 8
        fns = [lambda e, k=k, tt=tt, bank=bank: e.matmul(
            PS[bank][:, (tt % 8) * 36:(tt % 8 + 1) * 36], lhsT=x1T[:, k, tt * 128:(tt + 1) * 128], rhs=wrb[:, k, :],
            start=(k == 0), stop=(k == 7), skip_group_check=True) for k in range(8)]
        S.group("pe", fns, reads=[Bx1T, Bwr], writes=[Bps[bank]])
    for bank in range(2):
        S.op("dve", lambda e, bank=bank: e.tensor_tensor(
            out=lg[:, bank * 8:(bank + 1) * 8, :], in0=PS[bank][:, 0:288].rearrange("p (t c) -> p t c", c=36),
            in1=bc(brbc[:].rearrange("p (o c) -> p o c", o=1), [128, 8, 36]), op=ALU.add),
            reads=[Bps[bank], Bwr], writes=[BR])
    RB = [BR]
    def smv(r):
        return sm[:, r, :]

    def sm3(r, n):
        return bc(sm[:, r, :].rearrange("p (t o) -> p t o", o=1), [128, NT, n])

    S.op("dve", lambda e: e.tensor_reduce(out=smv(0), in_=lg[:, :, 0:4], axis=AX.X, op=ALU.max), reads=RB, writes=RB)
    S.op("dve", lambda e: e.tensor_tensor(out=pen[:], in0=lg[:, :, 0:4], in1=sm3(0, 4), op=ALU.is_lt), reads=RB, writes=RB)
    S.op("dve", lambda e: e.tensor_tensor(out=gex[:], in0=lg[:, :, 0:4], in1=sm3(0, 4), op=ALU.subtract), reads=RB, writes=RB)
    S.op("act", lambda e: e.activation(out=gex[:], in_=gex[:], func=AF.Exp), reads=RB, writes=RB)
    S.op("dve", lambda e: e.tensor_reduce(out=smv(1), in_=gex[:], axis=AX.X, op=ALU.add), reads=RB, writes=RB)
    S.op("dve", lambda e: e.tensor_scalar(out=pen[:], in0=pen[:], scalar1=NEG, scalar2=None, op0=ALU.mult), reads=RB, writes=RB)
    S.op("dve", lambda e: e.tensor_tensor(
        out=elm[:].rearrange("p t (g j) -> p t g j", g=4), in0=lg[:, :, 4:36].rearrange("p t (g j) -> p t g j", g=4),
        in1=bc(pen[:].rearrange("p t (g o) -> p t g o", o=1), [128, NT, 4, 8]), op=ALU.add), reads=RB, writes=RB)
    S.op("dve", lambda e: e.tensor_reduce(out=smv(2), in_=elm[:], axis=AX.X, op=ALU.max), reads=RB, writes=RB)
    S.op("dve", lambda e: e.tensor_tensor(out=tmpa[:], in0=elm[:], in1=sm3(2, 32), op=ALU.is_ge), reads=RB, writes=RB)
    S.op("dve", lambda e: e.scalar_tensor_tensor(out=tmpa[:], in0=tmpa[:], scalar=NEG, in1=elm[:], op0=ALU.mult, op1=ALU.add),
         reads=RB, writes=RB)
    S.op("dve", lambda e: e.tensor_reduce(out=smv(3), in_=tmpa[:], axis=AX.X, op=ALU.max), reads=RB, writes=RB)
    S.op("dve", lambda e: e.tensor_tensor(out=exr[:], in0=elm[:], in1=sm3(2, 32), op=ALU.subtract), reads=RB, writes=RB)
    S.op("act", lambda e: e.activation(out=exr[:], in_=exr[:], func=AF.Exp), reads=RB, writes=RB)
    S.op("dve", lambda e: e.tensor_tensor(out=sel[:], in0=elm[:], in1=sm3(3, 32), op=ALU.is_ge), reads=RB, writes=RB)
    S.op("dve", lambda e: e.tensor_copy(out=selb[:], in_=sel[:]), reads=RB, writes=RB)
    S.op("dve", lambda e: e.tensor_tensor(out=comb[:], in0=sel[:], in1=exr[:], op=ALU.mult), reads=RB, writes=RB)
    S.op("dve", lambda e: e.tensor_reduce(out=smv(4), in_=comb[:], axis=AX.X, op=ALU.add), reads=RB, writes=RB)
    S.op("dve", lambda e: e.tensor_tensor(out=smv(5), in0=smv(4), in1=smv(1), op=ALU.mult), reads=RB, writes=RB)
    S.op("dve", lambda e: e.reciprocal(out=smv(6), in_=smv(5)), reads=RB, writes=RB)
    S.op("dve", lambda e: e.tensor_tensor(out=comb[:], in0=comb[:], in1=sm3(6, 32), op=ALU.mult), reads=RB, writes=RB)
    if debug:
        S.dma("sp", lambda e: e.dma_start(out=dbg["d_comb"].rearrange("(t p) d -> p t d", p=128), in_=comb[:]),
              reads=RB, writes=[Bout])
    for tt in range(NT):
        fns = []
        for tp in range(tt):
            fns.append(lambda e, tt=tt, tp=tp: e.matmul(
                PS[2][:, tt * 32:(tt + 1) * 32], lhsT=onesb[:], rhs=selb[:, tp, :], start=(tp == 0), stop=False,
                skip_group_check=True))
        fns.append(lambda e, tt=tt: e.matmul(
            PS[2][:, tt * 32:(tt + 1) * 32], lhsT=ustr[:], rhs=selb[:, tt, :], start=(tt == 0), stop=True,
            skip_group_check=True))
        S.group("pe", fns, reads=RB + [Bc5], writes=[Bps[2]])
    fns = [lambda e, tt=tt: e.matmul(PS[3][0:32, 0:1], lhsT=selb[:, tt, :], rhs=onesb[:, 0:1],
                                     start=(tt == 0), stop=(tt == NT - 1)) for tt in range(NT)]
    S.group("pe", fns, reads=RB + [Bc5], writes=[Bps[3]])
    S.op("dve", lambda e: e.tensor_copy(out=cnt32[0:32, 0:1], in_=PS[3][0:32, 0:1]), reads=[Bps[3]], writes=RB)
    S.op("dve", lambda e: e.tensor_scalar(out=cnt32[0:32, 4:20], in0=thr16[0:32, :], scalar1=cnt32[0:32, 0:1], scalar2=None,
                                          op0=ALU.is_lt), reads=RB + [Bc5], writes=RB)
    S.op("dve", lambda e: e.tensor_reduce(out=cnt32[0:32, 1:2], in_=cnt32[0:32, 4:20], axis=AX.X, op=ALU.add), reads=RB, writes=RB)
    S.op("dve", lambda e: e.tensor_scalar(out=ntr[0:32, :], in0=onesb[0:32, :], scalar1=cnt32[0:32, 1:2], scalar2=None,
                                          op0=ALU.mult), reads=RB + [Bc5], writes=RB)
    S.op("pe", lambda e: e.matmul(PS[3][:, 64:96], lhsT=ntr[0:32, :], rhs=ustr[0:32, 0:32], start=True, stop=True),
         reads=RB + [Bc5], writes=[Bps[3]])
    S.op("pe", lambda e: e.matmul(PS[3][:, 96:128], lhsT=ntr[0:32, :], rhs=causal01[0:32, 0:32], start=True, stop=True),
         reads=RB + [Bconst], writes=[Bps[3]])
    S.op("dve", lambda e: e.tensor_copy(out=toffs[:], in_=PS[3][:, 64:128]), reads=[Bps[3]], writes=RB)
    S.op("dve", lambda e: e.scalar_tensor_tensor(
        out=posf[:], in0=bc(toffs[:, 0:32].rearrange("p (o c) -> p o c", o=1), [128, NT, 32]), scalar=128.0,
        in1=PS[2][:].rearrange("p (t c) -> p t c", c=32), op0=ALU.mult, op1=ALU.add), reads=RB + [Bps[2]], writes=RB)
    S.op("dve", lambda e: e.tensor_tensor(out=tmpa[:], in0=sel[:], in1=posf[:], op=ALU.mult), reads=RB, writes=RB)
    S.op("dve", lambda e: e.tensor_reduce(out=smv(8), in_=tmpa[:], axis=AX.X, op=ALU.max), reads=RB, writes=RB)
    S.op("dve", lambda e: e.tensor_scalar(out=tmpa[:], in0=sel[:], scalar1=-1.0e6, scalar2=1.0e6, op0=ALU.mult, op1=ALU.add),
         reads=RB, writes=RB)
    S.op("dve", lambda e: e.tensor_tensor(out=tmpa[:], in0=tmpa[:], in1=posf[:], op=ALU.add), reads=RB, writes=RB)
    S.op("dve", lambda e: e.tensor_reduce(out=smv(7), in_=tmpa[:], axis=AX.X, op=ALU.min), reads=RB, writes=RB)
    for r_pos, r_w in ((7, 9), (8, 10)):
        S.op("dve", lambda e, r_pos=r_pos: e.tensor_tensor(out=tmpa[:], in0=posf[:], in1=sm3(r_pos, 32), op=ALU.is_equal),
             reads=RB, writes=RB)
        S.op("dve", lambda e: e.tensor_tensor(out=tmpa[:], in0=tmpa[:], in1=comb[:], op=ALU.mult), reads=RB, writes=RB)
        S.op("dve", lambda e, r_w=r_w: e.tensor_reduce(out=smv(r_w), in_=tmpa[:], axis=AX.X, op=ALU.add), reads=RB, writes=RB)
    for slot in range(2):
        S.op("dve", lambda e, slot=slot: e.tensor_copy(out=posu[:, :, slot], in_=smv(7 + slot)), reads=RB, writes=[Bpos])
        S.op("dve", lambda e, slot=slot: e.tensor_copy(out=wsl[:, :, slot], in_=smv(9 + slot)), reads=RB, writes=[Bpos])
    S.op("dve", lambda e: e.tensor_tensor(
        out=tmpb[:], in0=bc(toffs[:, 32:64].rearrange("p (o c) -> p o c", o=1), [128, 64, 32]),
        in1=bc(jrow[:].rearrange("p (j o) -> p j o", o=1), [128, 64, 32]), op=ALU.is_le), reads=RB + [Bc5], writes=RB)
    S.op("dve", lambda e: e.tensor_reduce(out=ejf[:], in_=tmpb[:], axis=AX.X, op=ALU.add), reads=RB, writes=RB)
    S.op("dve", lambda e: e.scalar_tensor_tensor(
        out=ejf[:], in0=ejf[:], scalar=128.0, in1=bc(piota[:, 0:1], [128, 64]), op0=ALU.mult, op1=ALU.add),
        reads=RB + [Bc5], writes=RB)
    S.op("dve", lambda e: e.tensor_copy(out=widxu[:], in_=ejf[:]), reads=RB, writes=[Bpos])
    S.barrier()
    if stop_after <= 5:
        S.emit()
        return nc

    NTL = 64
    NB = 5
    NX = 6
    wgt = [M("wgt%d" % b, [128, 2048], BF16, 2048 + b * 4096, (6, 6)) for b in range(NB)]
    wut = [M("wut%d" % b, [128, 2048], BF16, 100864 + b * 4096, (6, 6)) for b in range(NB)]
    wdt = [M("wdt%d" % b, [128, 2048], BF16, 121344 + b * 4096, (6, 6)) for b in range(NB)]
    xst = M("xst", [128, NX, D], BF16, 22528, (6, 6))
    xsT = M("xsT", [128, 2, 8, 128], BF16, 71680, (6, 6))
    sgt = M("sgt", [128, 2, 256], BF16, 75776, (6, 6))
    hTt = M("hTt", [128, 2, 256], BF16, 76800, (6, 6))
    ysb = M("ysb", [128, 2, D], F32, 77824, (6, 6))
    Bwt = [[Buf("wt%d_%d" % (m, b)) for b in range(NB)] for m in range(3)]
    Bxst = [Buf("xst%d" % i) for i in range(NX)]; BxsT = [Buf("xsT0"), Buf("xsT1")]
    Bsgt = [Buf("sgt0"), Buf("sgt1")]; BhTt = [Buf("hTt0"), Buf("hTt1")]; Bysb = [Buf("ysb0"), Buf("ysb1")]
    Bxs = Buf("xs_d"); Bys = Buf("ys_d")
    for tt in range(NT):
        for slot in range(2):
            S.dma("pool", lambda e, tt=tt, slot=slot: e.indirect_dma_start(
                out=xs_d, out_offset=bass.IndirectOffsetOnAxis(ap=posu[:, tt, slot:slot + 1], axis=0),
                in_=x1tok[:, tt, :], in_offset=None), reads=[Bpos, Bx1tok[tt], Bzero], writes=[Bxs])

    def moe_load_w(j):
        b = j % NB
        for m, (wd_, wt_) in enumerate(((wgb_d, wgt), (wub_d, wut), (wdb_d, wdt))):
            S.dma("pool", lambda e, wd_=wd_, wt_=wt_: e.indirect_dma_start(
                out=wt_[b][:], out_offset=None, in_=wd_,
                in_offset=bass.IndirectOffsetOnAxis(ap=widxu[:, j:j + 1], axis=0), bounds_check=preg(e, NEXP * 128 - 1),
                oob_is_err=False), reads=[Bpos, Bwbf], writes=[Bwt[m][b]])

    def moe_load_x(j):
        bx = j % NX
        S.dma("sp", lambda e: e.dma_start(out=xst[:, bx, :], in_=xs_d[j * 128:(j + 1) * 128, :]), reads=[Bxs], writes=[Bxst[bx]])

    def moe_T(j):
        b2, bx = j % 2, j % NX
        pv = PS[b2][:].bitcast(BF16)
        fns = [lambda e, c=c: e.transpose(out=pv[:, c * 128:(c + 1) * 128], in_=xst[:, bx, c * 128:(c + 1) * 128],
                                         identity=ident[:]) for c in range(8)]
        S.group("pe", fns, reads=[Bxst[bx], Bconst], writes=[Bps[b2]])
        S.op("act", lambda e: e.activation(out=xsT[:, b2, :, :], in_=pv.rearrange("p (c t) -> p c t", c=8), func=AF.Copy),
             reads=[Bps[b2]], writes=[BxsT[b2]])

    def moe_GU(j):
        b2, b = j % 2, j % NB
        bank = 2 + b2
        fns = []
        for m, wt_ in ((0, wgt), (1, wut)):
            wv = wt_[b][:].rearrange("p (k n) -> p k n", k=8)
            for c in range(2):
                for k in range(8):
                    fns.append(lambda e, m=m, wv=wv, c=c, k=k: e.matmul(
                        PS[bank][:, (m * 2 + c) * 128:(m * 2 + c + 1) * 128], lhsT=wv[:, k, c * 128:(c + 1) * 128],
                        rhs=xsT[:, b2, k, :], start=(k == 0), stop=(k == 7), skip_group_check=True))
        S.group("pe", fns, reads=[BxsT[b2], Bwt[0][b], Bwt[1][b]], writes=[Bps[bank]])
        S.op("act", lambda e: e.activation(out=sgt[:, b2, :], in_=PS[bank][:, 0:256], func=AF.Silu),
             reads=[Bps[bank]], writes=[Bsgt[b2]])
        S.op("dve", lambda e: e.tensor_tensor(out=hTt[:, b2, :], in0=sgt[:, b2, :], in1=PS[bank][:, 256:512], op=ALU.mult),
             reads=[Bsgt[b2], Bps[bank]], writes=[BhTt[b2]])

    def moe_D(j):
        b2, b = j % 2, j % NB
        wv = wdt[b][:].rearrange("p (c n) -> p c n", c=2)
        for half in range(2):
            bank = 4 + b2 * 2 + half
            fns = [lambda e, c=c, half=half, bank=bank: e.matmul(
                PS[bank][:], lhsT=hTt[:, b2, c * 128:(c + 1) * 128], rhs=wv[:, c, half * 512:(half + 1) * 512],
                start=(c == 0), stop=(c == 1)) for c in range(2)]
            S.group("pe", fns, reads=[BhTt[b2], Bwt[2][b]], writes=[Bps[bank]])
            if half == 0:
                S.op("act", lambda e, bank=bank: e.activation(out=ysb[:, b2, 0:512], in_=PS[bank][:], func=AF.Copy),
                     reads=[Bps[bank]], writes=[Bysb[b2]])
            else:
                S.op("dve", lambda e, bank=bank: e.tensor_copy(out=ysb[:, b2, 512:1024], in_=PS[bank][:]),
                     reads=[Bps[bank]], writes=[Bysb[b2]])
        S.dma("sp", lambda e: e.dma_start(out=ys_d[j * 128:(j + 1) * 128, :], in_=ysb[:, b2, :]), reads=[Bysb[b2]], writes=[Bys])

    for j in range(NX):
        moe_load_x(j)
    for j in range(NB):
        moe_load_w(j)
    for s_ in range(NTL + 2):
        if s_ < NTL:
            moe_T(s_)
            if s_ + NX < NTL:
                moe_load_x(s_ + NX)
        if 1 <= s_ <= NTL:
            moe_GU(s_ - 1)
        if s_ >= 2:
            moe_D(s_ - 2)
            if s_ - 2 + NB < NTL:
                moe_load_w(s_ - 2 + NB)
    S.barrier()

    NYG = 12
    yg = M("yg", [128, NYG, D], F32, 34816, (7, 7))
    ln2g = M("ln2g", [128, D], F32, 2048, (7, 7))
    ln2bA = M("ln2bA", [128, D], F32, 6144, (7, 7))
    obuf = M("obuf", [128, 3, D], F32, 10240, (7, 7))
    lnst2 = M("lnst2", [128, 3, 32], F32, 22528, (7, 7))
    Bln2 = Buf("ln2"); Bob = [Buf("ob%d" % i) for i in range(3)]; Blnst2 = [Buf("ls%d" % i) for i in range(3)]
    Byg = [Buf("yg%d" % i) for i in range(NYG)]
    S.dma("sp", lambda e: e.dma_start(out=ln2g[:], in_=ln2g_d.to_broadcast([128, D])), writes=[Bln2])
    S.dma("sp", lambda e: e.dma_start(out=ln2bA[:], in_=ln2b_d.to_broadcast([128, D])), writes=[Bln2])

    def tail_gather(q):
        tt, slot = q // 2, q % 2
        yb_ = q % NYG
        S.dma("pool", lambda e: e.indirect_dma_start(
            out=yg[:, yb_, :], out_offset=None, in_=ys_d,
            in_offset=bass.IndirectOffsetOnAxis(ap=posu[:, tt, slot:slot + 1], axis=0)),
            reads=[Bpos, Bys], writes=[Byg[yb_]])

    for q in range(NYG):
        tail_gather(q)
    for tt in range(NT):
        b3 = tt % 3
        for slot in range(2):
            q = tt * 2 + slot
            yb_ = q % NYG
            S.op("dve", lambda e, tt=tt, slot=slot, yb_=yb_: e.scalar_tensor_tensor(
                out=acc[:, tt, :], in0=yg[:, yb_, :], scalar=wsl[:, tt, slot:slot + 1], in1=acc[:, tt, :],
                op0=ALU.mult, op1=ALU.add), reads=[Byg[yb_], Bpos, Bacc[tt]], writes=[Bacc[tt]])
            if q + NYG < 2 * NT:
                tail_gather(q + NYG)
        ln_scaled(acc[:, tt, :], [Bacc[tt]], lnst2[:, b3, :], Blnst2[b3], ln2g, ln2bA, Bln2, obuf[:, b3, :], [Bob[b3]],
                  alpha=1.0)
        S.dma("sp", lambda e, tt=tt, b3=b3: e.dma_start(out=out_d[tt * 128:(tt + 1) * 128, :], in_=obuf[:, b3, :]),
              reads=[Bob[b3]], writes=[Bout])
    S.barrier()
    S.emit()
    return nc


_NC_CACHE = {}


def _host_inputs(inp, b):
    f = np.float32
    w_in = inp["w_in"][0]
    cols = list(range(0, 1024))
    cols += list(range(1152, 1664))
    cols += list(range(1664, 1728)) * 2
    qb0 = 1736
    for j in range(4):
        cols += list(range(qb0 + j * 64, qb0 + (j + 1) * 64))
        cols += list(range(qb0 + (j + 4) * 64, qb0 + (j + 5) * 64))
    cols += list(range(2248, 2376))
    cols += list(range(1024, 1152))
    cols += list(range(2376, 2504))
    cols += list(range(1728, 1736))
    assert len(cols) == W1COLS
    rel = inp["rel_bias"].astype(f)
    s = np.arange(128)[:, None]
    t = np.arange(128)[None, :]
    bk_prev = t5_bucket_np(t - s + 128)
    bk_own = t5_bucket_np(t - s)
    swab = np.zeros((4, 128, 4, 128), f)
    for typ, bk in enumerate((bk_prev, bk_own)):
        for g in range(2):
            for j in range(4):
                swab[typ * 2 + g, :, j, :] = rel[bk, 8 + 4 * g + j]
    dsab = np.zeros((2, 128, 8, 128), f)
    for typ, bk in enumerate((bk_prev, bk_own)):
        for h in range(8):
            dsab[typ, :, h, :] = rel[bk, h]
    return {
        "xT": np.ascontiguousarray(inp["x"][b].T),
        "x": np.ascontiguousarray(inp["x"][b]),
        "w1": np.ascontiguousarray(w_in[:, cols]),
        "wg": np.ascontiguousarray(w_in[:, 2504:4552]),
        "kvg": np.ascontiguousarray(inp["kv_norm_g"][0].reshape(1, 128)),
        "wuv": np.ascontiguousarray(inp["w_uv"][0].transpose(1, 0, 2).reshape(128, 512)),
        "wa": np.ascontiguousarray(inp["w_branch_a"][0]),
        "wb": np.ascontiguousarray(inp["w_branch_b"][0]),
        "wo": np.ascontiguousarray(inp["w_out"][0]),
        "sinks": np.ascontiguousarray(inp["sinks"][0].reshape(1, 8)),
        "ln1g": np.ascontiguousarray(inp["ln1_g"][0].reshape(1, D)),
        "ln1b": np.ascontiguousarray(inp["ln1_b"][0].reshape(1, D)),
        "ln2g": np.ascontiguousarray(inp["ln2_g"][0].reshape(1, D)),
        "ln2b": np.ascontiguousarray(inp["ln2_b"][0].reshape(1, D)),
        "wr": np.ascontiguousarray(np.concatenate([inp["w_group"][0], inp["w_router"][0]], axis=1)),
        "br": np.ascontiguousarray(np.concatenate([inp["b_group"][0], inp["b_router"][0]]).reshape(1, 36)),
        "wgr": np.ascontiguousarray(inp["w_gate"][0].reshape(NEXP, 8, 128, DE).transpose(0, 2, 1, 3).reshape(NEXP * 128, 2048)),
        "wur": np.ascontiguousarray(inp["w_up"][0].reshape(NEXP, 8, 128, DE).transpose(0, 2, 1, 3).reshape(NEXP * 128, 2048)),
        "wdr": np.ascontiguousarray(inp["w_down"][0].reshape(NEXP, 2, 128, D).transpose(0, 2, 1, 3).reshape(NEXP * 128, 2048)),
        "swab": swab.reshape(4, 128, 512),
        "dsab": dsab.reshape(2, 128, 1024),
        "c31": np.ascontiguousarray(rel[31, 0:8].reshape(1, 8)),
    }


def kernel(**inputs):
    inp = {k: np.asarray(v, dtype=np.float32) for k, v in inputs.items()}
    n = 8
    if "nc" not in _NC_CACHE:
        _NC_CACHE["nc"] = build_nc(False)
    nc = _NC_CACHE["nc"]
    shared = None
    in_maps = []
    for b in range(n):
        m = _host_inputs(inp, b) if shared is None else dict(shared)
        if shared is None:
            shared = m
        else:
            m["xT"] = np.ascontiguousarray(inp["x"][b].T)
            m["x"] = np.ascontiguousarray(inp["x"][b])
        in_maps.append(m)
    res = run_bass_kernel_spmd(nc, in_maps, core_ids=list(range(n)))
    return np.stack([np.asarray(r["out"], dtype=np.float32) for r in res.results], axis=0)
```

```python
import math
import contextlib
import numpy as np
import concourse.bass as bass
import concourse.mybir as mybir
from concourse.bass_utils import run_bass_kernel_spmd

F32 = mybir.dt.float32
BF16 = mybir.dt.bfloat16
AF = mybir.ActivationFunctionType
ALU = mybir.AluOpType
AX = mybir.AxisListType

D = 1024
L = 2048
NT = 16
NEXP = 32
DE = 256
ALPHA = 2.0 ** 0.25
LN_EPS = 1e-5
RMS_EPS = 1e-6
ATT_SCALE = 128.0 ** -0.5
NEG = -30000.0
N_BISECT = 16
W1COLS = 2568
SB_BASE = 16640
SB_END = 229368


class Buf:
    __slots__ = ("name", "writer", "readers")

    def __init__(self, name):
        self.name = name
        self.writer = None
        self.readers = []


class Sched:
    ENGS = ("pe", "act", "dve", "pool", "sp")

    def __init__(self, nc, n_dma_sems=24):
        self.nc = nc
        self.ops = {e: [] for e in self.ENGS}
        self.cnt = {e: 0 for e in self.ENGS}
        self.seen = {e: {} for e in self.ENGS}
        self.n_dma_sems = n_dma_sems
        self.dma_i = {}
        self.dma_val = {}
        self.sems = {}

    def _deps(self, eng, reads, writes):
        need = {}

        def add(tok, skip_same):
            if tok is None:
                return
            e, key, val = tok
            if e == eng and skip_same:
                return
            if need.get(key, 0) < val:
                need[key] = val

        pe = eng == "pe"
        for b in reads:
            add(b.writer, pe)
        for b in writes:
            add(b.writer, pe)
            for r in b.readers:
                add(r, True)
        waits = []
        seen = self.seen[eng]
        for key, val in need.items():
            if seen.get(key, 0) < val:
                seen[key] = val
                waits.append((key, val))
        return waits

    def _commit(self, tok, reads, writes):
        for b in reads:
            b.readers.append(tok)
        for b in writes:
            b.writer = tok
            b.readers = []

    def group(self, eng, fns, reads=(), writes=()):
        waits = self._deps(eng, reads, writes)
        self.cnt[eng] += 1
        tok = (eng, "e_" + eng, self.cnt[eng])
        n = len(fns)
        for i, fn in enumerate(fns):
            self.ops[eng].append((fn, waits if i == 0 else (), ("e_" + eng, 1) if i == n - 1 else None))
        self._commit(tok, reads, writes)
        return tok

    def op(self, eng, fn, reads=(), writes=()):
        return self.group(eng, [fn], reads, writes)

    def dma(self, eng, fn, reads=(), writes=()):
        i = self.dma_i.get(eng, 0) % self.n_dma_sems
        self.dma_i[eng] = self.dma_i.get(eng, 0) + 1
        key = "d_%s_%d" % (eng, i)
        waits = list(self._deps(eng, reads, writes))
        prev = self.dma_val.get(key, 0)
        if prev > 0 and self.seen[eng].get(key, 0) < prev:
            self.seen[eng][key] = prev
            waits.append((key, prev))
        self.dma_val[key] = prev + 16
        tok = ("dma", key, self.dma_val[key])
        self.ops[eng].append((fn, waits, (key, 16)))
        self._commit(tok, reads, writes)
        return tok

    def barrier(self):
        targets = [("e_" + e, self.cnt[e]) for e in self.ENGS if self.cnt[e] > 0]
        targets += [(k, v) for k, v in self.dma_val.items() if v > 0]
        for e in self.ENGS:
            waits = []
            for key, val in targets:
                if key == "e_" + e and e != "pe":
                    pass
                if self.seen[e].get(key, 0) < val:
                    self.seen[e][key] = val
                    waits.append((key, val))
            if waits:
                self.ops[e].append((None, waits, None))

    def emit(self):
        nc = self.nc
        keys = ["e_" + e for e in self.ENGS] + sorted(self.dma_val.keys())
        with contextlib.ExitStack() as st:
            for k in keys:
                self.sems[k] = st.enter_context(nc.semaphore(k))
            block = st.enter_context(nc.Block())
            sems = self.sems

            def run(eng_name):
                def body(eng):
                    for fn, waits, inc in self.ops[eng_name]:
                        for key, val in waits:
                            eng.wait_ge(sems[key], val)
                        if fn is None:
                            continue
                        ins = fn(eng)
                        if inc is not None:
                            ins.then_inc(sems[inc[0]], inc[1])
                return body

            block.tensor(run("pe"))
            block.scalar(run("act"))
            block.vector(run("dve"))
            block.gpsimd(run("pool"))
            block.sync(run("sp"))


class Mem:
    def __init__(self, nc):
        self.nc = nc
        self.allocs = []

    def __call__(self, name, shape, dtype, off, life):
        esz = 4 if dtype == F32 else 2
        n = esz
        for s in shape[1:]:
            n *= s
        a0, a1 = SB_BASE + off, SB_BASE + off + n
        assert a1 <= SB_END, (name, a1)
        for (nm, b0, b1, lf) in self.allocs:
            if a0 < b1 and b0 < a1 and lf[0] <= life[1] and life[0] <= lf[1]:
                raise AssertionError("SBUF overlap %s vs %s" % (name, nm))
        self.allocs.append((name, a0, a1, life))
        return self.nc.alloc_sbuf_tensor_at(name, list(shape), dtype, offset=a0)


def t5_bucket_np(dist):
    n = np.maximum(dist, 0)
    nf = np.maximum(n, 1).astype(np.float32)
    large = 16 + (np.log(nf / np.float32(16)) / np.float32(math.log(128 / 16)) * np.float32(16)).astype(np.int32)
    large = np.minimum(large, 31)
    return np.where(n < 16, n, large).astype(np.int32)


def build_nc(debug=False, stop_after=99):
    nc = bass.Bass("TRN2", target_bir_lowering=False)

    def din(name, shape, dt=F32):
        return nc.dram_tensor(name, list(shape), dt, kind="ExternalInput").ap()

    xT_d = din("xT", [D, L])
    x_d = din("x", [L, D])
    w1_d = din("w1", [D, W1COLS])
    wg_d = din("wg", [D, 2048])
    kvg_d = din("kvg", [1, 128])
    wuv_d = din("wuv", [128, 512])
    wa_d = din("wa", [512, D])
    wb_d = din("wb", [512, D])
    wo_d = din("wo", [D, D])
    sinks_d = din("sinks", [1, 8])
    ln1g_d = din("ln1g", [1, D])
    ln1b_d = din("ln1b", [1, D])
    ln2g_d = din("ln2g", [1, D])
    ln2b_d = din("ln2b", [1, D])
    wr_d = din("wr", [D, 36])
    br_d = din("br", [1, 36])
    wgr_d = din("wgr", [NEXP * 128, 2048])
    wur_d = din("wur", [NEXP * 128, 2048])
    wdr_d = din("wdr", [NEXP * 128, 2048])
    wgb_d = nc.dram_tensor("wg_bf16", [NEXP * 128, 2048], BF16, kind="Internal").ap()
    wub_d = nc.dram_tensor("wu_bf16", [NEXP * 128, 2048], BF16, kind="Internal").ap()
    wdb_d = nc.dram_tensor("wd_bf16", [NEXP * 128, 2048], BF16, kind="Internal").ap()
    gates_d = nc.dram_tensor("gates_scr", [2048, L], BF16, kind="Internal").ap()
    xs_d = nc.dram_tensor("xs_scr", [64 * 128, D], BF16, kind="Internal").ap()
    ys_d = nc.dram_tensor("ys_scr", [64 * 128, D], F32, kind="Internal").ap()
    swab_d = din("swab", [4, 128, 512])
    dsab_d = din("dsab", [2, 128, 1024])
    c31_d = din("c31", [1, 8])
    out_d = nc.dram_tensor("out", [L, D], F32, kind="ExternalOutput").ap()
    dbg = {}
    if debug:
        for nm, shp in [("d_yb", [512, L]), ("d_ya", [512, L]), ("d_mergedT", [D, L]), ("d_acc", [L, D]),
                        ("d_comb", [L, 32]), ("d_ckvT", [128, L]), ("d_qlatT", [128, L])]:
            dbg[nm] = nc.dram_tensor(nm, shp, BF16 if nm in ("d_yb", "d_ya", "d_mergedT", "d_ckvT", "d_qlatT") else F32,
                                     kind="ExternalOutput").ap()

    S = Sched(nc)
    M = Mem(nc)
    _regs = {}

    def preg(e, val):
        if val not in _regs:
            _regs[val] = e.to_reg(val)
        return _regs[val]
    PS = [nc.alloc_psum_tensor("ps%d" % i, [128, 512], F32) for i in range(8)]
    Bps = [Buf("ps%d" % i) for i in range(8)]
    Bout = Buf("out")

    ident = M("ident", [128, 128], BF16, 0, (1, 7))
    gkv_bc = M("gkv_bc", [128, 128], F32, 256, (1, 7))
    esink = M("esink", [128, 8], F32, 768, (1, 7))
    negc31 = M("negc31", [128, 8], F32, 800, (1, 7))
    identf = M("identf", [128, 128], F32, 1024, (1, 7))
    causal01 = M("causal01", [128, 128], BF16, 1536, (1, 7))
    Bconst = Buf("const")

    S.op("pool", lambda e: e.memset(identf[:], 1.0), writes=[Bconst])
    S.op("pool", lambda e: e.affine_select(out=identf[:], in_=identf[:], pattern=[[1, 128]],
                                           compare_op=ALU.is_equal, fill=preg(e, 0.0), base=0, channel_multiplier=-1),
         reads=[Bconst], writes=[Bconst])
    S.op("pool", lambda e: e.tensor_copy(out=ident[:], in_=identf[:]), reads=[Bconst], writes=[Bconst])
    S.op("pool", lambda e: e.memset(causal01[:], 1.0), writes=[Bconst])
    S.op("pool", lambda e: e.affine_select(out=causal01[:], in_=causal01[:], pattern=[[1, 128]],
                                           compare_op=ALU.is_ge, fill=preg(e, 0.0), base=0, channel_multiplier=-1),
         reads=[Bconst], writes=[Bconst])
    S.dma("sp", lambda e: e.dma_start(out=gkv_bc[:], in_=kvg_d.to_broadcast([128, 128])), writes=[Bconst])
    S.dma("sp", lambda e: e.dma_start(out=esink[:], in_=sinks_d.to_broadcast([128, 8])), writes=[Bconst])
    S.dma("sp", lambda e: e.dma_start(out=negc31[:], in_=c31_d.to_broadcast([128, 8])), writes=[Bconst])
    S.op("act", lambda e: e.activation(out=esink[:], in_=esink[:], func=AF.Exp), reads=[Bconst], writes=[Bconst])
    S.op("dve", lambda e: e.tensor_scalar(out=negc31[:], in0=negc31[:], scalar1=-1.0, scalar2=None, op0=ALU.mult),
         reads=[Bconst], writes=[Bconst])

    xT = M("xT", [128, 8, L], BF16, 2048, (1, 1))
    o = 34816
    qlatT = M("qlatT", [128, 8, L], BF16, o, (1, 2.5)); o += 32768
    qidxT = M("qidxT", [128, 4, L], BF16, o, (1, 2.5)); o += 16384
    qbT = M("qbT", [128, 4, L], BF16, o, (1, 2)); o += 16384
    kidxT = M("kidxT", [128, L], BF16, o, (1, 2.5)); o += 4096
    kbT = M("kbT", [128, L], BF16, o, (1, 2)); o += 4096
    ckvT = M("ckvT", [128, L], BF16, o, (1, 2.5)); o += 4096
    kvW = M("kvW", [128, NT, 8, 65], BF16, o, (1, 2.5)); o += 16640
    vaug = M("vaug", [128, NT, 2, 65], BF16, o, (1, 2)); o += 4160
    widx = M("widx", [128, NT, 8], F32, o, (1, 2.5)); o += 512
    assert o == 133952
    R3 = 133952
    w1b = M("w1b", [128, 8, W1COLS], BF16, R3, (1, 1))
    wuvs = M("wuvs", [128, 512], F32, R3 + 41088, (1, 1))
    ckvtok = M("ckvtok", [128, NT, 128], BF16, R3 + 43136, (1, 1))
    wuvb = M("wuvb", [128, 512], BF16, R3 + 47232, (1, 1))
    p1tmp = M("p1tmp", [128, 128], F32, R3 + 48256, (1, 1))
    p1junk = M("p1junk", [128, 128], F32, R3 + 48768, (1, 1))

    BxT = [Buf("xT%d" % k) for k in range(8)]
    Bw1 = [Buf("w1_%d" % c) for c in range(6)]
    w1v = w1_d.rearrange("(k p) n -> p k n", p=128)
    xTv = xT_d.rearrange("(k p) n -> p k n", p=128)
    for k in range(8):
        S.dma("pool", lambda e, k=k: e.dma_start(out=xT[:, k, :], in_=xTv[:, k, :]), writes=[BxT[k]])
    w1blocks = [(0, 512), (512, 1024), (1024, 1536), (1536, 2048), (2048, 2304), (2304, W1COLS)]
    for c, (c0, c1) in enumerate(w1blocks):
        S.dma("pool", lambda e, c0=c0, c1=c1: e.dma_start(out=w1b[:, :, c0:c1], in_=w1v[:, :, c0:c1]),
              writes=[Bw1[c]])
    wgs = [M("wgs0", [128, 8, 512], BF16, 184320, (1, 1)), M("wgs1", [128, 8, 512], BF16, 192512, (1, 1))]
    gst = M("gst", [128, 2, 512], BF16, 200704, (1, 1))
    Bwgs = [Buf("wgs0"), Buf("wgs1")]; Bgst = [Buf("gst0"), Buf("gst1")]; Bgates = Buf("gates_d")
    wgv = wg_d.rearrange("(k p) n -> p k n", p=128)

    def load_wgs(c):
        wb_ = c % 2
        S.dma("pool", lambda e: e.dma_start(out=wgs[wb_][:], in_=wgv[:, :, c * 512:(c + 1) * 512]), writes=[Bwgs[wb_]])

    load_wgs(0)
    load_wgs(1)
    Bwuv = Buf("wuv")
    S.dma("sp", lambda e: e.dma_start(out=wuvs[:], in_=wuv_d), writes=[Bwuv])
    S.op("dve", lambda e: e.tensor_copy(out=wuvb[:], in_=wuvs[:]), reads=[Bwuv], writes=[Bwuv])

    def w1buf(col):
        for c, (c0, c1) in enumerate(w1blocks):
            if c0 <= col < c1:
                return Bw1[c]

    Bqlat = [Buf("qlat%d" % h) for h in range(8)]
    Bqidx = Buf("qidx"); Bqb = Buf("qb"); Bkidx = Buf("kidx"); Bkb = Buf("kb")
    Bckv = Buf("ckvT"); BkvW = Buf("kvW"); Bvaug = Buf("vaug"); Bwidx = Buf("widx"); Bcktok = Buf("ckvtok")

    fm_tiles = []
    for h in range(8):
        fm_tiles.append((lambda tb, h=h: qlatT[:, h, tb * 512:(tb + 1) * 512], ATT_SCALE, Bqlat[h]))
    for j in range(4):
        fm_tiles.append((lambda tb, j=j: qidxT[:, j, tb * 512:(tb + 1) * 512], 1.0, Bqidx))
    fm_tiles.append((lambda tb: kidxT[:, tb * 512:(tb + 1) * 512], 1.0, Bkidx))
    for j in range(4):
        fm_tiles.append((lambda tb, j=j: qbT[:, j, tb * 512:(tb + 1) * 512], 0.125, Bqb))
    fm_tiles.append((lambda tb: kbT[:, tb * 512:(tb + 1) * 512], 1.0, Bkb))
    ev = 0
    for ti, (dst, scale, bdst) in enumerate(fm_tiles):
        for tb in range(4):
            bank = ev % 4
            fns = []
            for k in range(8):
                fns.append(lambda e, k=k, ti=ti, tb=tb, bank=bank: e.matmul(
                    PS[bank][:], lhsT=w1b[:, k, ti * 128:(ti + 1) * 128], rhs=xT[:, k, tb * 512:(tb + 1) * 512],
                    start=(k == 0), stop=(k == 7)))
            S.group("pe", fns, reads=BxT + [w1buf(ti * 128)], writes=[Bps[bank]])
            if ev % 2 == 0:
                S.op("act", lambda e, dst=dst, tb=tb, bank=bank, scale=scale: e.activation(
                    out=dst(tb), in_=PS[bank][:], func=AF.Copy, scale=scale), reads=[Bps[bank]], writes=[bdst])
            else:
                S.op("dve", lambda e, dst=dst, tb=tb, bank=bank, scale=scale: e.tensor_scalar(
                    out=dst(tb), in0=PS[bank][:], scalar1=scale, scalar2=None, op0=ALU.mult),
                    reads=[Bps[bank]], writes=[bdst])
            ev += 1

    S.op("pool", lambda e: e.memset(vaug[:], 1.0), writes=[Bvaug])
    S.op("pool", lambda e: e.memset(kvW[:], 1.0), writes=[BkvW])
    Bp1tmp = Buf("p1tmp"); Bp1junk = Buf("p1junk")
    for tt in range(NT):
        bank = 4 + (tt % 2)
        fns = []
        for k in range(8):
            fns.append(lambda e, k=k, tt=tt, bank=bank: e.matmul(
                PS[bank][:, 0:264], lhsT=xT[:, k, tt * 128:(tt + 1) * 128], rhs=w1b[:, k, 2304:2568],
                start=(k == 0), stop=(k == 7)))
        S.group("pe", fns, reads=BxT + [Bw1[5]], writes=[Bps[bank]])
        S.op("act", lambda e, tt=tt, bank=bank: e.activation(
            out=p1junk[:, 0:128], in_=PS[bank][:, 0:128], func=AF.Square, accum_out=p1tmp[:, tt:tt + 1]),
            reads=[Bps[bank]], writes=[Bp1junk, Bp1tmp])
        S.op("act", lambda e, tt=tt, bank=bank: e.activation(
            out=vaug[:, tt, :, 0:64], in_=PS[bank][:, 128:256].rearrange("p (g d) -> p g d", g=2), func=AF.Copy),
            reads=[Bps[bank]], writes=[Bvaug])
        S.op("act", lambda e, tt=tt, bank=bank: e.activation(
            out=widx[:, tt, :], in_=PS[bank][:, 256:264], func=AF.Copy), reads=[Bps[bank]], writes=[Bwidx])
        S.op("dve", lambda e, tt=tt: e.tensor_scalar(
            out=p1tmp[:, 16 + tt:17 + tt], in0=p1tmp[:, tt:tt + 1], scalar1=1.0 / 128.0, scalar2=RMS_EPS,
            op0=ALU.mult, op1=ALU.add), reads=[Bp1tmp], writes=[Bp1tmp])
        S.op("act", lambda e, tt=tt: e.activation(
            out=p1tmp[:, 16 + tt:17 + tt], in_=p1tmp[:, 16 + tt:17 + tt], func=AF.Sqrt),
            reads=[Bp1tmp], writes=[Bp1tmp])
        S.op("dve", lambda e, tt=tt: e.reciprocal(
            out=p1tmp[:, 32 + tt:33 + tt], in_=p1tmp[:, 16 + tt:17 + tt]), reads=[Bp1tmp], writes=[Bp1tmp])
        S.op("dve", lambda e, tt=tt, bank=bank: e.scalar_tensor_tensor(
            out=ckvtok[:, tt, :], in0=PS[bank][:, 0:128], scalar=p1tmp[:, 32 + tt:33 + tt], in1=gkv_bc[:],
            op0=ALU.mult, op1=ALU.mult), reads=[Bps[bank], Bp1tmp, Bconst], writes=[Bcktok])
        tb_ = 6 + (tt % 2)
        S.op("pe", lambda e, tt=tt, tb_=tb_: e.transpose(
            out=PS[tb_][:].bitcast(BF16)[:, 0:128], in_=ckvtok[:, tt, :], identity=ident[:]),
            reads=[Bcktok, Bconst], writes=[Bps[tb_]])
        S.op("dve", lambda e, tt=tt, tb_=tb_: e.tensor_copy(
            out=ckvT[:, tt * 128:(tt + 1) * 128], in_=PS[tb_][:].bitcast(BF16)[:, 0:128]),
            reads=[Bps[tb_]], writes=[Bckv])
        S.op("pe", lambda e, tt=tt, bank=bank: e.matmul(
            PS[bank][:], lhsT=ckvT[:, tt * 128:(tt + 1) * 128], rhs=wuvb[:], start=True, stop=True),
            reads=[Bckv, Bwuv], writes=[Bps[bank]])
        S.op("act", lambda e, tt=tt, bank=bank: e.activation(
            out=kvW[:, tt, :, 0:64], in_=PS[bank][:].rearrange("p (h d) -> p h d", h=8), func=AF.Copy),
            reads=[Bps[bank]], writes=[BkvW])


    def gen_gates():
        gi_ = 0
        for c in range(4):
            wb_ = c % 2
            if c >= 1 and c + 1 < 4:
                load_wgs(c + 1)
            for j in range(4):
                for tb in range(4):
                    bank = 5 + (gi_ % 2)
                    sb_ = gi_ % 2
                    gi_ += 1
                    fns = [lambda e, k=k, j=j, tb=tb, bank=bank, wb_=wb_: e.matmul(
                        PS[bank][:], lhsT=wgs[wb_][:, k, j * 128:(j + 1) * 128], rhs=xT[:, k, tb * 512:(tb + 1) * 512],
                        start=(k == 0), stop=(k == 7)) for k in range(8)]
                    S.group("pe", fns, reads=BxT + [Bwgs[wb_]], writes=[Bps[bank]])
                    S.op("act", lambda e, bank=bank, sb_=sb_: e.activation(out=gst[:, sb_, :], in_=PS[bank][:], func=AF.Sigmoid),
                         reads=[Bps[bank]], writes=[Bgst[sb_]])
                    r0 = c * 512 + j * 128
                    S.dma("sp", lambda e, r0=r0, tb=tb, sb_=sb_: e.dma_start(
                        out=gates_d[r0:r0 + 128, tb * 512:(tb + 1) * 512], in_=gst[:, sb_, :]),
                        reads=[Bgst[sb_]], writes=[Bgates])
                    yield

    for _ in gen_gates():
        pass
    if debug:
        S.dma("sp", lambda e: e.dma_start(out=dbg["d_ckvT"], in_=ckvT[:]), reads=[Bckv], writes=[Bout])
        S.dma("sp", lambda e: e.dma_start(out=dbg["d_qlatT"], in_=qlatT[:, 0, :]), reads=Bqlat, writes=[Bout])
    S.barrier()
    if stop_after <= 1:
        S.emit()
        return nc

    yaT = M("yaT", [128, 4, L], BF16, 179904, (2, 3))
    ybT = M("ybT", [128, 4, L], BF16, 179904 + 16384, (2, 3))
    Eswa = M("Eswa", [128, 4, 512], BF16, R3, (2, 2))
    ytokA = M("ytokA", [128, 2, 512], BF16, R3 + 4096, (2, 2))
    st2a = M("st2a", [128, 128], F32, R3 + 6144, (2, 2))
    Edsa = M("Edsa", [128, 2, 1024], BF16, R3 + 20480, (2, 2.5))
    eTb = M("eTb", [128, 4, 512], BF16, R3 + 32768, (2, 2.5))
    pTb = M("pTb", [128, 4, 512], BF16, R3 + 36864, (2, 2.5))
    scr = M("scr", [128, 1024], F32, R3 + 40960, (2, 2))
    BEswa = Buf("Eswa"); BEdsa = Buf("Edsa"); Bscr = Buf("scr")
    BeT = [Buf("eT%d" % i) for i in range(4)]
    BpT = [Buf("pT%d" % i) for i in range(4)]
    BytokA = [Buf("ytokA0"), Buf("ytokA1")]
    Bst2a = Buf("st2a")
    ByaT = Buf("yaT"); BybT = Buf("ybT")

    for idx in range(4):
        typ = idx // 2
        S.dma("sp", lambda e, idx=idx: e.dma_start(out=scr[:, 0:512], in_=swab_d[idx]), writes=[Bscr])
        S.op("act", lambda e, idx=idx: e.activation(out=Eswa[:, idx, :], in_=scr[:, 0:512], func=AF.Exp),
             reads=[Bscr], writes=[BEswa])
        for j in range(4):
            if typ == 1:
                S.op("pool", lambda e, idx=idx, j=j: e.tensor_tensor(
                    out=Eswa[:, idx, j * 128:(j + 1) * 128], in0=Eswa[:, idx, j * 128:(j + 1) * 128],
                    in1=causal01[:], op=ALU.mult), reads=[BEswa, Bconst], writes=[BEswa])
            else:
                S.op("pool", lambda e, idx=idx, j=j: e.affine_select(
                    out=Eswa[:, idx, j * 128:(j + 1) * 128], in_=Eswa[:, idx, j * 128:(j + 1) * 128],
                    pattern=[[-1, 128]], compare_op=ALU.is_ge, fill=preg(e, 0.0), base=-1, channel_multiplier=1),
                    reads=[BEswa], writes=[BEswa])
    for typ in range(2):
        S.dma("sp", lambda e, typ=typ: e.dma_start(out=scr[:], in_=dsab_d[typ]), writes=[Bscr])
        for h in range(8):
            S.op("act", lambda e, typ=typ, h=h: e.activation(
                out=Edsa[:, typ, h * 128:(h + 1) * 128], in_=scr[:, h * 128:(h + 1) * 128], func=AF.Exp,
                bias=negc31[:, h:h + 1], scale=1.0), reads=[Bscr, Bconst], writes=[BEdsa])
            if typ == 1:
                S.op("pool", lambda e, h=h: e.tensor_tensor(
                    out=Edsa[:, 1, h * 128:(h + 1) * 128], in0=Edsa[:, 1, h * 128:(h + 1) * 128],
                    in1=causal01[:], op=ALU.mult), reads=[BEdsa, Bconst], writes=[BEdsa])

    def ytok_to_T(nblk, ysrc, ybufB, dstT, bdst):
        pv = PS[7][:].bitcast(BF16)
        fns = [lambda e, c=c: e.transpose(out=pv[:, c * 128:(c + 1) * 128],
                                         in_=ysrc[:, c * 128:(c + 1) * 128], identity=ident[:])
               for c in range(4)]
        S.group("pe", fns, reads=[ybufB, Bconst], writes=[Bps[7]])
        S.op("act", lambda e: e.activation(
            out=dstT[:, :, nblk * 128:(nblk + 1) * 128], in_=pv[:, 0:512].rearrange("p (c t) -> p c t", c=4),
            func=AF.Copy), reads=[Bps[7]], writes=[bdst])

    items = []
    for n in range(NT):
        for g in range(2):
            chunks = ([(n - 1, 0)] if n > 0 else []) + [(n, 1)]
            for ci, (kb, typ) in enumerate(chunks):
                items.append((n, g, kb, typ, ci == 0, ci == len(chunks) - 1))

    def swa_stage1(idx):
        n, g, kb, typ, first, last = items[idx]
        r = idx % 4
        lb = idx % 3
        S.op("pe", lambda e: e.matmul(
            PS[lb][:], lhsT=kbT[g * 64:(g + 1) * 64, kb * 128:(kb + 1) * 128],
            rhs=qbT[g * 64:(g + 1) * 64, :, n * 128:(n + 1) * 128], start=True, stop=True),
            reads=[Bkb, Bqb], writes=[Bps[lb]])
        S.op("act", lambda e: e.activation(out=eTb[:, r, :], in_=PS[lb][:], func=AF.Exp),
             reads=[Bps[lb]], writes=[BeT[r]])
        S.op("dve", lambda e: e.tensor_tensor(
            out=pTb[:, r, :], in0=eTb[:, r, :], in1=Eswa[:, typ * 2 + g, :], op=ALU.mult),
            reads=[BeT[r], BEswa], writes=[BpT[r]])

    def swa_stage2(idx):
        n, g, kb, typ, first, last = items[idx]
        r = idx % 4
        ybuf = n % 2
        obank = 3 + g
        fns = [lambda e, j=j: e.matmul(
            PS[obank][:, j * 65:(j + 1) * 65], lhsT=pTb[:, r, j * 128:(j + 1) * 128],
            rhs=vaug[:, kb, g, :], start=(first and j == 0), stop=(last and j == 3),
            skip_group_check=True) for j in range(4)]
        S.group("pe", fns, reads=[BpT[r], Bvaug], writes=[Bps[obank]])
        if not last:
            return
        ov = PS[obank][:, 0:260].rearrange("p (j c) -> p j c", j=4)
        c0 = ybuf * 16 + g * 4
        S.op("dve", lambda e: e.tensor_tensor(
            out=st2a[:, c0:c0 + 4].rearrange("p (j o) -> p j o", o=1), in0=ov[:, :, 64:65],
            in1=esink[:, g * 4:(g + 1) * 4].rearrange("p (j o) -> p j o", o=1), op=ALU.add),
            reads=[Bps[obank], Bconst], writes=[Bst2a])
        S.op("dve", lambda e: e.reciprocal(out=st2a[:, 32 + c0:32 + c0 + 4], in_=st2a[:, c0:c0 + 4]),
             reads=[Bst2a], writes=[Bst2a])
        S.op("dve", lambda e: e.tensor_tensor(
            out=ytokA[:, ybuf, g * 256:(g + 1) * 256].rearrange("p (j d) -> p j d", j=4), in0=ov[:, :, 0:64],
            in1=st2a[:, 32 + c0:32 + c0 + 4].rearrange("p (j o) -> p j o", o=1).to_broadcast([128, 4, 64]),
            op=ALU.mult), reads=[Bps[obank], Bst2a], writes=[BytokA[ybuf]])
        if g == 1:
            ytok_to_T(n, ytokA[:, ybuf, :], BytokA[ybuf], ybT, BybT)

    LAG = 2
    for idx in range(len(items) + LAG):
        if idx < len(items):
            swa_stage1(idx)
        if idx >= LAG:
            swa_stage2(idx - LAG)
    if debug:
        S.dma("sp", lambda e: e.dma_start(out=dbg["d_yb"].rearrange("(c p) t -> p c t", p=128), in_=ybT[:]),
              reads=[BybT], writes=[Bout])
    S.barrier()

    QB = 83968
    scoresA = M("scoresA", [128, L], F32, R3, (2.5, 2.5))
    scoresB = M("scoresB", [128, L], F32, QB, (2.5, 2.5))
    scoresC = M("scoresC", [128, L], F32, 2048, (2.5, 2.5))
    scoresD = M("scoresD", [128, L], F32, 2048 + 8192, (2.5, 2.5))
    maskbA = M("maskbA", [128, L], BF16, R3 + 8192, (2.5, 2.5))
    maskbB = M("maskbB", [128, L], BF16, 2048 + 16384, (2.5, 2.5))
    osb = M("osb", [128, 2, 520], F32, 129280, (2.5, 2.5))
    maskT_lo = M("maskT_lo", [128, 2, NT, 128], BF16, R3 + 12288, (2.5, 2.5))
    maskT_hi = M("maskT_hi", [128, 2, NT, 128], BF16, 2048 + 20480, (2.5, 2.5))
    rbuf = M("rbuf", [128, 4, 512], BF16, R3 + 40960, (2.5, 2.5))
    dg = M("dg", [128, 2, 8, 128], BF16, QB + 8192, (2.5, 2.5))
    ytokB = M("ytokB", [128, 2, 512], BF16, QB + 12288, (2.5, 2.5))
    st2 = M("st2", [128, 4, 64], F32, QB + 14336, (2.5, 2.5))
    steps = M("steps", [128, 32], F32, QB + 15360, (2.5, 2.5))
    sd0 = M("sd0", [128, 4, 32], F32, QB + 15488, (2.5, 2.5))
    zt = M("zt", [128, D], BF16, R3 + 24576, (2.5, 2.5))
    Bzero = Buf("zero")
    S.op("pool", lambda e: e.memset(zt[:], 0.0), writes=[Bzero])
    xs_v = xs_d.rearrange("(j p) n -> p j n", p=128)
    for j0 in range(0, 64, 8):
        S.dma("sp", lambda e, j0=j0: e.dma_start(
            out=xs_v[:, j0:j0 + 8, :], in_=zt[:].rearrange("p (o n) -> p o n", o=1).to_broadcast([128, 8, D])),
            reads=[Bzero], writes=[Bzero])
    Bwbf = Buf("wbf16")
    bg_jobs = []
    for r0 in range(0, NEXP * 128, 512):
        for src_, dst_ in ((wgr_d, wgb_d), (wur_d, wub_d), (wdr_d, wdb_d)):
            bg_jobs.append((src_, dst_, r0))

    def bg_convert(n=1):
        for _ in range(n):
            if not bg_jobs:
                return
            src_, dst_, r0 = bg_jobs.pop(0)
            S.dma("pool", lambda e, src_=src_, dst_=dst_, r0=r0: e.dma_start(
                out=dst_[r0:r0 + 512, :], in_=src_[r0:r0 + 512, :]), writes=[Bwbf])
    kblk0 = M("kblk0", [128, L], BF16, 104448, (2.5, 2.5))
    kblk1 = M("kblk1", [128, L], BF16, 2048 + 28672, (2.5, 2.5))
    kblk = [kblk0, kblk1]
    Bkblk = Buf("kblk")
    S.op("pool", lambda e: e.memset(kblk0[:], 0.0), writes=[Bkblk])
    S.op("pool", lambda e: e.memset(kblk1[:], 0.0), writes=[Bkblk])
    S.op("dve", lambda e: e.tensor_copy(out=kblk0[0:64, :], in_=kidxT[0:64, :]), reads=[Bkidx, Bkblk], writes=[Bkblk])
    S.op("dve", lambda e: e.tensor_copy(out=kblk1[64:128, :], in_=kidxT[64:128, :]), reads=[Bkidx, Bkblk], writes=[Bkblk])
    scoresX = [scoresA, scoresB, scoresC, scoresD]
    maskbX = [maskbA, maskbB]
    Bscore = [Buf("scores%d" % i) for i in range(4)]
    BmaskX = [Buf("mask0"), Buf("mask1")]; BmaskT = [Buf("maskT%d" % i) for i in range(4)]
    Brb = [Buf("rb%d" % i) for i in range(4)]
    Bdg = [Buf("dg0"), Buf("dg1")]
    BytokB = [Buf("ytokB0"), Buf("ytokB1")]
    Bbis = [Buf("bis%d" % i) for i in range(4)]
    Bst = [Buf("st%d" % i) for i in range(4)]
    Bosb = [Buf("osb0"), Buf("osb1")]
    Bsteps = Buf("steps")
    for k in range(N_BISECT):
        S.op("pool", lambda e, k=k: e.memset(steps[:, k:k + 1], 2.0 ** -(k + 1)), writes=[Bsteps])
    rotA = {"s1": 0, "rb": 0}

    def genS(i):
        Si = (i + 1) * 128
        sb = i % 2
        q4 = i % 4
        sc_t = scoresX[q4]
        for h in range(8):
            S.op("act", lambda e, h=h: e.activation(
                out=dg[:, sb, h, :], in_=ident[:], func=AF.Copy, scale=widx[:, i, h:h + 1]),
                reads=[Bconst, Bwidx], writes=[Bdg[sb]])
        yield
        nsc = (Si + 511) // 512
        stepsS = [(sc, h) for sc in range(nsc) for h in range(8)]
        slots = {}

        def S1(n):
            sc, h = stepsS[n]
            c0, c1 = sc * 512, min(Si, sc * 512 + 512)
            w = c1 - c0
            rr = rotA["rb"] % 4
            ab = 2 + (rotA["rb"] % 2)
            rotA["rb"] += 1
            slots[n] = rr
            hp = (h % 2) * 64
            S.op("pe", lambda e: e.matmul(
                PS[ab][:, 0:w], lhsT=qidxT[:, h // 2, i * 128:(i + 1) * 128],
                rhs=kblk[h % 2][:, c0:c1], start=True, stop=True),
                reads=[Bqidx, Bkblk], writes=[Bps[ab]])
            S.op("act", lambda e: e.activation(
                out=rbuf[:, rr, 0:w], in_=PS[ab][:, 0:w], func=AF.Relu), reads=[Bps[ab]], writes=[Brb[rr]])

        def S2(n):
            sc, h = stepsS[n]
            c0, c1 = sc * 512, min(Si, sc * 512 + 512)
            w = c1 - c0
            rr = slots[n]
            S.op("pe", lambda e: e.matmul(
                PS[6][:, 0:w], lhsT=dg[:, sb, h, :], rhs=rbuf[:, rr, 0:w], start=(h == 0), stop=(h == 7)),
                reads=[Bdg[sb], Brb[rr]], writes=[Bps[6]])
            if h == 7:
                S.op("act", lambda e: e.activation(out=sc_t[:, c0:c1], in_=PS[6][:, 0:w], func=AF.Copy),
                     reads=[Bps[6]], writes=[Bscore[q4]])

        S1(0)
        for n in range(len(stepsS)):
            if n + 1 < len(stepsS):
                S1(n + 1)
            S2(n)
            yield

    def genB(i):
        Si = (i + 1) * 128
        sb = i % 2
        q4 = i % 4
        sc_t = scoresX[q4]
        maskb = maskbX[sb]
        maskT = maskT_lo if q4 < 2 else maskT_hi
        Bmask = BmaskX[sb]
        stv = st2[:, q4, :]
        BB = [Bbis[q4]]
        S.op("dve", lambda e: e.tensor_reduce(out=stv[:, 0:1], in_=sc_t[:, 0:Si], axis=AX.X, op=ALU.max),
             reads=[Bscore[q4]], writes=BB)
        yield
        S.op("dve", lambda e: e.tensor_reduce(out=stv[:, 1:2], in_=sc_t[:, 0:Si], axis=AX.X, op=ALU.min),
             reads=[Bscore[q4]] + BB, writes=BB)
        yield
        S.op("dve", lambda e: e.scalar_tensor_tensor(
            out=stv[:, 2:3], in0=stv[:, 0:1], scalar=1.0, in1=stv[:, 1:2], op0=ALU.add, op1=ALU.subtract),
            reads=BB, writes=BB)
        S.op("dve", lambda e: e.tensor_scalar(
            out=sd0[:, q4, 0:N_BISECT], in0=steps[:, 0:N_BISECT], scalar1=stv[:, 2:3], scalar2=None, op0=ALU.mult),
            reads=BB + [Bsteps], writes=BB)
        S.op("dve", lambda e: e.tensor_tensor(out=stv[:, 3:4], in0=sd0[:, q4, 0:1], in1=stv[:, 1:2], op=ALU.add),
             reads=BB, writes=BB)
        S.op("pool", lambda e: e.affine_select(
            out=sc_t[:, i * 128:(i + 1) * 128], in_=sc_t[:, i * 128:(i + 1) * 128], pattern=[[-1, 128]],
            compare_op=ALU.is_ge, fill=preg(e, -1.0e30), base=0, channel_multiplier=1),
            reads=[Bscore[q4]] + BB, writes=[Bscore[q4]])
        yield
        for it in range(N_BISECT):
            S.op("dve", lambda e: e.tensor_scalar(
                out=maskb[:, 0:Si], in0=sc_t[:, 0:Si], scalar1=stv[:, 3:4], scalar2=None,
                op0=ALU.is_ge, op1=ALU.add, accum_out=stv[:, 4:5]),
                reads=[Bscore[q4]] + BB, writes=[Bmask] + BB)
            yield
            S.op("dve", lambda e: e.tensor_scalar(
                out=stv[:, 5:6], in0=stv[:, 4:5], scalar1=255.5, scalar2=0.5, op0=ALU.is_ge, op1=ALU.subtract),
                reads=BB, writes=BB)
            S.op("dve", lambda e, it=it: e.scalar_tensor_tensor(
                out=stv[:, 3:4], in0=stv[:, 5:6], scalar=sd0[:, q4, it:it + 1], in1=stv[:, 3:4],
                op0=ALU.mult, op1=ALU.add), reads=BB, writes=BB)
            yield
        S.op("dve", lambda e: e.scalar_tensor_tensor(
            out=stv[:, 6:7], in0=sd0[:, q4, N_BISECT - 1:N_BISECT], scalar=-0.5, in1=stv[:, 3:4],
            op0=ALU.mult, op1=ALU.add), reads=BB, writes=BB)
        S.op("dve", lambda e: e.tensor_scalar(
            out=maskb[:, 0:Si], in0=sc_t[:, 0:Si], scalar1=stv[:, 6:7], scalar2=None, op0=ALU.is_ge),
            reads=[Bscore[q4]] + BB, writes=[Bmask])
        yield
        pv = PS[7][:].bitcast(BF16)
        for q0 in range(0, i + 1, 4):
            q1 = min(i + 1, q0 + 4)
            fns = [lambda e, kb=kb, q0=q0: e.transpose(
                out=pv[:, (kb - q0) * 128:(kb - q0 + 1) * 128], in_=maskb[:, kb * 128:(kb + 1) * 128],
                identity=ident[:]) for kb in range(q0, q1)]
            S.group("pe", fns, reads=[Bmask, Bconst], writes=[Bps[7]])
            S.op("act", lambda e, q0=q0, q1=q1: e.activation(
                out=maskT[:, sb, q0:q1, :], in_=pv[:, 0:(q1 - q0) * 128].rearrange("p (c t) -> p c t", t=128),
                func=AF.Identity, scale=100.0, bias=-100.0), reads=[Bps[7]], writes=[BmaskT[q4]])
            yield

    def genA(i):
        sb = i % 2
        maskT = maskT_lo if (i % 4) < 2 else maskT_hi
        pairs = [(kb, hg) for kb in range(i + 1) for hg in range(2)]
        info = {}

        def stage1(pi):
            kb, hg = pairs[pi]
            near = kb >= i - 1
            typ = 1 if kb == i else 0
            r = rotA["s1"] % 4
            lb = rotA["s1"] % 2
            rotA["s1"] += 1
            info[pi] = r
            masked = i >= 2
            fns = [lambda e: e.matmul(
                PS[lb][:], lhsT=ckvT[:, kb * 128:(kb + 1) * 128],
                rhs=qlatT[:, hg * 4:(hg + 1) * 4, i * 128:(i + 1) * 128], start=True, stop=(not masked),
                skip_group_check=True)]
            rd = [Bckv] + Bqlat[hg * 4:(hg + 1) * 4]
            if masked:
                for j in range(4):
                    fns.append(lambda e, j=j: e.matmul(
                        PS[lb][:, j * 128:(j + 1) * 128], lhsT=ident[:], rhs=maskT[:, sb, kb, :], start=False,
                        stop=(j == 3), skip_group_check=True))
                rd = rd + [BmaskT[i % 4], Bconst]
            S.group("pe", fns, reads=rd, writes=[Bps[lb]])
            if near:
                S.op("act", lambda e: e.activation(out=eTb[:, r, :], in_=PS[lb][:], func=AF.Exp),
                     reads=[Bps[lb]], writes=[BeT[r]])
                S.op("pool", lambda e: e.tensor_tensor(out=pTb[:, r, :], in0=eTb[:, r, :],
                                                       in1=Edsa[:, typ, hg * 512:(hg + 1) * 512], op=ALU.mult),
                     reads=[BeT[r], BEdsa], writes=[BpT[r]])
            else:
                S.op("act", lambda e: e.activation(out=pTb[:, r, :], in_=PS[lb][:], func=AF.Exp),
                     reads=[Bps[lb]], writes=[BpT[r]])

        def stage2(pi):
            kb, hg = pairs[pi]
            r = info[pi]
            obank = 4 + hg
            fns = [lambda e, j=j: e.matmul(
                PS[obank][:, j * 65:(j + 1) * 65], lhsT=pTb[:, r, j * 128:(j + 1) * 128],
                rhs=kvW[:, kb, hg * 4 + j, :], start=(kb == 0 and j == 0), stop=(kb == i and j == 3),
                skip_group_check=True) for j in range(4)]
            S.group("pe", fns, reads=[BpT[r], BkvW], writes=[Bps[obank]])

        LAGA = 1
        for pi in range(len(pairs) + LAGA):
            if pi < len(pairs):
                stage1(pi)
            if pi >= LAGA:
                stage2(pi - LAGA)
            yield
        for hg in range(2):
            obank = 4 + hg
            S.op("act", lambda e, hg=hg, obank=obank: e.activation(
                out=osb[:, sb, hg * 260:(hg + 1) * 260], in_=PS[obank][:, 0:260], func=AF.Copy),
                reads=[Bps[obank]], writes=[Bosb[sb]])
        yield

        def finish():
            q4 = i % 4
            for hg in range(2):
                ov = osb[:, sb, hg * 260:(hg + 1) * 260].rearrange("p (j c) -> p j c", j=4)
                c0 = 32 + hg * 4
                S.op("dve", lambda e, ov=ov, c0=c0: e.reciprocal(
                    out=st2[:, q4, c0:c0 + 4].rearrange("p (j o) -> p j o", o=1), in_=ov[:, :, 64:65]),
                    reads=[Bosb[sb]], writes=[Bst[q4]])
                S.op("dve", lambda e, ov=ov, c0=c0, hg=hg: e.tensor_tensor(
                    out=ytokB[:, sb, hg * 256:(hg + 1) * 256].rearrange("p (j d) -> p j d", j=4), in0=ov[:, :, 0:64],
                    in1=st2[:, q4, c0:c0 + 4].rearrange("p (j o) -> p j o", o=1).to_broadcast([128, 4, 64]),
                    op=ALU.mult), reads=[Bosb[sb], Bst[q4]], writes=[BytokB[sb]])
            ytok_to_T(i, ytokB[:, sb, :], BytokB[sb], yaT, ByaT)
        post_round.append(finish)

    tick = {"n": 0}

    def interleave(gens):
        gens = [g for g in gens if g is not None]
        while gens:
            tick["n"] += 1
            if tick["n"] % 14 == 0:
                bg_convert(1)
            for g in list(gens):
                try:
                    next(g)
                except StopIteration:
                    gens.remove(g)

    import itertools
    Bjunk = Buf("junk")

    def warm_pe(n):
        fns = [lambda e: e.matmul(PS[7][:, 256:512], lhsT=ident[:], rhs=Edsa[:, 0, 0:256], start=True, stop=True,
                                  skip_group_check=True) for _ in range(n)]
        S.group("pe", fns, reads=[BEdsa, Bconst], writes=[Bjunk])

    post_round = []
    NP = NT // 2
    for rnd in range(0, NP + 2):
        gs = []
        if 1 <= rnd + 1 < NP:
            gs.append(itertools.chain(genS(2 * rnd + 2), genS(2 * rnd + 3)))
        if 1 <= rnd < NP:
            gs.append(genB(2 * rnd))
            gs.append(genB(2 * rnd + 1))
        if 0 <= rnd - 1 < NP:
            gs.append(itertools.chain(genA(2 * rnd - 2), genA(2 * rnd - 1)))
        interleave(gs)
        for f_ in post_round:
            f_()
        del post_round[:]

    bg_convert(len(bg_jobs))
    if debug:
        S.dma("sp", lambda e: e.dma_start(out=dbg["d_ya"].rearrange("(c p) t -> p c t", p=128), in_=yaT[:]),
              reads=[ByaT], writes=[Bout])
    S.barrier()
    if stop_after <= 2:
        S.emit()
        return nc

    wab = M("wab", [128, 4, D], BF16, 34816, (3, 3))
    wbb = M("wbb", [128, 4, D], BF16, 43008, (3, 3))
    mergedT = M("mergedT", [128, 8, L], BF16, 83968, (3, 4))
    sgate = M("sgate", [128, 6, 512], BF16, 51200, (3, 3))
    mtmp = M("mtmp", [128, 2, 2, 512], F32, 57344, (3, 3))
    Bwa = Buf("wa"); Bwb = Buf("wb")
    S.dma("pool", lambda e: e.dma_start(out=wab[:], in_=wa_d.rearrange("(k p) n -> p k n", p=128)), writes=[Bwa])
    S.dma("pool", lambda e: e.dma_start(out=wbb[:], in_=wb_d.rearrange("(k p) n -> p k n", p=128)), writes=[Bwb])
    woutb = M("woutb", [128, 8, D], BF16, 67584, (3, 4))
    ln1g = M("ln1g", [128, D], F32, 116736, (3, 4))
    ln1bA = M("ln1bA", [128, D], F32, 120832, (3, 4))
    Bwout = Buf("wout"); Bln1 = Buf("ln1")
    wov = wo_d.rearrange("(k p) n -> p k n", p=128)

    def prefetch_p3b():
        S.dma("pool", lambda e: e.dma_start(out=woutb[:, 0:4, :], in_=wov[:, 0:4, :]), writes=[Bwout])
        S.dma("pool", lambda e: e.dma_start(out=woutb[:, 4:8, :], in_=wov[:, 4:8, :]), writes=[Bwout])
        S.dma("sp", lambda e: e.dma_start(out=ln1g[:], in_=ln1g_d.to_broadcast([128, D])), writes=[Bln1])
        S.dma("sp", lambda e: e.dma_start(out=ln1bA[:], in_=ln1b_d.to_broadcast([128, D])), writes=[Bln1])
        S.op("act", lambda e: e.activation(out=ln1bA[:], in_=ln1bA[:], func=AF.Copy, scale=ALPHA),
             reads=[Bln1], writes=[Bln1])

    Bsg = [Buf("sg%d" % i) for i in range(6)]
    Bmt = [Buf("mt0"), Buf("mt1")]
    Bmerged = [Buf("merged%d" % f) for f in range(8)]

    def p3a_load(n):
        f, tb = n // 4, n % 4
        for gi in range(2):
            sl = (n % 3) * 2 + gi
            r0 = gi * 1024 + f * 128
            S.dma("sp", lambda e, r0=r0, tb=tb, sl=sl: e.dma_start(
                out=sgate[:, sl, :], in_=gates_d[r0:r0 + 128, tb * 512:(tb + 1) * 512]), reads=[Bgates], writes=[Bsg[sl]])

    p3a_load(0)
    p3a_load(1)
    it3 = 0
    for n in range(32):
        f, tb = n // 4, n % 4
        ts_ = slice(tb * 512, (tb + 1) * 512)
        if n + 2 < 32:
            p3a_load(n + 2)
        pb = (n % 2) * 2
        mi = n % 2
        for bi, (wt, yT, bw, by) in enumerate(((wab, yaT, Bwa, ByaT), (wbb, ybT, Bwb, BybT))):
            fns = [lambda e, k=k, wt=wt, yT=yT, f=f, ts_=ts_, pb=pb, bi=bi: e.matmul(
                PS[pb + bi][:], lhsT=wt[:, k, f * 128:(f + 1) * 128], rhs=yT[:, k, ts_],
                start=(k == 0), stop=(k == 3)) for k in range(4)]
            S.group("pe", fns, reads=[bw, by], writes=[Bps[pb + bi]])
            sl = (n % 3) * 2 + bi
            S.op("dve", lambda e, pb=pb, bi=bi, sl=sl, mi=mi: e.tensor_tensor(
                out=mtmp[:, mi, bi, :], in0=sgate[:, sl, :], in1=PS[pb + bi][:], op=ALU.mult),
                reads=[Bsg[sl], Bps[pb + bi]], writes=[Bmt[mi]])
        S.op("pool", lambda e, mi=mi, f=f, ts_=ts_: e.tensor_tensor(
            out=mergedT[:, f, ts_], in0=mtmp[:, mi, 0, :], in1=mtmp[:, mi, 1, :], op=ALU.add),
            reads=[Bmt[mi]], writes=[Bmerged[f]])
        if n == 2:
            prefetch_p3b()
    if debug:
        S.dma("sp", lambda e: e.dma_start(out=dbg["d_mergedT"].rearrange("(c p) t -> p c t", p=128), in_=mergedT[:]),
              reads=Bmerged, writes=[Bout])
    S.barrier()
    if stop_after <= 3:
        S.emit()
        return nc

    ACC_OFF = 147136
    acc = M("acc", [128, NT, D], F32, ACC_OFF, (4, 7))
    x1T = M("x1T", [128, 8, L], BF16, 2048, (4, 5))
    x1tok = M("x1tok", [128, NT, D], BF16, 34816, (4, 6))
    xtok = M("xtok", [128, 2, D], F32, 124928, (4, 4))
    rres = M("rres", [128, 3, D], F32, 133120, (4, 4))
    lnst = M("lnst", [128, 3, 32], F32, 145408, (4, 4))
    Bxtok = [Buf("xtok0"), Buf("xtok1")]
    Brres = [Buf("r%d" % i) for i in range(3)]
    Blnst = [Buf("lnst%d" % i) for i in range(3)]
    Bacc = [Buf("acc%d" % t) for t in range(NT)]
    Bx1tok = [Buf("x1tok%d" % t) for t in range(NT)]
    Bx1T = Buf("x1T")

    def ln_scaled(src, srcB, stv, stB, gt, btA, gB, dst, dstB, alpha=ALPHA):
        S.op("dve", lambda e: e.bn_stats(out=stv[:, 0:6], in_=src[:, 0:512]), reads=srcB, writes=[stB])
        S.op("dve", lambda e: e.bn_stats(out=stv[:, 6:12], in_=src[:, 512:1024]), reads=srcB + [stB], writes=[stB])
        S.op("dve", lambda e: e.bn_aggr(out=stv[:, 12:14], in_=stv[:, 0:12]), reads=[stB], writes=[stB])
        S.op("dve", lambda e: e.tensor_scalar(out=stv[:, 14:15], in0=stv[:, 13:14], scalar1=LN_EPS, scalar2=None,
                                              op0=ALU.add), reads=[stB], writes=[stB])
        S.op("act", lambda e: e.activation(out=stv[:, 14:15], in_=stv[:, 14:15], func=AF.Sqrt), reads=[stB], writes=[stB])
        S.op("dve", lambda e: e.reciprocal(out=stv[:, 15:16], in_=stv[:, 14:15]), reads=[stB], writes=[stB])
        S.op("dve", lambda e: e.tensor_scalar(out=stv[:, 16:17], in0=stv[:, 15:16], scalar1=alpha, scalar2=None,
                                              op0=ALU.mult), reads=[stB], writes=[stB])
        S.op("dve", lambda e: e.scalar_tensor_tensor(out=src, in0=src, scalar=stv[:, 12:13], in1=gt[:],
                                                     op0=ALU.subtract, op1=ALU.mult), reads=srcB + [stB, gB], writes=srcB)
        S.op("dve", lambda e: e.scalar_tensor_tensor(out=dst, in0=src, scalar=stv[:, 16:17], in1=btA[:],
                                                     op0=ALU.mult, op1=ALU.add), reads=srcB + [stB, gB], writes=dstB)

    def p3b_A(tt):
        b2, b3 = tt % 2, tt % 3
        S.dma("sp", lambda e: e.dma_start(out=xtok[:, b2, :], in_=x_d[tt * 128:(tt + 1) * 128, :]), writes=[Bxtok[b2]])
        for half in range(2):
            bank = b3 * 2 + half
            fns = [lambda e, k=k, half=half, bank=bank: e.matmul(
                PS[bank][:], lhsT=mergedT[:, k, tt * 128:(tt + 1) * 128], rhs=woutb[:, k, half * 512:(half + 1) * 512],
                start=(k == 0), stop=(k == 7)) for k in range(8)]
            S.group("pe", fns, reads=Bmerged + [Bwout], writes=[Bps[bank]])
            S.op("dve", lambda e, half=half, bank=bank: e.scalar_tensor_tensor(
                out=rres[:, b3, half * 512:(half + 1) * 512], in0=xtok[:, b2, half * 512:(half + 1) * 512], scalar=ALPHA,
                in1=PS[bank][:], op0=ALU.mult, op1=ALU.add), reads=[Bxtok[b2], Bps[bank]], writes=[Brres[b3]])

    def p3b_B(tt):
        b3 = tt % 3
        ln_scaled(rres[:, b3, :], [Brres[b3]], lnst[:, b3, :], Blnst[b3], ln1g, ln1bA, Bln1, acc[:, tt, :], [Bacc[tt]])
        S.op("act", lambda e: e.activation(out=x1tok[:, tt, :], in_=acc[:, tt, :], func=AF.Copy, scale=1.0 / ALPHA),
             reads=[Bacc[tt]], writes=[Bx1tok[tt]])

    def p3b_C(tt):
        for half in range(2):
            tbk = 6 + half
            pv = PS[tbk][:].bitcast(BF16)
            fns = [lambda e, c=c, half=half, pv=pv: e.transpose(
                out=pv[:, c * 128:(c + 1) * 128], in_=x1tok[:, tt, (half * 4 + c) * 128:(half * 4 + c + 1) * 128],
                identity=ident[:]) for c in range(4)]
            S.group("pe", fns, reads=[Bx1tok[tt], Bconst], writes=[Bps[tbk]])
            S.op("act", lambda e, half=half, pv=pv: e.activation(
                out=x1T[:, half * 4:(half + 1) * 4, tt * 128:(tt + 1) * 128],
                in_=pv[:, 0:512].rearrange("p (c t) -> p c t", c=4), func=AF.Copy), reads=[Bps[tbk]], writes=[Bx1T])

    for s_ in range(NT + 2):
        if s_ < NT:
            p3b_A(s_)
        if 1 <= s_ <= NT:
            p3b_B(s_ - 1)
        if s_ >= 2:
            p3b_C(s_ - 2)
    if debug:
        S.dma("sp", lambda e: e.dma_start(out=dbg["d_acc"].rearrange("(t p) d -> p t d", p=128), in_=acc[:]),
              reads=Bacc, writes=[Bout])
    S.barrier()
    if stop_after <= 4:
        S.emit()
        return nc

    bg_convert(len(bg_jobs))
    o = 67584
    wrb = M("wrb", [128, 8, 36], BF16, o, (5, 5)); o += 1024
    brbc = M("brbc", [128, 36], F32, o, (5, 5)); o += 256
    ustr = M("ustr", [128, 128], BF16, o, (5, 5)); o += 256
    onesb = M("onesb", [128, 128], BF16, o, (5, 5)); o += 256
    jrow = M("jrow", [128, 64], F32, o, (5, 5)); o += 256
    thr16 = M("thr16", [128, 16], F32, o, (5, 5)); o += 64
    piota = M("piota", [128, 2], F32, o, (5, 5)); o += 64
    lg = M("lg", [128, NT, 36], F32, o, (5, 5)); o += 2304
    elm = M("elm", [128, NT, 32], F32, o, (5, 5)); o += 2048
    exr = M("exr", [128, NT, 32], F32, o, (5, 5)); o += 2048
    sel = M("sel", [128, NT, 32], F32, o, (5, 5)); o += 2048
    selb = M("selb", [128, NT, 32], BF16, o, (5, 5)); o += 1024
    comb = M("comb", [128, NT, 32], F32, o, (5, 5)); o += 2048
    posf = M("posf", [128, NT, 32], F32, o, (5, 5)); o += 2048
    tmpa = M("tmpa", [128, NT, 32], F32, o, (5, 5)); o += 2048
    tmpb = M("tmpb", [128, 64, 32], F32, o, (5, 5)); o += 8192
    sm = M("sm", [128, 24, NT], F32, o, (5, 5)); o += 1536
    pen = M("pen", [128, NT, 4], F32, o, (5, 5)); o += 256
    gex = M("gex", [128, NT, 4], F32, o, (5, 5)); o += 256
    ntr = M("ntr", [128, 128], BF16, o, (5, 5)); o += 256
    cnt32 = M("cnt32", [128, 24], F32, o, (5, 5)); o += 96
    toffs = M("toffs", [128, 64], F32, o, (5, 5)); o += 256
    ejf = M("ejf", [128, 64], F32, o, (5, 5)); o += 256
    assert o < 116736
    posu = M("posu", [128, NT, 2], mybir.dt.uint32, 100352, (5, 7))
    wsl = M("wsl", [128, NT, 2], F32, 100480, (5, 7))
    widxu = M("widxu", [128, 64], mybir.dt.uint32, 100608, (5, 7))
    Bwr = Buf("wr"); Bc5 = Buf("c5"); BR = Buf("route"); Bpos = Buf("posu")
    S.dma("pool", lambda e: e.dma_start(out=wrb[:], in_=wr_d.rearrange("(k p) n -> p k n", p=128)), writes=[Bwr])
    S.dma("sp", lambda e: e.dma_start(out=brbc[:], in_=br_d.to_broadcast([128, 36])), writes=[Bwr])
    S.op("pool", lambda e: e.memset(ustr[:], 1.0), writes=[Bc5])
    S.op("pool", lambda e: e.affine_select(out=ustr[:], in_=ustr[:], pattern=[[1, 128]], compare_op=ALU.is_ge,
                                           fill=preg(e, 0.0), base=-1, channel_multiplier=-1), reads=[Bc5], writes=[Bc5])
    S.op("pool", lambda e: e.memset(onesb[:], 1.0), writes=[Bc5])
    S.op("pool", lambda e: e.iota(jrow[:], pattern=[[1, 64]], base=0, channel_multiplier=0,
                                  allow_small_or_imprecise_dtypes=True), writes=[Bc5])
    S.op("pool", lambda e: e.iota(thr16[:], pattern=[[128, 16]], base=0, channel_multiplier=0,
                                  allow_small_or_imprecise_dtypes=True), writes=[Bc5])
    S.op("pool", lambda e: e.iota(piota[:], pattern=[[0, 2]], base=0, channel_multiplier=1,
                                  allow_small_or_imprecise_dtypes=True), writes=[Bc5])

    def bc(ap, shape):
        return ap.to_broadcast(shape)

    for tt in range(NT):
        bank = tt // 8
        fns = [lambda e, k=k, tt=tt, bank=bank: e.matmul(
            PS[bank][:, (tt % 8) * 36:(tt % 8 + 1) * 36], lhsT=x1T[:, k, tt * 128:(tt + 1) * 128], rhs=wrb[:, k, :],
            start=(k == 0), stop=(k == 7), skip_group_check=True) for k in range(8)]
        S.group("pe", fns, reads=[Bx1T, Bwr], writes=[Bps[bank]])
    for bank in range(2):
        S.op("dve", lambda e, bank=bank: e.tensor_tensor(
            out=lg[:, bank * 8:(bank + 1) * 8, :], in0=PS[bank][:, 0:288].rearrange("p (t c) -> p t c", c=36),
            in1=bc(brbc[:].rearrange("p (o c) -> p o c", o=1), [128, 8, 36]), op=ALU.add),
            reads=[Bps[bank], Bwr], writes=[BR])
    RB = [BR]
    def smv(r):
        return sm[:, r, :]

    def sm3(r, n):
        return bc(sm[:, r, :].rearrange("p (t o) -> p t o", o=1), [128, NT, n])

    S.op("dve", lambda e: e.tensor_reduce(out=smv(0), in_=lg[:, :, 0:4], axis=AX.X, op=ALU.max), reads=RB, writes=RB)
    S.op("dve", lambda e: e.tensor_tensor(out=pen[:], in0=lg[:, :, 0:4], in1=sm3(0, 4), op=ALU.is_lt), reads=RB, writes=RB)
    S.op("dve", lambda e: e.tensor_tensor(out=gex[:], in0=lg[:, :, 0:4], in1=sm3(0, 4), op=ALU.subtract), reads=RB, writes=RB)
    S.op("act", lambda e: e.activation(out=gex[:], in_=gex[:], func=AF.Exp), reads=RB, writes=RB)
    S.op("dve", lambda e: e.tensor_reduce(out=smv(1), in_=gex[:], axis=AX.X, op=ALU.add), reads=RB, writes=RB)
    S.op("dve", lambda e: e.tensor_scalar(out=pen[:], in0=pen[:], scalar1=NEG, scalar2=None, op0=ALU.mult), reads=RB, writes=RB)
    S.op("dve", lambda e: e.tensor_tensor(
        out=elm[:].rearrange("p t (g j) -> p t g j", g=4), in0=lg[:, :, 4:36].rearrange("p t (g j) -> p t g j", g=4),
        in1=bc(pen[:].rearrange("p t (g o) -> p t g o", o=1), [128, NT, 4, 8]), op=ALU.add), reads=RB, writes=RB)
    S.op("dve", lambda e: e.tensor_reduce(out=smv(2), in_=elm[:], axis=AX.X, op=ALU.max), reads=RB, writes=RB)
    S.op("dve", lambda e: e.tensor_tensor(out=tmpa[:], in0=elm[:], in1=sm3(2, 32), op=ALU.is_ge), reads=RB, writes=RB)
    S.op("dve", lambda e: e.scalar_tensor_tensor(out=tmpa[:], in0=tmpa[:], scalar=NEG, in1=elm[:], op0=ALU.mult, op1=ALU.add),
         reads=RB, writes=RB)
    S.op("dve", lambda e: e.tensor_reduce(out=smv(3), in_=tmpa[:], axis=AX.X, op=ALU.max), reads=RB, writes=RB)
    S.op("dve", lambda e: e.tensor_tensor(out=exr[:], in0=elm[:], in1=sm3(2, 32), op=ALU.subtract), reads=RB, writes=RB)
    S.op("act", lambda e: e.activation(out=exr[:], in_=exr[:], func=AF.Exp), reads=RB, writes=RB)
    S.op("dve", lambda e: e.tensor_tensor(out=sel[:], in0=elm[:], in1=sm3(3, 32), op=ALU.is_ge), reads=RB, writes=RB)
    S.op("dve", lambda e: e.tensor_copy(out=selb[:], in_=sel[:]), reads=RB, writes=RB)
    S.op("dve", lambda e: e.tensor_tensor(out=comb[:], in0=sel[:], in1=exr[:], op=ALU.mult), reads=RB, writes=RB)
    S.op("dve", lambda e: e.tensor_reduce(out=smv(4), in_=comb[:], axis=AX.X, op=ALU.add), reads=RB, writes=RB)
    S.op("dve", lambda e: e.tensor_tensor(out=smv(5), in0=smv(4), in1=smv(1), op=ALU.mult), reads=RB, writes=RB)
    S.op("dve", lambda e: e.reciprocal(out=smv(6), in_=smv(5)), reads=RB, writes=RB)
    S.op("dve", lambda e: e.tensor_tensor(out=comb[:], in0=comb[:], in1=sm3(6, 32), op=ALU.mult), reads=RB, writes=RB)
    if debug:
        S.dma("sp", lambda e: e.dma_start(out=dbg["d_comb"].rearrange("(t p) d -> p t d", p=128), in_=comb[:]),
              reads=RB, writes=[Bout])
    for tt in range(NT):
        fns = []
        for tp in range(tt):
            fns.append(lambda e, tt=tt, tp=tp: e.matmul(
                PS[2][:, tt * 32:(tt + 1) * 32], lhsT=onesb[:], rhs=selb[:, tp, :], start=(tp == 0), stop=False,
                skip_group_check=True))
        fns.append(lambda e, tt=tt: e.matmul(
            PS[2][:, tt * 32:(tt + 1) * 32], lhsT=ustr[:], rhs=selb[:, tt, :], start=(tt == 0), stop=True,
            skip_group_check=True))
        S.group("pe", fns, reads=RB + [Bc5], writes=[Bps[2]])
    fns = [lambda e, tt=tt: e.matmul(PS[3][0:32, 0:1], lhsT=selb[:, tt, :], rhs=onesb[:, 0:1],
                                     start=(tt == 0), stop=(tt == NT - 1)) for tt in range(NT)]
    S.group("pe", fns, reads=RB + [Bc5], writes=[Bps[3]])
    S.op("dve", lambda e: e.tensor_copy(out=cnt32[0:32, 0:1], in_=PS[3][0:32, 0:1]), reads=[Bps[3]], writes=RB)
    S.op("dve", lambda e: e.tensor_scalar(out=cnt32[0:32, 4:20], in0=thr16[0:32, :], scalar1=cnt32[0:32, 0:1], scalar2=None,
                                          op0=ALU.is_lt), reads=RB + [Bc5], writes=RB)
    S.op("dve", lambda e: e.tensor_reduce(out=cnt32[0:32, 1:2], in_=cnt32[0:32, 4:20], axis=AX.X, op=ALU.add), reads=RB, writes=RB)
    S.op("dve", lambda e: e.tensor_scalar(out=ntr[0:32, :], in0=onesb[0:32, :], scalar1=cnt32[0:32, 1:2], scalar2=None,
                                          op0=ALU.mult), reads=RB + [Bc5], writes=RB)
    S.op("pe", lambda e: e.matmul(PS[3][:, 64:96], lhsT=ntr[0:32, :], rhs=ustr[0:32, 0:32], start=True, stop=True),
         reads=RB + [Bc5], writes=[Bps[3]])
    S.op("pe", lambda e: e.matmul(PS[3][:, 96:128], lhsT=ntr[0:32, :], rhs=causal01[0:32, 0:32], start=True, stop=True),
         reads=RB + [Bconst], writes=[Bps[3]])
    S.op("dve", lambda e: e.tensor_copy(out=toffs[:], in_=PS[3][:, 64:128]), reads=[Bps[3]], writes=RB)
    S.op("dve", lambda e: e.scalar_tensor_tensor(
        out=posf[:], in0=bc(toffs[:, 0:32].rearrange("p (o c) -> p o c", o=1), [128, NT, 32]), scalar=128.0,
        in1=PS[2][:].rearrange("p (t c) -> p t c", c=32), op0=ALU.mult, op1=ALU.add), reads=RB + [Bps[2]], writes=RB)
    S.op("dve", lambda e: e.tensor_tensor(out=tmpa[:], in0=sel[:], in1=posf[:], op=ALU.mult), reads=RB, writes=RB)
    S.op("dve", lambda e: e.tensor_reduce(out=smv(8), in_=tmpa[:], axis=AX.X, op=ALU.max), reads=RB, writes=RB)
    S.op("dve", lambda e: e.tensor_scalar(out=tmpa[:], in0=sel[:], scalar1=-1.0e6, scalar2=1.0e6, op0=ALU.mult, op1=ALU.add),
         reads=RB, writes=RB)
    S.op("dve", lambda e: e.tensor_tensor(out=tmpa[:], in0=tmpa[:], in1=posf[:], op=ALU.add), reads=RB, writes=RB)
    S.op("dve", lambda e: e.tensor_reduce(out=smv(7), in_=tmpa[:], axis=AX.X, op=ALU.min), reads=RB, writes=RB)
    for r_pos, r_w in ((7, 9), (8, 10)):
        S.op("dve", lambda e, r_pos=r_pos: e.tensor_tensor(out=tmpa[:], in0=posf[:], in1=sm3(r_pos, 32), op=ALU.is_equal),
             reads=RB, writes=RB)
        S.op("dve", lambda e: e.tensor_tensor(out=tmpa[:], in0=tmpa[:], in1=comb[:], op=ALU.mult), reads=RB, writes=RB)
        S.op("dve", lambda e, r_w=r_w: e.tensor_reduce(out=smv(r_w), in_=tmpa[:], axis=AX.X, op=ALU.add), reads=RB, writes=RB)
    for slot in range(2):
        S.op("dve", lambda e, slot=slot: e.tensor_copy(out=posu[:, :, slot], in_=smv(7 + slot)), reads=RB, writes=[Bpos])
        S.op("dve", lambda e, slot=slot: e.tensor_copy(out=wsl[:, :, slot], in_=smv(9 + slot)), reads=RB, writes=[Bpos])
    S.op("dve", lambda e: e.tensor_tensor(
        out=tmpb[:], in0=bc(toffs[:, 32:64].rearrange("p (o c) -> p o c", o=1), [128, 64, 32]),
        in1=bc(jrow[:].rearrange("p (j o) -> p j o", o=1), [128, 64, 32]), op=ALU.is_le), reads=RB + [Bc5], writes=RB)
    S.op("dve", lambda e: e.tensor_reduce(out=ejf[:], in_=tmpb[:], axis=AX.X, op=ALU.add), reads=RB, writes=RB)
    S.op("dve", lambda e: e.scalar_tensor_tensor(
        out=ejf[:], in0=ejf[:], scalar=128.0, in1=bc(piota[:, 0:1], [128, 64]), op0=ALU.mult, op1=ALU.add),
        reads=RB + [Bc5], writes=RB)
    S.op("dve", lambda e: e.tensor_copy(out=widxu[:], in_=ejf[:]), reads=RB, writes=[Bpos])
    S.barrier()
    if stop_after <= 5:
        S.emit()
        return nc

    NTL = 64
    NB = 5
    NX = 6
    wgt = [M("wgt%d" % b, [128, 2048], BF16, 2048 + b * 4096, (6, 6)) for b in range(NB)]
    wut = [M("wut%d" % b, [128, 2048], BF16, 100864 + b * 4096, (6, 6)) for b in range(NB)]
    wdt = [M("wdt%d" % b, [128, 2048], BF16, 121344 + b * 4096, (6, 6)) for b in range(NB)]
    xst = M("xst", [128, NX, D], BF16, 22528, (6, 6))
    xsT = M("xsT", [128, 2, 8, 128], BF16, 71680, (6, 6))
    sgt = M("sgt", [128, 2, 256], BF16, 75776, (6, 6))
    hTt = M("hTt", [128, 2, 256], BF16, 76800, (6, 6))
    ysb = M("ysb", [128, 2, D], F32, 77824, (6, 6))
    Bwt = [[Buf("wt%d_%d" % (m, b)) for b in range(NB)] for m in range(3)]
    Bxst = [Buf("xst%d" % i) for i in range(NX)]; BxsT = [Buf("xsT0"), Buf("xsT1")]
    Bsgt = [Buf("sgt0"), Buf("sgt1")]; BhTt = [Buf("hTt0"), Buf("hTt1")]; Bysb = [Buf("ysb0"), Buf("ysb1")]
    Bxs = Buf("xs_d"); Bys = Buf("ys_d")
    for tt in range(NT):
        for slot in range(2):
            S.dma("pool", lambda e, tt=tt, slot=slot: e.indirect_dma_start(
                out=xs_d, out_offset=bass.IndirectOffsetOnAxis(ap=posu[:, tt, slot:slot + 1], axis=0),
                in_=x1tok[:, tt, :], in_offset=None), reads=[Bpos, Bx1tok[tt], Bzero], writes=[Bxs])

    def moe_load_w(j):
        b = j % NB
        for m, (wd_, wt_) in enumerate(((wgb_d, wgt), (wub_d, wut), (wdb_d, wdt))):
            S.dma("pool", lambda e, wd_=wd_, wt_=wt_: e.indirect_dma_start(
                out=wt_[b][:], out_offset=None, in_=wd_,
                in_offset=bass.IndirectOffsetOnAxis(ap=widxu[:, j:j + 1], axis=0), bounds_check=preg(e, NEXP * 128 - 1),
                oob_is_err=False), reads=[Bpos, Bwbf], writes=[Bwt[m][b]])

    def moe_load_x(j):
        bx = j % NX
        S.dma("sp", lambda e: e.dma_start(out=xst[:, bx, :], in_=xs_d[j * 128:(j + 1) * 128, :]), reads=[Bxs], writes=[Bxst[bx]])

    def moe_T(j):
        b2, bx = j % 2, j % NX
        pv = PS[b2][:].bitcast(BF16)
        fns = [lambda e, c=c: e.transpose(out=pv[:, c * 128:(c + 1) * 128], in_=xst[:, bx, c * 128:(c + 1) * 128],
                                         identity=ident[:]) for c in range(8)]
        S.group("pe", fns, reads=[Bxst[bx], Bconst], writes=[Bps[b2]])
        S.op("act", lambda e: e.activation(out=xsT[:, b2, :, :], in_=pv.rearrange("p (c t) -> p c t", c=8), func=AF.Copy),
             reads=[Bps[b2]], writes=[BxsT[b2]])

    def moe_GU(j):
        b2, b = j % 2, j % NB
        bank = 2 + b2
        fns = []
        for m, wt_ in ((0, wgt), (1, wut)):
            wv = wt_[b][:].rearrange("p (k n) -> p k n", k=8)
            for c in range(2):
                for k in range(8):
                    fns.append(lambda e, m=m, wv=wv, c=c, k=k: e.matmul(
                        PS[bank][:, (m * 2 + c) * 128:(m * 2 + c + 1) * 128], lhsT=wv[:, k, c * 128:(c + 1) * 128],
                        rhs=xsT[:, b2, k, :], start=(k == 0), stop=(k == 7), skip_group_check=True))
        S.group("pe", fns, reads=[BxsT[b2], Bwt[0][b], Bwt[1][b]], writes=[Bps[bank]])
        S.op("act", lambda e: e.activation(out=sgt[:, b2, :], in_=PS[bank][:, 0:256], func=AF.Silu),
             reads=[Bps[bank]], writes=[Bsgt[b2]])
        S.op("dve", lambda e: e.tensor_tensor(out=hTt[:, b2, :], in0=sgt[:, b2, :], in1=PS[bank][:, 256:512], op=ALU.mult),
             reads=[Bsgt[b2], Bps[bank]], writes=[BhTt[b2]])

    def moe_D(j):
        b2, b = j % 2, j % NB
        wv = wdt[b][:].rearrange("p (c n) -> p c n", c=2)
        for half in range(2):
            bank = 4 + b2 * 2 + half
            fns = [lambda e, c=c, half=half, bank=bank: e.matmul(
                PS[bank][:], lhsT=hTt[:, b2, c * 128:(c + 1) * 128], rhs=wv[:, c, half * 512:(half + 1) * 512],
                start=(c == 0), stop=(c == 1)) for c in range(2)]
            S.group("pe", fns, reads=[BhTt[b2], Bwt[2][b]], writes=[Bps[bank]])
            if half == 0:
                S.op("act", lambda e, bank=bank: e.activation(out=ysb[:, b2, 0:512], in_=PS[bank][:], func=AF.Copy),
                     reads=[Bps[bank]], writes=[Bysb[b2]])
            else:
                S.op("dve", lambda e, bank=bank: e.tensor_copy(out=ysb[:, b2, 512:1024], in_=PS[bank][:]),
                     reads=[Bps[bank]], writes=[Bysb[b2]])
        S.dma("sp", lambda e: e.dma_start(out=ys_d[j * 128:(j + 1) * 128, :], in_=ysb[:, b2, :]), reads=[Bysb[b2]], writes=[Bys])

    for j in range(NX):
        moe_load_x(j)
    for j in range(NB):
        moe_load_w(j)
    for s_ in range(NTL + 2):
        if s_ < NTL:
            moe_T(s_)
            if s_ + NX < NTL:
                moe_load_x(s_ + NX)
        if 1 <= s_ <= NTL:
            moe_GU(s_ - 1)
        if s_ >= 2:
            moe_D(s_ - 2)
            if s_ - 2 + NB < NTL:
                moe_load_w(s_ - 2 + NB)
    S.barrier()

    NYG = 12
    yg = M("yg", [128, NYG, D], F32, 34816, (7, 7))
    ln2g = M("ln2g", [128, D], F32, 2048, (7, 7))
    ln2bA = M("ln2bA", [128, D], F32, 6144, (7, 7))
    obuf = M("obuf", [128, 3, D], F32, 10240, (7, 7))
    lnst2 = M("lnst2", [128, 3, 32], F32, 22528, (7, 7))
    Bln2 = Buf("ln2"); Bob = [Buf("ob%d" % i) for i in range(3)]; Blnst2 = [Buf("ls%d" % i) for i in range(3)]
    Byg = [Buf("yg%d" % i) for i in range(NYG)]
    S.dma("sp", lambda e: e.dma_start(out=ln2g[:], in_=ln2g_d.to_broadcast([128, D])), writes=[Bln2])
    S.dma("sp", lambda e: e.dma_start(out=ln2bA[:], in_=ln2b_d.to_broadcast([128, D])), writes=[Bln2])

    def tail_gather(q):
        tt, slot = q // 2, q % 2
        yb_ = q % NYG
        S.dma("pool", lambda e: e.indirect_dma_start(
            out=yg[:, yb_, :], out_offset=None, in_=ys_d,
            in_offset=bass.IndirectOffsetOnAxis(ap=posu[:, tt, slot:slot + 1], axis=0)),
            reads=[Bpos, Bys], writes=[Byg[yb_]])

    for q in range(NYG):
        tail_gather(q)
    for tt in range(NT):
        b3 = tt % 3
        for slot in range(2):
            q = tt * 2 + slot
            yb_ = q % NYG
            S.op("dve", lambda e, tt=tt, slot=slot, yb_=yb_: e.scalar_tensor_tensor(
                out=acc[:, tt, :], in0=yg[:, yb_, :], scalar=wsl[:, tt, slot:slot + 1], in1=acc[:, tt, :],
                op0=ALU.mult, op1=ALU.add), reads=[Byg[yb_], Bpos, Bacc[tt]], writes=[Bacc[tt]])
            if q + NYG < 2 * NT:
                tail_gather(q + NYG)
        ln_scaled(acc[:, tt, :], [Bacc[tt]], lnst2[:, b3, :], Blnst2[b3], ln2g, ln2bA, Bln2, obuf[:, b3, :], [Bob[b3]],
                  alpha=1.0)
        S.dma("sp", lambda e, tt=tt, b3=b3: e.dma_start(out=out_d[tt * 128:(tt + 1) * 128, :], in_=obuf[:, b3, :]),
              reads=[Bob[b3]], writes=[Bout])
    S.barrier()
    S.emit()
    return nc


_NC_CACHE = {}


def _host_inputs(inp, b):
    f = np.float32
    w_in = inp["w_in"][0]
    cols = list(range(0, 1024))
    cols += list(range(1152, 1664))
    cols += list(range(1664, 1728)) * 2
    qb0 = 1736
    for j in range(4):
        cols += list(range(qb0 + j * 64, qb0 + (j + 1) * 64))
        cols += list(range(qb0 + (j + 4) * 64, qb0 + (j + 5) * 64))
    cols += list(range(2248, 2376))
    cols += list(range(1024, 1152))
    cols += list(range(2376, 2504))
    cols += list(range(1728, 1736))
    assert len(cols) == W1COLS
    rel = inp["rel_bias"].astype(f)
    s = np.arange(128)[:, None]
    t = np.arange(128)[None, :]
    bk_prev = t5_bucket_np(t - s + 128)
    bk_own = t5_bucket_np(t - s)
    swab = np.zeros((4, 128, 4, 128), f)
    for typ, bk in enumerate((bk_prev, bk_own)):
        for g in range(2):
            for j in range(4):
                swab[typ * 2 + g, :, j, :] = rel[bk, 8 + 4 * g + j]
    dsab = np.zeros((2, 128, 8, 128), f)
    for typ, bk in enumerate((bk_prev, bk_own)):
        for h in range(8):
            dsab[typ, :, h, :] = rel[bk, h]
    return {
        "xT": np.ascontiguousarray(inp["x"][b].T),
        "x": np.ascontiguousarray(inp["x"][b]),
        "w1": np.ascontiguousarray(w_in[:, cols]),
        "wg": np.ascontiguousarray(w_in[:, 2504:4552]),
        "kvg": np.ascontiguousarray(inp["kv_norm_g"][0].reshape(1, 128)),
        "wuv": np.ascontiguousarray(inp["w_uv"][0].transpose(1, 0, 2).reshape(128, 512)),
        "wa": np.ascontiguousarray(inp["w_branch_a"][0]),
        "wb": np.ascontiguousarray(inp["w_branch_b"][0]),
        "wo": np.ascontiguousarray(inp["w_out"][0]),
        "sinks": np.ascontiguousarray(inp["sinks"][0].reshape(1, 8)),
        "ln1g": np.ascontiguousarray(inp["ln1_g"][0].reshape(1, D)),
        "ln1b": np.ascontiguousarray(inp["ln1_b"][0].reshape(1, D)),
        "ln2g": np.ascontiguousarray(inp["ln2_g"][0].reshape(1, D)),
        "ln2b": np.ascontiguousarray(inp["ln2_b"][0].reshape(1, D)),
        "wr": np.ascontiguousarray(np.concatenate([inp["w_group"][0], inp["w_router"][0]], axis=1)),
        "br": np.ascontiguousarray(np.concatenate([inp["b_group"][0], inp["b_router"][0]]).reshape(1, 36)),
        "wgr": np.ascontiguousarray(inp["w_gate"][0].reshape(NEXP, 8, 128, DE).transpose(0, 2, 1, 3).reshape(NEXP * 128, 2048)),
        "wur": np.ascontiguousarray(inp["w_up"][0].reshape(NEXP, 8, 128, DE).transpose(0, 2, 1, 3).reshape(NEXP * 128, 2048)),
        "wdr": np.ascontiguousarray(inp["w_down"][0].reshape(NEXP, 2, 128, D).transpose(0, 2, 1, 3).reshape(NEXP * 128, 2048)),
        "swab": swab.reshape(4, 128, 512),
        "dsab": dsab.reshape(2, 128, 1024),
        "c31": np.ascontiguousarray(rel[31, 0:8].reshape(1, 8)),
    }


def kernel(**inputs):
    inp = {k: np.asarray(v, dtype=np.float32) for k, v in inputs.items()}
    n = 8
    if "nc" not in _NC_CACHE:
        _NC_CACHE["nc"] = build_nc(False)
    nc = _NC_CACHE["nc"]
    shared = None
    in_maps = []
    for b in range(n):
        m = _host_inputs(inp, b) if shared is None else dict(shared)
        if shared is None:
            shared = m
        else:
            m["xT"] = np.ascontiguousarray(inp["x"][b].T)
            m["x"] = np.ascontiguousarray(inp["x"][b])
        in_maps.append(m)
    res = run_bass_kernel_spmd(nc, in_maps, core_ids=list(range(n)))
    return np.stack([np.asarray(r["out"], dtype=np.float32) for r in res.results], axis=0)
```

```python
import math
import contextlib
import numpy as np
import concourse.bass as bass
import concourse.mybir as mybir
from concourse.bass_utils import run_bass_kernel_spmd

F32 = mybir.dt.float32
BF16 = mybir.dt.bfloat16
AF = mybir.ActivationFunctionType
ALU = mybir.AluOpType
AX = mybir.AxisListType

D = 1024
L = 2048
NT = 16
NEXP = 32
DE = 256
ALPHA = 2.0 ** 0.25
LN_EPS = 1e-5
RMS_EPS = 1e-6
ATT_SCALE = 128.0 ** -0.5
NEG = -30000.0
N_BISECT = 16
W1COLS = 2568
SB_BASE = 16640
SB_END = 229368


class Buf:
    __slots__ = ("name", "writer", "readers")

    def __init__(self, name):
        self.name = name
        self.writer = None
        self.readers = []


class Sched:
    ENGS = ("pe", "act", "dve", "pool", "sp")

    def __init__(self, nc, n_dma_sems=24):
        self.nc = nc
        self.ops = {e: [] for e in self.ENGS}
        self.cnt = {e: 0 for e in self.ENGS}
        self.seen = {e: {} for e in self.ENGS}
        self.n_dma_sems = n_dma_sems
        self.dma_i = {}
        self.dma_val = {}
        self.sems = {}

    def _deps(self, eng, reads, writes):
        need = {}

        def add(tok, skip_same):
            if tok is None:
                return
            e, key, val = tok
            if e == eng and skip_same:
                return
            if need.get(key, 0) < val:
                need[key] = val

        pe = eng == "pe"
        for b in reads:
            add(b.writer, pe)
        for b in writes:
            add(b.writer, pe)
            for r in b.readers:
                add(r, True)
        waits = []
        seen = self.seen[eng]
        for key, val in need.items():
            if seen.get(key, 0) < val:
                seen[key] = val
                waits.append((key, val))
        return waits

    def _commit(self, tok, reads, writes):
        for b in reads:
            b.readers.append(tok)
        for b in writes:
            b.writer = tok
            b.readers = []

    def group(self, eng, fns, reads=(), writes=()):
        waits = self._deps(eng, reads, writes)
        self.cnt[eng] += 1
        tok = (eng, "e_" + eng, self.cnt[eng])
        n = len(fns)
        for i, fn in enumerate(fns):
            self.ops[eng].append((fn, waits if i == 0 else (), ("e_" + eng, 1) if i == n - 1 else None))
        self._commit(tok, reads, writes)
        return tok

    def op(self, eng, fn, reads=(), writes=()):
        return self.group(eng, [fn], reads, writes)

    def dma(self, eng, fn, reads=(), writes=()):
        i = self.dma_i.get(eng, 0) % self.n_dma_sems
        self.dma_i[eng] = self.dma_i.get(eng, 0) + 1
        key = "d_%s_%d" % (eng, i)
        waits = list(self._deps(eng, reads, writes))
        prev = self.dma_val.get(key, 0)
        if prev > 0 and self.seen[eng].get(key, 0) < prev:
            self.seen[eng][key] = prev
            waits.append((key, prev))
        self.dma_val[key] = prev + 16
        tok = ("dma", key, self.dma_val[key])
        self.ops[eng].append((fn, waits, (key, 16)))
        self._commit(tok, reads, writes)
        return tok

    def barrier(self):
        targets = [("e_" + e, self.cnt[e]) for e in self.ENGS if self.cnt[e] > 0]
        targets += [(k, v) for k, v in self.dma_val.items() if v > 0]
        for e in self.ENGS:
            waits = []
            for key, val in targets:
                if key == "e_" + e and e != "pe":
                    pass
                if self.seen[e].get(key, 0) < val:
                    self.seen[e][key] = val
                    waits.append((key, val))
            if waits:
                self.ops[e].append((None, waits, None))

    def emit(self):
        nc = self.nc
        keys = ["e_" + e for e in self.ENGS] + sorted(self.dma_val.keys())
        with contextlib.ExitStack() as st:
            for k in keys:
                self.sems[k] = st.enter_context(nc.semaphore(k))
            block = st.enter_context(nc.Block())
            sems = self.sems

            def run(eng_name):
                def body(eng):
                    for fn, waits, inc in self.ops[eng_name]:
                        for key, val in waits:
                            eng.wait_ge(sems[key], val)
                        if fn is None:
                            continue
                        ins = fn(eng)
                        if inc is not None:
                            ins.then_inc(sems[inc[0]], inc[1])
                return body

            block.tensor(run("pe"))
            block.scalar(run("act"))
            block.vector(run("dve"))
            block.gpsimd(run("pool"))
            block.sync(run("sp"))


class Mem:
    def __init__(self, nc):
        self.nc = nc
        self.allocs = []

    def __call__(self, name, shape, dtype, off, life):
        esz = 4 if dtype == F32 else 2
        n = esz
        for s in shape[1:]:
            n *= s
        a0, a1 = SB_BASE + off, SB_BASE + off + n
        assert a1 <= SB_END, (name, a1)
        for (nm, b0, b1, lf) in self.allocs:
            if a0 < b1 and b0 < a1 and lf[0] <= life[1] and life[0] <= lf[1]:
                raise AssertionError("SBUF overlap %s vs %s" % (name, nm))
        self.allocs.append((name, a0, a1, life))
        return self.nc.alloc_sbuf_tensor_at(name, list(shape), dtype, offset=a0)


def t5_bucket_np(dist):
    n = np.maximum(dist, 0)
    nf = np.maximum(n, 1).astype(np.float32)
    large = 16 + (np.log(nf / np.float32(16)) / np.float32(math.log(128 / 16)) * np.float32(16)).astype(np.int32)
    large = np.minimum(large, 31)
    return np.where(n < 16, n, large).astype(np.int32)


def build_nc(debug=False, stop_after=99):
    nc = bass.Bass("TRN2", target_bir_lowering=False)

    def din(name, shape, dt=F32):
        return nc.dram_tensor(name, list(shape), dt, kind="ExternalInput").ap()

    xT_d = din("xT", [D, L])
    x_d = din("x", [L, D])
    w1_d = din("w1", [D, W1COLS])
    wg_d = din("wg", [D, 2048])
    kvg_d = din("kvg", [1, 128])
    wuv_d = din("wuv", [128, 512])
    wa_d = din("wa", [512, D])
    wb_d = din("wb", [512, D])
    wo_d = din("wo", [D, D])
    sinks_d = din("sinks", [1, 8])
    ln1g_d = din("ln1g", [1, D])
    ln1b_d = din("ln1b", [1, D])
    ln2g_d = din("ln2g", [1, D])
    ln2b_d = din("ln2b", [1, D])
    wr_d = din("wr", [D, 36])
    br_d = din("br", [1, 36])
    wgr_d = din("wgr", [NEXP * 128, 2048])
    wur_d = din("wur", [NEXP * 128, 2048])
    wdr_d = din("wdr", [NEXP * 128, 2048])
    wgb_d = nc.dram_tensor("wg_bf16", [NEXP * 128, 2048], BF16, kind="Internal").ap()
    wub_d = nc.dram_tensor("wu_bf16", [NEXP * 128, 2048], BF16, kind="Internal").ap()
    wdb_d = nc.dram_tensor("wd_bf16", [NEXP * 128, 2048], BF16, kind="Internal").ap()
    gates_d = nc.dram_tensor("gates_scr", [2048, L], BF16, kind="Internal").ap()
    xs_d = nc.dram_tensor("xs_scr", [64 * 128, D], BF16, kind="Internal").ap()
    ys_d = nc.dram_tensor("ys_scr", [64 * 128, D], F32, kind="Internal").ap()
    swab_d = din("swab", [4, 128, 512])
    dsab_d = din("dsab", [2, 128, 1024])
    c31_d = din("c31", [1, 8])
    out_d = nc.dram_tensor("out", [L, D], F32, kind="ExternalOutput").ap()
    dbg = {}
    if debug:
        for nm, shp in [("d_yb", [512, L]), ("d_ya", [512, L]), ("d_mergedT", [D, L]), ("d_acc", [L, D]),
                        ("d_comb", [L, 32]), ("d_ckvT", [128, L]), ("d_qlatT", [128, L])]:
            dbg[nm] = nc.dram_tensor(nm, shp, BF16 if nm in ("d_yb", "d_ya", "d_mergedT", "d_ckvT", "d_qlatT") else F32,
                                     kind="ExternalOutput").ap()

    S = Sched(nc)
    M = Mem(nc)
    _regs = {}

    def preg(e, val):
        if val not in _regs:
            _regs[val] = e.to_reg(val)
        return _regs[val]
    PS = [nc.alloc_psum_tensor("ps%d" % i, [128, 512], F32) for i in range(8)]
    Bps = [Buf("ps%d" % i) for i in range(8)]
    Bout = Buf("out")

    ident = M("ident", [128, 128], BF16, 0, (1, 7))
    gkv_bc = M("gkv_bc", [128, 128], F32, 256, (1, 7))
    esink = M("esink", [128, 8], F32, 768, (1, 7))
    negc31 = M("negc31", [128, 8], F32, 800, (1, 7))
    identf = M("identf", [128, 128], F32, 1024, (1, 7))
    causal01 = M("causal01", [128, 128], BF16, 1536, (1, 7))
    Bconst = Buf("const")

    S.op("pool", lambda e: e.memset(identf[:], 1.0), writes=[Bconst])
    S.op("pool", lambda e: e.affine_select(out=identf[:], in_=identf[:], pattern=[[1, 128]],
                                           compare_op=ALU.is_equal, fill=preg(e, 0.0), base=0, channel_multiplier=-1),
         reads=[Bconst], writes=[Bconst])
    S.op("pool", lambda e: e.tensor_copy(out=ident[:], in_=identf[:]), reads=[Bconst], writes=[Bconst])
    S.op("pool", lambda e: e.memset(causal01[:], 1.0), writes=[Bconst])
    S.op("pool", lambda e: e.affine_select(out=causal01[:], in_=causal01[:], pattern=[[1, 128]],
                                           compare_op=ALU.is_ge, fill=preg(e, 0.0), base=0, channel_multiplier=-1),
         reads=[Bconst], writes=[Bconst])
    S.dma("sp", lambda e: e.dma_start(out=gkv_bc[:], in_=kvg_d.to_broadcast([128, 128])), writes=[Bconst])
    S.dma("sp", lambda e: e.dma_start(out=esink[:], in_=sinks_d.to_broadcast([128, 8])), writes=[Bconst])
    S.dma("sp", lambda e: e.dma_start(out=negc31[:], in_=c31_d.to_broadcast([128, 8])), writes=[Bconst])
    S.op("act", lambda e: e.activation(out=esink[:], in_=esink[:], func=AF.Exp), reads=[Bconst], writes=[Bconst])
    S.op("dve", lambda e: e.tensor_scalar(out=negc31[:], in0=negc31[:], scalar1=-1.0, scalar2=None, op0=ALU.mult),
         reads=[Bconst], writes=[Bconst])

    xT = M("xT", [128, 8, L], BF16, 2048, (1, 1))
    o = 34816
    qlatT = M("qlatT", [128, 8, L], BF16, o, (1, 2.5)); o += 32768
    qidxT = M("qidxT", [128, 4, L], BF16, o, (1, 2.5)); o += 16384
    qbT = M("qbT", [128, 4, L], BF16, o, (1, 2)); o += 16384
    kidxT = M("kidxT", [128, L], BF16, o, (1, 2.5)); o += 4096
    kbT = M("kbT", [128, L], BF16, o, (1, 2)); o += 4096
    ckvT = M("ckvT", [128, L], BF16, o, (1, 2.5)); o += 4096
    kvW = M("kvW", [128, NT, 8, 65], BF16, o, (1, 2.5)); o += 16640
    vaug = M("vaug", [128, NT, 2, 65], BF16, o, (1, 2)); o += 4160
    widx = M("widx", [128, NT, 8], F32, o, (1, 2.5)); o += 512
    assert o == 133952
    R3 = 133952
    w1b = M("w1b", [128, 8, W1COLS], BF16, R3, (1, 1))
    wuvs = M("wuvs", [128, 512], F32, R3 + 41088, (1, 1))
    ckvtok = M("ckvtok", [128, NT, 128], BF16, R3 + 43136, (1, 1))
    wuvb = M("wuvb", [128, 512], BF16, R3 + 47232, (1, 1))
    p1tmp = M("p1tmp", [128, 128], F32, R3 + 48256, (1, 1))
    p1junk = M("p1junk", [128, 128], F32, R3 + 48768, (1, 1))

    BxT = [Buf("xT%d" % k) for k in range(8)]
    Bw1 = [Buf("w1_%d" % c) for c in range(6)]
    w1v = w1_d.rearrange("(k p) n -> p k n", p=128)
    xTv = xT_d.rearrange("(k p) n -> p k n", p=128)
    for k in range(8):
        S.dma("pool", lambda e, k=k: e.dma_start(out=xT[:, k, :], in_=xTv[:, k, :]), writes=[BxT[k]])
    w1blocks = [(0, 512), (512, 1024), (1024, 1536), (1536, 2048), (2048, 2304), (2304, W1COLS)]
    for c, (c0, c1) in enumerate(w1blocks):
        S.dma("pool", lambda e, c0=c0, c1=c1: e.dma_start(out=w1b[:, :, c0:c1], in_=w1v[:, :, c0:c1]),
              writes=[Bw1[c]])
    wgs = [M("wgs0", [128, 8, 512], BF16, 184320, (1, 1)), M("wgs1", [128, 8, 512], BF16, 192512, (1, 1))]
    gst = M("gst", [128, 2, 512], BF16, 200704, (1, 1))
    Bwgs = [Buf("wgs0"), Buf("wgs1")]; Bgst = [Buf("gst0"), Buf("gst1")]; Bgates_l = [Buf("gates_d%d" % i) for i in range(64)]
    wgv = wg_d.rearrange("(k p) n -> p k n", p=128)

    def load_wgs(c):
        wb_ = c % 2
        S.dma("pool", lambda e: e.dma_start(out=wgs[wb_][:], in_=wgv[:, :, c * 512:(c + 1) * 512]), writes=[Bwgs[wb_]])

    load_wgs(0)
    load_wgs(1)
    Bwuv = Buf("wuv")
    S.dma("sp", lambda e: e.dma_start(out=wuvs[:], in_=wuv_d), writes=[Bwuv])
    S.op("dve", lambda e: e.tensor_copy(out=wuvb[:], in_=wuvs[:]), reads=[Bwuv], writes=[Bwuv])

    def w1buf(col):
        for c, (c0, c1) in enumerate(w1blocks):
            if c0 <= col < c1:
                return Bw1[c]

    Bqlat = [Buf("qlat%d" % h) for h in range(8)]
    Bqidx = Buf("qidx"); Bqb = Buf("qb"); Bkidx = Buf("kidx"); Bkb = Buf("kb")
    Bckv = Buf("ckvT"); BkvW = Buf("kvW"); Bvaug = Buf("vaug"); Bwidx = Buf("widx"); Bcktok = Buf("ckvtok")

    fm_tiles = []
    for h in range(8):
        fm_tiles.append((lambda tb, h=h: qlatT[:, h, tb * 512:(tb + 1) * 512], ATT_SCALE, Bqlat[h]))
    for j in range(4):
        fm_tiles.append((lambda tb, j=j: qidxT[:, j, tb * 512:(tb + 1) * 512], 1.0, Bqidx))
    fm_tiles.append((lambda tb: kidxT[:, tb * 512:(tb + 1) * 512], 1.0, Bkidx))
    for j in range(4):
        fm_tiles.append((lambda tb, j=j: qbT[:, j, tb * 512:(tb + 1) * 512], 0.125, Bqb))
    fm_tiles.append((lambda tb: kbT[:, tb * 512:(tb + 1) * 512], 1.0, Bkb))
    ev = 0
    for ti, (dst, scale, bdst) in enumerate(fm_tiles):
        for tb in range(4):
            bank = ev % 4
            fns = []
            for k in range(8):
                fns.append(lambda e, k=k, ti=ti, tb=tb, bank=bank: e.matmul(
                    PS[bank][:], lhsT=w1b[:, k, ti * 128:(ti + 1) * 128], rhs=xT[:, k, tb * 512:(tb + 1) * 512],
                    start=(k == 0), stop=(k == 7)))
            S.group("pe", fns, reads=BxT + [w1buf(ti * 128)], writes=[Bps[bank]])
            if ev % 2 == 0:
                S.op("act", lambda e, dst=dst, tb=tb, bank=bank, scale=scale: e.activation(
                    out=dst(tb), in_=PS[bank][:], func=AF.Copy, scale=scale), reads=[Bps[bank]], writes=[bdst])
            else:
                S.op("dve", lambda e, dst=dst, tb=tb, bank=bank, scale=scale: e.tensor_scalar(
                    out=dst(tb), in0=PS[bank][:], scalar1=scale, scalar2=None, op0=ALU.mult),
                    reads=[Bps[bank]], writes=[bdst])
            ev += 1

    S.op("pool", lambda e: e.memset(vaug[:], 1.0), writes=[Bvaug])
    S.op("pool", lambda e: e.memset(kvW[:], 1.0), writes=[BkvW])
    Bp1tmp = Buf("p1tmp"); Bp1junk = Buf("p1junk")
    for tt in range(NT):
        bank = 4 + (tt % 2)
        fns = []
        for k in range(8):
            fns.append(lambda e, k=k, tt=tt, bank=bank: e.matmul(
                PS[bank][:, 0:264], lhsT=xT[:, k, tt * 128:(tt + 1) * 128], rhs=w1b[:, k, 2304:2568],
                start=(k == 0), stop=(k == 7)))
        S.group("pe", fns, reads=BxT + [Bw1[5]], writes=[Bps[bank]])
        S.op("act", lambda e, tt=tt, bank=bank: e.activation(
            out=p1junk[:, 0:128], in_=PS[bank][:, 0:128], func=AF.Square, accum_out=p1tmp[:, tt:tt + 1]),
            reads=[Bps[bank]], writes=[Bp1junk, Bp1tmp])
        S.op("act", lambda e, tt=tt, bank=bank: e.activation(
            out=vaug[:, tt, :, 0:64], in_=PS[bank][:, 128:256].rearrange("p (g d) -> p g d", g=2), func=AF.Copy),
            reads=[Bps[bank]], writes=[Bvaug])
        S.op("act", lambda e, tt=tt, bank=bank: e.activation(
            out=widx[:, tt, :], in_=PS[bank][:, 256:264], func=AF.Copy), reads=[Bps[bank]], writes=[Bwidx])
        S.op("dve", lambda e, tt=tt: e.tensor_scalar(
            out=p1tmp[:, 16 + tt:17 + tt], in0=p1tmp[:, tt:tt + 1], scalar1=1.0 / 128.0, scalar2=RMS_EPS,
            op0=ALU.mult, op1=ALU.add), reads=[Bp1tmp], writes=[Bp1tmp])
        S.op("act", lambda e, tt=tt: e.activation(
            out=p1tmp[:, 16 + tt:17 + tt], in_=p1tmp[:, 16 + tt:17 + tt], func=AF.Sqrt),
            reads=[Bp1tmp], writes=[Bp1tmp])
        S.op("dve", lambda e, tt=tt: e.reciprocal(
            out=p1tmp[:, 32 + tt:33 + tt], in_=p1tmp[:, 16 + tt:17 + tt]), reads=[Bp1tmp], writes=[Bp1tmp])
        S.op("dve", lambda e, tt=tt, bank=bank: e.scalar_tensor_tensor(
            out=ckvtok[:, tt, :], in0=PS[bank][:, 0:128], scalar=p1tmp[:, 32 + tt:33 + tt], in1=gkv_bc[:],
            op0=ALU.mult, op1=ALU.mult), reads=[Bps[bank], Bp1tmp, Bconst], writes=[Bcktok])
        tb_ = 6 + (tt % 2)
        S.op("pe", lambda e, tt=tt, tb_=tb_: e.transpose(
            out=PS[tb_][:].bitcast(BF16)[:, 0:128], in_=ckvtok[:, tt, :], identity=ident[:]),
            reads=[Bcktok, Bconst], writes=[Bps[tb_]])
        S.op("dve", lambda e, tt=tt, tb_=tb_: e.tensor_copy(
            out=ckvT[:, tt * 128:(tt + 1) * 128], in_=PS[tb_][:].bitcast(BF16)[:, 0:128]),
            reads=[Bps[tb_]], writes=[Bckv])
        S.op("pe", lambda e, tt=tt, bank=bank: e.matmul(
            PS[bank][:], lhsT=ckvT[:, tt * 128:(tt + 1) * 128], rhs=wuvb[:], start=True, stop=True),
            reads=[Bckv, Bwuv], writes=[Bps[bank]])
        S.op("act", lambda e, tt=tt, bank=bank: e.activation(
            out=kvW[:, tt, :, 0:64], in_=PS[bank][:].rearrange("p (h d) -> p h d", h=8), func=AF.Copy),
            reads=[Bps[bank]], writes=[BkvW])


    def gen_gates():
        gi_ = 0
        for c in range(4):
            wb_ = c % 2
            if c >= 1 and c + 1 < 4:
                load_wgs(c + 1)
            for j in range(4):
                for tb in range(4):
                    bank = 5 + (gi_ % 2)
                    sb_ = gi_ % 2
                    gi_ += 1
                    fns = [lambda e, k=k, j=j, tb=tb, bank=bank, wb_=wb_: e.matmul(
                        PS[bank][:], lhsT=wgs[wb_][:, k, j * 128:(j + 1) * 128], rhs=xT[:, k, tb * 512:(tb + 1) * 512],
                        start=(k == 0), stop=(k == 7)) for k in range(8)]
                    S.group("pe", fns, reads=BxT + [Bwgs[wb_]], writes=[Bps[bank]])
                    S.op("act", lambda e, bank=bank, sb_=sb_: e.activation(out=gst[:, sb_, :], in_=PS[bank][:], func=AF.Sigmoid),
                         reads=[Bps[bank]], writes=[Bgst[sb_]])
                    r0 = c * 512 + j * 128
                    S.dma("sp", lambda e, r0=r0, tb=tb, sb_=sb_: e.dma_start(
                        out=gates_d[r0:r0 + 128, tb * 512:(tb + 1) * 512], in_=gst[:, sb_, :]),
                        reads=[Bgst[sb_]], writes=[Bgates_l[(r0 // 128) * 4 + tb]])
                    yield

    for _ in gen_gates():
        pass
    if debug:
        S.dma("sp", lambda e: e.dma_start(out=dbg["d_ckvT"], in_=ckvT[:]), reads=[Bckv], writes=[Bout])
        S.dma("sp", lambda e: e.dma_start(out=dbg["d_qlatT"], in_=qlatT[:, 0, :]), reads=Bqlat, writes=[Bout])
    S.barrier()
    if stop_after <= 1:
        S.emit()
        return nc

    yaT = M("yaT", [128, 4, L], BF16, 179904, (2, 3))
    ybT = M("ybT", [128, 4, L], BF16, 179904 + 16384, (2, 3))
    Eswa = M("Eswa", [128, 4, 512], BF16, R3, (2, 2))
    ytokA = M("ytokA", [128, 2, 512], BF16, R3 + 4096, (2, 2))
    st2a = M("st2a", [128, 128], F32, R3 + 6144, (2, 2))
    Edsa = M("Edsa", [128, 2, 1024], BF16, R3 + 20480, (2, 2.5))
    eTb = M("eTb", [128, 4, 512], BF16, R3 + 32768, (2, 2.5))
    pTb = M("pTb", [128, 4, 512], BF16, R3 + 36864, (2, 2.5))
    scr = M("scr", [128, 1024], F32, R3 + 40960, (2, 2))
    BEswa = Buf("Eswa"); BEdsa = Buf("Edsa"); Bscr = Buf("scr")
    BeT = [Buf("eT%d" % i) for i in range(4)]
    BpT = [Buf("pT%d" % i) for i in range(4)]
    BytokA = [Buf("ytokA0"), Buf("ytokA1")]
    Bst2a = Buf("st2a")
    ByaT = Buf("yaT"); BybT = Buf("ybT")

    for idx in range(4):
        typ = idx // 2
        S.dma("sp", lambda e, idx=idx: e.dma_start(out=scr[:, 0:512], in_=swab_d[idx]), writes=[Bscr])
        S.op("act", lambda e, idx=idx: e.activation(out=Eswa[:, idx, :], in_=scr[:, 0:512], func=AF.Exp),
             reads=[Bscr], writes=[BEswa])
        for j in range(4):
            if typ == 1:
                S.op("pool", lambda e, idx=idx, j=j: e.tensor_tensor(
                    out=Eswa[:, idx, j * 128:(j + 1) * 128], in0=Eswa[:, idx, j * 128:(j + 1) * 128],
                    in1=causal01[:], op=ALU.mult), reads=[BEswa, Bconst], writes=[BEswa])
            else:
                S.op("pool", lambda e, idx=idx, j=j: e.affine_select(
                    out=Eswa[:, idx, j * 128:(j + 1) * 128], in_=Eswa[:, idx, j * 128:(j + 1) * 128],
                    pattern=[[-1, 128]], compare_op=ALU.is_ge, fill=preg(e, 0.0), base=-1, channel_multiplier=1),
                    reads=[BEswa], writes=[BEswa])
    for typ in range(2):
        S.dma("sp", lambda e, typ=typ: e.dma_start(out=scr[:], in_=dsab_d[typ]), writes=[Bscr])
        for h in range(8):
            S.op("act", lambda e, typ=typ, h=h: e.activation(
                out=Edsa[:, typ, h * 128:(h + 1) * 128], in_=scr[:, h * 128:(h + 1) * 128], func=AF.Exp,
                bias=negc31[:, h:h + 1], scale=1.0), reads=[Bscr, Bconst], writes=[BEdsa])
            if typ == 1:
                S.op("pool", lambda e, h=h: e.tensor_tensor(
                    out=Edsa[:, 1, h * 128:(h + 1) * 128], in0=Edsa[:, 1, h * 128:(h + 1) * 128],
                    in1=causal01[:], op=ALU.mult), reads=[BEdsa, Bconst], writes=[BEdsa])

    def ytok_to_T(nblk, ysrc, ybufB, dstT, bdst):
        pv = PS[7][:].bitcast(BF16)
        fns = [lambda e, c=c: e.transpose(out=pv[:, c * 128:(c + 1) * 128],
                                         in_=ysrc[:, c * 128:(c + 1) * 128], identity=ident[:])
               for c in range(4)]
        S.group("pe", fns, reads=[ybufB, Bconst], writes=[Bps[7]])
        S.op("act", lambda e: e.activation(
            out=dstT[:, :, nblk * 128:(nblk + 1) * 128], in_=pv[:, 0:512].rearrange("p (c t) -> p c t", c=4),
            func=AF.Copy), reads=[Bps[7]], writes=[bdst])

    items = []
    for n in range(NT):
        for g in range(2):
            chunks = ([(n - 1, 0)] if n > 0 else []) + [(n, 1)]
            for ci, (kb, typ) in enumerate(chunks):
                items.append((n, g, kb, typ, ci == 0, ci == len(chunks) - 1))

    def swa_stage1(idx):
        n, g, kb, typ, first, last = items[idx]
        r = idx % 4
        lb = idx % 3
        S.op("pe", lambda e: e.matmul(
            PS[lb][:], lhsT=kbT[g * 64:(g + 1) * 64, kb * 128:(kb + 1) * 128],
            rhs=qbT[g * 64:(g + 1) * 64, :, n * 128:(n + 1) * 128], start=True, stop=True),
            reads=[Bkb, Bqb], writes=[Bps[lb]])
        S.op("act", lambda e: e.activation(out=eTb[:, r, :], in_=PS[lb][:], func=AF.Exp),
             reads=[Bps[lb]], writes=[BeT[r]])
        S.op("dve", lambda e: e.tensor_tensor(
            out=pTb[:, r, :], in0=eTb[:, r, :], in1=Eswa[:, typ * 2 + g, :], op=ALU.mult),
            reads=[BeT[r], BEswa], writes=[BpT[r]])

    def swa_stage2(idx):
        n, g, kb, typ, first, last = items[idx]
        r = idx % 4
        ybuf = n % 2
        obank = 3 + g
        fns = [lambda e, j=j: e.matmul(
            PS[obank][:, j * 65:(j + 1) * 65], lhsT=pTb[:, r, j * 128:(j + 1) * 128],
            rhs=vaug[:, kb, g, :], start=(first and j == 0), stop=(last and j == 3),
            skip_group_check=True) for j in range(4)]
        S.group("pe", fns, reads=[BpT[r], Bvaug], writes=[Bps[obank]])
        if not last:
            return
        ov = PS[obank][:, 0:260].rearrange("p (j c) -> p j c", j=4)
        c0 = ybuf * 16 + g * 4
        S.op("dve", lambda e: e.tensor_tensor(
            out=st2a[:, c0:c0 + 4].rearrange("p (j o) -> p j o", o=1), in0=ov[:, :, 64:65],
            in1=esink[:, g * 4:(g + 1) * 4].rearrange("p (j o) -> p j o", o=1), op=ALU.add),
            reads=[Bps[obank], Bconst], writes=[Bst2a])
        S.op("dve", lambda e: e.reciprocal(out=st2a[:, 32 + c0:32 + c0 + 4], in_=st2a[:, c0:c0 + 4]),
             reads=[Bst2a], writes=[Bst2a])
        S.op("dve", lambda e: e.tensor_tensor(
            out=ytokA[:, ybuf, g * 256:(g + 1) * 256].rearrange("p (j d) -> p j d", j=4), in0=ov[:, :, 0:64],
            in1=st2a[:, 32 + c0:32 + c0 + 4].rearrange("p (j o) -> p j o", o=1).to_broadcast([128, 4, 64]),
            op=ALU.mult), reads=[Bps[obank], Bst2a], writes=[BytokA[ybuf]])
        if g == 1:
            ytok_to_T(n, ytokA[:, ybuf, :], BytokA[ybuf], ybT, BybT)

    LAG = 2
    for idx in range(len(items) + LAG):
        if idx < len(items):
            swa_stage1(idx)
        if idx >= LAG:
            swa_stage2(idx - LAG)
    if debug:
        S.dma("sp", lambda e: e.dma_start(out=dbg["d_yb"].rearrange("(c p) t -> p c t", p=128), in_=ybT[:]),
              reads=[BybT], writes=[Bout])
    S.barrier()

    QB = 83968
    scoresA = M("scoresA", [128, L], F32, R3, (2.5, 2.5))
    scoresB = M("scoresB", [128, L], F32, QB, (2.5, 2.5))
    scoresC = M("scoresC", [128, L], F32, 2048, (2.5, 2.5))
    scoresD = M("scoresD", [128, L], F32, 2048 + 8192, (2.5, 2.5))
    maskbA = M("maskbA", [128, L], BF16, R3 + 8192, (2.5, 2.5))
    maskbB = M("maskbB", [128, L], BF16, 2048 + 16384, (2.5, 2.5))
    osb = M("osb", [128, 2, 520], F32, 129280, (2.5, 2.5))
    maskT_lo = M("maskT_lo", [128, 2, NT, 128], BF16, R3 + 12288, (2.5, 2.5))
    maskT_hi = M("maskT_hi", [128, 2, NT, 128], BF16, 2048 + 20480, (2.5, 2.5))
    rbuf = M("rbuf", [128, 4, 512], BF16, R3 + 40960, (2.5, 2.5))
    dg = M("dg", [128, 2, 8, 128], BF16, QB + 8192, (2.5, 2.5))
    ytokB = M("ytokB", [128, 2, 512], BF16, QB + 12288, (2.5, 2.5))
    st2 = M("st2", [128, 4, 64], F32, QB + 14336, (2.5, 2.5))
    steps = M("steps", [128, 32], F32, QB + 15360, (2.5, 2.5))
    sd0 = M("sd0", [128, 4, 32], F32, QB + 15488, (2.5, 2.5))
    zt = M("zt", [128, D], BF16, R3 + 24576, (2.5, 2.5))
    Bzero = Buf("zero")
    Bzero_l = [Buf("zero%d" % i) for i in range(8)]
    S.op("pool", lambda e: e.memset(zt[:], 0.0), writes=[Bzero])
    xs_v = xs_d.rearrange("(j p) n -> p j n", p=128)
    for j0 in range(0, 64, 8):
        S.dma("sp", lambda e, j0=j0: e.dma_start(
            out=xs_v[:, j0:j0 + 8, :], in_=zt[:].rearrange("p (o n) -> p o n", o=1).to_broadcast([128, 8, D])),
            reads=[Bzero], writes=[Bzero_l[j0 // 8]])
    Bwbf_l = []
    bg_jobs = []
    for r0 in range(0, NEXP * 128, 512):
        for src_, dst_ in ((wgr_d, wgb_d), (wur_d, wub_d), (wdr_d, wdb_d)):
            bg_jobs.append((src_, dst_, r0))

    def bg_convert(n=1):
        for _ in range(n):
            if not bg_jobs:
                return
            src_, dst_, r0 = bg_jobs.pop(0)
            bj = Buf("wbf%d" % len(Bwbf_l))
            Bwbf_l.append(bj)
            S.dma("pool", lambda e, src_=src_, dst_=dst_, r0=r0: e.dma_start(
                out=dst_[r0:r0 + 512, :], in_=src_[r0:r0 + 512, :]), writes=[bj])
    kblk0 = M("kblk0", [128, L], BF16, 104448, (2.5, 2.5))
    kblk1 = M("kblk1", [128, L], BF16, 2048 + 28672, (2.5, 2.5))
    kblk = [kblk0, kblk1]
    Bkblk = Buf("kblk")
    S.op("pool", lambda e: e.memset(kblk0[:], 0.0), writes=[Bkblk])
    S.op("pool", lambda e: e.memset(kblk1[:], 0.0), writes=[Bkblk])
    S.op("dve", lambda e: e.tensor_copy(out=kblk0[0:64, :], in_=kidxT[0:64, :]), reads=[Bkidx, Bkblk], writes=[Bkblk])
    S.op("dve", lambda e: e.tensor_copy(out=kblk1[64:128, :], in_=kidxT[64:128, :]), reads=[Bkidx, Bkblk], writes=[Bkblk])
    scoresX = [scoresA, scoresB, scoresC, scoresD]
    maskbX = [maskbA, maskbB]
    Bscore = [Buf("scores%d" % i) for i in range(4)]
    BmaskX = [Buf("mask0"), Buf("mask1")]; BmaskT = [Buf("maskT%d" % i) for i in range(4)]
    Brb = [Buf("rb%d" % i) for i in range(4)]
    Bdg = [Buf("dg0"), Buf("dg1")]
    BytokB = [Buf("ytokB0"), Buf("ytokB1")]
    Bbis = [Buf("bis%d" % i) for i in range(4)]
    Bst = [Buf("st%d" % i) for i in range(4)]
    Bosb = [Buf("osb0"), Buf("osb1")]
    Bsteps = Buf("steps")
    for k in range(N_BISECT):
        S.op("pool", lambda e, k=k: e.memset(steps[:, k:k + 1], 2.0 ** -(k + 1)), writes=[Bsteps])
    rotA = {"s1": 0, "rb": 0}

    def genS(i):
        Si = (i + 1) * 128
        sb = i % 2
        q4 = i % 4
        sc_t = scoresX[q4]
        for h in range(8):
            S.op("act", lambda e, h=h: e.activation(
                out=dg[:, sb, h, :], in_=ident[:], func=AF.Copy, scale=widx[:, i, h:h + 1]),
                reads=[Bconst, Bwidx], writes=[Bdg[sb]])
        yield
        nsc = (Si + 511) // 512
        stepsS = [(sc, h) for sc in range(nsc) for h in range(8)]
        slots = {}

        def S1(n):
            sc, h = stepsS[n]
            c0, c1 = sc * 512, min(Si, sc * 512 + 512)
            w = c1 - c0
            rr = rotA["rb"] % 4
            ab = 2 + (rotA["rb"] % 2)
            rotA["rb"] += 1
            slots[n] = rr
            hp = (h % 2) * 64
            S.op("pe", lambda e: e.matmul(
                PS[ab][:, 0:w], lhsT=qidxT[:, h // 2, i * 128:(i + 1) * 128],
                rhs=kblk[h % 2][:, c0:c1], start=True, stop=True),
                reads=[Bqidx, Bkblk], writes=[Bps[ab]])
            S.op("act", lambda e: e.activation(
                out=rbuf[:, rr, 0:w], in_=PS[ab][:, 0:w], func=AF.Relu), reads=[Bps[ab]], writes=[Brb[rr]])

        def S2(n):
            sc, h = stepsS[n]
            c0, c1 = sc * 512, min(Si, sc * 512 + 512)
            w = c1 - c0
            rr = slots[n]
            S.op("pe", lambda e: e.matmul(
                PS[6][:, 0:w], lhsT=dg[:, sb, h, :], rhs=rbuf[:, rr, 0:w], start=(h == 0), stop=(h == 7)),
                reads=[Bdg[sb], Brb[rr]], writes=[Bps[6]])
            if h == 7:
                S.op("act", lambda e: e.activation(out=sc_t[:, c0:c1], in_=PS[6][:, 0:w], func=AF.Copy),
                     reads=[Bps[6]], writes=[Bscore[q4]])

        S1(0)
        for n in range(len(stepsS)):
            if n + 1 < len(stepsS):
                S1(n + 1)
            S2(n)
            yield

    def genB(i):
        Si = (i + 1) * 128
        sb = i % 2
        q4 = i % 4
        sc_t = scoresX[q4]
        maskb = maskbX[sb]
        maskT = maskT_lo if q4 < 2 else maskT_hi
        Bmask = BmaskX[sb]
        stv = st2[:, q4, :]
        BB = [Bbis[q4]]
        S.op("dve", lambda e: e.tensor_reduce(out=stv[:, 0:1], in_=sc_t[:, 0:Si], axis=AX.X, op=ALU.max),
             reads=[Bscore[q4]], writes=BB)
        yield
        S.op("dve", lambda e: e.tensor_reduce(out=stv[:, 1:2], in_=sc_t[:, 0:Si], axis=AX.X, op=ALU.min),
             reads=[Bscore[q4]] + BB, writes=BB)
        yield
        S.op("dve", lambda e: e.scalar_tensor_tensor(
            out=stv[:, 2:3], in0=stv[:, 0:1], scalar=1.0, in1=stv[:, 1:2], op0=ALU.add, op1=ALU.subtract),
            reads=BB, writes=BB)
        S.op("dve", lambda e: e.tensor_scalar(
            out=sd0[:, q4, 0:N_BISECT], in0=steps[:, 0:N_BISECT], scalar1=stv[:, 2:3], scalar2=None, op0=ALU.mult),
            reads=BB + [Bsteps], writes=BB)
        S.op("dve", lambda e: e.tensor_tensor(out=stv[:, 3:4], in0=sd0[:, q4, 0:1], in1=stv[:, 1:2], op=ALU.add),
             reads=BB, writes=BB)
        S.op("pool", lambda e: e.affine_select(
            out=sc_t[:, i * 128:(i + 1) * 128], in_=sc_t[:, i * 128:(i + 1) * 128], pattern=[[-1, 128]],
            compare_op=ALU.is_ge, fill=preg(e, -1.0e30), base=0, channel_multiplier=1),
            reads=[Bscore[q4]] + BB, writes=[Bscore[q4]])
        yield
        for it in range(N_BISECT):
            S.op("dve", lambda e: e.tensor_scalar(
                out=maskb[:, 0:Si], in0=sc_t[:, 0:Si], scalar1=stv[:, 3:4], scalar2=None,
                op0=ALU.is_ge, op1=ALU.add, accum_out=stv[:, 4:5]),
                reads=[Bscore[q4]] + BB, writes=[Bmask] + BB)
            yield
            S.op("dve", lambda e: e.tensor_scalar(
                out=stv[:, 5:6], in0=stv[:, 4:5], scalar1=255.5, scalar2=0.5, op0=ALU.is_ge, op1=ALU.subtract),
                reads=BB, writes=BB)
            S.op("dve", lambda e, it=it: e.scalar_tensor_tensor(
                out=stv[:, 3:4], in0=stv[:, 5:6], scalar=sd0[:, q4, it:it + 1], in1=stv[:, 3:4],
                op0=ALU.mult, op1=ALU.add), reads=BB, writes=BB)
            yield
        S.op("dve", lambda e: e.scalar_tensor_tensor(
            out=stv[:, 6:7], in0=sd0[:, q4, N_BISECT - 1:N_BISECT], scalar=-0.5, in1=stv[:, 3:4],
            op0=ALU.mult, op1=ALU.add), reads=BB, writes=BB)
        S.op("dve", lambda e: e.tensor_scalar(
            out=maskb[:, 0:Si], in0=sc_t[:, 0:Si], scalar1=stv[:, 6:7], scalar2=None, op0=ALU.is_ge),
            reads=[Bscore[q4]] + BB, writes=[Bmask])
        yield
        pv = PS[7][:].bitcast(BF16)
        for q0 in range(0, i + 1, 4):
            q1 = min(i + 1, q0 + 4)
            fns = [lambda e, kb=kb, q0=q0: e.transpose(
                out=pv[:, (kb - q0) * 128:(kb - q0 + 1) * 128], in_=maskb[:, kb * 128:(kb + 1) * 128],
                identity=ident[:]) for kb in range(q0, q1)]
            S.group("pe", fns, reads=[Bmask, Bconst], writes=[Bps[7]])
            S.op("act", lambda e, q0=q0, q1=q1: e.activation(
                out=maskT[:, sb, q0:q1, :], in_=pv[:, 0:(q1 - q0) * 128].rearrange("p (c t) -> p c t", t=128),
                func=AF.Identity, scale=100.0, bias=-100.0), reads=[Bps[7]], writes=[BmaskT[q4]])
            yield

    def genA(i):
        sb = i % 2
        maskT = maskT_lo if (i % 4) < 2 else maskT_hi
        pairs = [(kb, hg) for kb in range(i + 1) for hg in range(2)]
        info = {}

        def stage1(pi):
            kb, hg = pairs[pi]
            near = kb >= i - 1
            typ = 1 if kb == i else 0
            r = rotA["s1"] % 4
            lb = rotA["s1"] % 2
            rotA["s1"] += 1
            info[pi] = r
            masked = i >= 2
            fns = [lambda e: e.matmul(
                PS[lb][:], lhsT=ckvT[:, kb * 128:(kb + 1) * 128],
                rhs=qlatT[:, hg * 4:(hg + 1) * 4, i * 128:(i + 1) * 128], start=True, stop=(not masked),
                skip_group_check=True)]
            rd = [Bckv] + Bqlat[hg * 4:(hg + 1) * 4]
            if masked:
                for j in range(4):
                    fns.append(lambda e, j=j: e.matmul(
                        PS[lb][:, j * 128:(j + 1) * 128], lhsT=ident[:], rhs=maskT[:, sb, kb, :], start=False,
                        stop=(j == 3), skip_group_check=True))
                rd = rd + [BmaskT[i % 4], Bconst]
            S.group("pe", fns, reads=rd, writes=[Bps[lb]])
            if near:
                S.op("act", lambda e: e.activation(out=eTb[:, r, :], in_=PS[lb][:], func=AF.Exp),
                     reads=[Bps[lb]], writes=[BeT[r]])
                S.op("pool", lambda e: e.tensor_tensor(out=pTb[:, r, :], in0=eTb[:, r, :],
                                                       in1=Edsa[:, typ, hg * 512:(hg + 1) * 512], op=ALU.mult),
                     reads=[BeT[r], BEdsa], writes=[BpT[r]])
            else:
                S.op("act", lambda e: e.activation(out=pTb[:, r, :], in_=PS[lb][:], func=AF.Exp),
                     reads=[Bps[lb]], writes=[BpT[r]])

        def stage2(pi):
            kb, hg = pairs[pi]
            r = info[pi]
            obank = 4 + hg
            fns = [lambda e, j=j: e.matmul(
                PS[obank][:, j * 65:(j + 1) * 65], lhsT=pTb[:, r, j * 128:(j + 1) * 128],
                rhs=kvW[:, kb, hg * 4 + j, :], start=(kb == 0 and j == 0), stop=(kb == i and j == 3),
                skip_group_check=True) for j in range(4)]
            S.group("pe", fns, reads=[BpT[r], BkvW], writes=[Bps[obank]])

        LAGA = 1
        for pi in range(len(pairs) + LAGA):
            if pi < len(pairs):
                stage1(pi)
            if pi >= LAGA:
                stage2(pi - LAGA)
            yield
        for hg in range(2):
            obank = 4 + hg
            S.op("act", lambda e, hg=hg, obank=obank: e.activation(
                out=osb[:, sb, hg * 260:(hg + 1) * 260], in_=PS[obank][:, 0:260], func=AF.Copy),
                reads=[Bps[obank]], writes=[Bosb[sb]])
        yield

        def finish():
            q4 = i % 4
            for hg in range(2):
                ov = osb[:, sb, hg * 260:(hg + 1) * 260].rearrange("p (j c) -> p j c", j=4)
                c0 = 32 + hg * 4
                S.op("dve", lambda e, ov=ov, c0=c0: e.reciprocal(
                    out=st2[:, q4, c0:c0 + 4].rearrange("p (j o) -> p j o", o=1), in_=ov[:, :, 64:65]),
                    reads=[Bosb[sb]], writes=[Bst[q4]])
                S.op("dve", lambda e, ov=ov, c0=c0, hg=hg: e.tensor_tensor(
                    out=ytokB[:, sb, hg * 256:(hg + 1) * 256].rearrange("p (j d) -> p j d", j=4), in0=ov[:, :, 0:64],
                    in1=st2[:, q4, c0:c0 + 4].rearrange("p (j o) -> p j o", o=1).to_broadcast([128, 4, 64]),
                    op=ALU.mult), reads=[Bosb[sb], Bst[q4]], writes=[BytokB[sb]])
            ytok_to_T(i, ytokB[:, sb, :], BytokB[sb], yaT, ByaT)
        post_round.append(finish)

    tick = {"n": 0}

    def interleave(gens):
        gens = [g for g in gens if g is not None]
        while gens:
            tick["n"] += 1
            if tick["n"] % 14 == 0:
                bg_convert(1)
            for g in list(gens):
                try:
                    next(g)
                except StopIteration:
                    gens.remove(g)

    import itertools
    Bjunk = Buf("junk")

    def warm_pe(n):
        fns = [lambda e: e.matmul(PS[7][:, 256:512], lhsT=ident[:], rhs=Edsa[:, 0, 0:256], start=True, stop=True,
                                  skip_group_check=True) for _ in range(n)]
        S.group("pe", fns, reads=[BEdsa, Bconst], writes=[Bjunk])

    post_round = []
    NP = NT // 2
    for rnd in range(0, NP + 2):
        gs = []
        if 1 <= rnd + 1 < NP:
            gs.append(itertools.chain(genS(2 * rnd + 2), genS(2 * rnd + 3)))
        if 1 <= rnd < NP:
            gs.append(genB(2 * rnd))
            gs.append(genB(2 * rnd + 1))
        if 0 <= rnd - 1 < NP:
            gs.append(itertools.chain(genA(2 * rnd - 2), genA(2 * rnd - 1)))
        interleave(gs)
        for f_ in post_round:
            f_()
        del post_round[:]

    bg_convert(len(bg_jobs))
    if debug:
        S.dma("sp", lambda e: e.dma_start(out=dbg["d_ya"].rearrange("(c p) t -> p c t", p=128), in_=yaT[:]),
              reads=[ByaT], writes=[Bout])
    S.barrier()
    if stop_after <= 2:
        S.emit()
        return nc

    wab = M("wab", [128, 4, D], BF16, 34816, (3, 3))
    wbb = M("wbb", [128, 4, D], BF16, 43008, (3, 3))
    mergedT = M("mergedT", [128, 8, L], BF16, 83968, (3, 4))
    sgate = M("sgate", [128, 6, 512], BF16, 51200, (3, 3))
    mtmp = M("mtmp", [128, 2, 2, 512], F32, 57344, (3, 3))
    Bwa = Buf("wa"); Bwb = Buf("wb")
    S.dma("pool", lambda e: e.dma_start(out=wab[:], in_=wa_d.rearrange("(k p) n -> p k n", p=128)), writes=[Bwa])
    S.dma("pool", lambda e: e.dma_start(out=wbb[:], in_=wb_d.rearrange("(k p) n -> p k n", p=128)), writes=[Bwb])
    woutb = M("woutb", [128, 8, D], BF16, 67584, (3, 4))
    ln1g = M("ln1g", [128, D], F32, 116736, (3, 4))
    ln1bA = M("ln1bA", [128, D], F32, 120832, (3, 4))
    Bwout = Buf("wout"); Bln1 = Buf("ln1")
    wov = wo_d.rearrange("(k p) n -> p k n", p=128)

    def prefetch_p3b():
        S.dma("pool", lambda e: e.dma_start(out=woutb[:, 0:4, :], in_=wov[:, 0:4, :]), writes=[Bwout])
        S.dma("pool", lambda e: e.dma_start(out=woutb[:, 4:8, :], in_=wov[:, 4:8, :]), writes=[Bwout])
        S.dma("sp", lambda e: e.dma_start(out=ln1g[:], in_=ln1g_d.to_broadcast([128, D])), writes=[Bln1])
        S.dma("sp", lambda e: e.dma_start(out=ln1bA[:], in_=ln1b_d.to_broadcast([128, D])), writes=[Bln1])
        S.op("act", lambda e: e.activation(out=ln1bA[:], in_=ln1bA[:], func=AF.Copy, scale=ALPHA),
             reads=[Bln1], writes=[Bln1])

    Bsg = [Buf("sg%d" % i) for i in range(6)]
    Bmt = [Buf("mt0"), Buf("mt1")]
    Bmerged = [Buf("merged%d" % f) for f in range(8)]

    def p3a_load(n):
        f, tb = n // 4, n % 4
        for gi in range(2):
            sl = (n % 3) * 2 + gi
            r0 = gi * 1024 + f * 128
            S.dma("sp", lambda e, r0=r0, tb=tb, sl=sl: e.dma_start(
                out=sgate[:, sl, :], in_=gates_d[r0:r0 + 128, tb * 512:(tb + 1) * 512]),
                reads=[Bgates_l[(r0 // 128) * 4 + tb]], writes=[Bsg[sl]])

    p3a_load(0)
    p3a_load(1)
    it3 = 0
    for n in range(32):
        f, tb = n // 4, n % 4
        ts_ = slice(tb * 512, (tb + 1) * 512)
        if n + 2 < 32:
            p3a_load(n + 2)
        pb = (n % 2) * 2
        mi = n % 2
        for bi, (wt, yT, bw, by) in enumerate(((wab, yaT, Bwa, ByaT), (wbb, ybT, Bwb, BybT))):
            fns = [lambda e, k=k, wt=wt, yT=yT, f=f, ts_=ts_, pb=pb, bi=bi: e.matmul(
                PS[pb + bi][:], lhsT=wt[:, k, f * 128:(f + 1) * 128], rhs=yT[:, k, ts_],
                start=(k == 0), stop=(k == 3)) for k in range(4)]
            S.group("pe", fns, reads=[bw, by], writes=[Bps[pb + bi]])
            sl = (n % 3) * 2 + bi
            S.op("dve", lambda e, pb=pb, bi=bi, sl=sl, mi=mi: e.tensor_tensor(
                out=mtmp[:, mi, bi, :], in0=sgate[:, sl, :], in1=PS[pb + bi][:], op=ALU.mult),
                reads=[Bsg[sl], Bps[pb + bi]], writes=[Bmt[mi]])
        S.op("pool", lambda e, mi=mi, f=f, ts_=ts_: e.tensor_tensor(
            out=mergedT[:, f, ts_], in0=mtmp[:, mi, 0, :], in1=mtmp[:, mi, 1, :], op=ALU.add),
            reads=[Bmt[mi]], writes=[Bmerged[f]])
        if n == 2:
            prefetch_p3b()
    if debug:
        S.dma("sp", lambda e: e.dma_start(out=dbg["d_mergedT"].rearrange("(c p) t -> p c t", p=128), in_=mergedT[:]),
              reads=Bmerged, writes=[Bout])
    S.barrier()
    if stop_after <= 3:
        S.emit()
        return nc

    ACC_OFF = 147136
    acc = M("acc", [128, NT, D], F32, ACC_OFF, (4, 7))
    x1T = M("x1T", [128, 8, L], BF16, 2048, (4, 5))
    x1tok = M("x1tok", [128, NT, D], BF16, 34816, (4, 6))
    xtok = M("xtok", [128, 2, D], F32, 124928, (4, 4))
    rres = M("rres", [128, 3, D], F32, 133120, (4, 4))
    lnst = M("lnst", [128, 3, 32], F32, 145408, (4, 4))
    Bxtok = [Buf("xtok0"), Buf("xtok1")]
    Brres = [Buf("r%d" % i) for i in range(3)]
    Blnst = [Buf("lnst%d" % i) for i in range(3)]
    Bacc = [Buf("acc%d" % t) for t in range(NT)]
    Bx1tok = [Buf("x1tok%d" % t) for t in range(NT)]
    Bx1T = Buf("x1T")

    def ln_scaled(src, srcB, stv, stB, gt, btA, gB, dst, dstB, alpha=ALPHA):
        S.op("dve", lambda e: e.bn_stats(out=stv[:, 0:6], in_=src[:, 0:512]), reads=srcB, writes=[stB])
        S.op("dve", lambda e: e.bn_stats(out=stv[:, 6:12], in_=src[:, 512:1024]), reads=srcB + [stB], writes=[stB])
        S.op("dve", lambda e: e.bn_aggr(out=stv[:, 12:14], in_=stv[:, 0:12]), reads=[stB], writes=[stB])
        S.op("dve", lambda e: e.tensor_scalar(out=stv[:, 14:15], in0=stv[:, 13:14], scalar1=LN_EPS, scalar2=None,
                                              op0=ALU.add), reads=[stB], writes=[stB])
        S.op("act", lambda e: e.activation(out=stv[:, 14:15], in_=stv[:, 14:15], func=AF.Sqrt), reads=[stB], writes=[stB])
        S.op("dve", lambda e: e.reciprocal(out=stv[:, 15:16], in_=stv[:, 14:15]), reads=[stB], writes=[stB])
        S.op("dve", lambda e: e.tensor_scalar(out=stv[:, 16:17], in0=stv[:, 15:16], scalar1=alpha, scalar2=None,
                                              op0=ALU.mult), reads=[stB], writes=[stB])
        S.op("dve", lambda e: e.scalar_tensor_tensor(out=src, in0=src, scalar=stv[:, 12:13], in1=gt[:],
                                                     op0=ALU.subtract, op1=ALU.mult), reads=srcB + [stB, gB], writes=srcB)
        S.op("dve", lambda e: e.scalar_tensor_tensor(out=dst, in0=src, scalar=stv[:, 16:17], in1=btA[:],
                                                     op0=ALU.mult, op1=ALU.add), reads=srcB + [stB, gB], writes=dstB)

    def p3b_A(tt):
        b2, b3 = tt % 2, tt % 3
        S.dma("sp", lambda e: e.dma_start(out=xtok[:, b2, :], in_=x_d[tt * 128:(tt + 1) * 128, :]), writes=[Bxtok[b2]])
        for half in range(2):
            bank = b3 * 2 + half
            fns = [lambda e, k=k, half=half, bank=bank: e.matmul(
                PS[bank][:], lhsT=mergedT[:, k, tt * 128:(tt + 1) * 128], rhs=woutb[:, k, half * 512:(half + 1) * 512],
                start=(k == 0), stop=(k == 7)) for k in range(8)]
            S.group("pe", fns, reads=Bmerged + [Bwout], writes=[Bps[bank]])
            S.op("dve", lambda e, half=half, bank=bank: e.scalar_tensor_tensor(
                out=rres[:, b3, half * 512:(half + 1) * 512], in0=xtok[:, b2, half * 512:(half + 1) * 512], scalar=ALPHA,
                in1=PS[bank][:], op0=ALU.mult, op1=ALU.add), reads=[Bxtok[b2], Bps[bank]], writes=[Brres[b3]])

    def p3b_B(tt):
        b3 = tt % 3
        ln_scaled(rres[:, b3, :], [Brres[b3]], lnst[:, b3, :], Blnst[b3], ln1g, ln1bA, Bln1, acc[:, tt, :], [Bacc[tt]])
        S.op("act", lambda e: e.activation(out=x1tok[:, tt, :], in_=acc[:, tt, :], func=AF.Copy, scale=1.0 / ALPHA),
             reads=[Bacc[tt]], writes=[Bx1tok[tt]])

    def p3b_C(tt):
        for half in range(2):
            tbk = 6 + half
            pv = PS[tbk][:].bitcast(BF16)
            fns = [lambda e, c=c, half=half, pv=pv: e.transpose(
                out=pv[:, c * 128:(c + 1) * 128], in_=x1tok[:, tt, (half * 4 + c) * 128:(half * 4 + c + 1) * 128],
                identity=ident[:]) for c in range(4)]
            S.group("pe", fns, reads=[Bx1tok[tt], Bconst], writes=[Bps[tbk]])
            S.op("act", lambda e, half=half, pv=pv: e.activation(
                out=x1T[:, half * 4:(half + 1) * 4, tt * 128:(tt + 1) * 128],
                in_=pv[:, 0:512].rearrange("p (c t) -> p c t", c=4), func=AF.Copy), reads=[Bps[tbk]], writes=[Bx1T])

    for s_ in range(NT + 2):
        if s_ < NT:
            p3b_A(s_)
        if 1 <= s_ <= NT:
            p3b_B(s_ - 1)
        if s_ >= 2:
            p3b_C(s_ - 2)
    if debug:
        S.dma("sp", lambda e: e.dma_start(out=dbg["d_acc"].rearrange("(t p) d -> p t d", p=128), in_=acc[:]),
              reads=Bacc, writes=[Bout])
    S.barrier()
    if stop_after <= 4:
        S.emit()
        return nc

    bg_convert(len(bg_jobs))
    o = 67584
    wrb = M("wrb", [128, 8, 36], BF16, o, (5, 5)); o += 1024
    brbc = M("brbc", [128, 36], F32, o, (5, 5)); o += 256
    ustr = M("ustr", [128, 128], BF16, o, (5, 5)); o += 256
    onesb = M("onesb", [128, 128], BF16, o, (5, 5)); o += 256
    jrow = M("jrow", [128, 64], F32, o, (5, 5)); o += 256
    thr16 = M("thr16", [128, 16], F32, o, (5, 5)); o += 64
    piota = M("piota", [128, 2], F32, o, (5, 5)); o += 64
    lg = M("lg", [128, NT, 36], F32, o, (5, 5)); o += 2304
    elm = M("elm", [128, NT, 32], F32, o, (5, 5)); o += 2048
    exr = M("exr", [128, NT, 32], F32, o, (5, 5)); o += 2048
    sel = M("sel", [128, NT, 32], F32, o, (5, 5)); o += 2048
    selb = M("selb", [128, NT, 32], BF16, o, (5, 5)); o += 1024
    comb = M("comb", [128, NT, 32], F32, o, (5, 5)); o += 2048
    posf = M("posf", [128, NT, 32], F32, o, (5, 5)); o += 2048
    tmpa = M("tmpa", [128, NT, 32], F32, o, (5, 5)); o += 2048
    tmpb = M("tmpb", [128, 64, 32], F32, o, (5, 5)); o += 8192
    sm = M("sm", [128, 24, NT], F32, o, (5, 5)); o += 1536
    pen = M("pen", [128, NT, 4], F32, o, (5, 5)); o += 256
    gex = M("gex", [128, NT, 4], F32, o, (5, 5)); o += 256
    ntr = M("ntr", [128, 128], BF16, o, (5, 5)); o += 256
    cnt32 = M("cnt32", [128, 24], F32, o, (5, 5)); o += 96
    toffs = M("toffs", [128, 64], F32, o, (5, 5)); o += 256
    ejf = M("ejf", [128, 64], F32, o, (5, 5)); o += 256
    assert o < 116736
    posu = M("posu", [128, NT, 2], mybir.dt.uint32, 100352, (5, 7))
    wsl = M("wsl", [128, NT, 2], F32, 100480, (5, 7))
    widxu = M("widxu", [128, 64], mybir.dt.uint32, 100608, (5, 7))
    Bwr = Buf("wr"); Bc5 = Buf("c5"); BR = Buf("route"); Bpos = Buf("posu")
    S.dma("pool", lambda e: e.dma_start(out=wrb[:], in_=wr_d.rearrange("(k p) n -> p k n", p=128)), writes=[Bwr])
    S.dma("sp", lambda e: e.dma_start(out=brbc[:], in_=br_d.to_broadcast([128, 36])), writes=[Bwr])
    S.op("pool", lambda e: e.memset(ustr[:], 1.0), writes=[Bc5])
    S.op("pool", lambda e: e.affine_select(out=ustr[:], in_=ustr[:], pattern=[[1, 128]], compare_op=ALU.is_ge,
                                           fill=preg(e, 0.0), base=-1, channel_multiplier=-1), reads=[Bc5], writes=[Bc5])
    S.op("pool", lambda e: e.memset(onesb[:], 1.0), writes=[Bc5])
    S.op("pool", lambda e: e.iota(jrow[:], pattern=[[1, 64]], base=0, channel_multiplier=0,
                                  allow_small_or_imprecise_dtypes=True), writes=[Bc5])
    S.op("pool", lambda e: e.iota(thr16[:], pattern=[[128, 16]], base=0, channel_multiplier=0,
                                  allow_small_or_imprecise_dtypes=True), writes=[Bc5])
    S.op("pool", lambda e: e.iota(piota[:], pattern=[[0, 2]], base=0, channel_multiplier=1,
                                  allow_small_or_imprecise_dtypes=True), writes=[Bc5])

    def bc(ap, shape):
        return ap.to_broadcast(shape)

    for tt in range(NT):
        bank = tt // 8
        fns = [lambda e, k=k, tt=tt, bank=bank: e.matmul(
            PS[bank][:, (tt % 8) * 36:(tt % 8 + 1) * 36], lhsT=x1T[:, k, tt * 128:(tt + 1) * 128], rhs=wrb[:, k, :],
            start=(k == 0), stop=(k == 7), skip_group_check=True) for k in range(8)]
        S.group("pe", fns, reads=[Bx1T, Bwr], writes=[Bps[bank]])
    for bank in range(2):
        S.op("dve", lambda e, bank=bank: e.tensor_tensor(
            out=lg[:, bank * 8:(bank + 1) * 8, :], in0=PS[bank][:, 0:288].rearrange("p (t c) -> p t c", c=36),
            in1=bc(brbc[:].rearrange("p (o c) -> p o c", o=1), [128, 8, 36]), op=ALU.add),
            reads=[Bps[bank], Bwr], writes=[BR])
    RB = [BR]
    def smv(r):
        return sm[:, r, :]

    def sm3(r, n):
        return bc(sm[:, r, :].rearrange("p (t o) -> p t o", o=1), [128, NT, n])

    S.op("dve", lambda e: e.tensor_reduce(out=smv(0), in_=lg[:, :, 0:4], axis=AX.X, op=ALU.max), reads=RB, writes=RB)
    S.op("dve", lambda e: e.tensor_tensor(out=pen[:], in0=lg[:, :, 0:4], in1=sm3(0, 4), op=ALU.is_lt), reads=RB, writes=RB)
    S.op("dve", lambda e: e.tensor_tensor(out=gex[:], in0=lg[:, :, 0:4], in1=sm3(0, 4), op=ALU.subtract), reads=RB, writes=RB)
    S.op("act", lambda e: e.activation(out=gex[:], in_=gex[:], func=AF.Exp), reads=RB, writes=RB)
    S.op("dve", lambda e: e.tensor_reduce(out=smv(1), in_=gex[:], axis=AX.X, op=ALU.add), reads=RB, writes=RB)
    S.op("dve", lambda e: e.tensor_scalar(out=pen[:], in0=pen[:], scalar1=NEG, scalar2=None, op0=ALU.mult), reads=RB, writes=RB)
    S.op("dve", lambda e: e.tensor_tensor(
        out=elm[:].rearrange("p t (g j) -> p t g j", g=4), in0=lg[:, :, 4:36].rearrange("p t (g j) -> p t g j", g=4),
        in1=bc(pen[:].rearrange("p t (g o) -> p t g o", o=1), [128, NT, 4, 8]), op=ALU.add), reads=RB, writes=RB)
    S.op("dve", lambda e: e.tensor_reduce(out=smv(2), in_=elm[:], axis=AX.X, op=ALU.max), reads=RB, writes=RB)
    S.op("dve", lambda e: e.tensor_tensor(out=tmpa[:], in0=elm[:], in1=sm3(2, 32), op=ALU.is_ge), reads=RB, writes=RB)
    S.op("dve", lambda e: e.scalar_tensor_tensor(out=tmpa[:], in0=tmpa[:], scalar=NEG, in1=elm[:], op0=ALU.mult, op1=ALU.add),
         reads=RB, writes=RB)
    S.op("dve", lambda e: e.tensor_reduce(out=smv(3), in_=tmpa[:], axis=AX.X, op=ALU.max), reads=RB, writes=RB)
    S.op("dve", lambda e: e.tensor_tensor(out=exr[:], in0=elm[:], in1=sm3(2, 32), op=ALU.subtract), reads=RB, writes=RB)
    S.op("act", lambda e: e.activation(out=exr[:], in_=exr[:], func=AF.Exp), reads=RB, writes=RB)
    S.op("dve", lambda e: e.tensor_tensor(out=sel[:], in0=elm[:], in1=sm3(3, 32), op=ALU.is_ge), reads=RB, writes=RB)
    S.op("dve", lambda e: e.tensor_copy(out=selb[:], in_=sel[:]), reads=RB, writes=RB)
    S.op("dve", lambda e: e.tensor_tensor(out=comb[:], in0=sel[:], in1=exr[:], op=ALU.mult), reads=RB, writes=RB)
    S.op("dve", lambda e: e.tensor_reduce(out=smv(4), in_=comb[:], axis=AX.X, op=ALU.add), reads=RB, writes=RB)
    S.op("dve", lambda e: e.tensor_tensor(out=smv(5), in0=smv(4), in1=smv(1), op=ALU.mult), reads=RB, writes=RB)
    S.op("dve", lambda e: e.reciprocal(out=smv(6), in_=smv(5)), reads=RB, writes=RB)
    S.op("dve", lambda e: e.tensor_tensor(out=comb[:], in0=comb[:], in1=sm3(6, 32), op=ALU.mult), reads=RB, writes=RB)
    if debug:
        S.dma("sp", lambda e: e.dma_start(out=dbg["d_comb"].rearrange("(t p) d -> p t d", p=128), in_=comb[:]),
              reads=RB, writes=[Bout])
    for tt in range(NT):
        fns = []
        for tp in range(tt):
            fns.append(lambda e, tt=tt, tp=tp: e.matmul(
                PS[2][:, tt * 32:(tt + 1) * 32], lhsT=onesb[:], rhs=selb[:, tp, :], start=(tp == 0), stop=False,
                skip_group_check=True))
        fns.append(lambda e, tt=tt: e.matmul(
            PS[2][:, tt * 32:(tt + 1) * 32], lhsT=ustr[:], rhs=selb[:, tt, :], start=(tt == 0), stop=True,
            skip_group_check=True))
        S.group("pe", fns, reads=RB + [Bc5], writes=[Bps[2]])
    fns = [lambda e, tt=tt: e.matmul(PS[3][0:32, 0:1], lhsT=selb[:, tt, :], rhs=onesb[:, 0:1],
                                     start=(tt == 0), stop=(tt == NT - 1)) for tt in range(NT)]
    S.group("pe", fns, reads=RB + [Bc5], writes=[Bps[3]])
    S.op("dve", lambda e: e.tensor_copy(out=cnt32[0:32, 0:1], in_=PS[3][0:32, 0:1]), reads=[Bps[3]], writes=RB)
    S.op("dve", lambda e: e.tensor_scalar(out=cnt32[0:32, 4:20], in0=thr16[0:32, :], scalar1=cnt32[0:32, 0:1], scalar2=None,
                                          op0=ALU.is_lt), reads=RB + [Bc5], writes=RB)
    S.op("dve", lambda e: e.tensor_reduce(out=cnt32[0:32, 1:2], in_=cnt32[0:32, 4:20], axis=AX.X, op=ALU.add), reads=RB, writes=RB)
    S.op("dve", lambda e: e.tensor_scalar(out=ntr[0:32, :], in0=onesb[0:32, :], scalar1=cnt32[0:32, 1:2], scalar2=None,
                                          op0=ALU.mult), reads=RB + [Bc5], writes=RB)
    S.op("pe", lambda e: e.matmul(PS[3][:, 64:96], lhsT=ntr[0:32, :], rhs=ustr[0:32, 0:32], start=True, stop=True),
         reads=RB + [Bc5], writes=[Bps[3]])
    S.op("pe", lambda e: e.matmul(PS[3][:, 96:128], lhsT=ntr[0:32, :], rhs=causal01[0:32, 0:32], start=True, stop=True),
         reads=RB + [Bconst], writes=[Bps[3]])
    S.op("dve", lambda e: e.tensor_copy(out=toffs[:], in_=PS[3][:, 64:128]), reads=[Bps[3]], writes=RB)
    S.op("dve", lambda e: e.scalar_tensor_tensor(
        out=posf[:], in0=bc(toffs[:, 0:32].rearrange("p (o c) -> p o c", o=1), [128, NT, 32]), scalar=128.0,
        in1=PS[2][:].rearrange("p (t c) -> p t c", c=32), op0=ALU.mult, op1=ALU.add), reads=RB + [Bps[2]], writes=RB)
    S.op("dve", lambda e: e.tensor_tensor(out=tmpa[:], in0=sel[:], in1=posf[:], op=ALU.mult), reads=RB, writes=RB)
    S.op("dve", lambda e: e.tensor_reduce(out=smv(8), in_=tmpa[:], axis=AX.X, op=ALU.max), reads=RB, writes=RB)
    S.op("dve", lambda e: e.tensor_scalar(out=tmpa[:], in0=sel[:], scalar1=-1.0e6, scalar2=1.0e6, op0=ALU.mult, op1=ALU.add),
         reads=RB, writes=RB)
    S.op("dve", lambda e: e.tensor_tensor(out=tmpa[:], in0=tmpa[:], in1=posf[:], op=ALU.add), reads=RB, writes=RB)
    S.op("dve", lambda e: e.tensor_reduce(out=smv(7), in_=tmpa[:], axis=AX.X, op=ALU.min), reads=RB, writes=RB)
    for r_pos, r_w in ((7, 9), (8, 10)):
        S.op("dve", lambda e, r_pos=r_pos: e.tensor_tensor(out=tmpa[:], in0=posf[:], in1=sm3(r_pos, 32), op=ALU.is_equal),
             reads=RB, writes=RB)
        S.op("dve", lambda e: e.tensor_tensor(out=tmpa[:], in0=tmpa[:], in1=comb[:], op=ALU.mult), reads=RB, writes=RB)
        S.op("dve", lambda e, r_w=r_w: e.tensor_reduce(out=smv(r_w), in_=tmpa[:], axis=AX.X, op=ALU.add), reads=RB, writes=RB)
    for slot in range(2):
        S.op("dve", lambda e, slot=slot: e.tensor_copy(out=posu[:, :, slot], in_=smv(7 + slot)), reads=RB, writes=[Bpos])
        S.op("dve", lambda e, slot=slot: e.tensor_copy(out=wsl[:, :, slot], in_=smv(9 + slot)), reads=RB, writes=[Bpos])
    S.op("dve", lambda e: e.tensor_tensor(
        out=tmpb[:], in0=bc(toffs[:, 32:64].rearrange("p (o c) -> p o c", o=1), [128, 64, 32]),
        in1=bc(jrow[:].rearrange("p (j o) -> p j o", o=1), [128, 64, 32]), op=ALU.is_le), reads=RB + [Bc5], writes=RB)
    S.op("dve", lambda e: e.tensor_reduce(out=ejf[:], in_=tmpb[:], axis=AX.X, op=ALU.add), reads=RB, writes=RB)
    S.op("dve", lambda e: e.scalar_tensor_tensor(
        out=ejf[:], in0=ejf[:], scalar=128.0, in1=bc(piota[:, 0:1], [128, 64]), op0=ALU.mult, op1=ALU.add),
        reads=RB + [Bc5], writes=RB)
    S.op("dve", lambda e: e.tensor_copy(out=widxu[:], in_=ejf[:]), reads=RB, writes=[Bpos])
    S.barrier()
    if stop_after <= 5:
        S.emit()
        return nc

    NTL = 64
    NB = 5
    NX = 6
    wgt = [M("wgt%d" % b, [128, 2048], BF16, 2048 + b * 4096, (6, 6)) for b in range(NB)]
    wut = [M("wut%d" % b, [128, 2048], BF16, 100864 + b * 4096, (6, 6)) for b in range(NB)]
    wdt = [M("wdt%d" % b, [128, 2048], BF16, 121344 + b * 4096, (6, 6)) for b in range(NB)]
    xst = M("xst", [128, NX, D], BF16, 22528, (6, 6))
    xsT = M("xsT", [128, 2, 8, 128], BF16, 71680, (6, 6))
    sgt = M("sgt", [128, 2, 256], BF16, 75776, (6, 6))
    hTt = M("hTt", [128, 2, 256], BF16, 76800, (6, 6))
    ysb = M("ysb", [128, 2, D], F32, 77824, (6, 6))
    Bwt = [[Buf("wt%d_%d" % (m, b)) for b in range(NB)] for m in range(3)]
    Bxst = [Buf("xst%d" % i) for i in range(NX)]; BxsT = [Buf("xsT0"), Buf("xsT1")]
    Bsgt = [Buf("sgt0"), Buf("sgt1")]; BhTt = [Buf("hTt0"), Buf("hTt1")]; Bysb = [Buf("ysb0"), Buf("ysb1")]
    Bxs_l = [Buf("xs_d%d" % i) for i in range(2 * NT)]; Bys = Buf("ys_d")
    for tt in range(NT):
        for slot in range(2):
            S.dma("pool", lambda e, tt=tt, slot=slot: e.indirect_dma_start(
                out=xs_d, out_offset=bass.IndirectOffsetOnAxis(ap=posu[:, tt, slot:slot + 1], axis=0),
                in_=x1tok[:, tt, :], in_offset=None), reads=[Bpos, Bx1tok[tt]] + Bzero_l, writes=[Bxs_l[tt * 2 + slot]])

    def moe_load_w(j):
        b = j % NB
        for m, (wd_, wt_) in enumerate(((wgb_d, wgt), (wub_d, wut), (wdb_d, wdt))):
            S.dma("pool", lambda e, wd_=wd_, wt_=wt_: e.indirect_dma_start(
                out=wt_[b][:], out_offset=None, in_=wd_,
                in_offset=bass.IndirectOffsetOnAxis(ap=widxu[:, j:j + 1], axis=0), bounds_check=preg(e, NEXP * 128 - 1),
                oob_is_err=False), reads=[Bpos] + Bwbf_l, writes=[Bwt[m][b]])

    def moe_load_x(j):
        bx = j % NX
        S.dma("sp", lambda e: e.dma_start(out=xst[:, bx, :], in_=xs_d[j * 128:(j + 1) * 128, :]), reads=Bxs_l, writes=[Bxst[bx]])

    def moe_T(j):
        b2, bx = j % 2, j % NX
        pv = PS[b2][:].bitcast(BF16)
        fns = [lambda e, c=c: e.transpose(out=pv[:, c * 128:(c + 1) * 128], in_=xst[:, bx, c * 128:(c + 1) * 128],
                                         identity=ident[:]) for c in range(8)]
        S.group("pe", fns, reads=[Bxst[bx], Bconst], writes=[Bps[b2]])
        S.op("act", lambda e: e.activation(out=xsT[:, b2, :, :], in_=pv.rearrange("p (c t) -> p c t", c=8), func=AF.Copy),
             reads=[Bps[b2]], writes=[BxsT[b2]])

    def moe_GU(j):
        b2, b = j % 2, j % NB
        bank = 2 + b2
        fns = []
        for m, wt_ in ((0, wgt), (1, wut)):
            wv = wt_[b][:].rearrange("p (k n) -> p k n", k=8)
            for c in range(2):
                for k in range(8):
                    fns.append(lambda e, m=m, wv=wv, c=c, k=k: e.matmul(
                        PS[bank][:, (m * 2 + c) * 128:(m * 2 + c + 1) * 128], lhsT=wv[:, k, c * 128:(c + 1) * 128],
                        rhs=xsT[:, b2, k, :], start=(k == 0), stop=(k == 7), skip_group_check=True))
        S.group("pe", fns, reads=[BxsT[b2], Bwt[0][b], Bwt[1][b]], writes=[Bps[bank]])
        S.op("act", lambda e: e.activation(out=sgt[:, b2, :], in_=PS[bank][:, 0:256], func=AF.Silu),
             reads=[Bps[bank]], writes=[Bsgt[b2]])
        S.op("dve", lambda e: e.tensor_tensor(out=hTt[:, b2, :], in0=sgt[:, b2, :], in1=PS[bank][:, 256:512], op=ALU.mult),
             reads=[Bsgt[b2], Bps[bank]], writes=[BhTt[b2]])

    def moe_D(j):
        b2, b = j % 2, j % NB
        wv = wdt[b][:].rearrange("p (c n) -> p c n", c=2)
        for half in range(2):
            bank = 4 + b2 * 2 + half
            fns = [lambda e, c=c, half=half, bank=bank: e.matmul(
                PS[bank][:], lhsT=hTt[:, b2, c * 128:(c + 1) * 128], rhs=wv[:, c, half * 512:(half + 1) * 512],
                start=(c == 0), stop=(c == 1)) for c in range(2)]
            S.group("pe", fns, reads=[BhTt[b2], Bwt[2][b]], writes=[Bps[bank]])
            if half == 0:
                S.op("act", lambda e, bank=bank: e.activation(out=ysb[:, b2, 0:512], in_=PS[bank][:], func=AF.Copy),
                     reads=[Bps[bank]], writes=[Bysb[b2]])
            else:
                S.op("dve", lambda e, bank=bank: e.tensor_copy(out=ysb[:, b2, 512:1024], in_=PS[bank][:]),
                     reads=[Bps[bank]], writes=[Bysb[b2]])
        S.dma("sp", lambda e: e.dma_start(out=ys_d[j * 128:(j + 1) * 128, :], in_=ysb[:, b2, :]), reads=[Bysb[b2]], writes=[Bys])

    for j in range(NX):
        moe_load_x(j)
    for j in range(NB):
        moe_load_w(j)
    for s_ in range(NTL + 2):
        if s_ < NTL:
            moe_T(s_)
            if s_ + NX < NTL:
                moe_load_x(s_ + NX)
        if 1 <= s_ <= NTL:
            moe_GU(s_ - 1)
        if s_ >= 2:
            moe_D(s_ - 2)
            if s_ - 2 + NB < NTL:
                moe_load_w(s_ - 2 + NB)
    S.barrier()

    NYG = 12
    yg = M("yg", [128, NYG, D], F32, 34816, (7, 7))
    ln2g = M("ln2g", [128, D], F32, 2048, (7, 7))
    ln2bA = M("ln2bA", [128, D], F32, 6144, (7, 7))
    obuf = M("obuf", [128, 3, D], F32, 10240, (7, 7))
    lnst2 = M("lnst2", [128, 3, 32], F32, 22528, (7, 7))
    Bln2 = Buf("ln2"); Bob = [Buf("ob%d" % i) for i in range(3)]; Blnst2 = [Buf("ls%d" % i) for i in range(3)]
    Byg = [Buf("yg%d" % i) for i in range(NYG)]
    S.dma("sp", lambda e: e.dma_start(out=ln2g[:], in_=ln2g_d.to_broadcast([128, D])), writes=[Bln2])
    S.dma("sp", lambda e: e.dma_start(out=ln2bA[:], in_=ln2b_d.to_broadcast([128, D])), writes=[Bln2])

    def tail_gather(q):
        tt, slot = q // 2, q % 2
        yb_ = q % NYG
        S.dma("pool", lambda e: e.indirect_dma_start(
            out=yg[:, yb_, :], out_offset=None, in_=ys_d,
            in_offset=bass.IndirectOffsetOnAxis(ap=posu[:, tt, slot:slot + 1], axis=0)),
            reads=[Bpos, Bys], writes=[Byg[yb_]])

    for q in range(NYG):
        tail_gather(q)
    for tt in range(NT):
        b3 = tt % 3
        for slot in range(2):
            q = tt * 2 + slot
            yb_ = q % NYG
            S.op("dve", lambda e, tt=tt, slot=slot, yb_=yb_: e.scalar_tensor_tensor(
                out=acc[:, tt, :], in0=yg[:, yb_, :], scalar=wsl[:, tt, slot:slot + 1], in1=acc[:, tt, :],
                op0=ALU.mult, op1=ALU.add), reads=[Byg[yb_], Bpos, Bacc[tt]], writes=[Bacc[tt]])
            if q + NYG < 2 * NT:
                tail_gather(q + NYG)
        ln_scaled(acc[:, tt, :], [Bacc[tt]], lnst2[:, b3, :], Blnst2[b3], ln2g, ln2bA, Bln2, obuf[:, b3, :], [Bob[b3]],
                  alpha=1.0)
        S.dma("sp", lambda e, tt=tt, b3=b3: e.dma_start(out=out_d[tt * 128:(tt + 1) * 128, :], in_=obuf[:, b3, :]),
              reads=[Bob[b3]], writes=[Bout])
    S.barrier()
    S.emit()
    return nc


_NC_CACHE = {}


def _host_inputs(inp, b):
    f = np.float32
    w_in = inp["w_in"][0]
    cols = list(range(0, 1024))
    cols += list(range(1152, 1664))
    cols += list(range(1664, 1728)) * 2
    qb0 = 1736
    for j in range(4):
        cols += list(range(qb0 + j * 64, qb0 + (j + 1) * 64))
        cols += list(range(qb0 + (j + 4) * 64, qb0 + (j + 5) * 64))
    cols += list(range(2248, 2376))
    cols += list(range(1024, 1152))
    cols += list(range(2376, 2504))
    cols += list(range(1728, 1736))
    assert len(cols) == W1COLS
    rel = inp["rel_bias"].astype(f)
    s = np.arange(128)[:, None]
    t = np.arange(128)[None, :]
    bk_prev = t5_bucket_np(t - s + 128)
    bk_own = t5_bucket_np(t - s)
    swab = np.zeros((4, 128, 4, 128), f)
    for typ, bk in enumerate((bk_prev, bk_own)):
        for g in range(2):
            for j in range(4):
                swab[typ * 2 + g, :, j, :] = rel[bk, 8 + 4 * g + j]
    dsab = np.zeros((2, 128, 8, 128), f)
    for typ, bk in enumerate((bk_prev, bk_own)):
        for h in range(8):
            dsab[typ, :, h, :] = rel[bk, h]
    return {
        "xT": np.ascontiguousarray(inp["x"][b].T),
        "x": np.ascontiguousarray(inp["x"][b]),
        "w1": np.ascontiguousarray(w_in[:, cols]),
        "wg": np.ascontiguousarray(w_in[:, 2504:4552]),
        "kvg": np.ascontiguousarray(inp["kv_norm_g"][0].reshape(1, 128)),
        "wuv": np.ascontiguousarray(inp["w_uv"][0].transpose(1, 0, 2).reshape(128, 512)),
        "wa": np.ascontiguousarray(inp["w_branch_a"][0]),
        "wb": np.ascontiguousarray(inp["w_branch_b"][0]),
        "wo": np.ascontiguousarray(inp["w_out"][0]),
        "sinks": np.ascontiguousarray(inp["sinks"][0].reshape(1, 8)),
        "ln1g": np.ascontiguousarray(inp["ln1_g"][0].reshape(1, D)),
        "ln1b": np.ascontiguousarray(inp["ln1_b"][0].reshape(1, D)),
        "ln2g": np.ascontiguousarray(inp["ln2_g"][0].reshape(1, D)),
        "ln2b": np.ascontiguousarray(inp["ln2_b"][0].reshape(1, D)),
        "wr": np.ascontiguousarray(np.concatenate([inp["w_group"][0], inp["w_router"][0]], axis=1)),
        "br": np.ascontiguousarray(np.concatenate([inp["b_group"][0], inp["b_router"][0]]).reshape(1, 36)),
        "wgr": np.ascontiguousarray(inp["w_gate"][0].reshape(NEXP, 8, 128, DE).transpose(0, 2, 1, 3).reshape(NEXP * 128, 2048)),
        "wur": np.ascontiguousarray(inp["w_up"][0].reshape(NEXP, 8, 128, DE).transpose(0, 2, 1, 3).reshape(NEXP * 128, 2048)),
        "wdr": np.ascontiguousarray(inp["w_down"][0].reshape(NEXP, 2, 128, D).transpose(0, 2, 1, 3).reshape(NEXP * 128, 2048)),
        "swab": swab.reshape(4, 128, 512),
        "dsab": dsab.reshape(2, 128, 1024),
        "c31": np.ascontiguousarray(rel[31, 0:8].reshape(1, 8)),
    }


def kernel(**inputs):
    inp = {k: np.asarray(v, dtype=np.float32) for k, v in inputs.items()}
    n = 8
    if "nc" not in _NC_CACHE:
        _NC_CACHE["nc"] = build_nc(False)
    nc = _NC_CACHE["nc"]
    shared = None
    in_maps = []
    for b in range(n):
        m = _host_inputs(inp, b) if shared is None else dict(shared)
        if shared is None:
            shared = m
        else:
            m["xT"] = np.ascontiguousarray(inp["x"][b].T)
            m["x"] = np.ascontiguousarray(inp["x"][b])
        in_maps.append(m)
    res = run_bass_kernel_spmd(nc, in_maps, core_ids=list(range(n)))
    return np.stack([np.asarray(r["out"], dtype=np.float32) for r in res.results], axis=0)
```

```python
import math
import contextlib
import numpy as np
import concourse.bass as bass
import concourse.mybir as mybir
from concourse.bass_utils import run_bass_kernel_spmd

F32 = mybir.dt.float32
BF16 = mybir.dt.bfloat16
AF = mybir.ActivationFunctionType
ALU = mybir.AluOpType
AX = mybir.AxisListType

D = 1024
L = 2048
NT = 16
NEXP = 32
DE = 256
ALPHA = 2.0 ** 0.25
LN_EPS = 1e-5
RMS_EPS = 1e-6
ATT_SCALE = 128.0 ** -0.5
NEG = -30000.0
N_BISECT = 16
W1COLS = 2568
SB_BASE = 16640
SB_END = 229368


class Buf:
    __slots__ = ("name", "writer", "readers")

    def __init__(self, name):
        self.name = name
        self.writer = None
        self.readers = []


class Sched:
    ENGS = ("pe", "act", "dve", "pool", "sp")

    def __init__(self, nc, n_dma_sems=24):
        self.nc = nc
        self.ops = {e: [] for e in self.ENGS}
        self.cnt = {e: 0 for e in self.ENGS}
        self.seen = {e: {} for e in self.ENGS}
        self.n_dma_sems = n_dma_sems
        self.dma_i = {}
        self.dma_val = {}
        self.sems = {}

    def _deps(self, eng, reads, writes):
        need = {}

        def add(tok, skip_same):
            if tok is None:
                return
            e, key, val = tok
            if e == eng and skip_same:
                return
            if need.get(key, 0) < val:
                need[key] = val

        pe = eng == "pe"
        for b in reads:
            add(b.writer, pe)
        for b in writes:
            add(b.writer, pe)
            for r in b.readers:
                add(r, True)
        waits = []
        seen = self.seen[eng]
        for key, val in need.items():
            if seen.get(key, 0) < val:
                seen[key] = val
                waits.append((key, val))
        return waits

    def _commit(self, tok, reads, writes):
        for b in reads:
            b.readers.append(tok)
        for b in writes:
            b.writer = tok
            b.readers = []

    def group(self, eng, fns, reads=(), writes=()):
        waits = self._deps(eng, reads, writes)
        self.cnt[eng] += 1
        tok = (eng, "e_" + eng, self.cnt[eng])
        n = len(fns)
        for i, fn in enumerate(fns):
            self.ops[eng].append((fn, waits if i == 0 else (), ("e_" + eng, 1) if i == n - 1 else None))
        self._commit(tok, reads, writes)
        return tok

    def op(self, eng, fn, reads=(), writes=()):
        return self.group(eng, [fn], reads, writes)

    def dma(self, eng, fn, reads=(), writes=()):
        i = self.dma_i.get(eng, 0) % self.n_dma_sems
        self.dma_i[eng] = self.dma_i.get(eng, 0) + 1
        key = "d_%s_%d" % (eng, i)
        waits = list(self._deps(eng, reads, writes))
        prev = self.dma_val.get(key, 0)
        if prev > 0 and self.seen[eng].get(key, 0) < prev:
            self.seen[eng][key] = prev
            waits.append((key, prev))
        self.dma_val[key] = prev + 16
        tok = ("dma", key, self.dma_val[key])
        self.ops[eng].append((fn, waits, (key, 16)))
        self._commit(tok, reads, writes)
        return tok

    def barrier(self):
        targets = [("e_" + e, self.cnt[e]) for e in self.ENGS if self.cnt[e] > 0]
        targets += [(k, v) for k, v in self.dma_val.items() if v > 0]
        for e in self.ENGS:
            waits = []
            for key, val in targets:
                if key == "e_" + e and e != "pe":
                    pass
                if self.seen[e].get(key, 0) < val:
                    self.seen[e][key] = val
                    waits.append((key, val))
            if waits:
                self.ops[e].append((None, waits, None))

    def emit(self):
        nc = self.nc
        keys = ["e_" + e for e in self.ENGS] + sorted(self.dma_val.keys())
        with contextlib.ExitStack() as st:
            for k in keys:
                self.sems[k] = st.enter_context(nc.semaphore(k))
            block = st.enter_context(nc.Block())
            sems = self.sems

            def run(eng_name):
                def body(eng):
                    for fn, waits, inc in self.ops[eng_name]:
                        for key, val in waits:
                            eng.wait_ge(sems[key], val)
                        if fn is None:
                            continue
                        ins = fn(eng)
                        if inc is not None:
                            ins.then_inc(sems[inc[0]], inc[1])
                return body

            block.tensor(run("pe"))
            block.scalar(run("act"))
            block.vector(run("dve"))
            block.gpsimd(run("pool"))
            block.sync(run("sp"))


class Mem:
    def __init__(self, nc):
        self.nc = nc
        self.allocs = []

    def __call__(self, name, shape, dtype, off, life):
        esz = 4 if dtype == F32 else 2
        n = esz
        for s in shape[1:]:
            n *= s
        a0, a1 = SB_BASE + off, SB_BASE + off + n
        assert a1 <= SB_END, (name, a1)
        for (nm, b0, b1, lf) in self.allocs:
            if a0 < b1 and b0 < a1 and lf[0] <= life[1] and life[0] <= lf[1]:
                raise AssertionError("SBUF overlap %s vs %s" % (name, nm))
        self.allocs.append((name, a0, a1, life))
        return self.nc.alloc_sbuf_tensor_at(name, list(shape), dtype, offset=a0)


def t5_bucket_np(dist):
    n = np.maximum(dist, 0)
    nf = np.maximum(n, 1).astype(np.float32)
    large = 16 + (np.log(nf / np.float32(16)) / np.float32(math.log(128 / 16)) * np.float32(16)).astype(np.int32)
    large = np.minimum(large, 31)
    return np.where(n < 16, n, large).astype(np.int32)


def build_nc(debug=False, stop_after=99):
    nc = bass.Bass("TRN2", target_bir_lowering=False)

    def din(name, shape, dt=F32):
        return nc.dram_tensor(name, list(shape), dt, kind="ExternalInput").ap()

    xT_d = din("xT", [D, L])
    x_d = din("x", [L, D])
    w1_d = din("w1", [D, W1COLS])
    wg_d = din("wg", [D, 2048])
    kvg_d = din("kvg", [1, 128])
    wuv_d = din("wuv", [128, 512])
    wa_d = din("wa", [512, D])
    wb_d = din("wb", [512, D])
    wo_d = din("wo", [D, D])
    sinks_d = din("sinks", [1, 8])
    ln1g_d = din("ln1g", [1, D])
    ln1b_d = din("ln1b", [1, D])
    ln2g_d = din("ln2g", [1, D])
    ln2b_d = din("ln2b", [1, D])
    wr_d = din("wr", [D, 36])
    br_d = din("br", [1, 36])
    wgr_d = din("wgr", [NEXP * 128, 2048])
    wur_d = din("wur", [NEXP * 128, 2048])
    wdr_d = din("wdr", [NEXP * 128, 2048])
    wgb_d = nc.dram_tensor("wg_bf16", [NEXP * 128, 2048], BF16, kind="Internal").ap()
    wub_d = nc.dram_tensor("wu_bf16", [NEXP * 128, 2048], BF16, kind="Internal").ap()
    wdb_d = nc.dram_tensor("wd_bf16", [NEXP * 128, 2048], BF16, kind="Internal").ap()
    gates_d = nc.dram_tensor("gates_scr", [2048, L], BF16, kind="Internal").ap()
    xs_d = nc.dram_tensor("xs_scr", [64 * 128, D], BF16, kind="Internal").ap()
    ys_d = nc.dram_tensor("ys_scr", [64 * 128, D], F32, kind="Internal").ap()
    swab_d = din("swab", [4, 128, 512])
    dsab_d = din("dsab", [2, 128, 1024])
    c31_d = din("c31", [1, 8])
    out_d = nc.dram_tensor("out", [L, D], F32, kind="ExternalOutput").ap()
    dbg = {}
    if debug:
        for nm, shp in [("d_yb", [512, L]), ("d_ya", [512, L]), ("d_mergedT", [D, L]), ("d_acc", [L, D]),
                        ("d_comb", [L, 32]), ("d_ckvT", [128, L]), ("d_qlatT", [128, L])]:
            dbg[nm] = nc.dram_tensor(nm, shp, BF16 if nm in ("d_yb", "d_ya", "d_mergedT", "d_ckvT", "d_qlatT") else F32,
                                     kind="ExternalOutput").ap()

    S = Sched(nc)
    M = Mem(nc)
    _regs = {}

    def preg(e, val):
        if val not in _regs:
            _regs[val] = e.to_reg(val)
        return _regs[val]
    PS = [nc.alloc_psum_tensor("ps%d" % i, [128, 512], F32) for i in range(8)]
    Bps = [Buf("ps%d" % i) for i in range(8)]
    Bout = Buf("out")

    ident = M("ident", [128, 128], BF16, 0, (1, 7))
    gkv_bc = M("gkv_bc", [128, 128], F32, 256, (1, 7))
    esink = M("esink", [128, 8], F32, 768, (1, 7))
    negc31 = M("negc31", [128, 8], F32, 800, (1, 7))
    identf = M("identf", [128, 128], F32, 1024, (1, 7))
    causal01 = M("causal01", [128, 128], BF16, 1536, (1, 7))
    Bconst = Buf("const")

    S.op("pool", lambda e: e.memset(identf[:], 1.0), writes=[Bconst])
    S.op("pool", lambda e: e.affine_select(out=identf[:], in_=identf[:], pattern=[[1, 128]],
                                           compare_op=ALU.is_equal, fill=preg(e, 0.0), base=0, channel_multiplier=-1),
         reads=[Bconst], writes=[Bconst])
    S.op("pool", lambda e: e.tensor_copy(out=ident[:], in_=identf[:]), reads=[Bconst], writes=[Bconst])
    S.op("pool", lambda e: e.memset(causal01[:], 1.0), writes=[Bconst])
    S.op("pool", lambda e: e.affine_select(out=causal01[:], in_=causal01[:], pattern=[[1, 128]],
                                           compare_op=ALU.is_ge, fill=preg(e, 0.0), base=0, channel_multiplier=-1),
         reads=[Bconst], writes=[Bconst])
    S.dma("sp", lambda e: e.dma_start(out=gkv_bc[:], in_=kvg_d.to_broadcast([128, 128])), writes=[Bconst])
    S.dma("sp", lambda e: e.dma_start(out=esink[:], in_=sinks_d.to_broadcast([128, 8])), writes=[Bconst])
    S.dma("sp", lambda e: e.dma_start(out=negc31[:], in_=c31_d.to_broadcast([128, 8])), writes=[Bconst])
    S.op("act", lambda e: e.activation(out=esink[:], in_=esink[:], func=AF.Exp), reads=[Bconst], writes=[Bconst])
    S.op("dve", lambda e: e.tensor_scalar(out=negc31[:], in0=negc31[:], scalar1=-1.0, scalar2=None, op0=ALU.mult),
         reads=[Bconst], writes=[Bconst])

    xT = M("xT", [128, 8, L], BF16, 2048, (1, 1))
    o = 34816
    qlatT = M("qlatT", [128, 8, L], BF16, o, (1, 2.5)); o += 32768
    qidxT = M("qidxT", [128, 4, L], BF16, o, (1, 2.5)); o += 16384
    qbT = M("qbT", [128, 4, L], BF16, o, (1, 2)); o += 16384
    kidxT = M("kidxT", [128, L], BF16, o, (1, 2.5)); o += 4096
    kbT = M("kbT", [128, L], BF16, o, (1, 2)); o += 4096
    ckvT = M("ckvT", [128, L], BF16, o, (1, 2.5)); o += 4096
    kvW = M("kvW", [128, NT, 8, 65], BF16, o, (1, 2.5)); o += 16640
    vaug = M("vaug", [128, NT, 2, 65], BF16, o, (1, 2)); o += 4160
    widx = M("widx", [128, NT, 8], F32, o, (1, 2.5)); o += 512
    assert o == 133952
    R3 = 133952
    w1b = M("w1b", [128, 8, W1COLS], BF16, R3, (1, 1))
    wuvs = M("wuvs", [128, 512], F32, R3 + 41088, (1, 1))
    ckvtok = M("ckvtok", [128, NT, 128], BF16, R3 + 43136, (1, 1))
    wuvb = M("wuvb", [128, 512], BF16, R3 + 47232, (1, 1))
    p1tmp = M("p1tmp", [128, 128], F32, R3 + 48256, (1, 1))
    p1junk = M("p1junk", [128, 128], F32, R3 + 48768, (1, 1))

    BxT = [Buf("xT%d" % k) for k in range(8)]
    Bw1 = [Buf("w1_%d" % c) for c in range(6)]
    w1v = w1_d.rearrange("(k p) n -> p k n", p=128)
    xTv = xT_d.rearrange("(k p) n -> p k n", p=128)
    for k in range(8):
        S.dma("pool", lambda e, k=k: e.dma_start(out=xT[:, k, :], in_=xTv[:, k, :]), writes=[BxT[k]])
    w1blocks = [(0, 512), (512, 1024), (1024, 1536), (1536, 2048), (2048, 2304), (2304, W1COLS)]
    for c, (c0, c1) in enumerate(w1blocks):
        S.dma("pool", lambda e, c0=c0, c1=c1: e.dma_start(out=w1b[:, :, c0:c1], in_=w1v[:, :, c0:c1]),
              writes=[Bw1[c]])
    wgs = [M("wgs0", [128, 8, 512], BF16, 184320, (1, 1)), M("wgs1", [128, 8, 512], BF16, 192512, (1, 1))]
    gst = M("gst", [128, 2, 512], BF16, 200704, (1, 1))
    Bwgs = [Buf("wgs0"), Buf("wgs1")]; Bgst = [Buf("gst0"), Buf("gst1")]; Bgates_l = [Buf("gates_d%d" % i) for i in range(64)]
    wgv = wg_d.rearrange("(k p) n -> p k n", p=128)

    def load_wgs(c):
        wb_ = c % 2
        S.dma("pool", lambda e: e.dma_start(out=wgs[wb_][:], in_=wgv[:, :, c * 512:(c + 1) * 512]), writes=[Bwgs[wb_]])

    load_wgs(0)
    load_wgs(1)
    Bwuv = Buf("wuv")
    S.dma("sp", lambda e: e.dma_start(out=wuvs[:], in_=wuv_d), writes=[Bwuv])
    S.op("dve", lambda e: e.tensor_copy(out=wuvb[:], in_=wuvs[:]), reads=[Bwuv], writes=[Bwuv])

    def w1buf(col):
        for c, (c0, c1) in enumerate(w1blocks):
            if c0 <= col < c1:
                return Bw1[c]

    Bqlat = [Buf("qlat%d" % h) for h in range(8)]
    Bqidx = Buf("qidx"); Bqb = Buf("qb"); Bkidx = Buf("kidx"); Bkb = Buf("kb")
    Bckv = Buf("ckvT"); BkvW = Buf("kvW"); Bvaug = Buf("vaug"); Bwidx = Buf("widx"); Bcktok = Buf("ckvtok")

    fm_tiles = []
    for h in range(8):
        fm_tiles.append((lambda tb, h=h: qlatT[:, h, tb * 512:(tb + 1) * 512], ATT_SCALE, Bqlat[h]))
    for j in range(4):
        fm_tiles.append((lambda tb, j=j: qidxT[:, j, tb * 512:(tb + 1) * 512], 1.0, Bqidx))
    fm_tiles.append((lambda tb: kidxT[:, tb * 512:(tb + 1) * 512], 1.0, Bkidx))
    for j in range(4):
        fm_tiles.append((lambda tb, j=j: qbT[:, j, tb * 512:(tb + 1) * 512], 0.125, Bqb))
    fm_tiles.append((lambda tb: kbT[:, tb * 512:(tb + 1) * 512], 1.0, Bkb))
    ev = 0
    for ti, (dst, scale, bdst) in enumerate(fm_tiles):
        for tb in range(4):
            bank = ev % 4
            fns = []
            for k in range(8):
                fns.append(lambda e, k=k, ti=ti, tb=tb, bank=bank: e.matmul(
                    PS[bank][:], lhsT=w1b[:, k, ti * 128:(ti + 1) * 128], rhs=xT[:, k, tb * 512:(tb + 1) * 512],
                    start=(k == 0), stop=(k == 7)))
            S.group("pe", fns, reads=BxT + [w1buf(ti * 128)], writes=[Bps[bank]])
            if ev % 2 == 0:
                S.op("act", lambda e, dst=dst, tb=tb, bank=bank, scale=scale: e.activation(
                    out=dst(tb), in_=PS[bank][:], func=AF.Copy, scale=scale), reads=[Bps[bank]], writes=[bdst])
            else:
                S.op("dve", lambda e, dst=dst, tb=tb, bank=bank, scale=scale: e.tensor_scalar(
                    out=dst(tb), in0=PS[bank][:], scalar1=scale, scalar2=None, op0=ALU.mult),
                    reads=[Bps[bank]], writes=[bdst])
            ev += 1


    def gen_gates():
        gi_ = 0
        for c in range(4):
            wb_ = c % 2
            if c >= 1 and c + 1 < 4:
                load_wgs(c + 1)
            for j in range(4):
                for tb in range(4):
                    bank = gi_ % 4
                    sb_ = gi_ % 2
                    gi_ += 1
                    fns = [lambda e, k=k, j=j, tb=tb, bank=bank, wb_=wb_: e.matmul(
                        PS[bank][:], lhsT=wgs[wb_][:, k, j * 128:(j + 1) * 128], rhs=xT[:, k, tb * 512:(tb + 1) * 512],
                        start=(k == 0), stop=(k == 7)) for k in range(8)]
                    S.group("pe", fns, reads=BxT + [Bwgs[wb_]], writes=[Bps[bank]])
                    S.op("act", lambda e, bank=bank, sb_=sb_: e.activation(out=gst[:, sb_, :], in_=PS[bank][:], func=AF.Sigmoid),
                         reads=[Bps[bank]], writes=[Bgst[sb_]])
                    r0 = c * 512 + j * 128
                    S.dma("sp", lambda e, r0=r0, tb=tb, sb_=sb_: e.dma_start(
                        out=gates_d[r0:r0 + 128, tb * 512:(tb + 1) * 512], in_=gst[:, sb_, :]),
                        reads=[Bgst[sb_]], writes=[Bgates_l[(r0 // 128) * 4 + tb]])
                    yield

    S.op("pool", lambda e: e.memset(vaug[:], 1.0), writes=[Bvaug])
    S.op("pool", lambda e: e.memset(kvW[:], 1.0), writes=[BkvW])
    Bp1tmp = Buf("p1tmp"); Bp1junk = Buf("p1junk")
    S.op("pool", lambda e: e.memset(p1tmp[:, 127:128], -0.5), writes=[Bp1tmp])
    gg_ = gen_gates()
    for tt in range(NT):
        for _ in range(4):
            next(gg_, None)
        bank = 4 + (tt % 2)
        fns = []
        for k in range(8):
            fns.append(lambda e, k=k, tt=tt, bank=bank: e.matmul(
                PS[bank][:, 0:264], lhsT=xT[:, k, tt * 128:(tt + 1) * 128], rhs=w1b[:, k, 2304:2568],
                start=(k == 0), stop=(k == 7)))
        S.group("pe", fns, reads=BxT + [Bw1[5]], writes=[Bps[bank]])
        S.op("act", lambda e, tt=tt, bank=bank: e.activation(
            out=p1junk[:, 0:128], in_=PS[bank][:, 0:128], func=AF.Square, accum_out=p1tmp[:, tt:tt + 1]),
            reads=[Bps[bank]], writes=[Bp1junk, Bp1tmp])
        S.op("act", lambda e, tt=tt, bank=bank: e.activation(
            out=vaug[:, tt, :, 0:64], in_=PS[bank][:, 128:256].rearrange("p (g d) -> p g d", g=2), func=AF.Copy),
            reads=[Bps[bank]], writes=[Bvaug])
        S.op("act", lambda e, tt=tt, bank=bank: e.activation(
            out=widx[:, tt, :], in_=PS[bank][:, 256:264], func=AF.Copy), reads=[Bps[bank]], writes=[Bwidx])
        S.op("dve", lambda e, tt=tt: e.tensor_scalar(
            out=p1tmp[:, 16 + tt:17 + tt], in0=p1tmp[:, tt:tt + 1], scalar1=1.0 / 128.0, scalar2=RMS_EPS,
            op0=ALU.mult, op1=ALU.add), reads=[Bp1tmp], writes=[Bp1tmp])
        S.op("pool", lambda e, tt=tt: e.tensor_tensor(
            out=p1tmp[:, 32 + tt:33 + tt], in0=p1tmp[:, 16 + tt:17 + tt], in1=p1tmp[:, 127:128], op=ALU.pow),
            reads=[Bp1tmp], writes=[Bp1tmp])
        S.op("dve", lambda e, tt=tt, bank=bank: e.scalar_tensor_tensor(
            out=ckvtok[:, tt, :], in0=PS[bank][:, 0:128], scalar=p1tmp[:, 32 + tt:33 + tt], in1=gkv_bc[:],
            op0=ALU.mult, op1=ALU.mult), reads=[Bps[bank], Bp1tmp, Bconst], writes=[Bcktok])
        tb_ = 6 + (tt % 2)
        S.op("pe", lambda e, tt=tt, tb_=tb_: e.transpose(
            out=PS[tb_][:].bitcast(BF16)[:, 0:128], in_=ckvtok[:, tt, :], identity=ident[:]),
            reads=[Bcktok, Bconst], writes=[Bps[tb_]])
        S.op("dve", lambda e, tt=tt, tb_=tb_: e.tensor_copy(
            out=ckvT[:, tt * 128:(tt + 1) * 128], in_=PS[tb_][:].bitcast(BF16)[:, 0:128]),
            reads=[Bps[tb_]], writes=[Bckv])
        S.op("pe", lambda e, tt=tt, bank=bank: e.matmul(
            PS[bank][:], lhsT=ckvT[:, tt * 128:(tt + 1) * 128], rhs=wuvb[:], start=True, stop=True),
            reads=[Bckv, Bwuv], writes=[Bps[bank]])
        S.op("act", lambda e, tt=tt, bank=bank: e.activation(
            out=kvW[:, tt, :, 0:64], in_=PS[bank][:].rearrange("p (h d) -> p h d", h=8), func=AF.Copy),
            reads=[Bps[bank]], writes=[BkvW])

    for _ in gg_:
        pass
    if debug:
        S.dma("sp", lambda e: e.dma_start(out=dbg["d_ckvT"], in_=ckvT[:]), reads=[Bckv], writes=[Bout])
        S.dma("sp", lambda e: e.dma_start(out=dbg["d_qlatT"], in_=qlatT[:, 0, :]), reads=Bqlat, writes=[Bout])
    S.barrier()
    if stop_after <= 1:
        S.emit()
        return nc

    yaT = M("yaT", [128, 4, L], BF16, 179904, (2, 3))
    ybT = M("ybT", [128, 4, L], BF16, 179904 + 16384, (2, 3))
    Eswa = M("Eswa", [128, 4, 512], BF16, R3, (2, 2))
    ytokA = M("ytokA", [128, 2, 512], BF16, R3 + 4096, (2, 2))
    st2a = M("st2a", [128, 128], F32, R3 + 6144, (2, 2))
    Edsa = M("Edsa", [128, 2, 1024], BF16, R3 + 20480, (2, 2.5))
    eTb = M("eTb", [128, 4, 512], BF16, R3 + 32768, (2, 2.5))
    pTb = M("pTb", [128, 4, 512], BF16, R3 + 36864, (2, 2.5))
    scr = M("scr", [128, 1024], F32, R3 + 40960, (2, 2))
    BEswa = Buf("Eswa"); BEdsa = Buf("Edsa"); Bscr = Buf("scr")
    BeT = [Buf("eT%d" % i) for i in range(4)]
    BpT = [Buf("pT%d" % i) for i in range(4)]
    BytokA = [Buf("ytokA0"), Buf("ytokA1")]
    Bst2a = Buf("st2a")
    ByaT = Buf("yaT"); BybT = Buf("ybT")

    for idx in range(4):
        typ = idx // 2
        S.dma("sp", lambda e, idx=idx: e.dma_start(out=scr[:, 0:512], in_=swab_d[idx]), writes=[Bscr])
        S.op("act", lambda e, idx=idx: e.activation(out=Eswa[:, idx, :], in_=scr[:, 0:512], func=AF.Exp),
             reads=[Bscr], writes=[BEswa])
        for j in range(4):
            if typ == 1:
                S.op("pool", lambda e, idx=idx, j=j: e.tensor_tensor(
                    out=Eswa[:, idx, j * 128:(j + 1) * 128], in0=Eswa[:, idx, j * 128:(j + 1) * 128],
                    in1=causal01[:], op=ALU.mult), reads=[BEswa, Bconst], writes=[BEswa])
            else:
                S.op("pool", lambda e, idx=idx, j=j: e.affine_select(
                    out=Eswa[:, idx, j * 128:(j + 1) * 128], in_=Eswa[:, idx, j * 128:(j + 1) * 128],
                    pattern=[[-1, 128]], compare_op=ALU.is_ge, fill=preg(e, 0.0), base=-1, channel_multiplier=1),
                    reads=[BEswa], writes=[BEswa])
    for typ in range(2):
        S.dma("sp", lambda e, typ=typ: e.dma_start(out=scr[:], in_=dsab_d[typ]), writes=[Bscr])
        for h in range(8):
            S.op("act", lambda e, typ=typ, h=h: e.activation(
                out=Edsa[:, typ, h * 128:(h + 1) * 128], in_=scr[:, h * 128:(h + 1) * 128], func=AF.Exp,
                bias=negc31[:, h:h + 1], scale=1.0), reads=[Bscr, Bconst], writes=[BEdsa])
            if typ == 1:
                S.op("pool", lambda e, h=h: e.tensor_tensor(
                    out=Edsa[:, 1, h * 128:(h + 1) * 128], in0=Edsa[:, 1, h * 128:(h + 1) * 128],
                    in1=causal01[:], op=ALU.mult), reads=[BEdsa, Bconst], writes=[BEdsa])

    def ytok_to_T(nblk, ysrc, ybufB, dstT, bdst):
        pv = PS[7][:].bitcast(BF16)
        fns = [lambda e, c=c: e.transpose(out=pv[:, c * 128:(c + 1) * 128],
                                         in_=ysrc[:, c * 128:(c + 1) * 128], identity=ident[:])
               for c in range(4)]
        S.group("pe", fns, reads=[ybufB, Bconst], writes=[Bps[7]])
        S.op("act", lambda e: e.activation(
            out=dstT[:, :, nblk * 128:(nblk + 1) * 128], in_=pv[:, 0:512].rearrange("p (c t) -> p c t", c=4),
            func=AF.Copy), reads=[Bps[7]], writes=[bdst])

    items = []
    for n in range(NT):
        for g in range(2):
            chunks = ([(n - 1, 0)] if n > 0 else []) + [(n, 1)]
            for ci, (kb, typ) in enumerate(chunks):
                items.append((n, g, kb, typ, ci == 0, ci == len(chunks) - 1))

    def swa_stage1(idx):
        n, g, kb, typ, first, last = items[idx]
        r = idx % 4
        lb = idx % 3
        S.op("pe", lambda e: e.matmul(
            PS[lb][:], lhsT=kbT[g * 64:(g + 1) * 64, kb * 128:(kb + 1) * 128],
            rhs=qbT[g * 64:(g + 1) * 64, :, n * 128:(n + 1) * 128], start=True, stop=True),
            reads=[Bkb, Bqb], writes=[Bps[lb]])
        S.op("act", lambda e: e.activation(out=eTb[:, r, :], in_=PS[lb][:], func=AF.Exp),
             reads=[Bps[lb]], writes=[BeT[r]])
        S.op("dve", lambda e: e.tensor_tensor(
            out=pTb[:, r, :], in0=eTb[:, r, :], in1=Eswa[:, typ * 2 + g, :], op=ALU.mult),
            reads=[BeT[r], BEswa], writes=[BpT[r]])

    def swa_stage2(idx):
        n, g, kb, typ, first, last = items[idx]
        r = idx % 4
        ybuf = n % 2
        obank = 3 + g
        fns = [lambda e, j=j: e.matmul(
            PS[obank][:, j * 65:(j + 1) * 65], lhsT=pTb[:, r, j * 128:(j + 1) * 128],
            rhs=vaug[:, kb, g, :], start=(first and j == 0), stop=(last and j == 3),
            skip_group_check=True) for j in range(4)]
        S.group("pe", fns, reads=[BpT[r], Bvaug], writes=[Bps[obank]])
        if not last:
            return
        ov = PS[obank][:, 0:260].rearrange("p (j c) -> p j c", j=4)
        c0 = ybuf * 16 + g * 4
        S.op("dve", lambda e: e.tensor_tensor(
            out=st2a[:, c0:c0 + 4].rearrange("p (j o) -> p j o", o=1), in0=ov[:, :, 64:65],
            in1=esink[:, g * 4:(g + 1) * 4].rearrange("p (j o) -> p j o", o=1), op=ALU.add),
            reads=[Bps[obank], Bconst], writes=[Bst2a])
        S.op("dve", lambda e: e.reciprocal(out=st2a[:, 32 + c0:32 + c0 + 4], in_=st2a[:, c0:c0 + 4]),
             reads=[Bst2a], writes=[Bst2a])
        S.op("dve", lambda e: e.tensor_tensor(
            out=ytokA[:, ybuf, g * 256:(g + 1) * 256].rearrange("p (j d) -> p j d", j=4), in0=ov[:, :, 0:64],
            in1=st2a[:, 32 + c0:32 + c0 + 4].rearrange("p (j o) -> p j o", o=1).to_broadcast([128, 4, 64]),
            op=ALU.mult), reads=[Bps[obank], Bst2a], writes=[BytokA[ybuf]])
        if g == 1:
            ytok_to_T(n, ytokA[:, ybuf, :], BytokA[ybuf], ybT, BybT)

    LAG = 2
    for idx in range(len(items) + LAG):
        if idx < len(items):
            swa_stage1(idx)
        if idx >= LAG:
            swa_stage2(idx - LAG)
    if debug:
        S.dma("sp", lambda e: e.dma_start(out=dbg["d_yb"].rearrange("(c p) t -> p c t", p=128), in_=ybT[:]),
              reads=[BybT], writes=[Bout])
    S.barrier()

    QB = 83968
    scoresA = M("scoresA", [128, L], F32, R3, (2.5, 2.5))
    scoresB = M("scoresB", [128, L], F32, QB, (2.5, 2.5))
    scoresC = M("scoresC", [128, L], F32, 2048, (2.5, 2.5))
    scoresD = M("scoresD", [128, L], F32, 2048 + 8192, (2.5, 2.5))
    maskbA = M("maskbA", [128, L], BF16, R3 + 8192, (2.5, 2.5))
    maskbB = M("maskbB", [128, L], BF16, 2048 + 16384, (2.5, 2.5))
    osb = M("osb", [128, 2, 520], F32, 129280, (2.5, 2.5))
    maskT_lo = M("maskT_lo", [128, 2, NT, 128], BF16, R3 + 12288, (2.5, 2.5))
    maskT_hi = M("maskT_hi", [128, 2, NT, 128], BF16, 2048 + 20480, (2.5, 2.5))
    rbuf = M("rbuf", [128, 4, 512], BF16, R3 + 40960, (2.5, 2.5))
    dg = M("dg", [128, 2, 8, 128], BF16, QB + 8192, (2.5, 2.5))
    ytokB = M("ytokB", [128, 2, 512], BF16, QB + 12288, (2.5, 2.5))
    st2 = M("st2", [128, 4, 64], F32, QB + 14336, (2.5, 2.5))
    steps = M("steps", [128, 32], F32, QB + 15360, (2.5, 2.5))
    sd0 = M("sd0", [128, 4, 32], F32, QB + 15488, (2.5, 2.5))
    zt = M("zt", [128, D], BF16, R3 + 24576, (2.5, 2.5))
    Bzero = Buf("zero")
    Bzero_l = [Buf("zero%d" % i) for i in range(8)]
    S.op("pool", lambda e: e.memset(zt[:], 0.0), writes=[Bzero])
    xs_v = xs_d.rearrange("(j p) n -> p j n", p=128)
    for j0 in range(0, 64, 8):
        S.dma("sp", lambda e, j0=j0: e.dma_start(
            out=xs_v[:, j0:j0 + 8, :], in_=zt[:].rearrange("p (o n) -> p o n", o=1).to_broadcast([128, 8, D])),
            reads=[Bzero], writes=[Bzero_l[j0 // 8]])
    Bwbf_l = []
    bg_jobs = []
    for r0 in range(0, NEXP * 128, 512):
        for src_, dst_ in ((wgr_d, wgb_d), (wur_d, wub_d), (wdr_d, wdb_d)):
            bg_jobs.append((src_, dst_, r0))

    def bg_convert(n=1):
        for _ in range(n):
            if not bg_jobs:
                return
            src_, dst_, r0 = bg_jobs.pop(0)
            bj = Buf("wbf%d" % len(Bwbf_l))
            Bwbf_l.append(bj)
            S.dma("pool", lambda e, src_=src_, dst_=dst_, r0=r0: e.dma_start(
                out=dst_[r0:r0 + 512, :], in_=src_[r0:r0 + 512, :]), writes=[bj])
    kblk0 = M("kblk0", [128, L], BF16, 104448, (2.5, 2.5))
    kblk1 = M("kblk1", [128, L], BF16, 2048 + 28672, (2.5, 2.5))
    kblk = [kblk0, kblk1]
    Bkblk = Buf("kblk")
    S.op("pool", lambda e: e.memset(kblk0[:], 0.0), writes=[Bkblk])
    S.op("pool", lambda e: e.memset(kblk1[:], 0.0), writes=[Bkblk])
    S.op("dve", lambda e: e.tensor_copy(out=kblk0[0:64, :], in_=kidxT[0:64, :]), reads=[Bkidx, Bkblk], writes=[Bkblk])
    S.op("dve", lambda e: e.tensor_copy(out=kblk1[64:128, :], in_=kidxT[64:128, :]), reads=[Bkidx, Bkblk], writes=[Bkblk])
    scoresX = [scoresA, scoresB, scoresC, scoresD]
    maskbX = [maskbA, maskbB]
    Bscore = [Buf("scores%d" % i) for i in range(4)]
    BmaskX = [Buf("mask0"), Buf("mask1")]; BmaskT = [Buf("maskT%d" % i) for i in range(4)]
    Brb = [Buf("rb%d" % i) for i in range(4)]
    Bdg = [Buf("dg0"), Buf("dg1")]
    BytokB = [Buf("ytokB0"), Buf("ytokB1")]
    Bbis = [Buf("bis%d" % i) for i in range(4)]
    Bst = [Buf("st%d" % i) for i in range(4)]
    Bosb = [Buf("osb0"), Buf("osb1")]
    Bsteps = Buf("steps")
    for k in range(N_BISECT):
        S.op("pool", lambda e, k=k: e.memset(steps[:, k:k + 1], 2.0 ** -(k + 1)), writes=[Bsteps])
    rotA = {"s1": 0, "rb": 0}

    def genS(i):
        Si = (i + 1) * 128
        sb = i % 2
        q4 = i % 4
        sc_t = scoresX[q4]
        for h in range(8):
            S.op("act", lambda e, h=h: e.activation(
                out=dg[:, sb, h, :], in_=ident[:], func=AF.Copy, scale=widx[:, i, h:h + 1]),
                reads=[Bconst, Bwidx], writes=[Bdg[sb]])
        yield
        nsc = (Si + 511) // 512
        stepsS = [(sc, h) for sc in range(nsc) for h in range(8)]
        slots = {}

        def S1(n):
            sc, h = stepsS[n]
            c0, c1 = sc * 512, min(Si, sc * 512 + 512)
            w = c1 - c0
            rr = rotA["rb"] % 4
            ab = 2 + (rotA["rb"] % 2)
            rotA["rb"] += 1
            slots[n] = rr
            hp = (h % 2) * 64
            S.op("pe", lambda e: e.matmul(
                PS[ab][:, 0:w], lhsT=qidxT[:, h // 2, i * 128:(i + 1) * 128],
                rhs=kblk[h % 2][:, c0:c1], start=True, stop=True),
                reads=[Bqidx, Bkblk], writes=[Bps[ab]])
            S.op("act", lambda e: e.activation(
                out=rbuf[:, rr, 0:w], in_=PS[ab][:, 0:w], func=AF.Relu), reads=[Bps[ab]], writes=[Brb[rr]])

        def S2(n):
            sc, h = stepsS[n]
            c0, c1 = sc * 512, min(Si, sc * 512 + 512)
            w = c1 - c0
            rr = slots[n]
            S.op("pe", lambda e: e.matmul(
                PS[6][:, 0:w], lhsT=dg[:, sb, h, :], rhs=rbuf[:, rr, 0:w], start=(h == 0), stop=(h == 7)),
                reads=[Bdg[sb], Brb[rr]], writes=[Bps[6]])
            if h == 7:
                S.op("act", lambda e: e.activation(out=sc_t[:, c0:c1], in_=PS[6][:, 0:w], func=AF.Copy),
                     reads=[Bps[6]], writes=[Bscore[q4]])

        S1(0)
        for n in range(len(stepsS)):
            if n + 1 < len(stepsS):
                S1(n + 1)
            S2(n)
            yield

    def genB(i):
        Si = (i + 1) * 128
        sb = i % 2
        q4 = i % 4
        sc_t = scoresX[q4]
        maskb = maskbX[sb]
        maskT = maskT_lo if q4 < 2 else maskT_hi
        Bmask = BmaskX[sb]
        stv = st2[:, q4, :]
        BB = [Bbis[q4]]
        S.op("dve", lambda e: e.tensor_reduce(out=stv[:, 0:1], in_=sc_t[:, 0:Si], axis=AX.X, op=ALU.max),
             reads=[Bscore[q4]], writes=BB)
        yield
        S.op("dve", lambda e: e.tensor_reduce(out=stv[:, 1:2], in_=sc_t[:, 0:Si], axis=AX.X, op=ALU.min),
             reads=[Bscore[q4]] + BB, writes=BB)
        yield
        S.op("dve", lambda e: e.scalar_tensor_tensor(
            out=stv[:, 2:3], in0=stv[:, 0:1], scalar=1.0, in1=stv[:, 1:2], op0=ALU.add, op1=ALU.subtract),
            reads=BB, writes=BB)
        S.op("dve", lambda e: e.tensor_scalar(
            out=sd0[:, q4, 0:N_BISECT], in0=steps[:, 0:N_BISECT], scalar1=stv[:, 2:3], scalar2=None, op0=ALU.mult),
            reads=BB + [Bsteps], writes=BB)
        S.op("dve", lambda e: e.tensor_tensor(out=stv[:, 3:4], in0=sd0[:, q4, 0:1], in1=stv[:, 1:2], op=ALU.add),
             reads=BB, writes=BB)
        S.op("pool", lambda e: e.affine_select(
            out=sc_t[:, i * 128:(i + 1) * 128], in_=sc_t[:, i * 128:(i + 1) * 128], pattern=[[-1, 128]],
            compare_op=ALU.is_ge, fill=preg(e, -1.0e30), base=0, channel_multiplier=1),
            reads=[Bscore[q4]] + BB, writes=[Bscore[q4]])
        yield
        for it in range(N_BISECT):
            S.op("dve", lambda e: e.tensor_scalar(
                out=maskb[:, 0:Si], in0=sc_t[:, 0:Si], scalar1=stv[:, 3:4], scalar2=None,
                op0=ALU.is_ge, op1=ALU.add, accum_out=stv[:, 4:5]),
                reads=[Bscore[q4]] + BB, writes=[Bmask] + BB)
            yield
            S.op("dve", lambda e: e.tensor_scalar(
                out=stv[:, 5:6], in0=stv[:, 4:5], scalar1=255.5, scalar2=0.5, op0=ALU.is_ge, op1=ALU.subtract),
                reads=BB, writes=BB)
            S.op("dve", lambda e, it=it: e.scalar_tensor_tensor(
                out=stv[:, 3:4], in0=stv[:, 5:6], scalar=sd0[:, q4, it:it + 1], in1=stv[:, 3:4],
                op0=ALU.mult, op1=ALU.add), reads=BB, writes=BB)
            yield
        S.op("dve", lambda e: e.scalar_tensor_tensor(
            out=stv[:, 6:7], in0=sd0[:, q4, N_BISECT - 1:N_BISECT], scalar=-0.5, in1=stv[:, 3:4],
            op0=ALU.mult, op1=ALU.add), reads=BB, writes=BB)
        S.op("dve", lambda e: e.tensor_scalar(
            out=maskb[:, 0:Si], in0=sc_t[:, 0:Si], scalar1=stv[:, 6:7], scalar2=None, op0=ALU.is_ge),
            reads=[Bscore[q4]] + BB, writes=[Bmask])
        yield
        pv = PS[7][:].bitcast(BF16)
        for q0 in range(0, i + 1, 4):
            q1 = min(i + 1, q0 + 4)
            fns = [lambda e, kb=kb, q0=q0: e.transpose(
                out=pv[:, (kb - q0) * 128:(kb - q0 + 1) * 128], in_=maskb[:, kb * 128:(kb + 1) * 128],
                identity=ident[:]) for kb in range(q0, q1)]
            S.group("pe", fns, reads=[Bmask, Bconst], writes=[Bps[7]])
            S.op("act", lambda e, q0=q0, q1=q1: e.activation(
                out=maskT[:, sb, q0:q1, :], in_=pv[:, 0:(q1 - q0) * 128].rearrange("p (c t) -> p c t", t=128),
                func=AF.Identity, scale=100.0, bias=-100.0), reads=[Bps[7]], writes=[BmaskT[q4]])
            yield

    def genA(i):
        sb = i % 2
        maskT = maskT_lo if (i % 4) < 2 else maskT_hi
        pairs = [(kb, hg) for kb in range(i + 1) for hg in range(2)]
        info = {}

        def stage1(pi):
            kb, hg = pairs[pi]
            near = kb >= i - 1
            typ = 1 if kb == i else 0
            r = rotA["s1"] % 4
            lb = rotA["s1"] % 2
            rotA["s1"] += 1
            info[pi] = r
            masked = i >= 2
            fns = [lambda e: e.matmul(
                PS[lb][:], lhsT=ckvT[:, kb * 128:(kb + 1) * 128],
                rhs=qlatT[:, hg * 4:(hg + 1) * 4, i * 128:(i + 1) * 128], start=True, stop=(not masked),
                skip_group_check=True)]
            rd = [Bckv] + Bqlat[hg * 4:(hg + 1) * 4]
            if masked:
                for j in range(4):
                    fns.append(lambda e, j=j: e.matmul(
                        PS[lb][:, j * 128:(j + 1) * 128], lhsT=ident[:], rhs=maskT[:, sb, kb, :], start=False,
                        stop=(j == 3), skip_group_check=True))
                rd = rd + [BmaskT[i % 4], Bconst]
            S.group("pe", fns, reads=rd, writes=[Bps[lb]])
            if near:
                S.op("act", lambda e: e.activation(out=eTb[:, r, :], in_=PS[lb][:], func=AF.Exp),
                     reads=[Bps[lb]], writes=[BeT[r]])
                S.op("pool", lambda e: e.tensor_tensor(out=pTb[:, r, :], in0=eTb[:, r, :],
                                                       in1=Edsa[:, typ, hg * 512:(hg + 1) * 512], op=ALU.mult),
                     reads=[BeT[r], BEdsa], writes=[BpT[r]])
            else:
                S.op("act", lambda e: e.activation(out=pTb[:, r, :], in_=PS[lb][:], func=AF.Exp),
                     reads=[Bps[lb]], writes=[BpT[r]])

        def stage2(pi):
            kb, hg = pairs[pi]
            r = info[pi]
            obank = 4 + hg
            fns = [lambda e, j=j: e.matmul(
                PS[obank][:, j * 65:(j + 1) * 65], lhsT=pTb[:, r, j * 128:(j + 1) * 128],
                rhs=kvW[:, kb, hg * 4 + j, :], start=(kb == 0 and j == 0), stop=(kb == i and j == 3),
                skip_group_check=True) for j in range(4)]
            S.group("pe", fns, reads=[BpT[r], BkvW], writes=[Bps[obank]])

        LAGA = 1
        for pi in range(len(pairs) + LAGA):
            if pi < len(pairs):
                stage1(pi)
            if pi >= LAGA:
                stage2(pi - LAGA)
            yield
        for hg in range(2):
            obank = 4 + hg
            S.op("act", lambda e, hg=hg, obank=obank: e.activation(
                out=osb[:, sb, hg * 260:(hg + 1) * 260], in_=PS[obank][:, 0:260], func=AF.Copy),
                reads=[Bps[obank]], writes=[Bosb[sb]])
        yield

        def finish():
            q4 = i % 4
            for hg in range(2):
                ov = osb[:, sb, hg * 260:(hg + 1) * 260].rearrange("p (j c) -> p j c", j=4)
                c0 = 32 + hg * 4
                S.op("dve", lambda e, ov=ov, c0=c0: e.reciprocal(
                    out=st2[:, q4, c0:c0 + 4].rearrange("p (j o) -> p j o", o=1), in_=ov[:, :, 64:65]),
                    reads=[Bosb[sb]], writes=[Bst[q4]])
                S.op("dve", lambda e, ov=ov, c0=c0, hg=hg: e.tensor_tensor(
                    out=ytokB[:, sb, hg * 256:(hg + 1) * 256].rearrange("p (j d) -> p j d", j=4), in0=ov[:, :, 0:64],
                    in1=st2[:, q4, c0:c0 + 4].rearrange("p (j o) -> p j o", o=1).to_broadcast([128, 4, 64]),
                    op=ALU.mult), reads=[Bosb[sb], Bst[q4]], writes=[BytokB[sb]])
            ytok_to_T(i, ytokB[:, sb, :], BytokB[sb], yaT, ByaT)
        post_round.append(finish)

    tick = {"n": 0}

    def interleave(gens):
        gens = [g for g in gens if g is not None]
        while gens:
            tick["n"] += 1
            if tick["n"] % 14 == 0:
                bg_convert(1)
            for g in list(gens):
                try:
                    next(g)
                except StopIteration:
                    gens.remove(g)

    import itertools
    Bjunk = Buf("junk")

    def warm_pe(n):
        fns = [lambda e: e.matmul(PS[7][:, 256:512], lhsT=ident[:], rhs=Edsa[:, 0, 0:256], start=True, stop=True,
                                  skip_group_check=True) for _ in range(n)]
        S.group("pe", fns, reads=[BEdsa, Bconst], writes=[Bjunk])

    post_round = []
    NP = NT // 2
    for rnd in range(0, NP + 2):
        gs = []
        if 1 <= rnd + 1 < NP:
            gs.append(itertools.chain(genS(2 * rnd + 2), genS(2 * rnd + 3)))
        if 1 <= rnd < NP:
            gs.append(genB(2 * rnd))
            gs.append(genB(2 * rnd + 1))
        if 0 <= rnd - 1 < NP:
            gs.append(itertools.chain(genA(2 * rnd - 2), genA(2 * rnd - 1)))
        interleave(gs)
        for f_ in post_round:
            f_()
        del post_round[:]

    bg_convert(len(bg_jobs))
    if debug:
        S.dma("sp", lambda e: e.dma_start(out=dbg["d_ya"].rearrange("(c p) t -> p c t", p=128), in_=yaT[:]),
              reads=[ByaT], writes=[Bout])
    S.barrier()
    if stop_after <= 2:
        S.emit()
        return nc

    wab = M("wab", [128, 4, D], BF16, 34816, (3, 3))
    wbb = M("wbb", [128, 4, D], BF16, 43008, (3, 3))
    mergedT = M("mergedT", [128, 8, L], BF16, 83968, (3, 4))
    sgate = M("sgate", [128, 6, 512], BF16, 51200, (3, 3))
    mtmp = M("mtmp", [128, 2, 2, 512], F32, 57344, (3, 3))
    Bwa = Buf("wa"); Bwb = Buf("wb")
    S.dma("pool", lambda e: e.dma_start(out=wab[:], in_=wa_d.rearrange("(k p) n -> p k n", p=128)), writes=[Bwa])
    S.dma("pool", lambda e: e.dma_start(out=wbb[:], in_=wb_d.rearrange("(k p) n -> p k n", p=128)), writes=[Bwb])
    woutb = M("woutb", [128, 8, D], BF16, 67584, (3, 4))
    ln1g = M("ln1g", [128, D], F32, 116736, (3, 4))
    ln1bA = M("ln1bA", [128, D], F32, 120832, (3, 4))
    Bwout = Buf("wout"); Bln1 = Buf("ln1")
    wov = wo_d.rearrange("(k p) n -> p k n", p=128)

    def prefetch_p3b():
        S.dma("pool", lambda e: e.dma_start(out=woutb[:, 0:4, :], in_=wov[:, 0:4, :]), writes=[Bwout])
        S.dma("pool", lambda e: e.dma_start(out=woutb[:, 4:8, :], in_=wov[:, 4:8, :]), writes=[Bwout])
        S.dma("sp", lambda e: e.dma_start(out=ln1g[:], in_=ln1g_d.to_broadcast([128, D])), writes=[Bln1])
        S.dma("sp", lambda e: e.dma_start(out=ln1bA[:], in_=ln1b_d.to_broadcast([128, D])), writes=[Bln1])
        S.op("act", lambda e: e.activation(out=ln1bA[:], in_=ln1bA[:], func=AF.Copy, scale=ALPHA),
             reads=[Bln1], writes=[Bln1])

    Bsg = [Buf("sg%d" % i) for i in range(6)]
    Bmt = [Buf("mt0"), Buf("mt1")]
    Bmerged = [Buf("merged%d" % f) for f in range(8)]

    def p3a_load(n):
        f, tb = n // 4, n % 4
        for gi in range(2):
            sl = (n % 3) * 2 + gi
            r0 = gi * 1024 + f * 128
            S.dma("sp", lambda e, r0=r0, tb=tb, sl=sl: e.dma_start(
                out=sgate[:, sl, :], in_=gates_d[r0:r0 + 128, tb * 512:(tb + 1) * 512]),
                reads=[Bgates_l[(r0 // 128) * 4 + tb]], writes=[Bsg[sl]])

    p3a_load(0)
    p3a_load(1)
    it3 = 0
    for n in range(32):
        f, tb = n // 4, n % 4
        ts_ = slice(tb * 512, (tb + 1) * 512)
        if n + 2 < 32:
            p3a_load(n + 2)
        pb = (n % 2) * 2
        mi = n % 2
        for bi, (wt, yT, bw, by) in enumerate(((wab, yaT, Bwa, ByaT), (wbb, ybT, Bwb, BybT))):
            fns = [lambda e, k=k, wt=wt, yT=yT, f=f, ts_=ts_, pb=pb, bi=bi: e.matmul(
                PS[pb + bi][:], lhsT=wt[:, k, f * 128:(f + 1) * 128], rhs=yT[:, k, ts_],
                start=(k == 0), stop=(k == 3)) for k in range(4)]
            S.group("pe", fns, reads=[bw, by], writes=[Bps[pb + bi]])
            sl = (n % 3) * 2 + bi
            S.op("dve", lambda e, pb=pb, bi=bi, sl=sl, mi=mi: e.tensor_tensor(
                out=mtmp[:, mi, bi, :], in0=sgate[:, sl, :], in1=PS[pb + bi][:], op=ALU.mult),
                reads=[Bsg[sl], Bps[pb + bi]], writes=[Bmt[mi]])
        S.op("pool", lambda e, mi=mi, f=f, ts_=ts_: e.tensor_tensor(
            out=mergedT[:, f, ts_], in0=mtmp[:, mi, 0, :], in1=mtmp[:, mi, 1, :], op=ALU.add),
            reads=[Bmt[mi]], writes=[Bmerged[f]])
        if n == 2:
            prefetch_p3b()
    if debug:
        S.dma("sp", lambda e: e.dma_start(out=dbg["d_mergedT"].rearrange("(c p) t -> p c t", p=128), in_=mergedT[:]),
              reads=Bmerged, writes=[Bout])
    S.barrier()
    if stop_after <= 3:
        S.emit()
        return nc

    ACC_OFF = 147136
    acc = M("acc", [128, NT, D], F32, ACC_OFF, (4, 7))
    x1T = M("x1T", [128, 8, L], BF16, 2048, (4, 5))
    x1tok = M("x1tok", [128, NT, D], BF16, 34816, (4, 6))
    xtok = M("xtok", [128, 2, D], F32, 124928, (4, 4))
    rres = M("rres", [128, 3, D], F32, 133120, (4, 4))
    lnst = M("lnst", [128, 3, 32], F32, 145408, (4, 4))
    Bxtok = [Buf("xtok0"), Buf("xtok1")]
    Brres = [Buf("r%d" % i) for i in range(3)]
    Blnst = [Buf("lnst%d" % i) for i in range(3)]
    Bacc = [Buf("acc%d" % t) for t in range(NT)]
    Bx1tok = [Buf("x1tok%d" % t) for t in range(NT)]
    Bx1T = Buf("x1T")

    def ln_scaled(src, srcB, stv, stB, gt, btA, gB, dst, dstB, alpha=ALPHA):
        S.op("dve", lambda e: e.bn_stats(out=stv[:, 0:6], in_=src[:, 0:512]), reads=srcB, writes=[stB])
        S.op("dve", lambda e: e.bn_stats(out=stv[:, 6:12], in_=src[:, 512:1024]), reads=srcB + [stB], writes=[stB])
        S.op("dve", lambda e: e.bn_aggr(out=stv[:, 12:14], in_=stv[:, 0:12]), reads=[stB], writes=[stB])
        S.op("dve", lambda e: e.tensor_scalar(out=stv[:, 14:15], in0=stv[:, 13:14], scalar1=LN_EPS, scalar2=None,
                                              op0=ALU.add), reads=[stB], writes=[stB])
        S.op("act", lambda e: e.activation(out=stv[:, 14:15], in_=stv[:, 14:15], func=AF.Sqrt), reads=[stB], writes=[stB])
        S.op("dve", lambda e: e.reciprocal(out=stv[:, 15:16], in_=stv[:, 14:15]), reads=[stB], writes=[stB])
        S.op("dve", lambda e: e.tensor_scalar(out=stv[:, 16:17], in0=stv[:, 15:16], scalar1=alpha, scalar2=None,
                                              op0=ALU.mult), reads=[stB], writes=[stB])
        S.op("dve", lambda e: e.scalar_tensor_tensor(out=src, in0=src, scalar=stv[:, 12:13], in1=gt[:],
                                                     op0=ALU.subtract, op1=ALU.mult), reads=srcB + [stB, gB], writes=srcB)
        S.op("dve", lambda e: e.scalar_tensor_tensor(out=dst, in0=src, scalar=stv[:, 16:17], in1=btA[:],
                                                     op0=ALU.mult, op1=ALU.add), reads=srcB + [stB, gB], writes=dstB)

    def p3b_A(tt):
        b2, b3 = tt % 2, tt % 3
        S.dma("sp", lambda e: e.dma_start(out=xtok[:, b2, :], in_=x_d[tt * 128:(tt + 1) * 128, :]), writes=[Bxtok[b2]])
        for half in range(2):
            bank = b3 * 2 + half
            fns = [lambda e, k=k, half=half, bank=bank: e.matmul(
                PS[bank][:], lhsT=mergedT[:, k, tt * 128:(tt + 1) * 128], rhs=woutb[:, k, half * 512:(half + 1) * 512],
                start=(k == 0), stop=(k == 7)) for k in range(8)]
            S.group("pe", fns, reads=Bmerged + [Bwout], writes=[Bps[bank]])
            S.op("dve", lambda e, half=half, bank=bank: e.scalar_tensor_tensor(
                out=rres[:, b3, half * 512:(half + 1) * 512], in0=xtok[:, b2, half * 512:(half + 1) * 512], scalar=ALPHA,
                in1=PS[bank][:], op0=ALU.mult, op1=ALU.add), reads=[Bxtok[b2], Bps[bank]], writes=[Brres[b3]])

    def p3b_B(tt):
        b3 = tt % 3
        ln_scaled(rres[:, b3, :], [Brres[b3]], lnst[:, b3, :], Blnst[b3], ln1g, ln1bA, Bln1, acc[:, tt, :], [Bacc[tt]])
        S.op("act", lambda e: e.activation(out=x1tok[:, tt, :], in_=acc[:, tt, :], func=AF.Copy, scale=1.0 / ALPHA),
             reads=[Bacc[tt]], writes=[Bx1tok[tt]])

    def p3b_C(tt):
        for half in range(2):
            tbk = 6 + half
            pv = PS[tbk][:].bitcast(BF16)
            fns = [lambda e, c=c, half=half, pv=pv: e.transpose(
                out=pv[:, c * 128:(c + 1) * 128], in_=x1tok[:, tt, (half * 4 + c) * 128:(half * 4 + c + 1) * 128],
                identity=ident[:]) for c in range(4)]
            S.group("pe", fns, reads=[Bx1tok[tt], Bconst], writes=[Bps[tbk]])
            S.op("act", lambda e, half=half, pv=pv: e.activation(
                out=x1T[:, half * 4:(half + 1) * 4, tt * 128:(tt + 1) * 128],
                in_=pv[:, 0:512].rearrange("p (c t) -> p c t", c=4), func=AF.Copy), reads=[Bps[tbk]], writes=[Bx1T])

    for s_ in range(NT + 2):
        if s_ < NT:
            p3b_A(s_)
        if 1 <= s_ <= NT:
            p3b_B(s_ - 1)
        if s_ >= 2:
            p3b_C(s_ - 2)
    if debug:
        S.dma("sp", lambda e: e.dma_start(out=dbg["d_acc"].rearrange("(t p) d -> p t d", p=128), in_=acc[:]),
              reads=Bacc, writes=[Bout])
    S.barrier()
    if stop_after <= 4:
        S.emit()
        return nc

    bg_convert(len(bg_jobs))
    o = 67584
    wrb = M("wrb", [128, 8, 36], BF16, o, (5, 5)); o += 1024
    brbc = M("brbc", [128, 36], F32, o, (5, 5)); o += 256
    ustr = M("ustr", [128, 128], BF16, o, (5, 5)); o += 256
    onesb = M("onesb", [128, 128], BF16, o, (5, 5)); o += 256
    jrow = M("jrow", [128, 64], F32, o, (5, 5)); o += 256
    thr16 = M("thr16", [128, 16], F32, o, (5, 5)); o += 64
    piota = M("piota", [128, 2], F32, o, (5, 5)); o += 64
    lg = M("lg", [128, NT, 36], F32, o, (5, 5)); o += 2304
    elm = M("elm", [128, NT, 32], F32, o, (5, 5)); o += 2048
    exr = M("exr", [128, NT, 32], F32, o, (5, 5)); o += 2048
    sel = M("sel", [128, NT, 32], F32, o, (5, 5)); o += 2048
    selb = M("selb", [128, NT, 32], BF16, o, (5, 5)); o += 1024
    comb = M("comb", [128, NT, 32], F32, o, (5, 5)); o += 2048
    posf = M("posf", [128, NT, 32], F32, o, (5, 5)); o += 2048
    tmpa = M("tmpa", [128, NT, 32], F32, o, (5, 5)); o += 2048
    tmpb = M("tmpb", [128, 64, 32], F32, o, (5, 5)); o += 8192
    sm = M("sm", [128, 24, NT], F32, o, (5, 5)); o += 1536
    pen = M("pen", [128, NT, 4], F32, o, (5, 5)); o += 256
    gex = M("gex", [128, NT, 4], F32, o, (5, 5)); o += 256
    ntr = M("ntr", [128, 128], BF16, o, (5, 5)); o += 256
    cnt32 = M("cnt32", [128, 24], F32, o, (5, 5)); o += 96
    toffs = M("toffs", [128, 64], F32, o, (5, 5)); o += 256
    ejf = M("ejf", [128, 64], F32, o, (5, 5)); o += 256
    assert o < 116736
    posu = M("posu", [128, NT, 2], mybir.dt.uint32, 100352, (5, 7))
    wsl = M("wsl", [128, NT, 2], F32, 100480, (5, 7))
    widxu = M("widxu", [128, 64], mybir.dt.uint32, 100608, (5, 7))
    Bwr = Buf("wr"); Bc5 = Buf("c5"); BR = Buf("route"); Bpos = Buf("posu")
    S.dma("pool", lambda e: e.dma_start(out=wrb[:], in_=wr_d.rearrange("(k p) n -> p k n", p=128)), writes=[Bwr])
    S.dma("sp", lambda e: e.dma_start(out=brbc[:], in_=br_d.to_broadcast([128, 36])), writes=[Bwr])
    S.op("pool", lambda e: e.memset(ustr[:], 1.0), writes=[Bc5])
    S.op("pool", lambda e: e.affine_select(out=ustr[:], in_=ustr[:], pattern=[[1, 128]], compare_op=ALU.is_ge,
                                           fill=preg(e, 0.0), base=-1, channel_multiplier=-1), reads=[Bc5], writes=[Bc5])
    S.op("pool", lambda e: e.memset(onesb[:], 1.0), writes=[Bc5])
    S.op("pool", lambda e: e.iota(jrow[:], pattern=[[1, 64]], base=0, channel_multiplier=0,
                                  allow_small_or_imprecise_dtypes=True), writes=[Bc5])
    S.op("pool", lambda e: e.iota(thr16[:], pattern=[[128, 16]], base=0, channel_multiplier=0,
                                  allow_small_or_imprecise_dtypes=True), writes=[Bc5])
    S.op("pool", lambda e: e.iota(piota[:], pattern=[[0, 2]], base=0, channel_multiplier=1,
                                  allow_small_or_imprecise_dtypes=True), writes=[Bc5])

    def bc(ap, shape):
        return ap.to_broadcast(shape)

    for tt in range(NT):
        bank = tt // 8
        fns = [lambda e, k=k, tt=tt, bank=bank: e.matmul(
            PS[bank][:, (tt % 8) * 36:(tt % 8 + 1) * 36], lhsT=x1T[:, k, tt * 128:(tt + 1) * 128], rhs=wrb[:, k, :],
            start=(k == 0), stop=(k == 7), skip_group_check=True) for k in range(8)]
        S.group("pe", fns, reads=[Bx1T, Bwr], writes=[Bps[bank]])
    for bank in range(2):
        S.op("dve", lambda e, bank=bank: e.tensor_tensor(
            out=lg[:, bank * 8:(bank + 1) * 8, :], in0=PS[bank][:, 0:288].rearrange("p (t c) -> p t c", c=36),
            in1=bc(brbc[:].rearrange("p (o c) -> p o c", o=1), [128, 8, 36]), op=ALU.add),
            reads=[Bps[bank], Bwr], writes=[BR])
    RB = [BR]
    def smv(r):
        return sm[:, r, :]

    def sm3(r, n):
        return bc(sm[:, r, :].rearrange("p (t o) -> p t o", o=1), [128, NT, n])

    S.op("dve", lambda e: e.tensor_reduce(out=smv(0), in_=lg[:, :, 0:4], axis=AX.X, op=ALU.max), reads=RB, writes=RB)
    S.op("dve", lambda e: e.tensor_tensor(out=pen[:], in0=lg[:, :, 0:4], in1=sm3(0, 4), op=ALU.is_lt), reads=RB, writes=RB)
    S.op("dve", lambda e: e.tensor_tensor(out=gex[:], in0=lg[:, :, 0:4], in1=sm3(0, 4), op=ALU.subtract), reads=RB, writes=RB)
    S.op("act", lambda e: e.activation(out=gex[:], in_=gex[:], func=AF.Exp), reads=RB, writes=RB)
    S.op("dve", lambda e: e.tensor_reduce(out=smv(1), in_=gex[:], axis=AX.X, op=ALU.add), reads=RB, writes=RB)
    S.op("dve", lambda e: e.tensor_scalar(out=pen[:], in0=pen[:], scalar1=NEG, scalar2=None, op0=ALU.mult), reads=RB, writes=RB)
    S.op("dve", lambda e: e.tensor_tensor(
        out=elm[:].rearrange("p t (g j) -> p t g j", g=4), in0=lg[:, :, 4:36].rearrange("p t (g j) -> p t g j", g=4),
        in1=bc(pen[:].rearrange("p t (g o) -> p t g o", o=1), [128, NT, 4, 8]), op=ALU.add), reads=RB, writes=RB)
    S.op("dve", lambda e: e.tensor_reduce(out=smv(2), in_=elm[:], axis=AX.X, op=ALU.max), reads=RB, writes=RB)
    S.op("dve", lambda e: e.tensor_tensor(out=tmpa[:], in0=elm[:], in1=sm3(2, 32), op=ALU.is_ge), reads=RB, writes=RB)
    S.op("dve", lambda e: e.scalar_tensor_tensor(out=tmpa[:], in0=tmpa[:], scalar=NEG, in1=elm[:], op0=ALU.mult, op1=ALU.add),
         reads=RB, writes=RB)
    S.op("dve", lambda e: e.tensor_reduce(out=smv(3), in_=tmpa[:], axis=AX.X, op=ALU.max), reads=RB, writes=RB)
    S.op("dve", lambda e: e.tensor_tensor(out=exr[:], in0=elm[:], in1=sm3(2, 32), op=ALU.subtract), reads=RB, writes=RB)
    S.op("act", lambda e: e.activation(out=exr[:], in_=exr[:], func=AF.Exp), reads=RB, writes=RB)
    S.op("dve", lambda e: e.tensor_tensor(out=sel[:], in0=elm[:], in1=sm3(3, 32), op=ALU.is_ge), reads=RB, writes=RB)
    S.op("dve", lambda e: e.tensor_copy(out=selb[:], in_=sel[:]), reads=RB, writes=RB)
    S.op("dve", lambda e: e.tensor_tensor(out=comb[:], in0=sel[:], in1=exr[:], op=ALU.mult), reads=RB, writes=RB)
    S.op("dve", lambda e: e.tensor_reduce(out=smv(4), in_=comb[:], axis=AX.X, op=ALU.add), reads=RB, writes=RB)
    S.op("dve", lambda e: e.tensor_tensor(out=smv(5), in0=smv(4), in1=smv(1), op=ALU.mult), reads=RB, writes=RB)
    S.op("dve", lambda e: e.reciprocal(out=smv(6), in_=smv(5)), reads=RB, writes=RB)
    S.op("dve", lambda e: e.tensor_tensor(out=comb[:], in0=comb[:], in1=sm3(6, 32), op=ALU.mult), reads=RB, writes=RB)
    if debug:
        S.dma("sp", lambda e: e.dma_start(out=dbg["d_comb"].rearrange("(t p) d -> p t d", p=128), in_=comb[:]),
              reads=RB, writes=[Bout])
    for tt in range(NT):
        fns = []
        for tp in range(tt):
            fns.append(lambda e, tt=tt, tp=tp: e.matmul(
                PS[2][:, tt * 32:(tt + 1) * 32], lhsT=onesb[:], rhs=selb[:, tp, :], start=(tp == 0), stop=False,
                skip_group_check=True))
        fns.append(lambda e, tt=tt: e.matmul(
            PS[2][:, tt * 32:(tt + 1) * 32], lhsT=ustr[:], rhs=selb[:, tt, :], start=(tt == 0), stop=True,
            skip_group_check=True))
        S.group("pe", fns, reads=RB + [Bc5], writes=[Bps[2]])
    fns = [lambda e, tt=tt: e.matmul(PS[3][0:32, 0:1], lhsT=selb[:, tt, :], rhs=onesb[:, 0:1],
                                     start=(tt == 0), stop=(tt == NT - 1)) for tt in range(NT)]
    S.group("pe", fns, reads=RB + [Bc5], writes=[Bps[3]])
    S.op("dve", lambda e: e.tensor_copy(out=cnt32[0:32, 0:1], in_=PS[3][0:32, 0:1]), reads=[Bps[3]], writes=RB)
    S.op("dve", lambda e: e.tensor_scalar(out=cnt32[0:32, 4:20], in0=thr16[0:32, :], scalar1=cnt32[0:32, 0:1], scalar2=None,
                                          op0=ALU.is_lt), reads=RB + [Bc5], writes=RB)
    S.op("dve", lambda e: e.tensor_reduce(out=cnt32[0:32, 1:2], in_=cnt32[0:32, 4:20], axis=AX.X, op=ALU.add), reads=RB, writes=RB)
    S.op("dve", lambda e: e.tensor_scalar(out=ntr[0:32, :], in0=onesb[0:32, :], scalar1=cnt32[0:32, 1:2], scalar2=None,
                                          op0=ALU.mult), reads=RB + [Bc5], writes=RB)
    S.op("pe", lambda e: e.matmul(PS[3][:, 64:96], lhsT=ntr[0:32, :], rhs=ustr[0:32, 0:32], start=True, stop=True),
         reads=RB + [Bc5], writes=[Bps[3]])
    S.op("pe", lambda e: e.matmul(PS[3][:, 96:128], lhsT=ntr[0:32, :], rhs=causal01[0:32, 0:32], start=True, stop=True),
         reads=RB + [Bconst], writes=[Bps[3]])
    S.op("dve", lambda e: e.tensor_copy(out=toffs[:], in_=PS[3][:, 64:128]), reads=[Bps[3]], writes=RB)
    S.op("dve", lambda e: e.scalar_tensor_tensor(
        out=posf[:], in0=bc(toffs[:, 0:32].rearrange("p (o c) -> p o c", o=1), [128, NT, 32]), scalar=128.0,
        in1=PS[2][:].rearrange("p (t c) -> p t c", c=32), op0=ALU.mult, op1=ALU.add), reads=RB + [Bps[2]], writes=RB)
    S.op("dve", lambda e: e.tensor_tensor(out=tmpa[:], in0=sel[:], in1=posf[:], op=ALU.mult), reads=RB, writes=RB)
    S.op("dve", lambda e: e.tensor_reduce(out=smv(8), in_=tmpa[:], axis=AX.X, op=ALU.max), reads=RB, writes=RB)
    S.op("dve", lambda e: e.tensor_scalar(out=tmpa[:], in0=sel[:], scalar1=-1.0e6, scalar2=1.0e6, op0=ALU.mult, op1=ALU.add),
         reads=RB, writes=RB)
    S.op("dve", lambda e: e.tensor_tensor(out=tmpa[:], in0=tmpa[:], in1=posf[:], op=ALU.add), reads=RB, writes=RB)
    S.op("dve", lambda e: e.tensor_reduce(out=smv(7), in_=tmpa[:], axis=AX.X, op=ALU.min), reads=RB, writes=RB)
    for r_pos, r_w in ((7, 9), (8, 10)):
        S.op("dve", lambda e, r_pos=r_pos: e.tensor_tensor(out=tmpa[:], in0=posf[:], in1=sm3(r_pos, 32), op=ALU.is_equal),
             reads=RB, writes=RB)
        S.op("dve", lambda e: e.tensor_tensor(out=tmpa[:], in0=tmpa[:], in1=comb[:], op=ALU.mult), reads=RB, writes=RB)
        S.op("dve", lambda e, r_w=r_w: e.tensor_reduce(out=smv(r_w), in_=tmpa[:], axis=AX.X, op=ALU.add), reads=RB, writes=RB)
    for slot in range(2):
        S.op("dve", lambda e, slot=slot: e.tensor_copy(out=posu[:, :, slot], in_=smv(7 + slot)), reads=RB, writes=[Bpos])
        S.op("dve", lambda e, slot=slot: e.tensor_copy(out=wsl[:, :, slot], in_=smv(9 + slot)), reads=RB, writes=[Bpos])
    S.op("dve", lambda e: e.tensor_tensor(
        out=tmpb[:], in0=bc(toffs[:, 32:64].rearrange("p (o c) -> p o c", o=1), [128, 64, 32]),
        in1=bc(jrow[:].rearrange("p (j o) -> p j o", o=1), [128, 64, 32]), op=ALU.is_le), reads=RB + [Bc5], writes=RB)
    S.op("dve", lambda e: e.tensor_reduce(out=ejf[:], in_=tmpb[:], axis=AX.X, op=ALU.add), reads=RB, writes=RB)
    S.op("dve", lambda e: e.scalar_tensor_tensor(
        out=ejf[:], in0=ejf[:], scalar=128.0, in1=bc(piota[:, 0:1], [128, 64]), op0=ALU.mult, op1=ALU.add),
        reads=RB + [Bc5], writes=RB)
    S.op("dve", lambda e: e.tensor_copy(out=widxu[:], in_=ejf[:]), reads=RB, writes=[Bpos])
    S.barrier()
    if stop_after <= 5:
        S.emit()
        return nc

    NTL = 64
    NB = 5
    NX = 6
    wgt = [M("wgt%d" % b, [128, 2048], BF16, 2048 + b * 4096, (6, 6)) for b in range(NB)]
    wut = [M("wut%d" % b, [128, 2048], BF16, 100864 + b * 4096, (6, 6)) for b in range(NB)]
    wdt = [M("wdt%d" % b, [128, 2048], BF16, 121344 + b * 4096, (6, 6)) for b in range(NB)]
    xst = M("xst", [128, NX, D], BF16, 22528, (6, 6))
    xsT = M("xsT", [128, 2, 8, 128], BF16, 71680, (6, 6))
    sgt = M("sgt", [128, 2, 256], BF16, 75776, (6, 6))
    hTt = M("hTt", [128, 2, 256], BF16, 76800, (6, 6))
    ysb = M("ysb", [128, 2, D], F32, 77824, (6, 6))
    Bwt = [[Buf("wt%d_%d" % (m, b)) for b in range(NB)] for m in range(3)]
    Bxst = [Buf("xst%d" % i) for i in range(NX)]; BxsT = [Buf("xsT0"), Buf("xsT1")]
    Bsgt = [Buf("sgt0"), Buf("sgt1")]; BhTt = [Buf("hTt0"), Buf("hTt1")]; Bysb = [Buf("ysb0"), Buf("ysb1")]
    Bxs_l = [Buf("xs_d%d" % i) for i in range(2 * NT)]; Bys = Buf("ys_d")
    for tt in range(NT):
        for slot in range(2):
            S.dma("pool", lambda e, tt=tt, slot=slot: e.indirect_dma_start(
                out=xs_d, out_offset=bass.IndirectOffsetOnAxis(ap=posu[:, tt, slot:slot + 1], axis=0),
                in_=x1tok[:, tt, :], in_offset=None), reads=[Bpos, Bx1tok[tt]] + Bzero_l, writes=[Bxs_l[tt * 2 + slot]])

    def moe_load_w(j):
        b = j % NB
        for m, (wd_, wt_) in enumerate(((wgb_d, wgt), (wub_d, wut), (wdb_d, wdt))):
            S.dma("pool", lambda e, wd_=wd_, wt_=wt_: e.indirect_dma_start(
                out=wt_[b][:], out_offset=None, in_=wd_,
                in_offset=bass.IndirectOffsetOnAxis(ap=widxu[:, j:j + 1], axis=0), bounds_check=preg(e, NEXP * 128 - 1),
                oob_is_err=False), reads=[Bpos] + Bwbf_l, writes=[Bwt[m][b]])

    def moe_load_x(j):
        bx = j % NX
        S.dma("sp", lambda e: e.dma_start(out=xst[:, bx, :], in_=xs_d[j * 128:(j + 1) * 128, :]), reads=Bxs_l, writes=[Bxst[bx]])

    def moe_T(j):
        b2, bx = j % 2, j % NX
        pv = PS[b2][:].bitcast(BF16)
        fns = [lambda e, c=c: e.transpose(out=pv[:, c * 128:(c + 1) * 128], in_=xst[:, bx, c * 128:(c + 1) * 128],
                                         identity=ident[:]) for c in range(8)]
        S.group("pe", fns, reads=[Bxst[bx], Bconst], writes=[Bps[b2]])
        S.op("act", lambda e: e.activation(out=xsT[:, b2, :, :], in_=pv.rearrange("p (c t) -> p c t", c=8), func=AF.Copy),
             reads=[Bps[b2]], writes=[BxsT[b2]])

    def moe_GU(j):
        b2, b = j % 2, j % NB
        bank = 2 + b2
        fns = []
        for m, wt_ in ((0, wgt), (1, wut)):
            wv = wt_[b][:].rearrange("p (k n) -> p k n", k=8)
            for c in range(2):
                for k in range(8):
                    fns.append(lambda e, m=m, wv=wv, c=c, k=k: e.matmul(
                        PS[bank][:, (m * 2 + c) * 128:(m * 2 + c + 1) * 128], lhsT=wv[:, k, c * 128:(c + 1) * 128],
                        rhs=xsT[:, b2, k, :], start=(k == 0), stop=(k == 7), skip_group_check=True))
        S.group("pe", fns, reads=[BxsT[b2], Bwt[0][b], Bwt[1][b]], writes=[Bps[bank]])
        S.op("act", lambda e: e.activation(out=sgt[:, b2, :], in_=PS[bank][:, 0:256], func=AF.Silu),
             reads=[Bps[bank]], writes=[Bsgt[b2]])
        S.op("dve", lambda e: e.tensor_tensor(out=hTt[:, b2, :], in0=sgt[:, b2, :], in1=PS[bank][:, 256:512], op=ALU.mult),
             reads=[Bsgt[b2], Bps[bank]], writes=[BhTt[b2]])

    def moe_D(j):
        b2, b = j % 2, j % NB
        wv = wdt[b][:].rearrange("p (c n) -> p c n", c=2)
        for half in range(2):
            bank = 4 + b2 * 2 + half
            fns = [lambda e, c=c, half=half, bank=bank: e.matmul(
                PS[bank][:], lhsT=hTt[:, b2, c * 128:(c + 1) * 128], rhs=wv[:, c, half * 512:(half + 1) * 512],
                start=(c == 0), stop=(c == 1)) for c in range(2)]
            S.group("pe", fns, reads=[BhTt[b2], Bwt[2][b]], writes=[Bps[bank]])
            if half == 0:
                S.op("act", lambda e, bank=bank: e.activation(out=ysb[:, b2, 0:512], in_=PS[bank][:], func=AF.Copy),
                     reads=[Bps[bank]], writes=[Bysb[b2]])
            else:
                S.op("dve", lambda e, bank=bank: e.tensor_copy(out=ysb[:, b2, 512:1024], in_=PS[bank][:]),
                     reads=[Bps[bank]], writes=[Bysb[b2]])
        S.dma("sp", lambda e: e.dma_start(out=ys_d[j * 128:(j + 1) * 128, :], in_=ysb[:, b2, :]), reads=[Bysb[b2]], writes=[Bys])

    for j in range(NX):
        moe_load_x(j)
    for j in range(NB):
        moe_load_w(j)
    for s_ in range(NTL + 2):
        if s_ < NTL:
            moe_T(s_)
            if s_ + NX < NTL:
                moe_load_x(s_ + NX)
        if 1 <= s_ <= NTL:
            moe_GU(s_ - 1)
        if s_ >= 2:
            moe_D(s_ - 2)
            if s_ - 2 + NB < NTL:
                moe_load_w(s_ - 2 + NB)
    S.barrier()

    NYG = 12
    yg = M("yg", [128, NYG, D], F32, 34816, (7, 7))
    ln2g = M("ln2g", [128, D], F32, 2048, (7, 7))
    ln2bA = M("ln2bA", [128, D], F32, 6144, (7, 7))
    obuf = M("obuf", [128, 3, D], F32, 10240, (7, 7))
    lnst2 = M("lnst2", [128, 3, 32], F32, 22528, (7, 7))
    Bln2 = Buf("ln2"); Bob = [Buf("ob%d" % i) for i in range(3)]; Blnst2 = [Buf("ls%d" % i) for i in range(3)]
    Byg = [Buf("yg%d" % i) for i in range(NYG)]
    S.dma("sp", lambda e: e.dma_start(out=ln2g[:], in_=ln2g_d.to_broadcast([128, D])), writes=[Bln2])
    S.dma("sp", lambda e: e.dma_start(out=ln2bA[:], in_=ln2b_d.to_broadcast([128, D])), writes=[Bln2])

    def tail_gather(q):
        tt, slot = q // 2, q % 2
        yb_ = q % NYG
        S.dma("pool", lambda e: e.indirect_dma_start(
            out=yg[:, yb_, :], out_offset=None, in_=ys_d,
            in_offset=bass.IndirectOffsetOnAxis(ap=posu[:, tt, slot:slot + 1], axis=0)),
            reads=[Bpos, Bys], writes=[Byg[yb_]])

    for q in range(NYG):
        tail_gather(q)
    for tt in range(NT):
        b3 = tt % 3
        for slot in range(2):
            q = tt * 2 + slot
            yb_ = q % NYG
            S.op("dve", lambda e, tt=tt, slot=slot, yb_=yb_: e.scalar_tensor_tensor(
                out=acc[:, tt, :], in0=yg[:, yb_, :], scalar=wsl[:, tt, slot:slot + 1], in1=acc[:, tt, :],
                op0=ALU.mult, op1=ALU.add), reads=[Byg[yb_], Bpos, Bacc[tt]], writes=[Bacc[tt]])
            if q + NYG < 2 * NT:
                tail_gather(q + NYG)
        ln_scaled(acc[:, tt, :], [Bacc[tt]], lnst2[:, b3, :], Blnst2[b3], ln2g, ln2bA, Bln2, obuf[:, b3, :], [Bob[b3]],
                  alpha=1.0)
        S.dma("sp", lambda e, tt=tt, b3=b3: e.dma_start(out=out_d[tt * 128:(tt + 1) * 128, :], in_=obuf[:, b3, :]),
              reads=[Bob[b3]], writes=[Bout])
    S.barrier()
    S.emit()
    return nc


_NC_CACHE = {}


def _host_inputs(inp, b):
    f = np.float32
    w_in = inp["w_in"][0]
    cols = list(range(0, 1024))
    cols += list(range(1152, 1664))
    cols += list(range(1664, 1728)) * 2
    qb0 = 1736
    for j in range(4):
        cols += list(range(qb0 + j * 64, qb0 + (j + 1) * 64))
        cols += list(range(qb0 + (j + 4) * 64, qb0 + (j + 5) * 64))
    cols += list(range(2248, 2376))
    cols += list(range(1024, 1152))
    cols += list(range(2376, 2504))
    cols += list(range(1728, 1736))
    assert len(cols) == W1COLS
    rel = inp["rel_bias"].astype(f)
    s = np.arange(128)[:, None]
    t = np.arange(128)[None, :]
    bk_prev = t5_bucket_np(t - s + 128)
    bk_own = t5_bucket_np(t - s)
    swab = np.zeros((4, 128, 4, 128), f)
    for typ, bk in enumerate((bk_prev, bk_own)):
        for g in range(2):
            for j in range(4):
                swab[typ * 2 + g, :, j, :] = rel[bk, 8 + 4 * g + j]
    dsab = np.zeros((2, 128, 8, 128), f)
    for typ, bk in enumerate((bk_prev, bk_own)):
        for h in range(8):
            dsab[typ, :, h, :] = rel[bk, h]
    return {
        "xT": np.ascontiguousarray(inp["x"][b].T),
        "x": np.ascontiguousarray(inp["x"][b]),
        "w1": np.ascontiguousarray(w_in[:, cols]),
        "wg": np.ascontiguousarray(w_in[:, 2504:4552]),
        "kvg": np.ascontiguousarray(inp["kv_norm_g"][0].reshape(1, 128)),
        "wuv": np.ascontiguousarray(inp["w_uv"][0].transpose(1, 0, 2).reshape(128, 512)),
        "wa": np.ascontiguousarray(inp["w_branch_a"][0]),
        "wb": np.ascontiguousarray(inp["w_branch_b"][0]),
        "wo": np.ascontiguousarray(inp["w_out"][0]),
        "sinks": np.ascontiguousarray(inp["sinks"][0].reshape(1, 8)),
        "ln1g": np.ascontiguousarray(inp["ln1_g"][0].reshape(1, D)),
        "ln1b": np.ascontiguousarray(inp["ln1_b"][0].reshape(1, D)),
        "ln2g": np.ascontiguousarray(inp["ln2_g"][0].reshape(1, D)),
        "ln2b": np.ascontiguousarray(inp["ln2_b"][0].reshape(1, D)),
        "wr": np.ascontiguousarray(np.concatenate([inp["w_group"][0], inp["w_router"][0]], axis=1)),
        "br": np.ascontiguousarray(np.concatenate([inp["b_group"][0], inp["b_router"][0]]).reshape(1, 36)),
        "wgr": np.ascontiguousarray(inp["w_gate"][0].reshape(NEXP, 8, 128, DE).transpose(0, 2, 1, 3).reshape(NEXP * 128, 2048)),
        "wur": np.ascontiguousarray(inp["w_up"][0].reshape(NEXP, 8, 128, DE).transpose(0, 2, 1, 3).reshape(NEXP * 128, 2048)),
        "wdr": np.ascontiguousarray(inp["w_down"][0].reshape(NEXP, 2, 128, D).transpose(0, 2, 1, 3).reshape(NEXP * 128, 2048)),
        "swab": swab.reshape(4, 128, 512),
        "dsab": dsab.reshape(2, 128, 1024),
        "c31": np.ascontiguousarray(rel[31, 0:8].reshape(1, 8)),
    }


def kernel(**inputs):
    inp = {k: np.asarray(v, dtype=np.float32) for k, v in inputs.items()}
    n = 8
    if "nc" not in _NC_CACHE:
        _NC_CACHE["nc"] = build_nc(False)
    nc = _NC_CACHE["nc"]
    shared = None
    in_maps = []
    for b in range(n):
        m = _host_inputs(inp, b) if shared is None else dict(shared)
        if shared is None:
            shared = m
        else:
            m["xT"] = np.ascontiguousarray(inp["x"][b].T)
            m["x"] = np.ascontiguousarray(inp["x"][b])
        in_maps.append(m)
    res = run_bass_kernel_spmd(nc, in_maps, core_ids=list(range(n)))
    return np.stack([np.asarray(r["out"], dtype=np.float32) for r in res.results], axis=0)
```

```python
import math
import contextlib
import numpy as np
import concourse.bass as bass
import concourse.mybir as mybir
from concourse.bass_utils import run_bass_kernel_spmd

F32 = mybir.dt.float32
BF16 = mybir.dt.bfloat16
AF = mybir.ActivationFunctionType
ALU = mybir.AluOpType
AX = mybir.AxisListType

D = 1024
L = 2048
NT = 16
NEXP = 32
DE = 256
ALPHA = 2.0 ** 0.25
LN_EPS = 1e-5
RMS_EPS = 1e-6
ATT_SCALE = 128.0 ** -0.5
NEG = -30000.0
N_BISECT = 16
W1COLS = 2568
SB_BASE = 16640
SB_END = 229368


class Buf:
    __slots__ = ("name", "writer", "readers")

    def __init__(self, name):
        self.name = name
        self.writer = None
        self.readers = []


class Sched:
    ENGS = ("pe", "act", "dve", "pool", "sp")

    def __init__(self, nc, n_dma_sems=24):
        self.nc = nc
        self.ops = {e: [] for e in self.ENGS}
        self.cnt = {e: 0 for e in self.ENGS}
        self.seen = {e: {} for e in self.ENGS}
        self.n_dma_sems = n_dma_sems
        self.dma_i = {}
        self.dma_val = {}
        self.sems = {}

    def _deps(self, eng, reads, writes):
        need = {}

        def add(tok, skip_same):
            if tok is None:
                return
            e, key, val = tok
            if e == eng and skip_same:
                return
            if need.get(key, 0) < val:
                need[key] = val

        pe = eng == "pe"
        for b in reads:
            add(b.writer, pe)
        for b in writes:
            add(b.writer, pe)
            for r in b.readers:
                add(r, True)
        waits = []
        seen = self.seen[eng]
        for key, val in need.items():
            if seen.get(key, 0) < val:
                seen[key] = val
                waits.append((key, val))
        return waits

    def _commit(self, tok, reads, writes):
        for b in reads:
            b.readers.append(tok)
        for b in writes:
            b.writer = tok
            b.readers = []

    def group(self, eng, fns, reads=(), writes=()):
        waits = self._deps(eng, reads, writes)
        self.cnt[eng] += 1
        tok = (eng, "e_" + eng, self.cnt[eng])
        n = len(fns)
        for i, fn in enumerate(fns):
            self.ops[eng].append((fn, waits if i == 0 else (), ("e_" + eng, 1) if i == n - 1 else None))
        self._commit(tok, reads, writes)
        return tok

    def op(self, eng, fn, reads=(), writes=()):
        return self.group(eng, [fn], reads, writes)

    def dma(self, eng, fn, reads=(), writes=()):
        i = self.dma_i.get(eng, 0) % self.n_dma_sems
        self.dma_i[eng] = self.dma_i.get(eng, 0) + 1
        key = "d_%s_%d" % (eng, i)
        waits = list(self._deps(eng, reads, writes))
        prev = self.dma_val.get(key, 0)
        if prev > 0 and self.seen[eng].get(key, 0) < prev:
            self.seen[eng][key] = prev
            waits.append((key, prev))
        self.dma_val[key] = prev + 16
        tok = ("dma", key, self.dma_val[key])
        self.ops[eng].append((fn, waits, (key, 16)))
        self._commit(tok, reads, writes)
        return tok

    def barrier(self):
        targets = [("e_" + e, self.cnt[e]) for e in self.ENGS if self.cnt[e] > 0]
        targets += [(k, v) for k, v in self.dma_val.items() if v > 0]
        for e in self.ENGS:
            waits = []
            for key, val in targets:
                if key == "e_" + e and e != "pe":
                    pass
                if self.seen[e].get(key, 0) < val:
                    self.seen[e][key] = val
                    waits.append((key, val))
            if waits:
                self.ops[e].append((None, waits, None))

    def emit(self):
        nc = self.nc
        keys = ["e_" + e for e in self.ENGS] + sorted(self.dma_val.keys())
        with contextlib.ExitStack() as st:
            for k in keys:
                self.sems[k] = st.enter_context(nc.semaphore(k))
            block = st.enter_context(nc.Block())
            sems = self.sems

            def run(eng_name):
                def body(eng):
                    for fn, waits, inc in self.ops[eng_name]:
                        for key, val in waits:
                            eng.wait_ge(sems[key], val)
                        if fn is None:
                            continue
                        ins = fn(eng)
                        if inc is not None:
                            ins.then_inc(sems[inc[0]], inc[1])
                return body

            block.tensor(run("pe"))
            block.scalar(run("act"))
            block.vector(run("dve"))
            block.gpsimd(run("pool"))
            block.sync(run("sp"))


class Mem:
    def __init__(self, nc):
        self.nc = nc
        self.allocs = []

    def __call__(self, name, shape, dtype, off, life):
        esz = 4 if dtype == F32 else 2
        n = esz
        for s in shape[1:]:
            n *= s
        a0, a1 = SB_BASE + off, SB_BASE + off + n
        assert a1 <= SB_END, (name, a1)
        for (nm, b0, b1, lf) in self.allocs:
            if a0 < b1 and b0 < a1 and lf[0] <= life[1] and life[0] <= lf[1]:
                raise AssertionError("SBUF overlap %s vs %s" % (name, nm))
        self.allocs.append((name, a0, a1, life))
        return self.nc.alloc_sbuf_tensor_at(name, list(shape), dtype, offset=a0)


def t5_bucket_np(dist):
    n = np.maximum(dist, 0)
    nf = np.maximum(n, 1).astype(np.float32)
    large = 16 + (np.log(nf / np.float32(16)) / np.float32(math.log(128 / 16)) * np.float32(16)).astype(np.int32)
    large = np.minimum(large, 31)
    return np.where(n < 16, n, large).astype(np.int32)


def build_nc(debug=False, stop_after=99):
    nc = bass.Bass("TRN2", target_bir_lowering=False)

    def din(name, shape, dt=F32):
        return nc.dram_tensor(name, list(shape), dt, kind="ExternalInput").ap()

    xT_d = din("xT", [D, L])
    x_d = din("x", [L, D])
    w1_d = din("w1", [D, W1COLS])
    wg_d = din("wg", [D, 2048])
    kvg_d = din("kvg", [1, 128])
    wuv_d = din("wuv", [128, 512])
    wa_d = din("wa", [512, D])
    wb_d = din("wb", [512, D])
    wo_d = din("wo", [D, D])
    sinks_d = din("sinks", [1, 8])
    ln1g_d = din("ln1g", [1, D])
    ln1b_d = din("ln1b", [1, D])
    ln2g_d = din("ln2g", [1, D])
    ln2b_d = din("ln2b", [1, D])
    wr_d = din("wr", [D, 36])
    br_d = din("br", [1, 36])
    wgur_d = din("wgur", [NEXP * 128, 4096])
    wdr_d = din("wdr", [NEXP * 128, 2048])
    wgub_d = nc.dram_tensor("wgu_bf16", [NEXP * 128, 4096], BF16, kind="Internal").ap()
    wdb_d = nc.dram_tensor("wd_bf16", [NEXP * 128, 2048], BF16, kind="Internal").ap()
    gates_d = nc.dram_tensor("gates_scr", [2048, L], BF16, kind="Internal").ap()
    xs_d = nc.dram_tensor("xs_scr", [64 * 128, D], BF16, kind="Internal").ap()
    ys_d = nc.dram_tensor("ys_scr", [64 * 128, D], F32, kind="Internal").ap()
    swab_d = din("swab", [4, 128, 512])
    dsab_d = din("dsab", [2, 128, 1024])
    c31_d = din("c31", [1, 8])
    out_d = nc.dram_tensor("out", [L, D], F32, kind="ExternalOutput").ap()
    dbg = {}
    if debug:
        for nm, shp in [("d_yb", [512, L]), ("d_ya", [512, L]), ("d_mergedT", [D, L]), ("d_acc", [L, D]),
                        ("d_comb", [L, 32]), ("d_ckvT", [128, L]), ("d_qlatT", [128, L])]:
            dbg[nm] = nc.dram_tensor(nm, shp, BF16 if nm in ("d_yb", "d_ya", "d_mergedT", "d_ckvT", "d_qlatT") else F32,
                                     kind="ExternalOutput").ap()

    S = Sched(nc)
    M = Mem(nc)
    _regs = {}

    def preg(e, val):
        if val not in _regs:
            _regs[val] = e.to_reg(val)
        return _regs[val]
    PS = [nc.alloc_psum_tensor("ps%d" % i, [128, 512], F32) for i in range(8)]
    Bps = [Buf("ps%d" % i) for i in range(8)]
    Bout = Buf("out")

    ident = M("ident", [128, 128], BF16, 0, (1, 7))
    gkv_bc = M("gkv_bc", [128, 128], F32, 256, (1, 7))
    esink = M("esink", [128, 8], F32, 768, (1, 7))
    negc31 = M("negc31", [128, 8], F32, 800, (1, 7))
    identf = M("identf", [128, 128], F32, 1024, (1, 7))
    causal01 = M("causal01", [128, 128], BF16, 1536, (1, 7))
    Bconst = Buf("const")

    S.op("pool", lambda e: e.memset(identf[:], 1.0), writes=[Bconst])
    S.op("pool", lambda e: e.affine_select(out=identf[:], in_=identf[:], pattern=[[1, 128]],
                                           compare_op=ALU.is_equal, fill=preg(e, 0.0), base=0, channel_multiplier=-1),
         reads=[Bconst], writes=[Bconst])
    S.op("pool", lambda e: e.tensor_copy(out=ident[:], in_=identf[:]), reads=[Bconst], writes=[Bconst])
    S.op("pool", lambda e: e.memset(causal01[:], 1.0), writes=[Bconst])
    S.op("pool", lambda e: e.affine_select(out=causal01[:], in_=causal01[:], pattern=[[1, 128]],
                                           compare_op=ALU.is_ge, fill=preg(e, 0.0), base=0, channel_multiplier=-1),
         reads=[Bconst], writes=[Bconst])
    S.dma("sp", lambda e: e.dma_start(out=gkv_bc[:], in_=kvg_d.to_broadcast([128, 128])), writes=[Bconst])
    S.dma("sp", lambda e: e.dma_start(out=esink[:], in_=sinks_d.to_broadcast([128, 8])), writes=[Bconst])
    S.dma("sp", lambda e: e.dma_start(out=negc31[:], in_=c31_d.to_broadcast([128, 8])), writes=[Bconst])
    S.op("act", lambda e: e.activation(out=esink[:], in_=esink[:], func=AF.Exp), reads=[Bconst], writes=[Bconst])
    S.op("dve", lambda e: e.tensor_scalar(out=negc31[:], in0=negc31[:], scalar1=-1.0, scalar2=None, op0=ALU.mult),
         reads=[Bconst], writes=[Bconst])

    xT = M("xT", [128, 8, L], BF16, 2048, (1, 1))
    o = 34816
    qlatT = M("qlatT", [128, 8, L], BF16, o, (1, 2.5)); o += 32768
    qidxT = M("qidxT", [128, 4, L], BF16, o, (1, 2.5)); o += 16384
    qbT = M("qbT", [128, 4, L], BF16, o, (1, 2)); o += 16384
    kidxT = M("kidxT", [128, L], BF16, o, (1, 2.5)); o += 4096
    kbT = M("kbT", [128, L], BF16, o, (1, 2)); o += 4096
    ckvT = M("ckvT", [128, L], BF16, o, (1, 2.5)); o += 4096
    kvW = M("kvW", [128, NT, 8, 65], BF16, o, (1, 2.5)); o += 16640
    vaug = M("vaug", [128, NT, 2, 65], BF16, o, (1, 2)); o += 4160
    widx = M("widx", [128, NT, 8], F32, o, (1, 2.5)); o += 512
    assert o == 133952
    R3 = 133952
    w1b = M("w1b", [128, 8, W1COLS], BF16, R3, (1, 1))
    wuvs = M("wuvs", [128, 512], F32, R3 + 41088, (1, 1))
    ckvtok = M("ckvtok", [128, NT, 128], BF16, R3 + 43136, (1, 1))
    wuvb = M("wuvb", [128, 512], BF16, R3 + 47232, (1, 1))
    p1tmp = M("p1tmp", [128, 128], F32, R3 + 48256, (1, 1))
    p1junk = M("p1junk", [128, 128], F32, R3 + 48768, (1, 1))

    BxT = [Buf("xT%d" % k) for k in range(8)]
    Bw1 = [Buf("w1_%d" % c) for c in range(6)]
    w1v = w1_d.rearrange("(k p) n -> p k n", p=128)
    xTv = xT_d.rearrange("(k p) n -> p k n", p=128)
    for k in range(8):
        S.dma("pool", lambda e, k=k: e.dma_start(out=xT[:, k, :], in_=xTv[:, k, :]), writes=[BxT[k]])
    w1blocks = [(0, 512), (512, 1024), (1024, 1536), (1536, 2048), (2048, 2304), (2304, W1COLS)]
    for c, (c0, c1) in enumerate(w1blocks):
        S.dma("pool", lambda e, c0=c0, c1=c1: e.dma_start(out=w1b[:, :, c0:c1], in_=w1v[:, :, c0:c1]),
              writes=[Bw1[c]])
    wgs = [M("wgs0", [128, 8, 512], BF16, 184320, (1, 1)), M("wgs1", [128, 8, 512], BF16, 192512, (1, 1))]
    gst = M("gst", [128, 2, 512], BF16, 200704, (1, 1))
    Bwgs = [Buf("wgs0"), Buf("wgs1")]; Bgst = [Buf("gst0"), Buf("gst1")]; Bgates_l = [Buf("gates_d%d" % i) for i in range(64)]
    wgv = wg_d.rearrange("(k p) n -> p k n", p=128)

    def load_wgs(c):
        wb_ = c % 2
        S.dma("pool", lambda e: e.dma_start(out=wgs[wb_][:], in_=wgv[:, :, c * 512:(c + 1) * 512]), writes=[Bwgs[wb_]])

    load_wgs(0)
    load_wgs(1)
    Bwuv = Buf("wuv")
    S.dma("sp", lambda e: e.dma_start(out=wuvs[:], in_=wuv_d), writes=[Bwuv])
    S.op("dve", lambda e: e.tensor_copy(out=wuvb[:], in_=wuvs[:]), reads=[Bwuv], writes=[Bwuv])

    def w1buf(col):
        for c, (c0, c1) in enumerate(w1blocks):
            if c0 <= col < c1:
                return Bw1[c]

    Bqlat = [Buf("qlat%d" % h) for h in range(8)]
    Bqidx = Buf("qidx"); Bqb = Buf("qb"); Bkidx = Buf("kidx"); Bkb = Buf("kb")
    Bckv = Buf("ckvT"); BkvW = Buf("kvW"); Bvaug = Buf("vaug"); Bwidx = Buf("widx"); Bcktok = Buf("ckvtok")

    fm_tiles = []
    for h in range(8):
        fm_tiles.append((lambda tb, h=h: qlatT[:, h, tb * 512:(tb + 1) * 512], ATT_SCALE, Bqlat[h]))
    for j in range(4):
        fm_tiles.append((lambda tb, j=j: qidxT[:, j, tb * 512:(tb + 1) * 512], 1.0, Bqidx))
    fm_tiles.append((lambda tb: kidxT[:, tb * 512:(tb + 1) * 512], 1.0, Bkidx))
    for j in range(4):
        fm_tiles.append((lambda tb, j=j: qbT[:, j, tb * 512:(tb + 1) * 512], 0.125, Bqb))
    fm_tiles.append((lambda tb: kbT[:, tb * 512:(tb + 1) * 512], 1.0, Bkb))
    ev = 0
    for ti, (dst, scale, bdst) in enumerate(fm_tiles):
        for tb in range(4):
            bank = ev % 4
            fns = []
            for k in range(8):
                fns.append(lambda e, k=k, ti=ti, tb=tb, bank=bank: e.matmul(
                    PS[bank][:], lhsT=w1b[:, k, ti * 128:(ti + 1) * 128], rhs=xT[:, k, tb * 512:(tb + 1) * 512],
                    start=(k == 0), stop=(k == 7)))
            S.group("pe", fns, reads=BxT + [w1buf(ti * 128)], writes=[Bps[bank]])
            if ev % 2 == 0:
                S.op("act", lambda e, dst=dst, tb=tb, bank=bank, scale=scale: e.activation(
                    out=dst(tb), in_=PS[bank][:], func=AF.Copy, scale=scale), reads=[Bps[bank]], writes=[bdst])
            else:
                S.op("dve", lambda e, dst=dst, tb=tb, bank=bank, scale=scale: e.tensor_scalar(
                    out=dst(tb), in0=PS[bank][:], scalar1=scale, scalar2=None, op0=ALU.mult),
                    reads=[Bps[bank]], writes=[bdst])
            ev += 1


    def gen_gates():
        gi_ = 0
        for c in range(4):
            wb_ = c % 2
            if c >= 1 and c + 1 < 4:
                load_wgs(c + 1)
            for j in range(4):
                for tb in range(4):
                    bank = gi_ % 4
                    sb_ = gi_ % 2
                    gi_ += 1
                    fns = [lambda e, k=k, j=j, tb=tb, bank=bank, wb_=wb_: e.matmul(
                        PS[bank][:], lhsT=wgs[wb_][:, k, j * 128:(j + 1) * 128], rhs=xT[:, k, tb * 512:(tb + 1) * 512],
                        start=(k == 0), stop=(k == 7)) for k in range(8)]
                    S.group("pe", fns, reads=BxT + [Bwgs[wb_]], writes=[Bps[bank]])
                    S.op("act", lambda e, bank=bank, sb_=sb_: e.activation(out=gst[:, sb_, :], in_=PS[bank][:], func=AF.Sigmoid),
                         reads=[Bps[bank]], writes=[Bgst[sb_]])
                    r0 = c * 512 + j * 128
                    S.dma("sp", lambda e, r0=r0, tb=tb, sb_=sb_: e.dma_start(
                        out=gates_d[r0:r0 + 128, tb * 512:(tb + 1) * 512], in_=gst[:, sb_, :]),
                        reads=[Bgst[sb_]], writes=[Bgates_l[(r0 // 128) * 4 + tb]])
                    yield

    S.op("pool", lambda e: e.memset(vaug[:], 1.0), writes=[Bvaug])
    S.op("pool", lambda e: e.memset(kvW[:], 1.0), writes=[BkvW])
    Bp1tmp = Buf("p1tmp"); Bp1junk = Buf("p1junk")
    S.op("pool", lambda e: e.memset(p1tmp[:, 127:128], -0.5), writes=[Bp1tmp])
    gg_ = gen_gates()
    for tt in range(NT):
        for _ in range(4):
            next(gg_, None)
        bank = 4 + (tt % 2)
        fns = []
        for k in range(8):
            fns.append(lambda e, k=k, tt=tt, bank=bank: e.matmul(
                PS[bank][:, 0:264], lhsT=xT[:, k, tt * 128:(tt + 1) * 128], rhs=w1b[:, k, 2304:2568],
                start=(k == 0), stop=(k == 7)))
        S.group("pe", fns, reads=BxT + [Bw1[5]], writes=[Bps[bank]])
        S.op("act", lambda e, tt=tt, bank=bank: e.activation(
            out=p1junk[:, 0:128], in_=PS[bank][:, 0:128], func=AF.Square, accum_out=p1tmp[:, tt:tt + 1]),
            reads=[Bps[bank]], writes=[Bp1junk, Bp1tmp])
        S.op("act", lambda e, tt=tt, bank=bank: e.activation(
            out=vaug[:, tt, :, 0:64], in_=PS[bank][:, 128:256].rearrange("p (g d) -> p g d", g=2), func=AF.Copy),
            reads=[Bps[bank]], writes=[Bvaug])
        S.op("act", lambda e, tt=tt, bank=bank: e.activation(
            out=widx[:, tt, :], in_=PS[bank][:, 256:264], func=AF.Copy), reads=[Bps[bank]], writes=[Bwidx])
        S.op("dve", lambda e, tt=tt: e.tensor_scalar(
            out=p1tmp[:, 16 + tt:17 + tt], in0=p1tmp[:, tt:tt + 1], scalar1=1.0 / 128.0, scalar2=RMS_EPS,
            op0=ALU.mult, op1=ALU.add), reads=[Bp1tmp], writes=[Bp1tmp])
        S.op("pool", lambda e, tt=tt: e.tensor_tensor(
            out=p1tmp[:, 32 + tt:33 + tt], in0=p1tmp[:, 16 + tt:17 + tt], in1=p1tmp[:, 127:128], op=ALU.pow),
            reads=[Bp1tmp], writes=[Bp1tmp])
        S.op("dve", lambda e, tt=tt, bank=bank: e.scalar_tensor_tensor(
            out=ckvtok[:, tt, :], in0=PS[bank][:, 0:128], scalar=p1tmp[:, 32 + tt:33 + tt], in1=gkv_bc[:],
            op0=ALU.mult, op1=ALU.mult), reads=[Bps[bank], Bp1tmp, Bconst], writes=[Bcktok])
        tb_ = 6 + (tt % 2)
        S.op("pe", lambda e, tt=tt, tb_=tb_: e.transpose(
            out=PS[tb_][:].bitcast(BF16)[:, 0:128], in_=ckvtok[:, tt, :], identity=ident[:]),
            reads=[Bcktok, Bconst], writes=[Bps[tb_]])
        S.op("dve", lambda e, tt=tt, tb_=tb_: e.tensor_copy(
            out=ckvT[:, tt * 128:(tt + 1) * 128], in_=PS[tb_][:].bitcast(BF16)[:, 0:128]),
            reads=[Bps[tb_]], writes=[Bckv])
        S.op("pe", lambda e, tt=tt, bank=bank: e.matmul(
            PS[bank][:], lhsT=ckvT[:, tt * 128:(tt + 1) * 128], rhs=wuvb[:], start=True, stop=True),
            reads=[Bckv, Bwuv], writes=[Bps[bank]])
        S.op("act", lambda e, tt=tt, bank=bank: e.activation(
            out=kvW[:, tt, :, 0:64], in_=PS[bank][:].rearrange("p (h d) -> p h d", h=8), func=AF.Copy),
            reads=[Bps[bank]], writes=[BkvW])

    for _ in gg_:
        pass
    if debug:
        S.dma("sp", lambda e: e.dma_start(out=dbg["d_ckvT"], in_=ckvT[:]), reads=[Bckv], writes=[Bout])
        S.dma("sp", lambda e: e.dma_start(out=dbg["d_qlatT"], in_=qlatT[:, 0, :]), reads=Bqlat, writes=[Bout])
    S.barrier()
    if stop_after <= 1:
        S.emit()
        return nc

    yaT = M("yaT", [128, 4, L], BF16, 179904, (2, 3))
    ybT = M("ybT", [128, 4, L], BF16, 179904 + 16384, (2, 3))
    Eswa = M("Eswa", [128, 4, 512], BF16, R3, (2, 2))
    ytokA = M("ytokA", [128, 2, 512], BF16, R3 + 4096, (2, 2))
    st2a = M("st2a", [128, 128], F32, R3 + 6144, (2, 2))
    Edsa = M("Edsa", [128, 2, 1024], BF16, R3 + 20480, (2, 2.5))
    eTb = M("eTb", [128, 4, 512], BF16, R3 + 32768, (2, 2.5))
    pTb = M("pTb", [128, 4, 512], BF16, R3 + 36864, (2, 2.5))
    scr = M("scr", [128, 1024], F32, R3 + 40960, (2, 2))
    BEswa = Buf("Eswa"); BEdsa = Buf("Edsa"); Bscr = Buf("scr")
    BeT = [Buf("eT%d" % i) for i in range(4)]
    BpT = [Buf("pT%d" % i) for i in range(4)]
    BytokA = [Buf("ytokA0"), Buf("ytokA1")]
    Bst2a = Buf("st2a")
    ByaT = Buf("yaT"); BybT = Buf("ybT")

    for idx in range(4):
        typ = idx // 2
        S.dma("sp", lambda e, idx=idx: e.dma_start(out=scr[:, 0:512], in_=swab_d[idx]), writes=[Bscr])
        S.op("act", lambda e, idx=idx: e.activation(out=Eswa[:, idx, :], in_=scr[:, 0:512], func=AF.Exp),
             reads=[Bscr], writes=[BEswa])
        for j in range(4):
            if typ == 1:
                S.op("pool", lambda e, idx=idx, j=j: e.tensor_tensor(
                    out=Eswa[:, idx, j * 128:(j + 1) * 128], in0=Eswa[:, idx, j * 128:(j + 1) * 128],
                    in1=causal01[:], op=ALU.mult), reads=[BEswa, Bconst], writes=[BEswa])
            else:
                S.op("pool", lambda e, idx=idx, j=j: e.affine_select(
                    out=Eswa[:, idx, j * 128:(j + 1) * 128], in_=Eswa[:, idx, j * 128:(j + 1) * 128],
                    pattern=[[-1, 128]], compare_op=ALU.is_ge, fill=preg(e, 0.0), base=-1, channel_multiplier=1),
                    reads=[BEswa], writes=[BEswa])
    for typ in range(2):
        S.dma("sp", lambda e, typ=typ: e.dma_start(out=scr[:], in_=dsab_d[typ]), writes=[Bscr])
        for h in range(8):
            S.op("act", lambda e, typ=typ, h=h: e.activation(
                out=Edsa[:, typ, h * 128:(h + 1) * 128], in_=scr[:, h * 128:(h + 1) * 128], func=AF.Exp,
                bias=negc31[:, h:h + 1], scale=1.0), reads=[Bscr, Bconst], writes=[BEdsa])
            if typ == 1:
                S.op("pool", lambda e, h=h: e.tensor_tensor(
                    out=Edsa[:, 1, h * 128:(h + 1) * 128], in0=Edsa[:, 1, h * 128:(h + 1) * 128],
                    in1=causal01[:], op=ALU.mult), reads=[BEdsa, Bconst], writes=[BEdsa])

    def ytok_to_T(nblk, ysrc, ybufB, dstT, bdst):
        pv = PS[7][:].bitcast(BF16)
        fns = [lambda e, c=c: e.transpose(out=pv[:, c * 128:(c + 1) * 128],
                                         in_=ysrc[:, c * 128:(c + 1) * 128], identity=ident[:])
               for c in range(4)]
        S.group("pe", fns, reads=[ybufB, Bconst], writes=[Bps[7]])
        S.op("act", lambda e: e.activation(
            out=dstT[:, :, nblk * 128:(nblk + 1) * 128], in_=pv[:, 0:512].rearrange("p (c t) -> p c t", c=4),
            func=AF.Copy), reads=[Bps[7]], writes=[bdst])

    items = []
    for n in range(NT):
        for g in range(2):
            chunks = ([(n - 1, 0)] if n > 0 else []) + [(n, 1)]
            for ci, (kb, typ) in enumerate(chunks):
                items.append((n, g, kb, typ, ci == 0, ci == len(chunks) - 1))

    def swa_stage1(idx):
        n, g, kb, typ, first, last = items[idx]
        r = idx % 4
        lb = idx % 3
        S.op("pe", lambda e: e.matmul(
            PS[lb][:], lhsT=kbT[g * 64:(g + 1) * 64, kb * 128:(kb + 1) * 128],
            rhs=qbT[g * 64:(g + 1) * 64, :, n * 128:(n + 1) * 128], start=True, stop=True),
            reads=[Bkb, Bqb], writes=[Bps[lb]])
        S.op("act", lambda e: e.activation(out=eTb[:, r, :], in_=PS[lb][:], func=AF.Exp),
             reads=[Bps[lb]], writes=[BeT[r]])
        S.op("dve", lambda e: e.tensor_tensor(
            out=pTb[:, r, :], in0=eTb[:, r, :], in1=Eswa[:, typ * 2 + g, :], op=ALU.mult),
            reads=[BeT[r], BEswa], writes=[BpT[r]])

    def swa_stage2(idx):
        n, g, kb, typ, first, last = items[idx]
        r = idx % 4
        ybuf = n % 2
        obank = 3 + g
        fns = [lambda e, j=j: e.matmul(
            PS[obank][:, j * 65:(j + 1) * 65], lhsT=pTb[:, r, j * 128:(j + 1) * 128],
            rhs=vaug[:, kb, g, :], start=(first and j == 0), stop=(last and j == 3),
            skip_group_check=True) for j in range(4)]
        S.group("pe", fns, reads=[BpT[r], Bvaug], writes=[Bps[obank]])
        if not last:
            return
        ov = PS[obank][:, 0:260].rearrange("p (j c) -> p j c", j=4)
        c0 = ybuf * 16 + g * 4
        S.op("dve", lambda e: e.tensor_tensor(
            out=st2a[:, c0:c0 + 4].rearrange("p (j o) -> p j o", o=1), in0=ov[:, :, 64:65],
            in1=esink[:, g * 4:(g + 1) * 4].rearrange("p (j o) -> p j o", o=1), op=ALU.add),
            reads=[Bps[obank], Bconst], writes=[Bst2a])
        S.op("dve", lambda e: e.reciprocal(out=st2a[:, 32 + c0:32 + c0 + 4], in_=st2a[:, c0:c0 + 4]),
             reads=[Bst2a], writes=[Bst2a])
        S.op("dve", lambda e: e.tensor_tensor(
            out=ytokA[:, ybuf, g * 256:(g + 1) * 256].rearrange("p (j d) -> p j d", j=4), in0=ov[:, :, 0:64],
            in1=st2a[:, 32 + c0:32 + c0 + 4].rearrange("p (j o) -> p j o", o=1).to_broadcast([128, 4, 64]),
            op=ALU.mult), reads=[Bps[obank], Bst2a], writes=[BytokA[ybuf]])
        if g == 1:
            ytok_to_T(n, ytokA[:, ybuf, :], BytokA[ybuf], ybT, BybT)

    LAG = 2
    for idx in range(len(items) + LAG):
        if idx < len(items):
            swa_stage1(idx)
        if idx >= LAG:
            swa_stage2(idx - LAG)
    if debug:
        S.dma("sp", lambda e: e.dma_start(out=dbg["d_yb"].rearrange("(c p) t -> p c t", p=128), in_=ybT[:]),
              reads=[BybT], writes=[Bout])
    S.barrier()

    QB = 83968
    scoresA = M("scoresA", [128, L], F32, R3, (2.5, 2.5))
    scoresB = M("scoresB", [128, L], F32, QB, (2.5, 2.5))
    scoresC = M("scoresC", [128, L], F32, 2048, (2.5, 2.5))
    scoresD = M("scoresD", [128, L], F32, 2048 + 8192, (2.5, 2.5))
    maskbA = M("maskbA", [128, L], BF16, R3 + 8192, (2.5, 2.5))
    maskbB = M("maskbB", [128, L], BF16, 2048 + 16384, (2.5, 2.5))
    osb = M("osb", [128, 2, 520], F32, 129280, (2.5, 2.5))
    maskT_lo = M("maskT_lo", [128, 2, NT, 128], BF16, R3 + 12288, (2.5, 2.5))
    maskT_hi = M("maskT_hi", [128, 2, NT, 128], BF16, 2048 + 20480, (2.5, 2.5))
    rbuf = M("rbuf", [128, 4, 512], BF16, R3 + 40960, (2.5, 2.5))
    dg = M("dg", [128, 2, 8, 128], BF16, QB + 8192, (2.5, 2.5))
    ytokB = M("ytokB", [128, 2, 512], BF16, QB + 12288, (2.5, 2.5))
    st2 = M("st2", [128, 4, 64], F32, QB + 14336, (2.5, 2.5))
    steps = M("steps", [128, 32], F32, QB + 15360, (2.5, 2.5))
    sd0 = M("sd0", [128, 4, 32], F32, QB + 15488, (2.5, 2.5))
    zt = M("zt", [128, D], BF16, R3 + 24576, (2.5, 2.5))
    Bzero = Buf("zero")
    Bzero_l = [Buf("zero%d" % i) for i in range(8)]
    S.op("pool", lambda e: e.memset(zt[:], 0.0), writes=[Bzero])
    xs_v = xs_d.rearrange("(j p) n -> p j n", p=128)
    for j0 in range(0, 64, 8):
        S.dma("sp", lambda e, j0=j0: e.dma_start(
            out=xs_v[:, j0:j0 + 8, :], in_=zt[:].rearrange("p (o n) -> p o n", o=1).to_broadcast([128, 8, D])),
            reads=[Bzero], writes=[Bzero_l[j0 // 8]])
    Bwbf_l = []
    bg_jobs = []
    for r0 in range(0, NEXP * 128, 512):
        for hh in range(2):
            bg_jobs.append((wgur_d[:, hh * 2048:(hh + 1) * 2048], wgub_d[:, hh * 2048:(hh + 1) * 2048], r0))
        bg_jobs.append((wdr_d, wdb_d, r0))

    def bg_convert(n=1):
        for _ in range(n):
            if not bg_jobs:
                return
            src_, dst_, r0 = bg_jobs.pop(0)
            bj = Buf("wbf%d" % len(Bwbf_l))
            Bwbf_l.append(bj)
            S.dma("pool", lambda e, src_=src_, dst_=dst_, r0=r0: e.dma_start(
                out=dst_[r0:r0 + 512, :], in_=src_[r0:r0 + 512, :]), writes=[bj])
    kblk0 = M("kblk0", [128, L], BF16, 104448, (2.5, 2.5))
    kblk1 = M("kblk1", [128, L], BF16, 2048 + 28672, (2.5, 2.5))
    kblk = [kblk0, kblk1]
    Bkblk = Buf("kblk")
    S.op("pool", lambda e: e.memset(kblk0[:], 0.0), writes=[Bkblk])
    S.op("pool", lambda e: e.memset(kblk1[:], 0.0), writes=[Bkblk])
    S.op("dve", lambda e: e.tensor_copy(out=kblk0[0:64, :], in_=kidxT[0:64, :]), reads=[Bkidx, Bkblk], writes=[Bkblk])
    S.op("dve", lambda e: e.tensor_copy(out=kblk1[64:128, :], in_=kidxT[64:128, :]), reads=[Bkidx, Bkblk], writes=[Bkblk])
    scoresX = [scoresA, scoresB, scoresC, scoresD]
    maskbX = [maskbA, maskbB]
    Bscore = [Buf("scores%d" % i) for i in range(4)]
    BmaskX = [Buf("mask0"), Buf("mask1")]; BmaskT = [Buf("maskT%d" % i) for i in range(4)]
    Brb = [Buf("rb%d" % i) for i in range(4)]
    Bdg = [Buf("dg0"), Buf("dg1")]
    BytokB = [Buf("ytokB0"), Buf("ytokB1")]
    Bbis = [Buf("bis%d" % i) for i in range(4)]
    Bst = [Buf("st%d" % i) for i in range(4)]
    Bosb = [Buf("osb0"), Buf("osb1")]
    Bsteps = Buf("steps")
    for k in range(N_BISECT):
        S.op("pool", lambda e, k=k: e.memset(steps[:, k:k + 1], 2.0 ** -(k + 1)), writes=[Bsteps])
    rotA = {"s1": 0, "rb": 0}

    def genS(i):
        Si = (i + 1) * 128
        sb = i % 2
        q4 = i % 4
        sc_t = scoresX[q4]
        for h in range(8):
            S.op("act", lambda e, h=h: e.activation(
                out=dg[:, sb, h, :], in_=ident[:], func=AF.Copy, scale=widx[:, i, h:h + 1]),
                reads=[Bconst, Bwidx], writes=[Bdg[sb]])
        yield
        nsc = (Si + 511) // 512
        stepsS = [(sc, h) for sc in range(nsc) for h in range(8)]
        slots = {}

        def S1(n):
            sc, h = stepsS[n]
            c0, c1 = sc * 512, min(Si, sc * 512 + 512)
            w = c1 - c0
            rr = rotA["rb"] % 4
            ab = 2 + (rotA["rb"] % 2)
            rotA["rb"] += 1
            slots[n] = rr
            hp = (h % 2) * 64
            S.op("pe", lambda e: e.matmul(
                PS[ab][:, 0:w], lhsT=qidxT[:, h // 2, i * 128:(i + 1) * 128],
                rhs=kblk[h % 2][:, c0:c1], start=True, stop=True),
                reads=[Bqidx, Bkblk], writes=[Bps[ab]])
            S.op("act", lambda e: e.activation(
                out=rbuf[:, rr, 0:w], in_=PS[ab][:, 0:w], func=AF.Relu), reads=[Bps[ab]], writes=[Brb[rr]])

        def S2(n):
            sc, h = stepsS[n]
            c0, c1 = sc * 512, min(Si, sc * 512 + 512)
            w = c1 - c0
            rr = slots[n]
            S.op("pe", lambda e: e.matmul(
                PS[6][:, 0:w], lhsT=dg[:, sb, h, :], rhs=rbuf[:, rr, 0:w], start=(h == 0), stop=(h == 7)),
                reads=[Bdg[sb], Brb[rr]], writes=[Bps[6]])
            if h == 7:
                S.op("act", lambda e: e.activation(out=sc_t[:, c0:c1], in_=PS[6][:, 0:w], func=AF.Copy),
                     reads=[Bps[6]], writes=[Bscore[q4]])

        S1(0)
        for n in range(len(stepsS)):
            if n + 1 < len(stepsS):
                S1(n + 1)
            S2(n)
            yield

    def genB(i):
        Si = (i + 1) * 128
        sb = i % 2
        q4 = i % 4
        sc_t = scoresX[q4]
        maskb = maskbX[sb]
        maskT = maskT_lo if q4 < 2 else maskT_hi
        Bmask = BmaskX[sb]
        stv = st2[:, q4, :]
        BB = [Bbis[q4]]
        S.op("dve", lambda e: e.tensor_reduce(out=stv[:, 0:1], in_=sc_t[:, 0:Si], axis=AX.X, op=ALU.max),
             reads=[Bscore[q4]], writes=BB)
        yield
        S.op("dve", lambda e: e.tensor_reduce(out=stv[:, 1:2], in_=sc_t[:, 0:Si], axis=AX.X, op=ALU.min),
             reads=[Bscore[q4]] + BB, writes=BB)
        yield
        S.op("dve", lambda e: e.scalar_tensor_tensor(
            out=stv[:, 2:3], in0=stv[:, 0:1], scalar=1.0, in1=stv[:, 1:2], op0=ALU.add, op1=ALU.subtract),
            reads=BB, writes=BB)
        S.op("dve", lambda e: e.tensor_scalar(
            out=sd0[:, q4, 0:N_BISECT], in0=steps[:, 0:N_BISECT], scalar1=stv[:, 2:3], scalar2=None, op0=ALU.mult),
            reads=BB + [Bsteps], writes=BB)
        S.op("dve", lambda e: e.tensor_tensor(out=stv[:, 3:4], in0=sd0[:, q4, 0:1], in1=stv[:, 1:2], op=ALU.add),
             reads=BB, writes=BB)
        S.op("pool", lambda e: e.affine_select(
            out=sc_t[:, i * 128:(i + 1) * 128], in_=sc_t[:, i * 128:(i + 1) * 128], pattern=[[-1, 128]],
            compare_op=ALU.is_ge, fill=preg(e, -1.0e30), base=0, channel_multiplier=1),
            reads=[Bscore[q4]] + BB, writes=[Bscore[q4]])
        yield
        for it in range(N_BISECT):
            S.op("dve", lambda e: e.tensor_scalar(
                out=maskb[:, 0:Si], in0=sc_t[:, 0:Si], scalar1=stv[:, 3:4], scalar2=None,
                op0=ALU.is_ge, op1=ALU.add, accum_out=stv[:, 4:5]),
                reads=[Bscore[q4]] + BB, writes=[Bmask] + BB)
            yield
            S.op("dve", lambda e: e.tensor_scalar(
                out=stv[:, 5:6], in0=stv[:, 4:5], scalar1=255.5, scalar2=0.5, op0=ALU.is_ge, op1=ALU.subtract),
                reads=BB, writes=BB)
            S.op("dve", lambda e, it=it: e.scalar_tensor_tensor(
                out=stv[:, 3:4], in0=stv[:, 5:6], scalar=sd0[:, q4, it:it + 1], in1=stv[:, 3:4],
                op0=ALU.mult, op1=ALU.add), reads=BB, writes=BB)
            yield
        S.op("dve", lambda e: e.scalar_tensor_tensor(
            out=stv[:, 6:7], in0=sd0[:, q4, N_BISECT - 1:N_BISECT], scalar=-0.5, in1=stv[:, 3:4],
            op0=ALU.mult, op1=ALU.add), reads=BB, writes=BB)
        S.op("dve", lambda e: e.tensor_scalar(
            out=maskb[:, 0:Si], in0=sc_t[:, 0:Si], scalar1=stv[:, 6:7], scalar2=None, op0=ALU.is_ge),
            reads=[Bscore[q4]] + BB, writes=[Bmask])
        yield
        pv = PS[7][:].bitcast(BF16)
        for q0 in range(0, i + 1, 4):
            q1 = min(i + 1, q0 + 4)
            fns = [lambda e, kb=kb, q0=q0: e.transpose(
                out=pv[:, (kb - q0) * 128:(kb - q0 + 1) * 128], in_=maskb[:, kb * 128:(kb + 1) * 128],
                identity=ident[:]) for kb in range(q0, q1)]
            S.group("pe", fns, reads=[Bmask, Bconst], writes=[Bps[7]])
            S.op("act", lambda e, q0=q0, q1=q1: e.activation(
                out=maskT[:, sb, q0:q1, :], in_=pv[:, 0:(q1 - q0) * 128].rearrange("p (c t) -> p c t", t=128),
                func=AF.Identity, scale=100.0, bias=-100.0), reads=[Bps[7]], writes=[BmaskT[q4]])
            yield

    def genA(i):
        sb = i % 2
        maskT = maskT_lo if (i % 4) < 2 else maskT_hi
        pairs = [(kb, hg) for kb in range(i + 1) for hg in range(2)]
        info = {}

        def stage1(pi):
            kb, hg = pairs[pi]
            near = kb >= i - 1
            typ = 1 if kb == i else 0
            r = rotA["s1"] % 4
            lb = rotA["s1"] % 2
            rotA["s1"] += 1
            info[pi] = r
            masked = i >= 2
            fns = [lambda e: e.matmul(
                PS[lb][:], lhsT=ckvT[:, kb * 128:(kb + 1) * 128],
                rhs=qlatT[:, hg * 4:(hg + 1) * 4, i * 128:(i + 1) * 128], start=True, stop=(not masked),
                skip_group_check=True)]
            rd = [Bckv] + Bqlat[hg * 4:(hg + 1) * 4]
            if masked:
                for j in range(4):
                    fns.append(lambda e, j=j: e.matmul(
                        PS[lb][:, j * 128:(j + 1) * 128], lhsT=ident[:], rhs=maskT[:, sb, kb, :], start=False,
                        stop=(j == 3), skip_group_check=True))
                rd = rd + [BmaskT[i % 4], Bconst]
            S.group("pe", fns, reads=rd, writes=[Bps[lb]])
            if near:
                S.op("act", lambda e: e.activation(out=eTb[:, r, :], in_=PS[lb][:], func=AF.Exp),
                     reads=[Bps[lb]], writes=[BeT[r]])
                S.op("pool", lambda e: e.tensor_tensor(out=pTb[:, r, :], in0=eTb[:, r, :],
                                                       in1=Edsa[:, typ, hg * 512:(hg + 1) * 512], op=ALU.mult),
                     reads=[BeT[r], BEdsa], writes=[BpT[r]])
            else:
                S.op("act", lambda e: e.activation(out=pTb[:, r, :], in_=PS[lb][:], func=AF.Exp),
                     reads=[Bps[lb]], writes=[BpT[r]])

        def stage2(pi):
            kb, hg = pairs[pi]
            r = info[pi]
            obank = 4 + hg
            fns = [lambda e, j=j: e.matmul(
                PS[obank][:, j * 65:(j + 1) * 65], lhsT=pTb[:, r, j * 128:(j + 1) * 128],
                rhs=kvW[:, kb, hg * 4 + j, :], start=(kb == 0 and j == 0), stop=(kb == i and j == 3),
                skip_group_check=True) for j in range(4)]
            S.group("pe", fns, reads=[BpT[r], BkvW], writes=[Bps[obank]])

        LAGA = 1
        for pi in range(len(pairs) + LAGA):
            if pi < len(pairs):
                stage1(pi)
            if pi >= LAGA:
                stage2(pi - LAGA)
            yield
        for hg in range(2):
            obank = 4 + hg
            S.op("act", lambda e, hg=hg, obank=obank: e.activation(
                out=osb[:, sb, hg * 260:(hg + 1) * 260], in_=PS[obank][:, 0:260], func=AF.Copy),
                reads=[Bps[obank]], writes=[Bosb[sb]])
        yield

        def finish():
            q4 = i % 4
            for hg in range(2):
                ov = osb[:, sb, hg * 260:(hg + 1) * 260].rearrange("p (j c) -> p j c", j=4)
                c0 = 32 + hg * 4
                S.op("dve", lambda e, ov=ov, c0=c0: e.reciprocal(
                    out=st2[:, q4, c0:c0 + 4].rearrange("p (j o) -> p j o", o=1), in_=ov[:, :, 64:65]),
                    reads=[Bosb[sb]], writes=[Bst[q4]])
                S.op("dve", lambda e, ov=ov, c0=c0, hg=hg: e.tensor_tensor(
                    out=ytokB[:, sb, hg * 256:(hg + 1) * 256].rearrange("p (j d) -> p j d", j=4), in0=ov[:, :, 0:64],
                    in1=st2[:, q4, c0:c0 + 4].rearrange("p (j o) -> p j o", o=1).to_broadcast([128, 4, 64]),
                    op=ALU.mult), reads=[Bosb[sb], Bst[q4]], writes=[BytokB[sb]])
            ytok_to_T(i, ytokB[:, sb, :], BytokB[sb], yaT, ByaT)
        post_round.append(finish)

    tick = {"n": 0}

    def interleave(gens):
        gens = [g for g in gens if g is not None]
        while gens:
            tick["n"] += 1
            if tick["n"] % 14 == 0:
                bg_convert(1)
            for g in list(gens):
                try:
                    next(g)
                except StopIteration:
                    gens.remove(g)

    import itertools
    Bjunk = Buf("junk")

    def warm_pe(n):
        fns = [lambda e: e.matmul(PS[7][:, 256:512], lhsT=ident[:], rhs=Edsa[:, 0, 0:256], start=True, stop=True,
                                  skip_group_check=True) for _ in range(n)]
        S.group("pe", fns, reads=[BEdsa, Bconst], writes=[Bjunk])

    post_round = []
    NP = NT // 2
    for rnd in range(0, NP + 2):
        gs = []
        if 1 <= rnd + 1 < NP:
            gs.append(itertools.chain(genS(2 * rnd + 2), genS(2 * rnd + 3)))
        if 1 <= rnd < NP:
            gs.append(genB(2 * rnd))
            gs.append(genB(2 * rnd + 1))
        if 0 <= rnd - 1 < NP:
            gs.append(itertools.chain(genA(2 * rnd - 2), genA(2 * rnd - 1)))
        interleave(gs)
        for f_ in post_round:
            f_()
        del post_round[:]

    bg_convert(len(bg_jobs))
    if debug:
        S.dma("sp", lambda e: e.dma_start(out=dbg["d_ya"].rearrange("(c p) t -> p c t", p=128), in_=yaT[:]),
              reads=[ByaT], writes=[Bout])
    S.barrier()
    if stop_after <= 2:
        S.emit()
        return nc

    wab = M("wab", [128, 4, D], BF16, 34816, (3, 3))
    wbb = M("wbb", [128, 4, D], BF16, 43008, (3, 3))
    mergedT = M("mergedT", [128, 8, L], BF16, 83968, (3, 4))
    sgate = M("sgate", [128, 6, 512], BF16, 51200, (3, 3))
    mtmp = M("mtmp", [128, 2, 2, 512], F32, 57344, (3, 3))
    Bwa = Buf("wa"); Bwb = Buf("wb")
    S.dma("pool", lambda e: e.dma_start(out=wab[:], in_=wa_d.rearrange("(k p) n -> p k n", p=128)), writes=[Bwa])
    S.dma("pool", lambda e: e.dma_start(out=wbb[:], in_=wb_d.rearrange("(k p) n -> p k n", p=128)), writes=[Bwb])
    woutb = M("woutb", [128, 8, D], BF16, 67584, (3, 4))
    ln1g = M("ln1g", [128, D], F32, 116736, (3, 4))
    ln1bA = M("ln1bA", [128, D], F32, 120832, (3, 4))
    Bwout = Buf("wout"); Bln1 = Buf("ln1")
    wov = wo_d.rearrange("(k p) n -> p k n", p=128)

    def prefetch_p3b():
        S.dma("pool", lambda e: e.dma_start(out=woutb[:, 0:4, :], in_=wov[:, 0:4, :]), writes=[Bwout])
        S.dma("pool", lambda e: e.dma_start(out=woutb[:, 4:8, :], in_=wov[:, 4:8, :]), writes=[Bwout])
        S.dma("sp", lambda e: e.dma_start(out=ln1g[:], in_=ln1g_d.to_broadcast([128, D])), writes=[Bln1])
        S.dma("sp", lambda e: e.dma_start(out=ln1bA[:], in_=ln1b_d.to_broadcast([128, D])), writes=[Bln1])
        S.op("act", lambda e: e.activation(out=ln1bA[:], in_=ln1bA[:], func=AF.Copy, scale=ALPHA),
             reads=[Bln1], writes=[Bln1])

    Bsg = [Buf("sg%d" % i) for i in range(6)]
    Bmt = [Buf("mt0"), Buf("mt1")]
    Bmerged = [Buf("merged%d" % f) for f in range(8)]

    def p3a_load(n):
        f, tb = n // 4, n % 4
        for gi in range(2):
            sl = (n % 3) * 2 + gi
            r0 = gi * 1024 + f * 128
            S.dma("sp", lambda e, r0=r0, tb=tb, sl=sl: e.dma_start(
                out=sgate[:, sl, :], in_=gates_d[r0:r0 + 128, tb * 512:(tb + 1) * 512]),
                reads=[Bgates_l[(r0 // 128) * 4 + tb]], writes=[Bsg[sl]])

    p3a_load(0)
    p3a_load(1)
    it3 = 0
    for n in range(32):
        f, tb = n // 4, n % 4
        ts_ = slice(tb * 512, (tb + 1) * 512)
        if n + 2 < 32:
            p3a_load(n + 2)
        pb = (n % 2) * 2
        mi = n % 2
        for bi, (wt, yT, bw, by) in enumerate(((wab, yaT, Bwa, ByaT), (wbb, ybT, Bwb, BybT))):
            fns = [lambda e, k=k, wt=wt, yT=yT, f=f, ts_=ts_, pb=pb, bi=bi: e.matmul(
                PS[pb + bi][:], lhsT=wt[:, k, f * 128:(f + 1) * 128], rhs=yT[:, k, ts_],
                start=(k == 0), stop=(k == 3)) for k in range(4)]
            S.group("pe", fns, reads=[bw, by], writes=[Bps[pb + bi]])
            sl = (n % 3) * 2 + bi
            S.op("dve", lambda e, pb=pb, bi=bi, sl=sl, mi=mi: e.tensor_tensor(
                out=mtmp[:, mi, bi, :], in0=sgate[:, sl, :], in1=PS[pb + bi][:], op=ALU.mult),
                reads=[Bsg[sl], Bps[pb + bi]], writes=[Bmt[mi]])
        S.op("pool", lambda e, mi=mi, f=f, ts_=ts_: e.tensor_tensor(
            out=mergedT[:, f, ts_], in0=mtmp[:, mi, 0, :], in1=mtmp[:, mi, 1, :], op=ALU.add),
            reads=[Bmt[mi]], writes=[Bmerged[f]])
        if n == 2:
            prefetch_p3b()
    if debug:
        S.dma("sp", lambda e: e.dma_start(out=dbg["d_mergedT"].rearrange("(c p) t -> p c t", p=128), in_=mergedT[:]),
              reads=Bmerged, writes=[Bout])
    S.barrier()
    if stop_after <= 3:
        S.emit()
        return nc

    ACC_OFF = 147136
    acc = M("acc", [128, NT, D], F32, ACC_OFF, (4, 7))
    x1T = M("x1T", [128, 8, L], BF16, 2048, (4, 5))
    x1tok = M("x1tok", [128, NT, D], BF16, 34816, (4, 6))
    xtok = M("xtok", [128, 2, D], F32, 124928, (4, 4))
    rres = M("rres", [128, 3, D], F32, 133120, (4, 4))
    lnst = M("lnst", [128, 3, 32], F32, 145408, (4, 4))
    Bxtok = [Buf("xtok0"), Buf("xtok1")]
    Brres = [Buf("r%d" % i) for i in range(3)]
    Blnst = [Buf("lnst%d" % i) for i in range(3)]
    Bacc = [Buf("acc%d" % t) for t in range(NT)]
    Bx1tok = [Buf("x1tok%d" % t) for t in range(NT)]
    Bx1T = Buf("x1T")

    def ln_scaled(src, srcB, stv, stB, gt, btA, gB, dst, dstB, alpha=ALPHA):
        S.op("dve", lambda e: e.bn_stats(out=stv[:, 0:6], in_=src[:, 0:512]), reads=srcB, writes=[stB])
        S.op("dve", lambda e: e.bn_stats(out=stv[:, 6:12], in_=src[:, 512:1024]), reads=srcB + [stB], writes=[stB])
        S.op("dve", lambda e: e.bn_aggr(out=stv[:, 12:14], in_=stv[:, 0:12]), reads=[stB], writes=[stB])
        S.op("dve", lambda e: e.tensor_scalar(out=stv[:, 14:15], in0=stv[:, 13:14], scalar1=LN_EPS, scalar2=None,
                                              op0=ALU.add), reads=[stB], writes=[stB])
        S.op("act", lambda e: e.activation(out=stv[:, 14:15], in_=stv[:, 14:15], func=AF.Sqrt), reads=[stB], writes=[stB])
        S.op("dve", lambda e: e.reciprocal(out=stv[:, 15:16], in_=stv[:, 14:15]), reads=[stB], writes=[stB])
        S.op("dve", lambda e: e.tensor_scalar(out=stv[:, 16:17], in0=stv[:, 15:16], scalar1=alpha, scalar2=None,
                                              op0=ALU.mult), reads=[stB], writes=[stB])
        S.op("dve", lambda e: e.scalar_tensor_tensor(out=src, in0=src, scalar=stv[:, 12:13], in1=gt[:],
                                                     op0=ALU.subtract, op1=ALU.mult), reads=srcB + [stB, gB], writes=srcB)
        S.op("dve", lambda e: e.scalar_tensor_tensor(out=dst, in0=src, scalar=stv[:, 16:17], in1=btA[:],
                                                     op0=ALU.mult, op1=ALU.add), reads=srcB + [stB, gB], writes=dstB)

    def p3b_A(tt):
        b2, b3 = tt % 2, tt % 3
        S.dma("sp", lambda e: e.dma_start(out=xtok[:, b2, :], in_=x_d[tt * 128:(tt + 1) * 128, :]), writes=[Bxtok[b2]])
        for half in range(2):
            bank = b3 * 2 + half
            fns = [lambda e, k=k, half=half, bank=bank: e.matmul(
                PS[bank][:], lhsT=mergedT[:, k, tt * 128:(tt + 1) * 128], rhs=woutb[:, k, half * 512:(half + 1) * 512],
                start=(k == 0), stop=(k == 7)) for k in range(8)]
            S.group("pe", fns, reads=Bmerged + [Bwout], writes=[Bps[bank]])
            S.op("dve", lambda e, half=half, bank=bank: e.scalar_tensor_tensor(
                out=rres[:, b3, half * 512:(half + 1) * 512], in0=xtok[:, b2, half * 512:(half + 1) * 512], scalar=ALPHA,
                in1=PS[bank][:], op0=ALU.mult, op1=ALU.add), reads=[Bxtok[b2], Bps[bank]], writes=[Brres[b3]])

    def p3b_B(tt):
        b3 = tt % 3
        ln_scaled(rres[:, b3, :], [Brres[b3]], lnst[:, b3, :], Blnst[b3], ln1g, ln1bA, Bln1, acc[:, tt, :], [Bacc[tt]])
        S.op("act", lambda e: e.activation(out=x1tok[:, tt, :], in_=acc[:, tt, :], func=AF.Copy, scale=1.0 / ALPHA),
             reads=[Bacc[tt]], writes=[Bx1tok[tt]])

    def p3b_C(tt):
        for half in range(2):
            tbk = 6 + half
            pv = PS[tbk][:].bitcast(BF16)
            fns = [lambda e, c=c, half=half, pv=pv: e.transpose(
                out=pv[:, c * 128:(c + 1) * 128], in_=x1tok[:, tt, (half * 4 + c) * 128:(half * 4 + c + 1) * 128],
                identity=ident[:]) for c in range(4)]
            S.group("pe", fns, reads=[Bx1tok[tt], Bconst], writes=[Bps[tbk]])
            S.op("act", lambda e, half=half, pv=pv: e.activation(
                out=x1T[:, half * 4:(half + 1) * 4, tt * 128:(tt + 1) * 128],
                in_=pv[:, 0:512].rearrange("p (c t) -> p c t", c=4), func=AF.Copy), reads=[Bps[tbk]], writes=[Bx1T])

    for s_ in range(NT + 2):
        if s_ < NT:
            p3b_A(s_)
        if 1 <= s_ <= NT:
            p3b_B(s_ - 1)
        if s_ >= 2:
            p3b_C(s_ - 2)
    if debug:
        S.dma("sp", lambda e: e.dma_start(out=dbg["d_acc"].rearrange("(t p) d -> p t d", p=128), in_=acc[:]),
              reads=Bacc, writes=[Bout])
    S.barrier()
    if stop_after <= 4:
        S.emit()
        return nc

    bg_convert(len(bg_jobs))
    o = 67584
    wrb = M("wrb", [128, 8, 36], BF16, o, (5, 5)); o += 1024
    brbc = M("brbc", [128, 36], F32, o, (5, 5)); o += 256
    ustr = M("ustr", [128, 128], BF16, o, (5, 5)); o += 256
    onesb = M("onesb", [128, 128], BF16, o, (5, 5)); o += 256
    jrow = M("jrow", [128, 64], F32, o, (5, 5)); o += 256
    thr16 = M("thr16", [128, 16], F32, o, (5, 5)); o += 64
    piota = M("piota", [128, 2], F32, o, (5, 5)); o += 64
    lg = M("lg", [128, NT, 36], F32, o, (5, 5)); o += 2304
    elm = M("elm", [128, NT, 32], F32, o, (5, 5)); o += 2048
    exr = M("exr", [128, NT, 32], F32, o, (5, 5)); o += 2048
    sel = M("sel", [128, NT, 32], F32, o, (5, 5)); o += 2048
    selb = M("selb", [128, NT, 32], BF16, o, (5, 5)); o += 1024
    comb = M("comb", [128, NT, 32], F32, o, (5, 5)); o += 2048
    posf = M("posf", [128, NT, 32], F32, o, (5, 5)); o += 2048
    tmpa = M("tmpa", [128, NT, 32], F32, o, (5, 5)); o += 2048
    tmpb = M("tmpb", [128, 64, 32], F32, o, (5, 5)); o += 8192
    sm = M("sm", [128, 24, NT], F32, o, (5, 5)); o += 1536
    pen = M("pen", [128, NT, 4], F32, o, (5, 5)); o += 256
    gex = M("gex", [128, NT, 4], F32, o, (5, 5)); o += 256
    ntr = M("ntr", [128, 128], BF16, o, (5, 5)); o += 256
    cnt32 = M("cnt32", [128, 24], F32, o, (5, 5)); o += 96
    toffs = M("toffs", [128, 64], F32, o, (5, 5)); o += 256
    ejf = M("ejf", [128, 64], F32, o, (5, 5)); o += 256
    assert o < 116736
    posu = M("posu", [128, NT, 2], mybir.dt.uint32, 100352, (5, 7))
    wsl = M("wsl", [128, NT, 2], F32, 100480, (5, 7))
    widxu = M("widxu", [128, 64], mybir.dt.uint32, 100608, (5, 7))
    Bwr = Buf("wr"); Bc5 = Buf("c5"); BR = Buf("route"); Bpos = Buf("posu")
    S.dma("pool", lambda e: e.dma_start(out=wrb[:], in_=wr_d.rearrange("(k p) n -> p k n", p=128)), writes=[Bwr])
    S.dma("sp", lambda e: e.dma_start(out=brbc[:], in_=br_d.to_broadcast([128, 36])), writes=[Bwr])
    S.op("pool", lambda e: e.memset(ustr[:], 1.0), writes=[Bc5])
    S.op("pool", lambda e: e.affine_select(out=ustr[:], in_=ustr[:], pattern=[[1, 128]], compare_op=ALU.is_ge,
                                           fill=preg(e, 0.0), base=-1, channel_multiplier=-1), reads=[Bc5], writes=[Bc5])
    S.op("pool", lambda e: e.memset(onesb[:], 1.0), writes=[Bc5])
    S.op("pool", lambda e: e.iota(jrow[:], pattern=[[1, 64]], base=0, channel_multiplier=0,
                                  allow_small_or_imprecise_dtypes=True), writes=[Bc5])
    S.op("pool", lambda e: e.iota(thr16[:], pattern=[[128, 16]], base=0, channel_multiplier=0,
                                  allow_small_or_imprecise_dtypes=True), writes=[Bc5])
    S.op("pool", lambda e: e.iota(piota[:], pattern=[[0, 2]], base=0, channel_multiplier=1,
                                  allow_small_or_imprecise_dtypes=True), writes=[Bc5])

    def bc(ap, shape):
        return ap.to_broadcast(shape)

    for tt in range(NT):
        bank = tt // 8
        fns = [lambda e, k=k, tt=tt, bank=bank: e.matmul(
            PS[bank][:, (tt % 8) * 36:(tt % 8 + 1) * 36], lhsT=x1T[:, k, tt * 128:(tt + 1) * 128], rhs=wrb[:, k, :],
            start=(k == 0), stop=(k == 7), skip_group_check=True) for k in range(8)]
        S.group("pe", fns, reads=[Bx1T, Bwr], writes=[Bps[bank]])
    for bank in range(2):
        S.op("dve", lambda e, bank=bank: e.tensor_tensor(
            out=lg[:, bank * 8:(bank + 1) * 8, :], in0=PS[bank][:, 0:288].rearrange("p (t c) -> p t c", c=36),
            in1=bc(brbc[:].rearrange("p (o c) -> p o c", o=1), [128, 8, 36]), op=ALU.add),
            reads=[Bps[bank], Bwr], writes=[BR])
    RB = [BR]
    def smv(r):
        return sm[:, r, :]

    def sm3(r, n):
        return bc(sm[:, r, :].rearrange("p (t o) -> p t o", o=1), [128, NT, n])

    S.op("dve", lambda e: e.tensor_reduce(out=smv(0), in_=lg[:, :, 0:4], axis=AX.X, op=ALU.max), reads=RB, writes=RB)
    S.op("dve", lambda e: e.tensor_tensor(out=pen[:], in0=lg[:, :, 0:4], in1=sm3(0, 4), op=ALU.is_lt), reads=RB, writes=RB)
    S.op("dve", lambda e: e.tensor_tensor(out=gex[:], in0=lg[:, :, 0:4], in1=sm3(0, 4), op=ALU.subtract), reads=RB, writes=RB)
    S.op("act", lambda e: e.activation(out=gex[:], in_=gex[:], func=AF.Exp), reads=RB, writes=RB)
    S.op("dve", lambda e: e.tensor_reduce(out=smv(1), in_=gex[:], axis=AX.X, op=ALU.add), reads=RB, writes=RB)
    S.op("dve", lambda e: e.tensor_scalar(out=pen[:], in0=pen[:], scalar1=NEG, scalar2=None, op0=ALU.mult), reads=RB, writes=RB)
    S.op("dve", lambda e: e.tensor_tensor(
        out=elm[:].rearrange("p t (g j) -> p t g j", g=4), in0=lg[:, :, 4:36].rearrange("p t (g j) -> p t g j", g=4),
        in1=bc(pen[:].rearrange("p t (g o) -> p t g o", o=1), [128, NT, 4, 8]), op=ALU.add), reads=RB, writes=RB)
    S.op("dve", lambda e: e.tensor_reduce(out=smv(2), in_=elm[:], axis=AX.X, op=ALU.max), reads=RB, writes=RB)
    S.op("dve", lambda e: e.tensor_tensor(out=tmpa[:], in0=elm[:], in1=sm3(2, 32), op=ALU.is_ge), reads=RB, writes=RB)
    S.op("dve", lambda e: e.scalar_tensor_tensor(out=tmpa[:], in0=tmpa[:], scalar=NEG, in1=elm[:], op0=ALU.mult, op1=ALU.add),
         reads=RB, writes=RB)
    S.op("dve", lambda e: e.tensor_reduce(out=smv(3), in_=tmpa[:], axis=AX.X, op=ALU.max), reads=RB, writes=RB)
    S.op("dve", lambda e: e.tensor_tensor(out=exr[:], in0=elm[:], in1=sm3(2, 32), op=ALU.subtract), reads=RB, writes=RB)
    S.op("act", lambda e: e.activation(out=exr[:], in_=exr[:], func=AF.Exp), reads=RB, writes=RB)
    S.op("dve", lambda e: e.tensor_tensor(out=sel[:], in0=elm[:], in1=sm3(3, 32), op=ALU.is_ge), reads=RB, writes=RB)
    S.op("dve", lambda e: e.tensor_copy(out=selb[:], in_=sel[:]), reads=RB, writes=RB)
    S.op("dve", lambda e: e.tensor_tensor(out=comb[:], in0=sel[:], in1=exr[:], op=ALU.mult), reads=RB, writes=RB)
    S.op("dve", lambda e: e.tensor_reduce(out=smv(4), in_=comb[:], axis=AX.X, op=ALU.add), reads=RB, writes=RB)
    S.op("dve", lambda e: e.tensor_tensor(out=smv(5), in0=smv(4), in1=smv(1), op=ALU.mult), reads=RB, writes=RB)
    S.op("dve", lambda e: e.reciprocal(out=smv(6), in_=smv(5)), reads=RB, writes=RB)
    S.op("dve", lambda e: e.tensor_tensor(out=comb[:], in0=comb[:], in1=sm3(6, 32), op=ALU.mult), reads=RB, writes=RB)
    if debug:
        S.dma("sp", lambda e: e.dma_start(out=dbg["d_comb"].rearrange("(t p) d -> p t d", p=128), in_=comb[:]),
              reads=RB, writes=[Bout])
    for tt in range(NT):
        fns = []
        for tp in range(tt):
            fns.append(lambda e, tt=tt, tp=tp: e.matmul(
                PS[2][:, tt * 32:(tt + 1) * 32], lhsT=onesb[:], rhs=selb[:, tp, :], start=(tp == 0), stop=False,
                skip_group_check=True))
        fns.append(lambda e, tt=tt: e.matmul(
            PS[2][:, tt * 32:(tt + 1) * 32], lhsT=ustr[:], rhs=selb[:, tt, :], start=(tt == 0), stop=True,
            skip_group_check=True))
        S.group("pe", fns, reads=RB + [Bc5], writes=[Bps[2]])
    fns = [lambda e, tt=tt: e.matmul(PS[3][0:32, 0:1], lhsT=selb[:, tt, :], rhs=onesb[:, 0:1],
                                     start=(tt == 0), stop=(tt == NT - 1)) for tt in range(NT)]
    S.group("pe", fns, reads=RB + [Bc5], writes=[Bps[3]])
    S.op("dve", lambda e: e.tensor_copy(out=cnt32[0:32, 0:1], in_=PS[3][0:32, 0:1]), reads=[Bps[3]], writes=RB)
    S.op("dve", lambda e: e.tensor_scalar(out=cnt32[0:32, 4:20], in0=thr16[0:32, :], scalar1=cnt32[0:32, 0:1], scalar2=None,
                                          op0=ALU.is_lt), reads=RB + [Bc5], writes=RB)
    S.op("dve", lambda e: e.tensor_reduce(out=cnt32[0:32, 1:2], in_=cnt32[0:32, 4:20], axis=AX.X, op=ALU.add), reads=RB, writes=RB)
    S.op("dve", lambda e: e.tensor_scalar(out=ntr[0:32, :], in0=onesb[0:32, :], scalar1=cnt32[0:32, 1:2], scalar2=None,
                                          op0=ALU.mult), reads=RB + [Bc5], writes=RB)
    S.op("pe", lambda e: e.matmul(PS[3][:, 64:96], lhsT=ntr[0:32, :], rhs=ustr[0:32, 0:32], start=True, stop=True),
         reads=RB + [Bc5], writes=[Bps[3]])
    S.op("pe", lambda e: e.matmul(PS[3][:, 96:128], lhsT=ntr[0:32, :], rhs=causal01[0:32, 0:32], start=True, stop=True),
         reads=RB + [Bconst], writes=[Bps[3]])
    S.op("dve", lambda e: e.tensor_copy(out=toffs[:], in_=PS[3][:, 64:128]), reads=[Bps[3]], writes=RB)
    S.op("dve", lambda e: e.scalar_tensor_tensor(
        out=posf[:], in0=bc(toffs[:, 0:32].rearrange("p (o c) -> p o c", o=1), [128, NT, 32]), scalar=128.0,
        in1=PS[2][:].rearrange("p (t c) -> p t c", c=32), op0=ALU.mult, op1=ALU.add), reads=RB + [Bps[2]], writes=RB)
    S.op("dve", lambda e: e.tensor_tensor(out=tmpa[:], in0=sel[:], in1=posf[:], op=ALU.mult), reads=RB, writes=RB)
    S.op("dve", lambda e: e.tensor_reduce(out=smv(8), in_=tmpa[:], axis=AX.X, op=ALU.max), reads=RB, writes=RB)
    S.op("dve", lambda e: e.tensor_scalar(out=tmpa[:], in0=sel[:], scalar1=-1.0e6, scalar2=1.0e6, op0=ALU.mult, op1=ALU.add),
         reads=RB, writes=RB)
    S.op("dve", lambda e: e.tensor_tensor(out=tmpa[:], in0=tmpa[:], in1=posf[:], op=ALU.add), reads=RB, writes=RB)
    S.op("dve", lambda e: e.tensor_reduce(out=smv(7), in_=tmpa[:], axis=AX.X, op=ALU.min), reads=RB, writes=RB)
    for r_pos, r_w in ((7, 9), (8, 10)):
        S.op("dve", lambda e, r_pos=r_pos: e.tensor_tensor(out=tmpa[:], in0=posf[:], in1=sm3(r_pos, 32), op=ALU.is_equal),
             reads=RB, writes=RB)
        S.op("dve", lambda e: e.tensor_tensor(out=tmpa[:], in0=tmpa[:], in1=comb[:], op=ALU.mult), reads=RB, writes=RB)
        S.op("dve", lambda e, r_w=r_w: e.tensor_reduce(out=smv(r_w), in_=tmpa[:], axis=AX.X, op=ALU.add), reads=RB, writes=RB)
    for slot in range(2):
        S.op("dve", lambda e, slot=slot: e.tensor_copy(out=posu[:, :, slot], in_=smv(7 + slot)), reads=RB, writes=[Bpos])
        S.op("dve", lambda e, slot=slot: e.tensor_copy(out=wsl[:, :, slot], in_=smv(9 + slot)), reads=RB, writes=[Bpos])
    S.op("dve", lambda e: e.tensor_tensor(
        out=tmpb[:], in0=bc(toffs[:, 32:64].rearrange("p (o c) -> p o c", o=1), [128, 64, 32]),
        in1=bc(jrow[:].rearrange("p (j o) -> p j o", o=1), [128, 64, 32]), op=ALU.is_le), reads=RB + [Bc5], writes=RB)
    S.op("dve", lambda e: e.tensor_reduce(out=ejf[:], in_=tmpb[:], axis=AX.X, op=ALU.add), reads=RB, writes=RB)
    S.op("dve", lambda e: e.scalar_tensor_tensor(
        out=ejf[:], in0=ejf[:], scalar=128.0, in1=bc(piota[:, 0:1], [128, 64]), op0=ALU.mult, op1=ALU.add),
        reads=RB + [Bc5], writes=RB)
    S.op("dve", lambda e: e.tensor_copy(out=widxu[:], in_=ejf[:]), reads=RB, writes=[Bpos])
    S.barrier()
    if stop_after <= 5:
        S.emit()
        return nc

    NTL = 64
    NB = 5
    NX = 6
    wgut = [M("wgut%d" % b, [128, 4096], BF16, 100864 + b * 8192, (6, 6)) for b in range(NB)]
    wdt = [M("wdt%d" % b, [128, 2048], BF16, 2048 + b * 4096, (6, 6)) for b in range(NB)]
    xst = M("xst", [128, NX, D], BF16, 22528, (6, 6))
    xsT = M("xsT", [128, 2, 8, 128], BF16, 71680, (6, 6))
    sgt = M("sgt", [128, 2, 256], BF16, 75776, (6, 6))
    htok = M("htok", [128, 2, 256], BF16, 76800, (6, 6))
    ysb = M("ysb", [128, 2, D], F32, 77824, (6, 6))
    hTt = M("hTt", [128, 2, 256], BF16, 86016, (6, 6))
    Bwt = [[Buf("wt%d_%d" % (m, b)) for b in range(NB)] for m in range(2)]
    Bxst = [Buf("xst%d" % i) for i in range(NX)]; BxsT = [Buf("xsT0"), Buf("xsT1")]
    Bsgt = [Buf("sgt0"), Buf("sgt1")]; BhTt = [Buf("hTt0"), Buf("hTt1")]; Bysb = [Buf("ysb0"), Buf("ysb1")]
    Bhtok = [Buf("htok0"), Buf("htok1")]
    Bxs_l = [Buf("xs_d%d" % i) for i in range(2 * NT)]; Bys = Buf("ys_d")
    for tt in range(NT):
        for slot in range(2):
            S.dma("pool", lambda e, tt=tt, slot=slot: e.indirect_dma_start(
                out=xs_d, out_offset=bass.IndirectOffsetOnAxis(ap=posu[:, tt, slot:slot + 1], axis=0),
                in_=x1tok[:, tt, :], in_offset=None), reads=[Bpos, Bx1tok[tt]] + Bzero_l, writes=[Bxs_l[tt * 2 + slot]])

    def moe_load_w(j):
        b = j % NB
        for m, (wd_, wt_) in enumerate(((wgub_d, wgut), (wdb_d, wdt))):
            S.dma("pool", lambda e, wd_=wd_, wt_=wt_: e.indirect_dma_start(
                out=wt_[b][:], out_offset=None, in_=wd_,
                in_offset=bass.IndirectOffsetOnAxis(ap=widxu[:, j:j + 1], axis=0), bounds_check=preg(e, NEXP * 128 - 1),
                oob_is_err=False), reads=[Bpos] + Bwbf_l, writes=[Bwt[m][b]])

    def moe_load_x(j):
        bx = j % NX
        S.dma("sp", lambda e: e.dma_start(out=xst[:, bx, :], in_=xs_d[j * 128:(j + 1) * 128, :]), reads=Bxs_l, writes=[Bxst[bx]])

    def moe_T(j):
        bx, b2 = j % NX, j % 2
        pv = PS[0][:].bitcast(BF16)
        fns = [lambda e, c=c: e.transpose(out=pv[:, c * 128:(c + 1) * 128], in_=xst[:, bx, c * 128:(c + 1) * 128],
                                         identity=ident[:]) for c in range(8)]
        S.group("pe", fns, reads=[Bxst[bx], Bconst], writes=[Bps[0]])
        S.op("act", lambda e: e.activation(out=xsT[:, b2, :, :], in_=pv.rearrange("p (c t) -> p c t", c=8), func=AF.Copy),
             reads=[Bps[0]], writes=[BxsT[b2]])

    def moe_GU(j):
        b2, b = j % 2, j % NB
        bank = 1 + b2
        fns = [lambda e, k=k: e.matmul(PS[bank][:], lhsT=xsT[:, b2, k, :], rhs=wgut[b][:, k * 512:(k + 1) * 512],
                                       start=(k == 0), stop=(k == 7)) for k in range(8)]
        S.group("pe", fns, reads=[BxsT[b2], Bwt[0][b]], writes=[Bps[bank]])
        S.op("act", lambda e: e.activation(out=sgt[:, b2, :], in_=PS[bank][:, 0:256], func=AF.Silu),
             reads=[Bps[bank]], writes=[Bsgt[b2]])
        S.op("dve", lambda e: e.tensor_tensor(out=htok[:, b2, :], in0=sgt[:, b2, :], in1=PS[bank][:, 256:512], op=ALU.mult),
             reads=[Bsgt[b2], Bps[bank]], writes=[Bhtok[b2]])

    def moe_HT(j):
        b2 = j % 2
        pv = PS[3][:].bitcast(BF16)
        fns = [lambda e, c=c: e.transpose(out=pv[:, c * 128:(c + 1) * 128], in_=htok[:, b2, c * 128:(c + 1) * 128],
                                         identity=ident[:]) for c in range(2)]
        S.group("pe", fns, reads=[Bhtok[b2], Bconst], writes=[Bps[3]])
        S.op("dve", lambda e: e.tensor_copy(out=hTt[:, b2, :], in_=pv[:, 0:256]), reads=[Bps[3]], writes=[BhTt[b2]])

    def moe_D(j):
        b2, b = j % 2, j % NB
        wv = wdt[b][:].rearrange("p (c n) -> p c n", c=2)
        for half in range(2):
            bank = 4 + b2 * 2 + half
            fns = [lambda e, c=c, half=half, bank=bank: e.matmul(
                PS[bank][:], lhsT=hTt[:, b2, c * 128:(c + 1) * 128], rhs=wv[:, c, half * 512:(half + 1) * 512],
                start=(c == 0), stop=(c == 1)) for c in range(2)]
            S.group("pe", fns, reads=[BhTt[b2], Bwt[1][b]], writes=[Bps[bank]])
            if half == 0:
                S.op("act", lambda e, bank=bank: e.activation(out=ysb[:, b2, 0:512], in_=PS[bank][:], func=AF.Copy),
                     reads=[Bps[bank]], writes=[Bysb[b2]])
            else:
                S.op("dve", lambda e, bank=bank: e.tensor_copy(out=ysb[:, b2, 512:1024], in_=PS[bank][:]),
                     reads=[Bps[bank]], writes=[Bysb[b2]])
        S.dma("sp", lambda e: e.dma_start(out=ys_d[j * 128:(j + 1) * 128, :], in_=ysb[:, b2, :]), reads=[Bysb[b2]], writes=[Bys])

    for j in range(NX):
        moe_load_x(j)
    for j in range(NB):
        moe_load_w(j)
    for s_ in range(NTL + 3):
        if s_ < NTL:
            moe_T(s_)
            if s_ + NX < NTL:
                moe_load_x(s_ + NX)
        if 1 <= s_ <= NTL:
            moe_GU(s_ - 1)
        if 2 <= s_ <= NTL + 1:
            moe_HT(s_ - 2)
        if s_ >= 3:
            moe_D(s_ - 3)
            if s_ - 3 + NB < NTL:
                moe_load_w(s_ - 3 + NB)
    S.barrier()

    NYG = 12
    yg = M("yg", [128, NYG, D], F32, 34816, (7, 7))
    ln2g = M("ln2g", [128, D], F32, 2048, (7, 7))
    ln2bA = M("ln2bA", [128, D], F32, 6144, (7, 7))
    obuf = M("obuf", [128, 3, D], F32, 10240, (7, 7))
    lnst2 = M("lnst2", [128, 3, 32], F32, 22528, (7, 7))
    Bln2 = Buf("ln2"); Bob = [Buf("ob%d" % i) for i in range(3)]; Blnst2 = [Buf("ls%d" % i) for i in range(3)]
    Byg = [Buf("yg%d" % i) for i in range(NYG)]
    S.dma("sp", lambda e: e.dma_start(out=ln2g[:], in_=ln2g_d.to_broadcast([128, D])), writes=[Bln2])
    S.dma("sp", lambda e: e.dma_start(out=ln2bA[:], in_=ln2b_d.to_broadcast([128, D])), writes=[Bln2])

    def tail_gather(q):
        tt, slot = q // 2, q % 2
        yb_ = q % NYG
        S.dma("pool", lambda e: e.indirect_dma_start(
            out=yg[:, yb_, :], out_offset=None, in_=ys_d,
            in_offset=bass.IndirectOffsetOnAxis(ap=posu[:, tt, slot:slot + 1], axis=0)),
            reads=[Bpos, Bys], writes=[Byg[yb_]])

    for q in range(NYG):
        tail_gather(q)
    for tt in range(NT):
        b3 = tt % 3
        for slot in range(2):
            q = tt * 2 + slot
            yb_ = q % NYG
            S.op("dve", lambda e, tt=tt, slot=slot, yb_=yb_: e.scalar_tensor_tensor(
                out=acc[:, tt, :], in0=yg[:, yb_, :], scalar=wsl[:, tt, slot:slot + 1], in1=acc[:, tt, :],
                op0=ALU.mult, op1=ALU.add), reads=[Byg[yb_], Bpos, Bacc[tt]], writes=[Bacc[tt]])
            if q + NYG < 2 * NT:
                tail_gather(q + NYG)
        ln_scaled(acc[:, tt, :], [Bacc[tt]], lnst2[:, b3, :], Blnst2[b3], ln2g, ln2bA, Bln2, obuf[:, b3, :], [Bob[b3]],
                  alpha=1.0)
        S.dma("sp", lambda e, tt=tt, b3=b3: e.dma_start(out=out_d[tt * 128:(tt + 1) * 128, :], in_=obuf[:, b3, :]),
              reads=[Bob[b3]], writes=[Bout])
    S.barrier()
    S.emit()
    return nc


_NC_CACHE = {}


def _host_inputs(inp, b):
    f = np.float32
    w_in = inp["w_in"][0]
    cols = list(range(0, 1024))
    cols += list(range(1152, 1664))
    cols += list(range(1664, 1728)) * 2
    qb0 = 1736
    for j in range(4):
        cols += list(range(qb0 + j * 64, qb0 + (j + 1) * 64))
        cols += list(range(qb0 + (j + 4) * 64, qb0 + (j + 5) * 64))
    cols += list(range(2248, 2376))
    cols += list(range(1024, 1152))
    cols += list(range(2376, 2504))
    cols += list(range(1728, 1736))
    assert len(cols) == W1COLS
    rel = inp["rel_bias"].astype(f)
    s = np.arange(128)[:, None]
    t = np.arange(128)[None, :]
    bk_prev = t5_bucket_np(t - s + 128)
    bk_own = t5_bucket_np(t - s)
    swab = np.zeros((4, 128, 4, 128), f)
    for typ, bk in enumerate((bk_prev, bk_own)):
        for g in range(2):
            for j in range(4):
                swab[typ * 2 + g, :, j, :] = rel[bk, 8 + 4 * g + j]
    dsab = np.zeros((2, 128, 8, 128), f)
    for typ, bk in enumerate((bk_prev, bk_own)):
        for h in range(8):
            dsab[typ, :, h, :] = rel[bk, h]
    return {
        "xT": np.ascontiguousarray(inp["x"][b].T),
        "x": np.ascontiguousarray(inp["x"][b]),
        "w1": np.ascontiguousarray(w_in[:, cols]),
        "wg": np.ascontiguousarray(w_in[:, 2504:4552]),
        "kvg": np.ascontiguousarray(inp["kv_norm_g"][0].reshape(1, 128)),
        "wuv": np.ascontiguousarray(inp["w_uv"][0].transpose(1, 0, 2).reshape(128, 512)),
        "wa": np.ascontiguousarray(inp["w_branch_a"][0]),
        "wb": np.ascontiguousarray(inp["w_branch_b"][0]),
        "wo": np.ascontiguousarray(inp["w_out"][0]),
        "sinks": np.ascontiguousarray(inp["sinks"][0].reshape(1, 8)),
        "ln1g": np.ascontiguousarray(inp["ln1_g"][0].reshape(1, D)),
        "ln1b": np.ascontiguousarray(inp["ln1_b"][0].reshape(1, D)),
        "ln2g": np.ascontiguousarray(inp["ln2_g"][0].reshape(1, D)),
        "ln2b": np.ascontiguousarray(inp["ln2_b"][0].reshape(1, D)),
        "wr": np.ascontiguousarray(np.concatenate([inp["w_group"][0], inp["w_router"][0]], axis=1)),
        "br": np.ascontiguousarray(np.concatenate([inp["b_group"][0], inp["b_router"][0]]).reshape(1, 36)),
        "wgur": np.ascontiguousarray(np.concatenate(
            [inp["w_gate"][0].reshape(NEXP, 8, 128, DE), inp["w_up"][0].reshape(NEXP, 8, 128, DE)], axis=-1)
            .transpose(0, 2, 1, 3).reshape(NEXP * 128, 4096)),
        "wdr": np.ascontiguousarray(inp["w_down"][0].reshape(NEXP, 2, 128, D).transpose(0, 2, 1, 3).reshape(NEXP * 128, 2048)),
        "swab": swab.reshape(4, 128, 512),
        "dsab": dsab.reshape(2, 128, 1024),
        "c31": np.ascontiguousarray(rel[31, 0:8].reshape(1, 8)),
    }


def kernel(**inputs):
    inp = {k: np.asarray(v, dtype=np.float32) for k, v in inputs.items()}
    n = 8
    if "nc" not in _NC_CACHE:
        _NC_CACHE["nc"] = build_nc(False)
    nc = _NC_CACHE["nc"]
    shared = None
    in_maps = []
    for b in range(n):
        m = _host_inputs(inp, b) if shared is None else dict(shared)
        if shared is None:
            shared = m
        else:
            m["xT"] = np.ascontiguousarray(inp["x"][b].T)
            m["x"] = np.ascontiguousarray(inp["x"][b])
        in_maps.append(m)
    res = run_bass_kernel_spmd(nc, in_maps, core_ids=list(range(n)))
    return np.stack([np.asarray(r["out"], dtype=np.float32) for r in res.results], axis=0)
```
